# Optimizing a Trainium2 kernel written in Bass

```python
import math
import jax, jax.numpy as jnp
from jax import lax
import numpy as np


D_MODEL = 1024
BATCH = 8
SEQ = 4096
DEPTH = 1

PLE_DIM = 256
RMS_EPS = 1e-6

RET_HEADS = 8
RET_HEAD_DIM = 64
RET_WIDTH = RET_HEADS * RET_HEAD_DIM
RET_CHUNK = 128
RET_GN_EPS = 1e-5
ROPE_BASE = 10000.0

RWKV_HEADS = 8
RWKV_HEAD_DIM = 64
RWKV_WIDTH = RWKV_HEADS * RWKV_HEAD_DIM
DECAY_LORA = 64
AAA_LORA = 64
GATE_LORA = 128
RWKV_GN_EPS = 64e-5
L2_EPS = 1e-12

PEER_HEADS = 8
PEER_N_KEYS = 128
PEER_N_EXPERTS = PEER_N_KEYS * PEER_N_KEYS
PEER_QUERY_DIM = 256
PEER_HALF = PEER_QUERY_DIM // 2
PEER_TOPK = 16
PEER_TOKEN_BLOCK = 128

RET_SIZES = (RET_WIDTH, RET_WIDTH, RET_WIDTH, RET_WIDTH)
RWKV_SIZES = (RWKV_WIDTH, RWKV_WIDTH, RWKV_WIDTH, DECAY_LORA, AAA_LORA, GATE_LORA)
GATE_SIZES = (D_MODEL, D_MODEL)
RET_COLS = sum(RET_SIZES)
RWKV_COLS = sum(RWKV_SIZES)
GATE_COLS = sum(GATE_SIZES)
IN_COLS = RET_COLS + RWKV_COLS + GATE_COLS

kernel_name = 'hybrid_retention_rwkv7_peer_block'


def _split(z, sizes):
    offs = [sum(sizes[:i + 1]) for i in range(len(sizes) - 1)]
    return jnp.split(z, offs, axis=-1)


def _rmsnorm(x, g):
    xf = x.astype(jnp.float32)
    y = xf * lax.rsqrt(jnp.mean(xf * xf, axis=-1, keepdims=True) + RMS_EPS)
    return (y * g.astype(jnp.float32)).astype(x.dtype)


def _head_norm(o, eps):
    mu = jnp.mean(o, axis=-1, keepdims=True)
    oc = o - mu
    var = jnp.mean(oc * oc, axis=-1, keepdims=True)
    y = oc * lax.rsqrt(var + eps)
    return y.reshape(o.shape[0], o.shape[1], -1)


def _rotary(x):
    S, d = x.shape[1], x.shape[-1]
    half = d // 2
    inv_freq = ROPE_BASE ** (-jnp.arange(half, dtype=jnp.float32) * 2.0 / d)
    ang = jnp.arange(S, dtype=jnp.float32)[:, None] * inv_freq[None, :]
    cos = jnp.cos(ang)[None, :, None, :]
    sin = jnp.sin(ang)[None, :, None, :]
    x1, x2 = x[..., :half], x[..., half:]
    return jnp.concatenate([x1 * cos - x2 * sin, x1 * sin + x2 * cos], axis=-1)


def _retention_chunkwise(q, k, v):
    B, S, H, d = q.shape
    C = RET_CHUNK
    NC = S // C
    log_gamma = jnp.log1p(-(2.0 ** (-5.0 - jnp.arange(H, dtype=jnp.float32))))
    idx = jnp.arange(C, dtype=jnp.float32)
    diff = idx[:, None] - idx[None, :]
    causal = diff >= 0
    intra_decay = jnp.where(causal[None], jnp.exp(log_gamma[:, None, None] * jnp.where(causal, diff, 0.0)[None]), 0.0)
    zeta = jnp.exp(log_gamma[:, None] * (C - 1.0 - idx)[None, :])
    xi = jnp.exp(log_gamma[:, None] * (idx + 1.0)[None, :])
    chunk_decay = jnp.exp(log_gamma * C)

    qc = q.reshape(B, NC, C, H, d)
    kc = k.reshape(B, NC, C, H, d)
    vc = v.reshape(B, NC, C, H, d)
    scores = jnp.einsum('bnihd,bnjhd->bnhij', qc, kc) * intra_decay[None, None]
    intra = jnp.einsum('bnhij,bnjhd->bnihd', scores, vc)
    chunk_kv = jnp.einsum('bnjhd,hj,bnjhe->bnhde', kc, zeta, vc)

    def step(R, kv):
        return R * chunk_decay[None, :, None, None] + kv, R

    R0 = jnp.zeros((B, H, d, d), chunk_kv.dtype)
    _, R_prev = lax.scan(step, R0, jnp.moveaxis(chunk_kv, 1, 0))
    R_prev = jnp.moveaxis(R_prev, 0, 1)
    cross = jnp.einsum('bnihd,bnhde,hi->bnihe', qc, R_prev, xi)
    return (intra + cross).reshape(B, S, H, d)


def _wkv7(r, w, k, v, kk, a):
    B, S, H, d = r.shape

    def step(state, inp):
        r_t, w_t, k_t, v_t, kk_t, a_t = inp
        sa = jnp.einsum('bhvk,bhk->bhv', state, -kk_t)
        state = (state * w_t[:, :, None, :]
                 + sa[..., None] * (kk_t * a_t)[:, :, None, :]
                 + v_t[..., None] * k_t[:, :, None, :])
        y = jnp.einsum('bhvk,bhk->bhv', state, r_t)
        return state, y

    xs = (jnp.moveaxis(r, 1, 0), jnp.moveaxis(w, 1, 0), jnp.moveaxis(k, 1, 0),
          jnp.moveaxis(v, 1, 0), jnp.moveaxis(kk, 1, 0), jnp.moveaxis(a, 1, 0))
    _, ys = lax.scan(step, jnp.zeros((B, H, d, d), jnp.float32), xs)
    return jnp.moveaxis(ys, 0, 1)


def _token_shift(z, mu):
    z_prev = jnp.concatenate([jnp.zeros_like(z[:, :1]), z[:, :-1]], axis=1)
    return z + (z_prev - z) * mu


def _token_mixers(h, w_in, ret_gn_g, rwkv_mu, rwkv_w0, rwkv_w_up, rwkv_a0, rwkv_a_up,
                  rwkv_g_up, rwkv_k_k, rwkv_k_a, rwkv_r_k, rwkv_gn_g, rwkv_gn_b,
                  w_ret_br, w_rwkv_br, w_o):
    B, S, _ = h.shape
    f32 = jnp.float32
    z = h @ w_in
    z_ret = z[..., :RET_COLS]
    z_rwkv = z[..., RET_COLS:RET_COLS + RWKV_COLS]
    z_gate = z[..., RET_COLS + RWKV_COLS:]

    q, k, v, gr = _split(z_ret, RET_SIZES)
    rh = lambda t: t.astype(f32).reshape(B, S, RET_HEADS, RET_HEAD_DIM)
    q = _rotary(rh(q))
    k = _rotary(rh(k)) * (RET_HEAD_DIM ** -0.5)
    o_ret = _retention_chunkwise(q, k, rh(v))
    y_ret = jax.nn.silu(gr.astype(f32)) * (_head_norm(o_ret, RET_GN_EPS) * ret_gn_g)
    branch_ret = (y_ret @ w_ret_br.astype(f32)).astype(h.dtype)

    zs = _token_shift(z_rwkv, rwkv_mu).astype(f32)
    r, kr, vr, wl, al, gl = _split(zs, RWKV_SIZES)
    w_log = -jax.nn.softplus(-(rwkv_w0 + jnp.tanh(wl) @ rwkv_w_up)) - 0.5
    decay = jnp.exp(-jnp.exp(w_log))
    a = jax.nn.sigmoid(rwkv_a0 + al @ rwkv_a_up)
    g = jax.nn.sigmoid(gl) @ rwkv_g_up
    hh = lambda t: t.reshape(B, S, RWKV_HEADS, RWKV_HEAD_DIM)
    kk = hh(kr * rwkv_k_k)
    kk = kk / jnp.maximum(jnp.sqrt(jnp.sum(kk * kk, axis=-1, keepdims=True)), L2_EPS)
    kr = kr * (1.0 + (a - 1.0) * rwkv_k_a)
    r_h, k_h, v_h = hh(r), hh(kr), hh(vr)
    o = _wkv7(r_h, hh(decay), k_h, v_h, kk, hh(a))
    bonus = jnp.sum(r_h * k_h * rwkv_r_k, axis=-1, keepdims=True) * v_h
    y_rwkv = (_head_norm(o, RWKV_GN_EPS) * rwkv_gn_g + rwkv_gn_b + bonus.reshape(B, S, -1)) * g
    branch_rwkv = (y_rwkv @ w_rwkv_br.astype(f32)).astype(h.dtype)

    gate_ret, gate_rwkv = _split(z_gate, GATE_SIZES)
    merged = jax.nn.sigmoid(gate_ret) * branch_ret + jax.nn.sigmoid(gate_rwkv) * branch_rwkv
    return merged @ w_o


def _peer(h, w_pq, sub_keys, expert_u, expert_v):
    B, S, D = h.shape
    K = PEER_TOPK
    q = (h @ w_pq).reshape(B, S, PEER_HEADS, 2, PEER_HALF)
    s = jnp.einsum('bshcd,hckd->bshck', q, sub_keys)
    top_s, top_i = lax.top_k(s, K)
    cand_s = (top_s[..., 0, :, None] + top_s[..., 1, None, :]).reshape(B, S, PEER_HEADS, K * K)
    cand_i = (top_i[..., 0, :, None] * PEER_N_KEYS + top_i[..., 1, None, :]).reshape(B, S, PEER_HEADS, K * K)
    best_s, best_pos = lax.top_k(cand_s, K)
    ids = jnp.take_along_axis(cand_i, best_pos, axis=-1)
    gates = jax.nn.softmax(best_s.astype(jnp.float32), axis=-1).astype(h.dtype)

    T = B * S
    NB = T // PEER_TOKEN_BLOCK
    hb = h.reshape(NB, PEER_TOKEN_BLOCK, D)
    ib = ids.reshape(NB, PEER_TOKEN_BLOCK, PEER_HEADS * K)
    gb = gates.reshape(NB, PEER_TOKEN_BLOCK, PEER_HEADS * K)

    def block(args):
        hx, ix, gx = args
        u = expert_u[ix]
        act = jax.nn.gelu(jnp.einsum('td,tkd->tk', hx, u))
        vv = expert_v[ix]
        return jnp.einsum('tk,tkd->td', gx * act, vv)

    out = lax.map(block, (hb, ib, gb))
    return out.reshape(B, S, D)


def setup_inputs(seed: int = 0) -> dict:
    key = jax.random.key(seed)
    ks = iter(jax.random.split(key, 32))
    nrm = lambda shape, scale: jax.random.normal(next(ks), shape, jnp.float32) * scale
    uni = lambda shape, lo, hi: jax.random.uniform(next(ks), shape, jnp.float32, lo, hi)
    L = DEPTH
    return {
        'x': nrm((BATCH, SEQ, D_MODEL), 1.0),
        'p': nrm((DEPTH, BATCH, SEQ, PLE_DIM), 1.0),
        'g_mix': 1.0 + nrm((L, D_MODEL), 0.02),
        'w_in': nrm((L, D_MODEL, IN_COLS), D_MODEL ** -0.5),
        'ret_gn_g': 1.0 + nrm((L, RET_WIDTH), 0.02),
        'rwkv_mu': uni((L, RWKV_COLS), 0.0, 1.0),
        'rwkv_w0': uni((L, RWKV_WIDTH), -6.0, -1.0),
        'rwkv_w_up': nrm((L, DECAY_LORA, RWKV_WIDTH), 0.5 * DECAY_LORA ** -0.5),
        'rwkv_a0': nrm((L, RWKV_WIDTH), 0.1),
        'rwkv_a_up': nrm((L, AAA_LORA, RWKV_WIDTH), AAA_LORA ** -0.5),
        'rwkv_g_up': nrm((L, GATE_LORA, RWKV_WIDTH), GATE_LORA ** -0.5),
        'rwkv_k_k': 0.85 + nrm((L, RWKV_WIDTH), 0.05),
        'rwkv_k_a': 1.0 + nrm((L, RWKV_WIDTH), 0.05),
        'rwkv_r_k': nrm((L, RWKV_HEADS, RWKV_HEAD_DIM), 0.1),
        'rwkv_gn_g': 1.0 + nrm((L, RWKV_WIDTH), 0.02),
        'rwkv_gn_b': nrm((L, RWKV_WIDTH), 0.02),
        'w_ret_br': nrm((L, RET_WIDTH, D_MODEL), RET_WIDTH ** -0.5),
        'w_rwkv_br': nrm((L, RWKV_WIDTH, D_MODEL), RWKV_WIDTH ** -0.5),
        'w_o': nrm((L, D_MODEL, D_MODEL), D_MODEL ** -0.5),
        'g_ffn': 1.0 + nrm((L, D_MODEL), 0.02),
        'w_pq': nrm((L, D_MODEL, PEER_HEADS * PEER_QUERY_DIM), D_MODEL ** -0.5),
        'peer_sub_keys': nrm((L, PEER_HEADS, 2, PEER_N_KEYS, PEER_HALF), PEER_HALF ** -0.5),
        'peer_u': nrm((L, PEER_N_EXPERTS, D_MODEL), D_MODEL ** -0.5),
        'peer_v': nrm((L, PEER_N_EXPERTS, D_MODEL), PEER_HEADS ** -0.5),
        'g_ple': 1.0 + nrm((L, D_MODEL), 0.02),
        'w_ple_gate': nrm((L, D_MODEL, D_MODEL), D_MODEL ** -0.5),
        'w_ple_up': nrm((L, PLE_DIM, D_MODEL), PLE_DIM ** -0.5),
        'g_final': 1.0 + nrm((D_MODEL,), 0.02),
    }


def reference(x, p, g_mix, w_in, ret_gn_g, rwkv_mu, rwkv_w0, rwkv_w_up, rwkv_a0, rwkv_a_up,
              rwkv_g_up, rwkv_k_k, rwkv_k_a, rwkv_r_k, rwkv_gn_g, rwkv_gn_b, w_ret_br,
              w_rwkv_br, w_o, g_ffn, w_pq, peer_sub_keys, peer_u, peer_v, g_ple,
              w_ple_gate, w_ple_up, g_final):
    for i in range(DEPTH):
        h = _rmsnorm(x, g_mix[i])
        x = x + _token_mixers(h, w_in[i], ret_gn_g[i], rwkv_mu[i], rwkv_w0[i], rwkv_w_up[i],
                              rwkv_a0[i], rwkv_a_up[i], rwkv_g_up[i], rwkv_k_k[i], rwkv_k_a[i],
                              rwkv_r_k[i], rwkv_gn_g[i], rwkv_gn_b[i], w_ret_br[i],
                              w_rwkv_br[i], w_o[i]).astype(x.dtype)
        h2 = _rmsnorm(x, g_ffn[i])
        x = x + _peer(h2, w_pq[i], peer_sub_keys[i], peer_u[i], peer_v[i]).astype(x.dtype)
        ple_gate = jax.nn.sigmoid(_rmsnorm(x, g_ple[i]) @ w_ple_gate[i])
        x = x + (ple_gate * (p[i] @ w_ple_up[i])).astype(x.dtype)
    return _rmsnorm(x, g_final)
```

```python
import math
import numpy as np
from contextlib import ExitStack
import concourse.bass as bass
import concourse.mybir as mybir
from concourse.bass_utils import run_bass_kernel_spmd

F32 = mybir.dt.float32
BF16 = mybir.dt.bfloat16
U32 = mybir.dt.uint32
I32 = mybir.dt.int32
AF = mybir.ActivationFunctionType
ALU = mybir.AluOpType
AX = mybir.AxisListType

D = 1024
SEQ = 4096
NCORES = 8
IN_COLS = 5888
C0 = math.exp(-0.5)


class Sched:
    SELF_SYNC = {'pe': False, 'act': True, 'dve': True, 'pool': True, 'sp': True}

    def __init__(self, nc, es, n_dma_sems=8):
        self.nc = nc
        self.ops = {e: [] for e in ('pe', 'act', 'dve', 'pool', 'sp')}
        self.sem = {e: es.enter_context(nc.semaphore('prog_' + e)) for e in self.ops}
        self.cnt = {e: 0 for e in self.ops}
        self.waited = {e: {} for e in self.ops}
        self.last_w = {}
        self.readers = {}
        self.dsem = {}
        self.dcnt = {}
        self.drr = {}
        for q in ('sp', 'act', 'pool'):
            self.dsem[q] = [es.enter_context(nc.semaphore('dma_%s_%d' % (q, i)))
                            for i in range(n_dma_sems)]
            self.dcnt[q] = [0] * n_dma_sems
            self.drr[q] = 0
        self.sem_id = {}
        self.pending = {e: [] for e in self.ops}

    def barrier(self):
        toks = list(self.last_w.values())
        for ts_ in self.readers.values():
            toks.extend(ts_)
        for e in self.ops:
            self.pending[e] = list(toks)

    def _deps(self, r, w):
        toks = []
        for k in r:
            t = self.last_w.get(k)
            if t is not None:
                toks.append(t)
        for k in w:
            t = self.last_w.get(k)
            if t is not None:
                toks.append(t)
            toks.extend(self.readers.get(k, ()))
        return toks

    def _waits(self, e, toks):
        need = {}
        for (sem, val, src) in toks:
            if src == e and not self.SELF_SYNC[e]:
                continue
            key = id(sem)
            self.sem_id[key] = sem
            if self.waited[e].get(key, 0) >= val:
                continue
            if need.get(key, 0) < val:
                need[key] = val
        out = []
        for key, val in need.items():
            self.waited[e][key] = val
            out.append((self.sem_id[key], val))
        return out

    def _commit(self, tok, r, w):
        for k in w:
            self.last_w[k] = tok
            self.readers[k] = []
        for k in r:
            if k in w:
                continue
            self.readers.setdefault(k, []).append(tok)

    def op(self, e, fn, r=(), w=()):
        r = list(r); w = list(w)
        toks = self._deps(r, w) + self.pending[e]
        self.pending[e] = []
        waits = self._waits(e, toks)
        self.cnt[e] += 1
        tok = (self.sem[e], self.cnt[e], e)
        self.ops[e].append((waits, fn, (self.sem[e], 1)))
        self._commit(tok, r, w)
        return tok

    def dma(self, q, fn, r=(), w=()):
        r = list(r); w = list(w)
        j = self.drr[q]
        self.drr[q] = (j + 1) % len(self.dsem[q])
        sem = self.dsem[q][j]
        toks = self._deps(r, w) + self.pending[q]
        self.pending[q] = []
        if self.dcnt[q][j] > 0:
            toks.append((sem, 16 * self.dcnt[q][j], None))
        waits = self._waits(q, toks)
        self.dcnt[q][j] += 1
        tok = (sem, 16 * self.dcnt[q][j], None)
        self.ops[q].append((waits, fn, (sem, 16)))
        self._commit(tok, r, w)
        return tok

    def wait_all(self, e):
        toks = list(self.last_w.values())
        for ts in self.readers.values():
            toks.extend(ts)
        waits = self._waits(e, toks)
        self.ops[e].append((waits, None, None))

    def emit(self):
        nc = self.nc
        with nc.Block() as block:
            def run(e, eng):
                for waits, fn, inc in self.ops[e]:
                    for sem, val in waits:
                        eng.wait_ge(sem, val)
                    if fn is not None:
                        ins = fn(eng)
                        ins.then_inc(inc[0], inc[1])

            @block.sync
            def _(eng):
                run('sp', eng)

            @block.scalar
            def _(eng):
                run('act', eng)

            @block.vector
            def _(eng):
                run('dve', eng)

            @block.gpsimd
            def _(eng):
                run('pool', eng)

            @block.tensor
            def _(eng):
                run('pe', eng)


def host_consts(nt):
    f = np.float32
    S = nt * 128
    half = 32
    inv_freq = (10000.0 ** (-np.arange(half, dtype=f) * f(2.0) / f(64))).astype(f)
    ang = (np.arange(S, dtype=f)[:, None] * inv_freq[None, :]).astype(f)
    cos = np.cos(ang).astype(f); sin = np.sin(ang).astype(f)
    rot = np.zeros((nt, 128, 128), f)
    rot[:, :, 0:32] = cos.reshape(nt, 128, 32)
    rot[:, :, 32:64] = sin.reshape(nt, 128, 32)
    rot[:, :, 64:96] = cos.reshape(nt, 128, 32) * f(0.125)
    rot[:, :, 96:128] = sin.reshape(nt, 128, 32) * f(0.125)
    H = 8
    lg = np.log1p(-(2.0 ** (-5.0 - np.arange(H, dtype=np.float64))))
    idx = np.arange(128, dtype=np.float64)
    diff = idx[None, :] - idx[:, None]
    DT = np.where(diff[:, None, :] >= 0, np.exp(lg[None, :, None] * np.maximum(diff, 0)[:, None, :]), 0.0).astype(f)
    xiT = np.zeros((128, 4, 128), f)
    CDb = np.zeros((128, 4, 64), f)
    for c in range(4):
        for p in range(128):
            h = 2 * c + p // 64
            xiT[p, c, :] = np.exp(lg[h] * (idx + 1.0))
            CDb[p, c, :] = np.exp(lg[h] * 128.0)
    ZT = np.exp(lg[None, :] * (127.0 - idx)[:, None]).astype(f)
    s = np.arange(128)
    SU = (s[None, :] > s[:, None]).astype(f)
    SUI = (s[None, :] >= s[:, None]).astype(f)
    SL = SU.T.copy()
    BO = np.zeros((128, 128), f); BO[:64, :64] = 1; BO[64:, 64:] = 1
    HS = np.zeros((128, 2), f); HS[:64, 0] = 1; HS[64:, 1] = 1
    ident = np.eye(128, dtype=f)
    io16 = np.tile(np.arange(16, dtype=f)[None, :], (128, 1))
    cm = np.concatenate([ident, SU, SUI, SL, BO, ZT, HS, io16, io16 * 16], axis=1)
    return dict(rot=rot, DT=DT.reshape(128, 1024), xiT=xiT.reshape(128, 512),
                CDb=CDb.reshape(128, 256), cm=np.ascontiguousarray(cm))


CM_W = 128 * 5 + 8 + 2 + 16 + 16


class _Stop(Exception):
    pass


def build(nt, stage='full', stop_after=None):
    nc = bass.Bass("TRN2", target_bir_lowering=False)
    S_ = nt * 128
    WDT = BF16
    dram = lambda n, s, d, k="ExternalInput": nc.dram_tensor(n, s, d, kind=k).ap()
    x_d = dram("x", [S_, D], F32)
    p_d = dram("p", [S_, 256], F32)
    w_in_d = dram("w_in", [D, IN_COLS], F32)
    pp_d = dram("pp", [128, 34], F32)
    g4_d = dram("g4", [4, D], F32)
    gn3_d = dram("gn3", [3, 512], F32)
    lora_d = dram("lora", [128, 3, 512], F32)
    wbr_d = dram("wbr", [2, 512, D], F32)
    wo_d = dram("w_o", [D, D], F32)
    wpq_d = dram("w_pq", [D, 2048], F32)
    sk_d = dram("sk", [16, 128, 128], F32)
    pu_d = dram("peer_u", [16384, D], F32)
    pv_d = dram("peer_v", [16384, D], F32)
    wpg_d = dram("w_ple_gate", [D, D], F32)
    wpu_d = dram("w_ple_up", [256, D], F32)
    rot_d = dram("rot", [nt, 128, 128], F32)
    DT_d = dram("DT", [128, 1024], F32)
    xiT_d = dram("xiT", [128, 512], F32)
    CDb_d = dram("CDb", [128, 256], F32)
    cm_d = dram("cm", [128, CM_W], F32)
    out_d = dram("out", [S_, D], F32, "ExternalOutput")
    winb_d = nc.dram_tensor("winb", [D, IN_COLS], BF16, kind="Internal").ap()

    es = ExitStack()
    with es:
        S = Sched(nc, es)
        sb = lambda n, s, d: es.enter_context(nc.sbuf_tensor("sb_" + n, s, d))
        ps = lambda n, s, d: es.enter_context(nc.psum_tensor(n, s, d))

        def mm(out, lhsT, rhs, start, stop, r, w):
            S.op('pe', lambda e: e.matmul(out, lhsT=lhsT, rhs=rhs, start=start, stop=stop), r=r, w=w)

        def tr(out, in_, ident, r, w):
            S.op('pe', lambda e: e.transpose(out=out, in_=in_, identity=ident), r=r, w=w)

        def act(out, in_, func, r, w, **kw):
            S.op('act', lambda e: e.activation(out=out, in_=in_, func=func, **kw), r=r, w=w)

        def cp(eng, out, in_, r, w):
            if eng == 'act':
                S.op('act', lambda e: e.copy(out=out, in_=in_), r=r, w=w)
            else:
                S.op(eng, lambda e: e.tensor_copy(out=out, in_=in_), r=r, w=w)

        def tt(eng, out, in0, in1, op, r, w):
            S.op(eng, lambda e: e.tensor_tensor(out=out, in0=in0, in1=in1, op=op), r=r, w=w)

        def ts(eng, out, in0, s1, s2, op0, op1, r, w):
            if op1 is None:
                S.op(eng, lambda e: e.tensor_scalar(out=out, in0=in0, scalar1=s1, scalar2=None, op0=op0), r=r, w=w)
            else:
                S.op(eng, lambda e: e.tensor_scalar(out=out, in0=in0, scalar1=s1, scalar2=s2, op0=op0, op1=op1), r=r, w=w)

        def stt(out, in0, scalar, in1, op0, op1, r, w):
            S.op('dve', lambda e: e.scalar_tensor_tensor(out=out, in0=in0, scalar=scalar, in1=in1, op0=op0, op1=op1), r=r, w=w)

        def red(out, in_, op, r, w, axis=AX.X):
            S.op('dve', lambda e: e.tensor_reduce(out=out, in_=in_, axis=axis, op=op), r=r, w=w)

        def rcp(t, k):
            S.op('dve', lambda e: e.reciprocal(out=t, in_=t), r=[k], w=[k])

        def ld(q, out, in_, w, r=()):
            S.dma(q, lambda e: e.dma_start(out=out, in_=in_), r=r, w=w)

        def bc(ap, axis, shape):
            return ap.unsqueeze(axis).to_broadcast(shape)

        ptb = ps("ptb", [128, 8, 128], BF16)
        pbk = [ps("pb%d" % i, [128, 512], F32) for i in range(7)]
        PB = ['pb%d' % i for i in range(7)]

        cm = sb("cm", [128, CM_W], F32)
        ld('sp', cm[:], cm_d[:, :], ['cm'])
        identf = cm[:, 0:128]
        SU = cm[:, 128:256]; SUI = cm[:, 256:384]; SL = cm[:, 384:512]; BO = cm[:, 512:640]
        ZT = cm[:, 640:648]; HS = cm[:, 648:650]; IO16 = cm[:, 650:666]; IO16X = cm[:, 666:682]
        identb = sb("identb", [128, 128], BF16)
        cp('dve', identb[:], identf, ['cm'], ['identb'])
        eps_t = sb("eps_t", [128, 4], F32)
        S.op('dve', lambda e: e.memset(eps_t[:, 0:1], 1e-6), w=['eps'])
        S.op('dve', lambda e: e.memset(eps_t[:, 1:2], 1e-5), w=['eps'])
        S.op('dve', lambda e: e.memset(eps_t[:, 2:3], 64e-5), w=['eps'])
        sq_junk = sb("sq_junk", [128, D], BF16)
        rs_ss = sb("rs_ss", [128, 1], F32)
        rs_rstd = sb("rs_rstd", [128, 1], F32)

        def rmsnorm(src, src_key, gtab, gkey, dst, dst_key):
            act(sq_junk[:], src, AF.Square, [src_key], ['sq_junk', 'rs_ss'], accum_out=rs_ss[:])
            act(rs_rstd[:], rs_ss[:], AF.Sqrt, ['rs_ss', 'eps'], ['rs_rstd'], scale=1.0 / D, bias=eps_t[:, 0:1])
            rcp(rs_rstd[:], 'rs_rstd')
            stt(dst, src, rs_rstd[:, 0:1], gtab, ALU.mult, ALU.mult, [src_key, 'rs_rstd', gkey], [dst_key])

        def ck(k):
            if stop_after == k:
                raise _Stop()
        esA = ExitStack()
        with esA:
          try:
            sbA = lambda n, s, d: esA.enter_context(nc.sbuf_tensor("sa_" + n, s, d))
            pp = sbA("pp", [128, 34], F32)
            ld('sp', pp[:], pp_d[:, :], ['pp'])
            MU = pp[:, 0:14]; W0 = pp[:, 14:18]; A0 = pp[:, 18:22]; KK_ = pp[:, 22:26]; KA = pp[:, 26:30]; RK = pp[:, 30:34]
            omm = sbA("omm", [128, 14], F32)
            ts('dve', omm[:], MU, -1.0, 1.0, ALU.mult, ALU.add, ['pp'], ['omm'])
            omka = sbA("omka", [128, 4], F32)
            ts('dve', omka[:], KA, -1.0, 1.0, ALU.mult, ALU.add, ['pp'], ['omka'])
            gmix = sbA("gmix", [128, D], F32)
            ld('sp', gmix[:], g4_d[0, :].partition_broadcast(128), ['gmix'])
            gn3 = sbA("gn3", [128, 3, 512], F32)
            for i in range(3):
                ld('sp', gn3[:, i, :], gn3_d[i, :].partition_broadcast(128), ['gn3_%d' % i])

            NWB = 3
            wch = [sbA("wch%d" % i, [128, 8, 512], BF16) for i in range(NWB)]
            k = 0
            for kc in range(8):
                for cb in range(4):
                    b = k % NWB
                    stg = wch[b][:].rearrange("p a n -> p (a n)")[:, 0:1472]
                    S.dma('pool', lambda e, stg=stg, kc=kc, cb=cb: e.dma_start(out=stg, in_=w_in_d[kc * 128:(kc + 1) * 128, cb * 1472:(cb + 1) * 1472]), w=['wch%d' % b])
                    S.dma('sp', lambda e, stg=stg, kc=kc, cb=cb: e.dma_start(out=winb_d[kc * 128:(kc + 1) * 128, cb * 1472:(cb + 1) * 1472], in_=stg), r=['wch%d' % b], w=['winb'])
                    k += 1
            wbr = sbA("wbr", [128, 2, 4, D], BF16)
            for i in range(2):
                for c in range(4):
                    S.dma('pool', lambda e, i=i, c=c: e.dma_start(out=wbr[:, i, c, :], in_=wbr_d[i, c * 128:(c + 1) * 128, :]), w=['wbr%d%d' % (i, c)])
            wo = sbA("wo", [128, 8, D], BF16)
            for c in range(8):
                S.dma('pool', lambda e, c=c: e.dma_start(out=wo[:, c, :], in_=wo_d[c * 128:(c + 1) * 128, :]), w=['wo%d' % c])
            WBR = ['wbr%d%d' % (i, c) for i in range(2) for c in range(4)]
            WO = ['wo%d' % c for c in range(8)]
            lora = sbA("lora", [128, 3, 512], BF16)
            S.dma('pool', lambda e: e.dma_start(out=lora[:], in_=lora_d[:, :, :]), w=['lora'])
            DT = sbA("DT", [128, 8, 128], F32)
            ld('sp', DT[:].rearrange("p h i -> p (h i)"), DT_d[:, :], ['DT'])
            xiT = sbA("xiT", [128, 4, 128], F32)
            ld('sp', xiT[:].rearrange("p c i -> p (c i)"), xiT_d[:, :], ['xiT'])
            CDb = sbA("CDb", [128, 4, 64], F32)
            ld('sp', CDb[:].rearrange("p c i -> p (c i)"), CDb_d[:, :], ['CDb'])

            CH = [(0, 512), (512, 512), (1024, 512), (1536, 512),
                  (2048, 512), (2560, 512), (3072, 512), (3584, 256),
                  (3840, 512), (4352, 512), (4864, 512), (5376, 512)]
            wctr = [k]

            def load_chunk(ci):
                b = wctr[0] % NWB
                wctr[0] += 1
                c0, cw = CH[ci]
                S.dma('sp', lambda e, b=b, c0=c0, cw=cw: e.dma_start(
                    out=wch[b][:, :, 0:cw], in_=winb_d[:, c0:c0 + cw].rearrange("(kc p) n -> p kc n", p=128)),
                    r=['winb'], w=['wch%d' % b])
                return b

            xt = [sbA("xt%d" % i, [128, D], F32) for i in range(2)]
            rot_t = [sbA("rot%d" % i, [128, 128], F32) for i in range(2)]
            h = sbA("h", [128, D], BF16)
            hT = sbA("hT", [128, 8, 128], BF16)
            qk_rot = sbA("qk_rot", [128, 2, 512], BF16)
            rt = [sbA("rt%d" % i, [128, 8, 32], F32) for i in range(4)]
            v_tok = sbA("v_tok", [128, 512], BF16)
            gr_s = sbA("gr_s", [128, 512], BF16)
            gate_s = sbA("gate_s", [128, 2048], BF16)
            qkT = sbA("qkT", [128, 8, 128], BF16)
            qxT = sbA("qxT", [128, 4, 128], BF16)
            kz = sbA("kz", [128, 8, 64], BF16)
            PT = sbA("PT", [128, 8, 128], BF16)
            R32 = sbA("R32", [128, 4, 64], F32)
            Rb = sbA("Rb", [128, 4, 128], BF16)
            qTm = sbA("qTm", [128, 2, 4, 128], BF16)
            S.op('dve', lambda e: e.memset(qTm[:], 0.0), w=['qTm'])
            Rtmp = sbA("Rtmp", [128, 4, 64], F32)
            S.op('dve', lambda e: e.memset(R32[:], 0.0), w=['R32'])
            S.op('dve', lambda e: e.memset(Rb[:], 0.0), w=['Rb'])
            hn_sq = sbA("hn_sq", [128, 8, 64], F32)
            hn_c = sbA("hn_c", [128, 8, 64], F32)
            hn_s = sbA("hn_s", [128, 8], F32)
            hn_q = sbA("hn_q", [128, 8], F32)
            hn_m = sbA("hn_m", [128, 8], F32)
            hn_r = sbA("hn_r", [128, 8], F32)
            y_bf = sbA("y_bf", [128, 512], BF16)
            yT = sbA("yT", [128, 4, 128], BF16)
            ZB = sbA("ZB", [128, 14, 129], F32)
            zlast = sbA("zlast", [128, 14, 1], F32)
            S.op('dve', lambda e: e.memset(zlast[:], 0.0), w=['zlast'])
            zs = sbA("zs", [128, 14, 128], F32)
            lor_in = sbA("lor_in", [128, 2, 128], BF16)
            f4 = [sbA("f4_%d" % i, [128, 4, 128], F32) for i in range(8)]
            F4 = ['f4_%d' % i for i in range(8)]
            f4ones = sbA("f4ones", [128, 128], F32)
            S.op('dve', lambda e: e.memset(f4ones[:], 1.0), w=['f4ones'])
            wk = {n_: sbA("wk_" + n_, [128, 4, 128], WDT) for n_ in ('ab', 'rb', 'bb', 'kb', 'bt', 'kt', 'vT')}
            tok3 = sbA("tok3", [128, 3, 512], WDT)
            Am = {n_: sbA("Am_" + n_, [128, 4, 128], WDT) for n_ in ('akT', 'rbT', 'rkT')}
            Nb = [sbA("Nb%d" % i, [128, 4, 128], WDT) for i in range(2)]
            NTb = [sbA("NTb%d" % i, [128, 4, 128], WDT) for i in range(2)]
            Qb = [sbA("Qb%d" % i, [128, 4, 128], WDT) for i in range(2)]
            BOw = sbA("BOw", [128, 128], F32)
            cp('dve', BOw[:], BO, ['cm'], ['BOw'])
            Xs = sbA("Xs", [128, 256], WDT)
            Us = sbA("Us", [128, 512], WDT)
            ST32 = sbA("ST32", [128, 4, 64], F32)
            STb = sbA("STb", [128, 4, 128], WDT)
            wkm = {n_: sbA("wkm_" + n_, [128, 2, 4, 128], WDT) for n_ in ('ab', 'bb', 'rb')}
            for n_ in ('ab', 'bb', 'rb'):
                S.op('pool', lambda e, n_=n_: e.memset(wkm[n_][:], 0.0), w=['wkm_' + n_])
            STtmp = sbA("STtmp", [128, 4, 64], F32)
            S.op('dve', lambda e: e.memset(ST32[:], 0.0), w=['ST32'])
            S.op('dve', lambda e: e.memset(STb[:], 0.0), w=['STb'])
            PCt = sbA("PCt", [128, 4], F32)
            g_tok = sbA("g_tok", [128, 512], F32)
            cbt = sbA("cbt", [128, 8], F32)
            merged = sbA("merged", [128, D], BF16)
            mT = sbA("mT", [128, 8, 128], BF16)
            x1 = sbA("x1", [128, D], F32)

            def headnorm(ops_ap, ops_key, eps_col, dst_c):
                red(hn_s[:], ops_ap, ALU.add, [ops_key], ['hn_s'])
                act(hn_sq[:], ops_ap, AF.Square, [ops_key], ['hn_sq'])
                red(hn_q[:], hn_sq[:], ALU.add, ['hn_sq'], ['hn_q'])
                ts('dve', hn_m[:], hn_s[:], 1.0 / 64, None, ALU.mult, None, ['hn_s'], ['hn_m'])
                tt('dve', hn_r[:], hn_m[:], hn_m[:], ALU.mult, ['hn_m'], ['hn_r'])
                stt(hn_r[:], hn_q[:], 1.0 / 64, hn_r[:], ALU.mult, ALU.subtract, ['hn_q', 'hn_r'], ['hn_r'])
                act(hn_r[:], hn_r[:], AF.Sqrt, ['hn_r', 'eps'], ['hn_r'], bias=eps_t[:, eps_col:eps_col + 1])
                rcp(hn_r[:], 'hn_r')
                tt('dve', dst_c, ops_ap, bc(hn_m[:], 2, [128, 8, 64]), ALU.subtract, [ops_key, 'hn_m'], ['hn_c'])
                tt('dve', dst_c, dst_c, bc(hn_r[:], 2, [128, 8, 64]), ALU.mult, ['hn_c', 'hn_r'], ['hn_c'])

            ck(1)
            for n in range(nt):
                par = n % 2
                X, XK = xt[par], 'xt%d' % par
                ld('sp', X[:], x_d[n * 128:(n + 1) * 128, :], [XK])
                ld('sp', rot_t[par][:], rot_d[n, :, :], ['rot%d' % par])
                ROT = 'rot%d' % par
                rmsnorm(X[:], XK, gmix[:], 'gmix', h[:], 'h')
                for c in range(8):
                    tr(ptb[:, c, :], h[:, c * 128:(c + 1) * 128], identb[:], ['h', 'identb'], ['ptb'])
                cp('act', hT[:], ptb[:], ['ptb'], ['hT'])

                ck(2)
                def inproj_tok(ci, bank):
                    b = load_chunk(ci)
                    for kc in range(8):
                        mm(pbk[bank][:], hT[:, kc, :], wch[b][:, kc, :], kc == 0, kc == 7, ['hT', 'wch%d' % b], [PB[bank]])

                Cc = rot_t[par][:, 0:32]; Sc = rot_t[par][:, 32:64]
                kCc = rot_t[par][:, 64:96]; kSc = rot_t[par][:, 96:128]
                for qi in range(2):
                    bank = qi
                    inproj_tok(qi, bank)
                    pv = pbk[bank][:].rearrange("p (h two f) -> p h two f", two=2, f=32)
                    q1 = pv[:, :, 0, :]; q2 = pv[:, :, 1, :]
                    cc, ssn = (Cc, Sc) if qi == 0 else (kCc, kSc)
                    cb_ = bc(cc, 1, [128, 8, 32]); sb_ = bc(ssn, 1, [128, 8, 32])
                    ov = qk_rot[:, qi, :].rearrange("p (h two f) -> p h two f", two=2, f=32)
                    tt('dve', rt[0][:], q1, cb_, ALU.mult, [PB[bank], ROT], ['rt0'])
                    tt('dve', rt[1][:], q2, sb_, ALU.mult, [PB[bank], ROT], ['rt1'])
                    tt('dve', rt[2][:], q1, sb_, ALU.mult, [PB[bank], ROT], ['rt2'])
                    tt('dve', rt[3][:], q2, cb_, ALU.mult, [PB[bank], ROT], ['rt3'])
                    tt('pool', ov[:, :, 0, :], rt[0][:], rt[1][:], ALU.subtract, ['rt0', 'rt1'], ['qk_rot'])
                    tt('pool', ov[:, :, 1, :], rt[2][:], rt[3][:], ALU.add, ['rt2', 'rt3'], ['qk_rot'])
                inproj_tok(2, 2)
                cp('act', v_tok[:], pbk[2][:], [PB[2]], ['v_tok'])
                inproj_tok(3, 3)
                act(gr_s[:], pbk[3][:], AF.Silu, [PB[3]], ['gr_s'])

                ck(3)
                for c in range(8):
                    tr(ptb[:, c, :], qk_rot[:, c // 4, (c % 4) * 128:(c % 4 + 1) * 128], identb[:], ['qk_rot', 'identb'], ['ptb'])
                cp('act', qkT[:, 4:8, :], ptb[:, 4:8, :], ['ptb'], ['qkT'])
                cp('act', qTm[0:64, 0, :, :], ptb[0:64, 0:4, :], ['ptb'], ['qTm'])
                cp('act', qTm[64:128, 1, :, :], ptb[64:128, 0:4, :], ['ptb'], ['qTm'])
                tt('dve', qxT[:], ptb[:, 0:4, :], xiT[:], ALU.mult, ['ptb', 'xiT'], ['qxT'])
                tt('pool', kz[:], qk_rot[:, 1, :].rearrange("p (h d) -> p h d", d=64), bc(ZT, 2, [128, 8, 64]), ALU.mult, ['qk_rot', 'cm'], ['kz'])

                ck(31)
                for hh in range(8):
                    c, base = hh // 2, (hh % 2) * 64
                    bank = 4 + hh // 4
                    mm(pbk[bank][:, (hh % 4) * 128:(hh % 4 + 1) * 128], qkT[:, 4 + c, :], qTm[:, hh % 2, c, :], True, True, ['qkT', 'qTm'], [PB[bank]])
                for g in range(2):
                    tt('dve', PT[:, 4 * g:4 * g + 4, :], pbk[4 + g][:].rearrange("p (h i) -> p h i", i=128), DT[:, 4 * g:4 * g + 4, :], ALU.mult, [PB[4 + g], 'DT'], ['PT'])
                ck(32)
                for c in range(4):
                    mm(pbk[6][:, c * 128:(c + 1) * 128], qxT[:, c, :], Rb[:, c, :], True, False, ['qxT', 'Rb'], [PB[6]])
                    for w_ in range(2):
                        hh = 2 * c + w_
                        mm(pbk[6][:, hh * 64:(hh + 1) * 64], PT[:, hh, :], v_tok[:, hh * 64:(hh + 1) * 64], False, w_ == 1, ['PT', 'v_tok'], [PB[6]])
                ck(33)
                for c in range(4):
                    mm(pbk[4][:, c * 128:(c + 1) * 128], kz[:, 2 * c:2 * c + 2, :].rearrange("p a d -> p (a d)"), v_tok[:, c * 128:(c + 1) * 128], True, True, ['kz', 'v_tok'], [PB[4]])
                tt('pool', Rtmp[:], R32[:], CDb[:], ALU.mult, ['R32', 'CDb'], ['Rtmp'])
                p4v = pbk[4][:].rearrange("p (c x) -> p c x", x=128)
                tt('dve', R32[0:64, :, :], Rtmp[0:64, :, :], p4v[0:64, :, 0:64], ALU.add, ['Rtmp', PB[4]], ['R32'])
                tt('dve', R32[64:128, :, :], Rtmp[64:128, :, :], p4v[64:128, :, 64:128], ALU.add, ['Rtmp', PB[4]], ['R32'])
                cp('pool', Rb[0:64, :, 0:64], R32[0:64, :, :], ['R32'], ['Rb'])
                cp('pool', Rb[64:128, :, 64:128], R32[64:128, :, :], ['R32'], ['Rb'])
                ck(34)
                o3 = pbk[6][:].rearrange("p (h e) -> p h e", e=64)
                headnorm(o3, PB[6], 1, hn_c[:])
                hc2 = hn_c[:].rearrange("p h e -> p (h e)")
                tt('dve', hc2, hc2, gn3[:, 0, :], ALU.mult, ['hn_c', 'gn3_0'], ['hn_c'])
                tt('dve', y_bf[:], hc2, gr_s[:], ALU.mult, ['hn_c', 'gr_s'], ['y_bf'])
                ck(35)
                for c in range(4):
                    tr(ptb[:, c, :], y_bf[:, c * 128:(c + 1) * 128], identb[:], ['y_bf', 'identb'], ['ptb'])
                cp('act', yT[:], ptb[:, 0:4, :], ['ptb'], ['yT'])
                for hf in range(2):
                    for c in range(4):
                        mm(pbk[hf][:], yT[:, c, :], wbr[:, 0, c, hf * 512:(hf + 1) * 512], c == 0, c == 3, ['yT'] + WBR, [PB[hf]])

                ck(4)
                for j in range(4):
                    b = load_chunk(4 + j)
                    nm = 4 if j < 3 else 2
                    bank = 2 + (j % 2)
                    for m in range(nm):
                        for kc in range(8):
                            mm(pbk[bank][:, m * 128:(m + 1) * 128], wch[b][:, kc, m * 128:(m + 1) * 128], hT[:, kc, :], kc == 0, kc == 7, ['hT', 'wch%d' % b], [PB[bank]])
                    cp('act', ZB[:, 4 * j:4 * j + nm, 1:129], pbk[bank][:, 0:nm * 128].rearrange("p (m t) -> p m t", t=128), [PB[bank]], ['ZB'])
                cp('pool', ZB[:, :, 0:1], zlast[:], ['zlast'], ['ZB'])
                for (m0, m1) in ((0, 4), (4, 8), (8, 12), (12, 14)):
                    nm = m1 - m0
                    tmp = f4[7][:, 0:nm, :]
                    tt('pool', tmp, ZB[:, m0:m1, 0:128], bc(MU[:, m0:m1], 2, [128, nm, 128]), ALU.mult, ['ZB', 'pp'], [F4[7]])
                    tt('dve', zs[:, m0:m1, :], ZB[:, m0:m1, 1:129], bc(omm[:, m0:m1], 2, [128, nm, 128]), ALU.mult, ['ZB', 'omm'], ['zs'])
                    tt('dve', zs[:, m0:m1, :], zs[:, m0:m1, :], tmp, ALU.add, ['zs', F4[7]], ['zs'])
                cp('pool', zlast[:], ZB[:, :, 128:129], ['ZB'], ['zlast'])
                ck(5)
                rF = zs[:, 0:4, :]; krF = zs[:, 4:8, :]; vF = zs[:, 8:12, :]
                act(lor_in[0:64, 0, :], zs[0:64, 12, :], AF.Tanh, ['zs'], ['lor_in'])
                cp('act', lor_in[64:128, 0, :], zs[64:128, 12, :], ['zs'], ['lor_in'])
                act(lor_in[:, 1, :], zs[:, 13, :], AF.Sigmoid, ['zs'], ['lor_in'])
                for m in range(4):
                    mm(pbk[2][:, m * 128:(m + 1) * 128], lora[:, 0, m * 128:(m + 1) * 128], lor_in[:, 0, :], True, True, ['lora', 'lor_in'], [PB[2]])
                for m in range(4):
                    mm(pbk[3][:, m * 128:(m + 1) * 128], lora[:, 1, m * 128:(m + 1) * 128], lor_in[:, 0, :], True, True, ['lora', 'lor_in'], [PB[3]])
                mm(pbk[4][:], lor_in[:, 1, :], lora[:, 2, :], True, True, ['lora', 'lor_in'], [PB[4]])
                cp('act', g_tok[:], pbk[4][:], [PB[4]], ['g_tok'])
                sg, asig, kkF, kkn, kpr, bF, csF, tmpF = f4
                for m in range(4):
                    act(sg[:, m, :], pbk[2][:, m * 128:(m + 1) * 128], AF.Sigmoid, [PB[2], 'pp'], [F4[0]], bias=W0[:, m:m + 1])
                for m in range(4):
                    act(asig[:, m, :], pbk[3][:, m * 128:(m + 1) * 128], AF.Sigmoid, [PB[3], 'pp'], [F4[1]], bias=A0[:, m:m + 1])
                b4 = lambda t: bc(t, 2, [128, 4, 128])
                f2 = lambda t: t[:].rearrange("p m t -> p (m t)")
                tt('dve', kkF[:], krF, b4(KK_), ALU.mult, ['zs', 'pp'], [F4[2]])
                tt('pool', tmpF[:], kkF[:], kkF[:], ALU.mult, [F4[2]], [F4[7]])
                mm(pbk[2][:], BOw[:], f2(tmpF), True, True, ['BOw', F4[7]], [PB[2]])
                act(f2(kkn), pbk[2][:], AF.Sqrt, [PB[2]], [F4[3]])
                ts('dve', kkn[:], kkn[:], 1e-12, None, ALU.max, None, [F4[3]], [F4[3]])
                rcp(kkn[:], F4[3])
                tt('dve', kkn[:], kkn[:], kkF[:], ALU.mult, [F4[3], F4[2]], [F4[3]])
                tt('pool', kpr[:], asig[:], b4(KA), ALU.mult, [F4[1], 'pp'], [F4[4]])
                tt('pool', kpr[:], kpr[:], b4(omka[:]), ALU.add, [F4[4], 'omka'], [F4[4]])
                tt('dve', kpr[:], kpr[:], krF, ALU.mult, [F4[4], 'zs'], [F4[4]])
                tt('pool', bF[:], kkn[:], asig[:], ALU.mult, [F4[3], F4[1]], [F4[5]])
                tt('pool', tmpF[:], rF, kpr[:], ALU.mult, ['zs', F4[4]], [F4[7]])
                tt('pool', tmpF[:], tmpF[:], b4(RK), ALU.mult, [F4[7], 'pp'], [F4[7]])
                for c in range(4):
                    mm(pbk[3][:, 2 * c:2 * c + 2], tmpF[:, c, :], HS, True, True, [F4[7], 'cm'], [PB[3]])
                cp('act', cbt[:], pbk[3][:, 0:8], [PB[3]], ['cbt'])
                for m in range(4):
                    S.op('dve', lambda e, m=m: e.tensor_tensor_scan(out=csF[:, m, :], data0=f4ones[:], data1=sg[:, m, :], initial=0.0, op0=ALU.mult, op1=ALU.add), r=[F4[0], 'f4ones'], w=[F4[6]])
                E1, E2 = kkF, tmpF
                act(E1[:], csF[:], AF.Exp, [F4[6]], [F4[2]], scale=-C0)
                act(E2[:], csF[:], AF.Exp, [F4[6]], [F4[7]], scale=C0)
                tt('dve', csF[:], csF[:], sg[:], ALU.subtract, [F4[6], F4[0]], [F4[6]])
                act(sg[:], csF[:], AF.Exp, [F4[6]], [F4[0]], scale=-C0)
                E3 = sg
                cp('dve', PCt[:], E1[:, :, 127], [F4[2]], ['PCt'])
                stt(wk['ab'][:], kkn[:], -1.0, E3[:], ALU.mult, ALU.mult, [F4[3], F4[0]], ['wk_ab'])
                tt('dve', wk['rb'][:], rF, E1[:], ALU.mult, ['zs', F4[2]], ['wk_rb'])
                tt('pool', csF[:], bF[:], E2[:], ALU.mult, [F4[5], F4[7]], [F4[6]])
                cp('act', wk['bb'][:], csF[:], [F4[6]], ['wk_bb'])
                tt('pool', wk['bt'][:], csF[:], bc(PCt[:], 2, [128, 4, 128]), ALU.mult, [F4[6], 'PCt'], ['wk_bt'])
                tt('dve', bF[:], kpr[:], E2[:], ALU.mult, [F4[4], F4[7]], [F4[5]])
                cp('act', wk['kb'][:], bF[:], [F4[5]], ['wk_kb'])
                tt('pool', wk['kt'][:], bF[:], bc(PCt[:], 2, [128, 4, 128]), ALU.mult, [F4[5], 'PCt'], ['wk_kt'])
                cp('act', wk['vT'][:], vF, ['zs'], ['wk_vT'])
                for n_ in ('ab', 'bb', 'rb'):
                    cp('pool', wkm[n_][0:64, 0, :, :], wk[n_][0:64, :, :], ['wk_' + n_], ['wkm_' + n_])
                    cp('pool', wkm[n_][64:128, 1, :, :], wk[n_][64:128, :, :], ['wk_' + n_], ['wkm_' + n_])
                ck(6)
                for i, nmk in enumerate(('bt', 'kt', 'vT')):
                    for c in range(4):
                        tr(ptb[:, c, :], wk[nmk][:, c, :], identb[:], ['wk_' + nmk, 'identb'], ['ptb'])
                    cp('act', tok3[:, i, :].rearrange("p (c x) -> p c x", x=128), ptb[:, 0:4, :], ['ptb'], ['tok3_%d' % i])
                Btok = tok3[:, 0, :]; Ktok = tok3[:, 1, :]; Vtok = tok3[:, 2, :]

                ck(7)
                def hop(hh):
                    return hh // 2, (hh % 2) * 64
                for g in range(2):
                    hs = [4 * g + i for i in range(4)]
                    specs = [('bb', 'ab', SU, Nb[0], 'Nb0'), ('ab', 'bb', SL, NTb[0], 'NTb0'),
                             ('kb', 'ab', SU, Am['akT'], 'Am_akT'), ('bb', 'rb', SUI, Am['rbT'], 'Am_rbT'),
                             ('kb', 'rb', SUI, Am['rkT'], 'Am_rkT')]
                    for si, (l_, r_, msk, dst, dk) in enumerate(specs):
                        bank = 2 + (si % 3)
                        for i, hh in enumerate(hs):
                            c, base = hop(hh)
                            mm(pbk[bank][:, i * 128:(i + 1) * 128], wk[l_][:, c, :], wkm[r_][:, hh % 2, c, :], True, True, ['wk_' + l_, 'wkm_' + r_], [PB[bank]])
                        tt('dve', dst[:], pbk[bank][:].rearrange("p (h t) -> p h t", t=128), bc(msk, 1, [128, 4, 128]), ALU.mult, [PB[bank], 'cm'], [dk])
                    tt('pool', Qb[0][:], Nb[0][:], bc(identb[:], 1, [128, 4, 128]), ALU.add, ['Nb0', 'identb'], ['Qb0'])
                    cur = 0
                    for lvl in range(6):
                        nx = 1 - cur
                        last = (lvl == 5)
                        if not last:
                            for i in range(4):
                                mm(pbk[2][:, i * 128:(i + 1) * 128], NTb[cur][:, i, :], Nb[cur][:, i, :], True, True, ['NTb%d' % cur, 'Nb%d' % cur], [PB[2]])
                        for i in range(4):
                            mm(pbk[3][:, i * 128:(i + 1) * 128], Nb[cur][:, i, :], NTb[cur][:, i, :], True, True, ['NTb%d' % cur, 'Nb%d' % cur], [PB[3]])
                        if not last:
                            cp('dve', Nb[nx][:].rearrange("p h t -> p (h t)"), pbk[2][:], [PB[2]], ['Nb%d' % nx])
                        cp('act', NTb[nx][:].rearrange("p h t -> p (h t)"), pbk[3][:], [PB[3]], ['NTb%d' % nx])
                        for i in range(4):
                            mm(pbk[4][:, i * 128:(i + 1) * 128], identb[:], Qb[cur][:, i, :], True, False, ['identb', 'Qb%d' % cur], [PB[4]])
                            mm(pbk[4][:, i * 128:(i + 1) * 128], NTb[nx][:, i, :], Qb[cur][:, i, :], False, True, ['NTb%d' % nx, 'Qb%d' % cur], [PB[4]])
                        cp('act' if lvl % 2 else 'dve', Qb[nx][:].rearrange("p h t -> p (h t)"), pbk[4][:], [PB[4]], ['Qb%d' % nx])
                        cur = nx
                    Qf, QK = Qb[cur], 'Qb%d' % cur
                    for ci in range(2):
                        c = 2 * g + ci
                        mm(pbk[5][:, ci * 128:(ci + 1) * 128], wk['ab'][:, c, :], STb[:, c, :], True, False, ['wk_ab', 'STb'], [PB[5]])
                        for w_ in range(2):
                            i = 2 * ci + w_
                            hh = hs[i]
                            mm(pbk[5][:, i * 64:(i + 1) * 64], Am['akT'][:, i, :], Vtok[:, hh * 64:(hh + 1) * 64], False, w_ == 1, ['Am_akT', 'tok3_2'], [PB[5]])
                    cp('act', Xs[:], pbk[5][:, 0:256], [PB[5]], ['Xs'])
                    for i, hh in enumerate(hs):
                        oc = slice(i * 64, (i + 1) * 64)
                        mm(pbk[5][:, 256 + i * 64:256 + (i + 1) * 64], Qf[:, i, :], Xs[:, oc], True, True, [QK, 'Xs'], [PB[5]])
                    cp('act', Us[:, g * 256:(g + 1) * 256], pbk[5][:, 256:512], [PB[5]], ['Us'])
                    for ci in range(2):
                        c = 2 * g + ci
                        mm(pbk[6][:, c * 128:(c + 1) * 128], wk['rb'][:, c, :], STb[:, c, :], True, False, ['wk_rb', 'STb'], [PB[6]])
                        for w_ in range(2):
                            i = 2 * ci + w_
                            hh = hs[i]
                            hc = slice(hh * 64, (hh + 1) * 64)
                            mm(pbk[6][:, hc], Am['rkT'][:, i, :], Vtok[:, hc], False, False, ['Am_rkT', 'tok3_2'], [PB[6]])
                            mm(pbk[6][:, hc], Am['rbT'][:, i, :], Us[:, hc], False, w_ == 1, ['Am_rbT', 'Us'], [PB[6]])
                for c in range(4):
                    cs_ = slice(c * 128, (c + 1) * 128)
                    mm(pbk[5][:, cs_], Btok[:, cs_], Us[:, cs_], True, False, ['tok3_0', 'Us'], [PB[5]])
                    mm(pbk[5][:, cs_], Ktok[:, cs_], Vtok[:, cs_], False, True, ['tok3_1', 'tok3_2'], [PB[5]])
                tt('pool', STtmp[:], ST32[:], bc(PCt[:], 2, [128, 4, 64]), ALU.mult, ['ST32', 'PCt'], ['STtmp'])
                p5v = pbk[5][:].rearrange("p (c x) -> p c x", x=128)
                tt('dve', ST32[0:64, :, :], STtmp[0:64, :, :], p5v[0:64, :, 0:64], ALU.add, ['STtmp', PB[5]], ['ST32'])
                tt('dve', ST32[64:128, :, :], STtmp[64:128, :, :], p5v[64:128, :, 64:128], ALU.add, ['STtmp', PB[5]], ['ST32'])
                cp('pool', STb[0:64, :, 0:64], ST32[0:64, :, :], ['ST32'], ['STb'])
                cp('pool', STb[64:128, :, 64:128], ST32[64:128, :, :], ['ST32'], ['STb'])
                ck(8)
                o3 = pbk[6][:].rearrange("p (h e) -> p h e", e=64)
                headnorm(o3, PB[6], 2, hn_c[:])
                hc2 = hn_c[:].rearrange("p h e -> p (h e)")
                tt('dve', hc2, hc2, gn3[:, 1, :], ALU.mult, ['hn_c', 'gn3_1'], ['hn_c'])
                tt('dve', hc2, hc2, gn3[:, 2, :], ALU.add, ['hn_c', 'gn3_2'], ['hn_c'])
                tt('pool', hn_sq[:], Vtok.rearrange("p (h e) -> p h e", e=64), bc(cbt[:], 2, [128, 8, 64]), ALU.mult, ['tok3_2', 'cbt'], ['hn_sq'])
                tt('dve', hc2, hc2, hn_sq[:].rearrange("p h e -> p (h e)"), ALU.add, ['hn_c', 'hn_sq'], ['hn_c'])
                tt('dve', y_bf[:], hc2, g_tok[:], ALU.mult, ['hn_c', 'g_tok'], ['y_bf'])
                for c in range(4):
                    tr(ptb[:, c, :], y_bf[:, c * 128:(c + 1) * 128], identb[:], ['y_bf', 'identb'], ['ptb'])
                cp('act', yT[:], ptb[:, 0:4, :], ['ptb'], ['yT'])
                for hf in range(2):
                    for c in range(4):
                        mm(pbk[2 + hf][:], yT[:, c, :], wbr[:, 1, c, hf * 512:(hf + 1) * 512], c == 0, c == 3, ['yT'] + WBR, [PB[2 + hf]])

                ck(9)
                for gi in range(4):
                    bank = 4 + (gi % 2)
                    inproj_tok(8 + gi, bank)
                    act(gate_s[:, gi * 512:(gi + 1) * 512], pbk[bank][:], AF.Sigmoid, [PB[bank]], ['gate_s'])
                for hf in range(2):
                    sl = slice(hf * 512, (hf + 1) * 512)
                    tt('dve', x1[:, sl], pbk[hf][:], gate_s[:, sl], ALU.mult, [PB[hf], 'gate_s'], ['x1'])
                    tt('dve', merged[:, sl], pbk[2 + hf][:], gate_s[:, 1024 + hf * 512:1024 + (hf + 1) * 512], ALU.mult, [PB[2 + hf], 'gate_s'], ['merged'])
                    tt('pool', merged[:, sl], merged[:, sl], x1[:, sl], ALU.add, ['merged', 'x1'], ['merged'])
                for c in range(8):
                    tr(ptb[:, c, :], merged[:, c * 128:(c + 1) * 128], identb[:], ['merged', 'identb'], ['ptb'])
                cp('act', mT[:], ptb[:], ['ptb'], ['mT'])
                for hf in range(2):
                    for c in range(8):
                        mm(pbk[4 + hf][:], mT[:, c, :], wo[:, c, hf * 512:(hf + 1) * 512], c == 0, c == 7, ['mT'] + WO, [PB[4 + hf]])
                    tt('dve', x1[:, hf * 512:(hf + 1) * 512], pbk[4 + hf][:], X[:, hf * 512:(hf + 1) * 512], ALU.add, [PB[4 + hf], XK], ['x1'])
                S.dma('act', lambda e, n=n: e.dma_start(out=out_d[n * 128:(n + 1) * 128, :], in_=x1[:]), r=['x1'], w=['out%d' % n])
          except _Stop:
            pass

        if stage == 'A':
            S.wait_all('sp')
            S.emit()
            return nc
        S.barrier()

        esB = ExitStack()
        with esB:
          try:
            sbB = lambda n, s, d: esB.enter_context(nc.sbuf_tensor("sc_" + n, s, d))
            g3 = sbB("g3", [128, 3, D], F32)
            for i in range(3):
                ld('sp', g3[:, i, :], g4_d[1 + i, :].partition_broadcast(128), ['g3_%d' % i])
            wpq = sbB("wpq", [128, 8, 2048], BF16)
            for kc in range(8):
                for hf in range(2):
                    S.dma('pool', lambda e, kc=kc, hf=hf: e.dma_start(out=wpq[:, kc, hf * 1024:(hf + 1) * 1024], in_=wpq_d[kc * 128:(kc + 1) * 128, hf * 1024:(hf + 1) * 1024]), w=['wpq%d' % kc])
            WPQ = ['wpq%d' % kc for kc in range(8)]
            wpg = sbB("wpg", [128, 8, D], BF16)
            for c in range(8):
                S.dma('pool', lambda e, c=c: e.dma_start(out=wpg[:, c, :], in_=wpg_d[c * 128:(c + 1) * 128, :]), w=['wpg%d' % c])
            WPG = ['wpg%d' % c for c in range(8)]
            wpu = sbB("wpu", [128, 2, D], BF16)
            for c in range(2):
                S.dma('pool', lambda e, c=c: e.dma_start(out=wpu[:, c, :], in_=wpu_d[c * 128:(c + 1) * 128, :]), w=['wpu%d' % c])
            WPU = ['wpu0', 'wpu1']
            skT = sbB("skT", [128, 16, 128], F32)
            s_sb = sbB("s_sb", [128, 16, 128], F32)
            for g in range(16):
                ld('sp', s_sb[:, g, :], sk_d[g, :, :], ['s_sb'])
            for g4i in range(4):
                bank = g4i % 2
                for i in range(4):
                    g = g4i * 4 + i
                    tr(pbk[bank][:, i * 128:(i + 1) * 128], s_sb[:, g, :], identf, ['s_sb', 'cm'], [PB[bank]])
                cp('act', skT[:, g4i * 4:g4i * 4 + 4, :].rearrange("p g k -> p (g k)"), pbk[bank][:], [PB[bank]], ['skT'])

            ck(101)
            x1b = [sbB("x1b%d" % i, [128, D], F32) for i in range(2)]
            ptl = [sbB("ptl%d" % i, [128, 256], F32) for i in range(2)]
            h2 = sbB("h2", [128, D], F32)
            h2b = sbB("h2b", [128, D], BF16)
            h2T = sbB("h2T", [128, 8, 128], BF16)
            qT = sbB("qT", [128, 16, 128], F32)
            s_rp = sbB("s_rp", [128, 128], F32)
            vals = sbB("vals", [128, 16, 16], F32)
            idxu = sbB("idxu", [128, 16, 16], U32)
            idxf = sbB("idxf", [128, 16, 16], F32)
            cand = sbB("cand", [128, 8, 256], F32)
            cand2 = sbB("cand2", [128, 256], F32)
            best = sbB("best", [128, 8, 16], F32)
            posu = sbB("posu", [128, 8, 16], U32)
            posf = sbB("posf", [128, 128], F32)
            pa_f = sbB("pa_f", [128, 128], F32)
            pb_f = sbB("pb_f", [128, 128], F32)
            oh = sbB("oh", [128, 128, 16], F32)
            isel = sbB("isel", [128, 128], F32)
            jsel = sbB("jsel", [128, 128], F32)
            ids_f = sbB("ids_f", [128, 128], F32)
            ids_u = sbB("ids_u", [128, 128], U32)
            gat = sbB("gat", [128, 8, 16], F32)
            gsum = sbB("gsum", [128, 8], F32)
            dots = sbB("dots", [128, 128], F32)
            coef = sbB("coef", [128, 128], F32)
            NGB = 4
            gbuf = [sbB("gbuf%d" % i, [128, D], F32) for i in range(NGB)]
            djunk = sbB("djunk", [128, D], BF16)
            acc = sbB("acc", [128, D], F32)
            x2 = sbB("x2", [128, D], F32)
            h3 = sbB("h3", [128, D], BF16)
            h3T = sbB("h3T", [128, 8, 128], BF16)
            p_bf = sbB("p_bf", [128, 256], BF16)
            pTt = sbB("pTt", [128, 2, 128], BF16)
            pg_s = sbB("pg_s", [128, D], F32)
            gctr = [0]

            for n in range(nt):
                par = n % 2
                X1, X1K = x1b[par], 'x1b%d' % par
                ld('sp', X1[:], out_d[n * 128:(n + 1) * 128, :], [X1K], r=['out%d' % n])
                ld('sp', ptl[par][:], p_d[n * 128:(n + 1) * 128, :], ['ptl%d' % par])
                ck(102)
                rmsnorm(X1[:], X1K, g3[:, 0, :], 'g3_0', h2[:], 'h2')
                cp('pool', h2b[:], h2[:], ['h2'], ['h2b'])
                for c in range(8):
                    tr(ptb[:, c, :], h2b[:, c * 128:(c + 1) * 128], identb[:], ['h2b', 'identb'], ['ptb'])
                cp('act', h2T[:], ptb[:], ['ptb'], ['h2T'])
                for g4i in range(4):
                    bank = 2 + (g4i % 2)
                    for i in range(4):
                        m = g4i * 4 + i
                        for kc in range(8):
                            mm(pbk[bank][:, i * 128:(i + 1) * 128], wpq[:, kc, m * 128:(m + 1) * 128], h2T[:, kc, :], kc == 0, kc == 7, ['h2T'] + WPQ, [PB[bank]])
                    cp('act', qT[:, g4i * 4:g4i * 4 + 4, :].rearrange("p g t -> p (g t)"), pbk[bank][:], [PB[bank]], ['qT'])
                for g4i in range(4):
                    bank = 4 + (g4i % 2)
                    for i in range(4):
                        g = g4i * 4 + i
                        mm(pbk[bank][:, i * 128:(i + 1) * 128], qT[:, g, :], skT[:, g, :], True, True, ['qT', 'skT'], [PB[bank]])
                    cp('act', s_sb[:, g4i * 4:g4i * 4 + 4, :].rearrange("p g k -> p (g k)"), pbk[bank][:], [PB[bank]], ['s_sb'])
                ck(103)
                for g in range(16):
                    S.op('dve', lambda e, g=g: e.max(out=vals[:, g, 0:8], in_=s_sb[:, g, :]), r=['s_sb'], w=['vals'])
                    S.op('dve', lambda e, g=g: e.max_index(out=idxu[:, g, 0:8], in_max=vals[:, g, 0:8], in_values=s_sb[:, g, :]), r=['s_sb', 'vals'], w=['idxu'])
                    S.op('dve', lambda e, g=g: e.match_replace(out=s_rp[:], in_to_replace=vals[:, g, 0:8], in_values=s_sb[:, g, :], imm_value=-1e30), r=['s_sb', 'vals'], w=['s_rp'])
                    S.op('dve', lambda e, g=g: e.max(out=vals[:, g, 8:16], in_=s_rp[:]), r=['s_rp'], w=['vals'])
                    S.op('dve', lambda e, g=g: e.max_index(out=idxu[:, g, 8:16], in_max=vals[:, g, 8:16], in_values=s_rp[:]), r=['s_rp', 'vals'], w=['idxu'])
                cp('dve', idxf[:], idxu[:], ['idxu'], ['idxf'])
                v4 = vals[:].rearrange("p (h c) a -> p h c a", c=2)
                for hh in range(8):
                    tt('dve', cand[:, hh, :].rearrange("p (a b) -> p a b", b=16), bc(v4[:, hh, 0, :], 2, [128, 16, 16]), bc(v4[:, hh, 1, :], 1, [128, 16, 16]), ALU.add, ['vals'], ['cand'])
                for hh in range(8):
                    S.op('dve', lambda e, hh=hh: e.max(out=best[:, hh, 0:8], in_=cand[:, hh, :]), r=['cand'], w=['best'])
                    S.op('dve', lambda e, hh=hh: e.max_index(out=posu[:, hh, 0:8], in_max=best[:, hh, 0:8], in_values=cand[:, hh, :]), r=['cand', 'best'], w=['posu'])
                    S.op('dve', lambda e, hh=hh: e.match_replace(out=cand2[:], in_to_replace=best[:, hh, 0:8], in_values=cand[:, hh, :], imm_value=-1e30), r=['cand', 'best'], w=['cand2'])
                    S.op('dve', lambda e, hh=hh: e.max(out=best[:, hh, 8:16], in_=cand2[:]), r=['cand2'], w=['best'])
                    S.op('dve', lambda e, hh=hh: e.max_index(out=posu[:, hh, 8:16], in_max=best[:, hh, 8:16], in_values=cand2[:]), r=['cand2', 'best'], w=['posu'])
                ck(104)
                tt('dve', gat[:], best[:], bc(best[:, :, 0], 2, [128, 8, 16]), ALU.subtract, ['best'], ['gat'])
                act(gat[:], gat[:], AF.Exp, ['gat'], ['gat'])
                red(gsum[:], gat[:], ALU.add, ['gat'], ['gsum'])
                rcp(gsum[:], 'gsum')
                tt('dve', gat[:], gat[:], bc(gsum[:], 2, [128, 8, 16]), ALU.mult, ['gat', 'gsum'], ['gat'])
                cp('dve', posf[:], posu[:].rearrange("p h n -> p (h n)"), ['posu'], ['posf'])
                tt('dve', oh[:], bc(posf[:], 2, [128, 128, 16]), bc(IO16X, 1, [128, 128, 16]), ALU.is_ge, ['posf', 'cm'], ['oh'])
                red(pa_f[:], oh[:], ALU.add, ['oh'], ['pa_f'])
                ts('dve', pa_f[:], pa_f[:], -1.0, None, ALU.add, None, ['pa_f'], ['pa_f'])
                stt(pb_f[:], pa_f[:], -16.0, posf[:], ALU.mult, ALU.add, ['pa_f', 'posf'], ['pb_f'])
                i4 = idxf[:].rearrange("p (h c) a -> p h c a", c=2)
                for (pf, pk, cidx, dst, dk) in ((pa_f, 'pa_f', 0, isel, 'isel'), (pb_f, 'pb_f', 1, jsel, 'jsel')):
                    tt('dve', oh[:], bc(pf[:], 2, [128, 128, 16]), bc(IO16, 1, [128, 128, 16]), ALU.is_equal, [pk, 'cm'], ['oh'])
                    ohv = oh[:].rearrange("p (h n) a -> p h n a", n=16)
                    for hh in range(8):
                        tt('dve', ohv[:, hh, :, :], ohv[:, hh, :, :], bc(i4[:, hh, cidx, :], 1, [128, 16, 16]), ALU.mult, ['oh', 'idxf'], ['oh'])
                    red(dst[:], oh[:], ALU.add, ['oh'], [dk])
                stt(ids_f[:], isel[:], 128.0, jsel[:], ALU.mult, ALU.add, ['isel', 'jsel'], ['ids_f'])
                cp('dve', ids_u[:], ids_f[:], ['ids_f'], ['ids_u'])

                ck(105)
                def gather(tbl, col):
                    b = gctr[0] % NGB
                    gctr[0] += 1
                    S.dma('pool', lambda e, b=b, col=col: e.indirect_dma_start(
                        out=gbuf[b][:], out_offset=None, in_=tbl[:, :],
                        in_offset=bass.IndirectOffsetOnAxis(ap=ids_u[:, col:col + 1], axis=0)),
                        r=['ids_u'], w=['gbuf%d' % b])
                    return b
                for col in range(128):
                    b = gather(pu_d, col)
                    S.op('dve', lambda e, b=b, col=col: e.scalar_tensor_tensor(
                        out=djunk[:], in0=gbuf[b][:], scalar=1.0, in1=h2[:],
                        op0=ALU.mult, op1=ALU.mult, accum_out=dots[:, col:col + 1]),
                        r=['gbuf%d' % b, 'h2'], w=['djunk', 'dots'])
                ck(106)
                act(coef[:], dots[:], AF.Gelu_apprx_tanh, ['dots'], ['coef'])
                tt('dve', coef[:], coef[:], gat[:].rearrange("p h n -> p (h n)"), ALU.mult, ['coef', 'gat'], ['coef'])
                for col in range(128):
                    b = gather(pv_d, col)
                    if col == 0:
                        ts('dve', acc[:], gbuf[b][:], coef[:, 0:1], None, ALU.mult, None, ['gbuf%d' % b, 'coef'], ['acc'])
                    else:
                        stt(acc[:], gbuf[b][:], coef[:, col:col + 1], acc[:], ALU.mult, ALU.add, ['gbuf%d' % b, 'coef', 'acc'], ['acc'])
                ck(107)
                tt('dve', x2[:], acc[:], X1[:], ALU.add, ['acc', X1K], ['x2'])
                rmsnorm(x2[:], 'x2', g3[:, 1, :], 'g3_1', h3[:], 'h3')
                for c in range(8):
                    tr(ptb[:, c, :], h3[:, c * 128:(c + 1) * 128], identb[:], ['h3', 'identb'], ['ptb'])
                cp('act', h3T[:], ptb[:], ['ptb'], ['h3T'])
                cp('pool', p_bf[:], ptl[par][:], ['ptl%d' % par], ['p_bf'])
                for c in range(2):
                    tr(ptb[:, c, :], p_bf[:, c * 128:(c + 1) * 128], identb[:], ['p_bf', 'identb'], ['ptb'])
                cp('act', pTt[:], ptb[:, 0:2, :], ['ptb'], ['pTt'])
                for hf in range(2):
                    sl = slice(hf * 512, (hf + 1) * 512)
                    for c in range(8):
                        mm(pbk[hf][:], h3T[:, c, :], wpg[:, c, sl], c == 0, c == 7, ['h3T'] + WPG, [PB[hf]])
                    act(pg_s[:, sl], pbk[hf][:], AF.Sigmoid, [PB[hf]], ['pg_s'])
                    for c in range(2):
                        mm(pbk[2 + hf][:], pTt[:, c, :], wpu[:, c, sl], c == 0, c == 1, ['pTt'] + WPU, [PB[2 + hf]])
                    tt('dve', pg_s[:, sl], pg_s[:, sl], pbk[2 + hf][:], ALU.mult, ['pg_s', PB[2 + hf]], ['pg_s'])
                tt('dve', pg_s[:], x2[:], pg_s[:], ALU.add, ['x2', 'pg_s'], ['pg_s'])
                rmsnorm(pg_s[:], 'pg_s', g3[:, 2, :], 'g3_2', x2[:], 'x2')
                S.dma('act', lambda e, n=n: e.dma_start(out=out_d[n * 128:(n + 1) * 128, :], in_=x2[:]), r=['x2'], w=['out%d' % n])
          except _Stop:
            pass

        S.wait_all('sp')
        S.emit()
    return nc


def make_in_maps(inputs, nt, ncores):
    f = np.float32
    c = host_consts(nt)
    g = lambda k: np.asarray(inputs[k], dtype=f)
    S_ = nt * 128
    mu = g('rwkv_mu')[0]
    pp = np.zeros((128, 34), f)
    pp[:, 0:14] = mu.reshape(14, 128).T
    for j, k in enumerate(('rwkv_w0', 'rwkv_a0', 'rwkv_k_k', 'rwkv_k_a')):
        pp[:, 14 + 4 * j:18 + 4 * j] = g(k)[0].reshape(4, 128).T
    pp[:, 30:34] = g('rwkv_r_k')[0].reshape(4, 128).T
    g4 = np.stack([g('g_mix')[0], g('g_ffn')[0], g('g_ple')[0], g('g_final')], 0)
    gn3 = np.stack([g('ret_gn_g')[0], g('rwkv_gn_g')[0], g('rwkv_gn_b')[0]], 0)
    lora = np.zeros((128, 3, 512), f)
    lora[0:64, 0, :] = g('rwkv_w_up')[0]
    lora[64:128, 1, :] = g('rwkv_a_up')[0]
    lora[:, 2, :] = g('rwkv_g_up')[0]
    wbr = np.stack([g('w_ret_br')[0], g('w_rwkv_br')[0]], 0)
    shared = dict(w_in=g('w_in')[0], pp=pp, g4=np.ascontiguousarray(g4), gn3=np.ascontiguousarray(gn3),
                  lora=lora, wbr=np.ascontiguousarray(wbr), w_o=g('w_o')[0], w_pq=g('w_pq')[0],
                  sk=np.ascontiguousarray(g('peer_sub_keys')[0].reshape(16, 128, 128)),
                  peer_u=g('peer_u')[0], peer_v=g('peer_v')[0], w_ple_gate=g('w_ple_gate')[0],
                  w_ple_up=g('w_ple_up')[0], rot=c['rot'], DT=c['DT'], xiT=c['xiT'], CDb=c['CDb'], cm=c['cm'])
    x = g('x'); p = g('p')[0]
    maps = []
    for i in range(ncores):
        m = dict(shared)
        m['x'] = np.ascontiguousarray(x[i, :S_])
        m['p'] = np.ascontiguousarray(p[i, :S_])
        maps.append(m)
    return maps


def kernel(**inputs):
    nt = SEQ // 128
    nc = build(nt)
    in_maps = make_in_maps(inputs, nt, NCORES)
    res = run_bass_kernel_spmd(nc, in_maps, core_ids=list(range(NCORES)))
    out = np.stack([np.asarray(r["out"], dtype=np.float32) for r in res.results], axis=0)
    return out
```

```python
import math
import numpy as np
from contextlib import ExitStack
import concourse.bass as bass
import concourse.mybir as mybir
from concourse.bass_utils import run_bass_kernel_spmd

F32 = mybir.dt.float32
BF16 = mybir.dt.bfloat16
U32 = mybir.dt.uint32
I32 = mybir.dt.int32
AF = mybir.ActivationFunctionType
ALU = mybir.AluOpType
AX = mybir.AxisListType

D = 1024
SEQ = 4096
NCORES = 8
IN_COLS = 5888
C0 = math.exp(-0.5)


class Sched:
    SELF_SYNC = {'pe': False, 'act': True, 'dve': True, 'pool': True, 'sp': True}

    def __init__(self, nc, es, n_dma_sems=8):
        self.nc = nc
        self.ops = {e: [] for e in ('pe', 'act', 'dve', 'pool', 'sp')}
        self.sem = {e: es.enter_context(nc.semaphore('prog_' + e)) for e in self.ops}
        self.cnt = {e: 0 for e in self.ops}
        self.waited = {e: {} for e in self.ops}
        self.last_w = {}
        self.readers = {}
        self.dsem = {}
        self.dcnt = {}
        self.drr = {}
        for q in ('sp', 'act', 'pool'):
            self.dsem[q] = [es.enter_context(nc.semaphore('dma_%s_%d' % (q, i)))
                            for i in range(n_dma_sems)]
            self.dcnt[q] = [0] * n_dma_sems
            self.drr[q] = 0
        self.sem_id = {}
        self.pending = {e: [] for e in self.ops}

    def barrier(self):
        toks = list(self.last_w.values())
        for ts_ in self.readers.values():
            toks.extend(ts_)
        for e in self.ops:
            self.pending[e] = list(toks)

    def _deps(self, r, w):
        toks = []
        for k in r:
            t = self.last_w.get(k)
            if t is not None:
                toks.append(t)
        for k in w:
            t = self.last_w.get(k)
            if t is not None:
                toks.append(t)
            toks.extend(self.readers.get(k, ()))
        return toks

    def _waits(self, e, toks):
        need = {}
        for (sem, val, src) in toks:
            if src == e and not self.SELF_SYNC[e]:
                continue
            key = id(sem)
            self.sem_id[key] = sem
            if self.waited[e].get(key, 0) >= val:
                continue
            if need.get(key, 0) < val:
                need[key] = val
        out = []
        for key, val in need.items():
            self.waited[e][key] = val
            out.append((self.sem_id[key], val))
        return out

    def _commit(self, tok, r, w):
        for k in w:
            self.last_w[k] = tok
            self.readers[k] = []
        for k in r:
            if k in w:
                continue
            self.readers.setdefault(k, []).append(tok)

    def op(self, e, fn, r=(), w=()):
        r = list(r); w = list(w)
        toks = self._deps(r, w) + self.pending[e]
        self.pending[e] = []
        waits = self._waits(e, toks)
        self.cnt[e] += 1
        tok = (self.sem[e], self.cnt[e], e)
        self.ops[e].append((waits, fn, (self.sem[e], 1)))
        self._commit(tok, r, w)
        return tok

    def dma(self, q, fn, r=(), w=()):
        r = list(r); w = list(w)
        j = self.drr[q]
        self.drr[q] = (j + 1) % len(self.dsem[q])
        sem = self.dsem[q][j]
        toks = self._deps(r, w) + self.pending[q]
        self.pending[q] = []
        if self.dcnt[q][j] > 0:
            toks.append((sem, 16 * self.dcnt[q][j], None))
        waits = self._waits(q, toks)
        self.dcnt[q][j] += 1
        tok = (sem, 16 * self.dcnt[q][j], None)
        self.ops[q].append((waits, fn, (sem, 16)))
        self._commit(tok, r, w)
        return tok

    def wait_all(self, e):
        toks = list(self.last_w.values())
        for ts in self.readers.values():
            toks.extend(ts)
        waits = self._waits(e, toks)
        self.ops[e].append((waits, None, None))

    def emit(self):
        nc = self.nc
        with nc.Block() as block:
            def run(e, eng):
                for waits, fn, inc in self.ops[e]:
                    for sem, val in waits:
                        eng.wait_ge(sem, val)
                    if fn is not None:
                        ins = fn(eng)
                        ins.then_inc(inc[0], inc[1])

            @block.sync
            def _(eng):
                run('sp', eng)

            @block.scalar
            def _(eng):
                run('act', eng)

            @block.vector
            def _(eng):
                run('dve', eng)

            @block.gpsimd
            def _(eng):
                run('pool', eng)

            @block.tensor
            def _(eng):
                run('pe', eng)


def host_consts(nt):
    f = np.float32
    S = nt * 128
    half = 32
    inv_freq = (10000.0 ** (-np.arange(half, dtype=f) * f(2.0) / f(64))).astype(f)
    ang = (np.arange(S, dtype=f)[:, None] * inv_freq[None, :]).astype(f)
    cos = np.cos(ang).astype(f); sin = np.sin(ang).astype(f)
    rot = np.zeros((nt, 128, 128), f)
    rot[:, :, 0:32] = cos.reshape(nt, 128, 32)
    rot[:, :, 32:64] = sin.reshape(nt, 128, 32)
    rot[:, :, 64:96] = cos.reshape(nt, 128, 32) * f(0.125)
    rot[:, :, 96:128] = sin.reshape(nt, 128, 32) * f(0.125)
    H = 8
    lg = np.log1p(-(2.0 ** (-5.0 - np.arange(H, dtype=np.float64))))
    idx = np.arange(128, dtype=np.float64)
    diff = idx[None, :] - idx[:, None]
    DT = np.where(diff[:, None, :] >= 0, np.exp(lg[None, :, None] * np.maximum(diff, 0)[:, None, :]), 0.0).astype(f)
    xiT = np.zeros((128, 4, 128), f)
    CDb = np.zeros((128, 4, 64), f)
    for c in range(4):
        for p in range(128):
            h = 2 * c + p // 64
            xiT[p, c, :] = np.exp(lg[h] * (idx + 1.0))
            CDb[p, c, :] = np.exp(lg[h] * 128.0)
    ZT = np.exp(lg[None, :] * (127.0 - idx)[:, None]).astype(f)
    s = np.arange(128)
    SU = (s[None, :] > s[:, None]).astype(f)
    SUI = (s[None, :] >= s[:, None]).astype(f)
    SL = SU.T.copy()
    BO = np.zeros((128, 128), f); BO[:64, :64] = 1; BO[64:, 64:] = 1
    HS = np.zeros((128, 2), f); HS[:64, 0] = 1; HS[64:, 1] = 1
    ident = np.eye(128, dtype=f)
    io16 = np.tile(np.arange(16, dtype=f)[None, :], (128, 1))
    cm = np.concatenate([ident, SU, SUI, SL, BO, ZT, HS, io16, io16 * 16], axis=1)
    return dict(rot=rot, DT=DT.reshape(128, 1024), xiT=xiT.reshape(128, 512),
                CDb=CDb.reshape(128, 256), cm=np.ascontiguousarray(cm))


CM_W = 128 * 5 + 8 + 2 + 16 + 16


class _Stop(Exception):
    pass


def build(nt, stage='full', stop_after=None):
    nc = bass.Bass("TRN2", target_bir_lowering=False)
    S_ = nt * 128
    WDT = BF16
    dram = lambda n, s, d, k="ExternalInput": nc.dram_tensor(n, s, d, kind=k).ap()
    x_d = dram("x", [S_, D], F32)
    p_d = dram("p", [S_, 256], F32)
    w_in_d = dram("w_in", [D, IN_COLS], F32)
    pp_d = dram("pp", [128, 34], F32)
    g4_d = dram("g4", [4, D], F32)
    gn3_d = dram("gn3", [3, 512], F32)
    lora_d = dram("lora", [128, 3, 512], F32)
    wbr_d = dram("wbr", [2, 512, D], F32)
    wo_d = dram("w_o", [D, D], F32)
    wpq_d = dram("w_pq", [D, 2048], F32)
    sk_d = dram("sk", [16, 128, 128], F32)
    pu_d = dram("peer_u", [16384, D], F32)
    pv_d = dram("peer_v", [16384, D], F32)
    wpg_d = dram("w_ple_gate", [D, D], F32)
    wpu_d = dram("w_ple_up", [256, D], F32)
    rot_d = dram("rot", [nt, 128, 128], F32)
    DT_d = dram("DT", [128, 1024], F32)
    xiT_d = dram("xiT", [128, 512], F32)
    CDb_d = dram("CDb", [128, 256], F32)
    cm_d = dram("cm", [128, CM_W], F32)
    out_d = dram("out", [S_, D], F32, "ExternalOutput")
    winb_d = nc.dram_tensor("winb", [D, IN_COLS], BF16, kind="Internal").ap()
    utb_d = nc.dram_tensor("utb", [128, 128, D], BF16, kind="Internal").ap()
    vtb_d = nc.dram_tensor("vtb", [128, 128, D], BF16, kind="Internal").ap()
    wpqb_d = nc.dram_tensor("wpqb", [D, 2048], BF16, kind="Internal").ap()

    es = ExitStack()
    with es:
        S = Sched(nc, es)
        sb = lambda n, s, d: es.enter_context(nc.sbuf_tensor("sb_" + n, s, d))
        ps = lambda n, s, d: es.enter_context(nc.psum_tensor(n, s, d))

        def mm(out, lhsT, rhs, start, stop, r, w):
            S.op('pe', lambda e: e.matmul(out, lhsT=lhsT, rhs=rhs, start=start, stop=stop), r=r, w=w)

        def tr(out, in_, ident, r, w):
            S.op('pe', lambda e: e.transpose(out=out, in_=in_, identity=ident), r=r, w=w)

        def act(out, in_, func, r, w, **kw):
            S.op('act', lambda e: e.activation(out=out, in_=in_, func=func, **kw), r=r, w=w)

        def cp(eng, out, in_, r, w):
            if eng == 'act':
                S.op('act', lambda e: e.copy(out=out, in_=in_), r=r, w=w)
            else:
                S.op(eng, lambda e: e.tensor_copy(out=out, in_=in_), r=r, w=w)

        def tt(eng, out, in0, in1, op, r, w):
            S.op(eng, lambda e: e.tensor_tensor(out=out, in0=in0, in1=in1, op=op), r=r, w=w)

        def ts(eng, out, in0, s1, s2, op0, op1, r, w):
            if op1 is None:
                S.op(eng, lambda e: e.tensor_scalar(out=out, in0=in0, scalar1=s1, scalar2=None, op0=op0), r=r, w=w)
            else:
                S.op(eng, lambda e: e.tensor_scalar(out=out, in0=in0, scalar1=s1, scalar2=s2, op0=op0, op1=op1), r=r, w=w)

        def stt(out, in0, scalar, in1, op0, op1, r, w):
            S.op('dve', lambda e: e.scalar_tensor_tensor(out=out, in0=in0, scalar=scalar, in1=in1, op0=op0, op1=op1), r=r, w=w)

        def red(out, in_, op, r, w, axis=AX.X):
            S.op('dve', lambda e: e.tensor_reduce(out=out, in_=in_, axis=axis, op=op), r=r, w=w)

        def rcp(t, k):
            S.op('dve', lambda e: e.reciprocal(out=t, in_=t), r=[k], w=[k])

        def ld(q, out, in_, w, r=()):
            S.dma(q, lambda e: e.dma_start(out=out, in_=in_), r=r, w=w)

        def bc(ap, axis, shape):
            return ap.unsqueeze(axis).to_broadcast(shape)

        ptb = ps("ptb", [128, 8, 128], BF16)
        pbk = [ps("pb%d" % i, [128, 512], F32) for i in range(7)]
        PB = ['pb%d' % i for i in range(7)]

        cm = sb("cm", [128, CM_W], F32)
        ld('sp', cm[:], cm_d[:, :], ['cm'])
        identf = cm[:, 0:128]
        SU = cm[:, 128:256]; SUI = cm[:, 256:384]; SL = cm[:, 384:512]; BO = cm[:, 512:640]
        ZT = cm[:, 640:648]; HS = cm[:, 648:650]; IO16 = cm[:, 650:666]; IO16X = cm[:, 666:682]
        identb = sb("identb", [128, 128], BF16)
        cp('dve', identb[:], identf, ['cm'], ['identb'])
        eps_t = sb("eps_t", [128, 4], F32)
        S.op('dve', lambda e: e.memset(eps_t[:, 0:1], 1e-6), w=['eps'])
        S.op('dve', lambda e: e.memset(eps_t[:, 1:2], 1e-5), w=['eps'])
        S.op('dve', lambda e: e.memset(eps_t[:, 2:3], 64e-5), w=['eps'])
        sq_junk = sb("sq_junk", [128, D], BF16)
        rs_ss = sb("rs_ss", [128, 1], F32)
        rs_rstd = sb("rs_rstd", [128, 1], F32)

        def rmsnorm(src, src_key, gtab, gkey, dst, dst_key):
            act(sq_junk[:], src, AF.Square, [src_key], ['sq_junk', 'rs_ss'], accum_out=rs_ss[:])
            act(rs_rstd[:], rs_ss[:], AF.Sqrt, ['rs_ss', 'eps'], ['rs_rstd'], scale=1.0 / D, bias=eps_t[:, 0:1])
            rcp(rs_rstd[:], 'rs_rstd')
            stt(dst, src, rs_rstd[:, 0:1], gtab, ALU.mult, ALU.mult, [src_key, 'rs_rstd', gkey], [dst_key])

        def ck(k):
            if stop_after == k:
                raise _Stop()
        esA = ExitStack()
        with esA:
          try:
            sbA = lambda n, s, d: esA.enter_context(nc.sbuf_tensor("sa_" + n, s, d))
            pp = sbA("pp", [128, 34], F32)
            ld('sp', pp[:], pp_d[:, :], ['pp'])
            MU = pp[:, 0:14]; W0 = pp[:, 14:18]; A0 = pp[:, 18:22]; KK_ = pp[:, 22:26]; KA = pp[:, 26:30]; RK = pp[:, 30:34]
            omm = sbA("omm", [128, 14], F32)
            ts('dve', omm[:], MU, -1.0, 1.0, ALU.mult, ALU.add, ['pp'], ['omm'])
            omka = sbA("omka", [128, 4], F32)
            ts('dve', omka[:], KA, -1.0, 1.0, ALU.mult, ALU.add, ['pp'], ['omka'])
            gmix = sbA("gmix", [128, D], F32)
            ld('sp', gmix[:], g4_d[0, :].partition_broadcast(128), ['gmix'])
            gn3 = sbA("gn3", [128, 3, 512], F32)
            for i in range(3):
                ld('sp', gn3[:, i, :], gn3_d[i, :].partition_broadcast(128), ['gn3_%d' % i])

            NWB = 3
            wch = [sbA("wch%d" % i, [128, 8, 512], BF16) for i in range(NWB)]
            k = 0
            for kc in range(8):
                for cb in range(4):
                    b = k % NWB
                    stg = wch[b][:].rearrange("p a n -> p (a n)")[:, 0:1472]
                    S.dma('pool', lambda e, stg=stg, kc=kc, cb=cb: e.dma_start(out=stg, in_=w_in_d[kc * 128:(kc + 1) * 128, cb * 1472:(cb + 1) * 1472]), w=['wch%d' % b])
                    S.dma('sp', lambda e, stg=stg, kc=kc, cb=cb: e.dma_start(out=winb_d[kc * 128:(kc + 1) * 128, cb * 1472:(cb + 1) * 1472], in_=stg), r=['wch%d' % b], w=['winb'])
                    k += 1
            wbr = sbA("wbr", [128, 2, 4, D], BF16)
            for i in range(2):
                for c in range(4):
                    S.dma('pool', lambda e, i=i, c=c: e.dma_start(out=wbr[:, i, c, :], in_=wbr_d[i, c * 128:(c + 1) * 128, :]), w=['wbr%d%d' % (i, c)])
            wo = sbA("wo", [128, 8, D], BF16)
            for c in range(8):
                S.dma('pool', lambda e, c=c: e.dma_start(out=wo[:, c, :], in_=wo_d[c * 128:(c + 1) * 128, :]), w=['wo%d' % c])
            WBR = ['wbr%d%d' % (i, c) for i in range(2) for c in range(4)]
            WO = ['wo%d' % c for c in range(8)]
            lora = sbA("lora", [128, 3, 512], BF16)
            S.dma('pool', lambda e: e.dma_start(out=lora[:], in_=lora_d[:, :, :]), w=['lora'])
            DT = sbA("DT", [128, 8, 128], F32)
            ld('sp', DT[:].rearrange("p h i -> p (h i)"), DT_d[:, :], ['DT'])
            xiT = sbA("xiT", [128, 4, 128], F32)
            ld('sp', xiT[:].rearrange("p c i -> p (c i)"), xiT_d[:, :], ['xiT'])
            CDb = sbA("CDb", [128, 4, 64], F32)
            ld('sp', CDb[:].rearrange("p c i -> p (c i)"), CDb_d[:, :], ['CDb'])

            CH = [(0, 512), (512, 512), (1024, 512), (1536, 512),
                  (2048, 512), (2560, 512), (3072, 512), (3584, 256),
                  (3840, 512), (4352, 512), (4864, 512), (5376, 512)]
            wctr = [k]

            def load_chunk(ci):
                b = wctr[0] % NWB
                wctr[0] += 1
                c0, cw = CH[ci]
                S.dma('sp', lambda e, b=b, c0=c0, cw=cw: e.dma_start(
                    out=wch[b][:, :, 0:cw], in_=winb_d[:, c0:c0 + cw].rearrange("(kc p) n -> p kc n", p=128)),
                    r=['winb'], w=['wch%d' % b])
                return b

            xt = [sbA("xt%d" % i, [128, D], F32) for i in range(2)]
            rot_t = [sbA("rot%d" % i, [128, 128], F32) for i in range(2)]
            h = sbA("h", [128, D], BF16)
            hT = sbA("hT", [128, 8, 128], BF16)
            qk_rot = sbA("qk_rot", [128, 2, 512], BF16)
            rt = [sbA("rt%d" % i, [128, 8, 32], F32) for i in range(4)]
            v_tok = sbA("v_tok", [128, 512], BF16)
            gr_s = sbA("gr_s", [128, 512], BF16)
            gate_s = sbA("gate_s", [128, 2048], BF16)
            qkT = sbA("qkT", [128, 8, 128], BF16)
            qxT = sbA("qxT", [128, 4, 128], BF16)
            kz = sbA("kz", [128, 8, 64], BF16)
            PT = sbA("PT", [128, 8, 128], BF16)
            R32 = sbA("R32", [128, 4, 64], F32)
            Rb = sbA("Rb", [128, 4, 128], BF16)
            qTm = sbA("qTm", [128, 2, 4, 128], BF16)
            S.op('dve', lambda e: e.memset(qTm[:], 0.0), w=['qTm'])
            Rtmp = sbA("Rtmp", [128, 4, 64], F32)
            S.op('dve', lambda e: e.memset(R32[:], 0.0), w=['R32'])
            S.op('dve', lambda e: e.memset(Rb[:], 0.0), w=['Rb'])
            hn_sq = sbA("hn_sq", [128, 8, 64], F32)
            hn_c = sbA("hn_c", [128, 8, 64], F32)
            hn_s = sbA("hn_s", [128, 8], F32)
            hn_q = sbA("hn_q", [128, 8], F32)
            hn_m = sbA("hn_m", [128, 8], F32)
            hn_r = sbA("hn_r", [128, 8], F32)
            y_bf = sbA("y_bf", [128, 512], BF16)
            yT = sbA("yT", [128, 4, 128], BF16)
            ZB = sbA("ZB", [128, 14, 129], F32)
            zlast = sbA("zlast", [128, 14, 1], F32)
            S.op('dve', lambda e: e.memset(zlast[:], 0.0), w=['zlast'])
            zs = sbA("zs", [128, 14, 128], F32)
            lor_in = sbA("lor_in", [128, 2, 128], BF16)
            f4 = [sbA("f4_%d" % i, [128, 4, 128], F32) for i in range(8)]
            F4 = ['f4_%d' % i for i in range(8)]
            f4ones = sbA("f4ones", [128, 128], F32)
            S.op('dve', lambda e: e.memset(f4ones[:], 1.0), w=['f4ones'])
            wk = {n_: sbA("wk_" + n_, [128, 4, 128], WDT) for n_ in ('ab', 'rb', 'bb', 'kb', 'bt', 'kt', 'vT')}
            tok3 = sbA("tok3", [128, 3, 512], WDT)
            Am = {n_: sbA("Am_" + n_, [128, 4, 128], WDT) for n_ in ('akT', 'rbT', 'rkT')}
            Nb = [sbA("Nb%d" % i, [128, 4, 128], WDT) for i in range(2)]
            NTb = [sbA("NTb%d" % i, [128, 4, 128], WDT) for i in range(2)]
            Qb = [sbA("Qb%d" % i, [128, 4, 128], WDT) for i in range(2)]
            BOw = sbA("BOw", [128, 128], F32)
            cp('dve', BOw[:], BO, ['cm'], ['BOw'])
            Xs = sbA("Xs", [128, 256], WDT)
            Us = sbA("Us", [128, 512], WDT)
            ST32 = sbA("ST32", [128, 4, 64], F32)
            STb = sbA("STb", [128, 4, 128], WDT)
            wkm = {n_: sbA("wkm_" + n_, [128, 2, 4, 128], WDT) for n_ in ('ab', 'bb', 'rb')}
            for n_ in ('ab', 'bb', 'rb'):
                S.op('pool', lambda e, n_=n_: e.memset(wkm[n_][:], 0.0), w=['wkm_' + n_])
            STtmp = sbA("STtmp", [128, 4, 64], F32)
            S.op('dve', lambda e: e.memset(ST32[:], 0.0), w=['ST32'])
            S.op('dve', lambda e: e.memset(STb[:], 0.0), w=['STb'])
            PCt = sbA("PCt", [128, 4], F32)
            g_tok = sbA("g_tok", [128, 512], F32)
            cbt = sbA("cbt", [128, 8], F32)
            merged = sbA("merged", [128, D], BF16)
            mT = sbA("mT", [128, 8, 128], BF16)
            x1 = sbA("x1", [128, D], F32)

            def headnorm(ops_ap, ops_key, eps_col, dst_c):
                red(hn_s[:], ops_ap, ALU.add, [ops_key], ['hn_s'])
                act(hn_sq[:], ops_ap, AF.Square, [ops_key], ['hn_sq'])
                red(hn_q[:], hn_sq[:], ALU.add, ['hn_sq'], ['hn_q'])
                ts('dve', hn_m[:], hn_s[:], 1.0 / 64, None, ALU.mult, None, ['hn_s'], ['hn_m'])
                tt('dve', hn_r[:], hn_m[:], hn_m[:], ALU.mult, ['hn_m'], ['hn_r'])
                stt(hn_r[:], hn_q[:], 1.0 / 64, hn_r[:], ALU.mult, ALU.subtract, ['hn_q', 'hn_r'], ['hn_r'])
                act(hn_r[:], hn_r[:], AF.Sqrt, ['hn_r', 'eps'], ['hn_r'], bias=eps_t[:, eps_col:eps_col + 1])
                rcp(hn_r[:], 'hn_r')
                tt('dve', dst_c, ops_ap, bc(hn_m[:], 2, [128, 8, 64]), ALU.subtract, [ops_key, 'hn_m'], ['hn_c'])
                tt('dve', dst_c, dst_c, bc(hn_r[:], 2, [128, 8, 64]), ALU.mult, ['hn_c', 'hn_r'], ['hn_c'])

            ck(1)
            for n in range(nt):
                par = n % 2
                X, XK = xt[par], 'xt%d' % par
                ld('sp', X[:], x_d[n * 128:(n + 1) * 128, :], [XK])
                ld('sp', rot_t[par][:], rot_d[n, :, :], ['rot%d' % par])
                ROT = 'rot%d' % par
                rmsnorm(X[:], XK, gmix[:], 'gmix', h[:], 'h')
                for c in range(8):
                    tr(ptb[:, c, :], h[:, c * 128:(c + 1) * 128], identb[:], ['h', 'identb'], ['ptb'])
                cp('act', hT[:], ptb[:], ['ptb'], ['hT'])

                ck(2)
                def inproj_tok(ci, bank):
                    b = load_chunk(ci)
                    for kc in range(8):
                        mm(pbk[bank][:], hT[:, kc, :], wch[b][:, kc, :], kc == 0, kc == 7, ['hT', 'wch%d' % b], [PB[bank]])

                Cc = rot_t[par][:, 0:32]; Sc = rot_t[par][:, 32:64]
                kCc = rot_t[par][:, 64:96]; kSc = rot_t[par][:, 96:128]
                for qi in range(2):
                    bank = qi
                    inproj_tok(qi, bank)
                    pv = pbk[bank][:].rearrange("p (h two f) -> p h two f", two=2, f=32)
                    q1 = pv[:, :, 0, :]; q2 = pv[:, :, 1, :]
                    cc, ssn = (Cc, Sc) if qi == 0 else (kCc, kSc)
                    cb_ = bc(cc, 1, [128, 8, 32]); sb_ = bc(ssn, 1, [128, 8, 32])
                    ov = qk_rot[:, qi, :].rearrange("p (h two f) -> p h two f", two=2, f=32)
                    tt('dve', rt[0][:], q1, cb_, ALU.mult, [PB[bank], ROT], ['rt0'])
                    tt('dve', rt[1][:], q2, sb_, ALU.mult, [PB[bank], ROT], ['rt1'])
                    tt('dve', rt[2][:], q1, sb_, ALU.mult, [PB[bank], ROT], ['rt2'])
                    tt('dve', rt[3][:], q2, cb_, ALU.mult, [PB[bank], ROT], ['rt3'])
                    tt('pool', ov[:, :, 0, :], rt[0][:], rt[1][:], ALU.subtract, ['rt0', 'rt1'], ['qk_rot'])
                    tt('pool', ov[:, :, 1, :], rt[2][:], rt[3][:], ALU.add, ['rt2', 'rt3'], ['qk_rot'])
                inproj_tok(2, 2)
                cp('act', v_tok[:], pbk[2][:], [PB[2]], ['v_tok'])
                inproj_tok(3, 3)
                act(gr_s[:], pbk[3][:], AF.Silu, [PB[3]], ['gr_s'])

                ck(3)
                for c in range(8):
                    tr(ptb[:, c, :], qk_rot[:, c // 4, (c % 4) * 128:(c % 4 + 1) * 128], identb[:], ['qk_rot', 'identb'], ['ptb'])
                cp('act', qkT[:, 4:8, :], ptb[:, 4:8, :], ['ptb'], ['qkT'])
                cp('act', qTm[0:64, 0, :, :], ptb[0:64, 0:4, :], ['ptb'], ['qTm'])
                cp('act', qTm[64:128, 1, :, :], ptb[64:128, 0:4, :], ['ptb'], ['qTm'])
                tt('dve', qxT[:], ptb[:, 0:4, :], xiT[:], ALU.mult, ['ptb', 'xiT'], ['qxT'])
                tt('pool', kz[:], qk_rot[:, 1, :].rearrange("p (h d) -> p h d", d=64), bc(ZT, 2, [128, 8, 64]), ALU.mult, ['qk_rot', 'cm'], ['kz'])

                ck(31)
                for hh in range(8):
                    c, base = hh // 2, (hh % 2) * 64
                    bank = 4 + hh // 4
                    mm(pbk[bank][:, (hh % 4) * 128:(hh % 4 + 1) * 128], qkT[:, 4 + c, :], qTm[:, hh % 2, c, :], True, True, ['qkT', 'qTm'], [PB[bank]])
                for g in range(2):
                    tt('dve', PT[:, 4 * g:4 * g + 4, :], pbk[4 + g][:].rearrange("p (h i) -> p h i", i=128), DT[:, 4 * g:4 * g + 4, :], ALU.mult, [PB[4 + g], 'DT'], ['PT'])
                ck(32)
                for c in range(4):
                    mm(pbk[6][:, c * 128:(c + 1) * 128], qxT[:, c, :], Rb[:, c, :], True, False, ['qxT', 'Rb'], [PB[6]])
                    for w_ in range(2):
                        hh = 2 * c + w_
                        mm(pbk[6][:, hh * 64:(hh + 1) * 64], PT[:, hh, :], v_tok[:, hh * 64:(hh + 1) * 64], False, w_ == 1, ['PT', 'v_tok'], [PB[6]])
                ck(33)
                for c in range(4):
                    mm(pbk[4][:, c * 128:(c + 1) * 128], kz[:, 2 * c:2 * c + 2, :].rearrange("p a d -> p (a d)"), v_tok[:, c * 128:(c + 1) * 128], True, True, ['kz', 'v_tok'], [PB[4]])
                tt('pool', Rtmp[:], R32[:], CDb[:], ALU.mult, ['R32', 'CDb'], ['Rtmp'])
                p4v = pbk[4][:].rearrange("p (c x) -> p c x", x=128)
                tt('dve', R32[0:64, :, :], Rtmp[0:64, :, :], p4v[0:64, :, 0:64], ALU.add, ['Rtmp', PB[4]], ['R32'])
                tt('dve', R32[64:128, :, :], Rtmp[64:128, :, :], p4v[64:128, :, 64:128], ALU.add, ['Rtmp', PB[4]], ['R32'])
                cp('pool', Rb[0:64, :, 0:64], R32[0:64, :, :], ['R32'], ['Rb'])
                cp('pool', Rb[64:128, :, 64:128], R32[64:128, :, :], ['R32'], ['Rb'])
                ck(34)
                o3 = pbk[6][:].rearrange("p (h e) -> p h e", e=64)
                headnorm(o3, PB[6], 1, hn_c[:])
                hc2 = hn_c[:].rearrange("p h e -> p (h e)")
                tt('dve', hc2, hc2, gn3[:, 0, :], ALU.mult, ['hn_c', 'gn3_0'], ['hn_c'])
                tt('dve', y_bf[:], hc2, gr_s[:], ALU.mult, ['hn_c', 'gr_s'], ['y_bf'])
                ck(35)
                for c in range(4):
                    tr(ptb[:, c, :], y_bf[:, c * 128:(c + 1) * 128], identb[:], ['y_bf', 'identb'], ['ptb'])
                cp('act', yT[:], ptb[:, 0:4, :], ['ptb'], ['yT'])
                for hf in range(2):
                    for c in range(4):
                        mm(pbk[hf][:], yT[:, c, :], wbr[:, 0, c, hf * 512:(hf + 1) * 512], c == 0, c == 3, ['yT'] + WBR, [PB[hf]])

                ck(4)
                for j in range(4):
                    b = load_chunk(4 + j)
                    nm = 4 if j < 3 else 2
                    bank = 2 + (j % 2)
                    for m in range(nm):
                        for kc in range(8):
                            mm(pbk[bank][:, m * 128:(m + 1) * 128], wch[b][:, kc, m * 128:(m + 1) * 128], hT[:, kc, :], kc == 0, kc == 7, ['hT', 'wch%d' % b], [PB[bank]])
                    cp('act', ZB[:, 4 * j:4 * j + nm, 1:129], pbk[bank][:, 0:nm * 128].rearrange("p (m t) -> p m t", t=128), [PB[bank]], ['ZB'])
                cp('pool', ZB[:, :, 0:1], zlast[:], ['zlast'], ['ZB'])
                for (m0, m1) in ((0, 4), (4, 8), (8, 12), (12, 14)):
                    nm = m1 - m0
                    tmp = f4[7][:, 0:nm, :]
                    tt('pool', tmp, ZB[:, m0:m1, 0:128], bc(MU[:, m0:m1], 2, [128, nm, 128]), ALU.mult, ['ZB', 'pp'], [F4[7]])
                    tt('dve', zs[:, m0:m1, :], ZB[:, m0:m1, 1:129], bc(omm[:, m0:m1], 2, [128, nm, 128]), ALU.mult, ['ZB', 'omm'], ['zs'])
                    tt('dve', zs[:, m0:m1, :], zs[:, m0:m1, :], tmp, ALU.add, ['zs', F4[7]], ['zs'])
                cp('pool', zlast[:], ZB[:, :, 128:129], ['ZB'], ['zlast'])
                ck(5)
                rF = zs[:, 0:4, :]; krF = zs[:, 4:8, :]; vF = zs[:, 8:12, :]
                act(lor_in[0:64, 0, :], zs[0:64, 12, :], AF.Tanh, ['zs'], ['lor_in'])
                cp('act', lor_in[64:128, 0, :], zs[64:128, 12, :], ['zs'], ['lor_in'])
                act(lor_in[:, 1, :], zs[:, 13, :], AF.Sigmoid, ['zs'], ['lor_in'])
                for m in range(4):
                    mm(pbk[2][:, m * 128:(m + 1) * 128], lora[:, 0, m * 128:(m + 1) * 128], lor_in[:, 0, :], True, True, ['lora', 'lor_in'], [PB[2]])
                for m in range(4):
                    mm(pbk[3][:, m * 128:(m + 1) * 128], lora[:, 1, m * 128:(m + 1) * 128], lor_in[:, 0, :], True, True, ['lora', 'lor_in'], [PB[3]])
                mm(pbk[4][:], lor_in[:, 1, :], lora[:, 2, :], True, True, ['lora', 'lor_in'], [PB[4]])
                cp('act', g_tok[:], pbk[4][:], [PB[4]], ['g_tok'])
                sg, asig, kkF, kkn, kpr, bF, csF, tmpF = f4
                for m in range(4):
                    act(sg[:, m, :], pbk[2][:, m * 128:(m + 1) * 128], AF.Sigmoid, [PB[2], 'pp'], [F4[0]], bias=W0[:, m:m + 1])
                for m in range(4):
                    act(asig[:, m, :], pbk[3][:, m * 128:(m + 1) * 128], AF.Sigmoid, [PB[3], 'pp'], [F4[1]], bias=A0[:, m:m + 1])
                b4 = lambda t: bc(t, 2, [128, 4, 128])
                f2 = lambda t: t[:].rearrange("p m t -> p (m t)")
                tt('dve', kkF[:], krF, b4(KK_), ALU.mult, ['zs', 'pp'], [F4[2]])
                tt('pool', tmpF[:], kkF[:], kkF[:], ALU.mult, [F4[2]], [F4[7]])
                mm(pbk[2][:], BOw[:], f2(tmpF), True, True, ['BOw', F4[7]], [PB[2]])
                act(f2(kkn), pbk[2][:], AF.Sqrt, [PB[2]], [F4[3]])
                ts('dve', kkn[:], kkn[:], 1e-12, None, ALU.max, None, [F4[3]], [F4[3]])
                rcp(kkn[:], F4[3])
                tt('dve', kkn[:], kkn[:], kkF[:], ALU.mult, [F4[3], F4[2]], [F4[3]])
                tt('pool', kpr[:], asig[:], b4(KA), ALU.mult, [F4[1], 'pp'], [F4[4]])
                tt('pool', kpr[:], kpr[:], b4(omka[:]), ALU.add, [F4[4], 'omka'], [F4[4]])
                tt('dve', kpr[:], kpr[:], krF, ALU.mult, [F4[4], 'zs'], [F4[4]])
                tt('pool', bF[:], kkn[:], asig[:], ALU.mult, [F4[3], F4[1]], [F4[5]])
                tt('pool', tmpF[:], rF, kpr[:], ALU.mult, ['zs', F4[4]], [F4[7]])
                tt('pool', tmpF[:], tmpF[:], b4(RK), ALU.mult, [F4[7], 'pp'], [F4[7]])
                for c in range(4):
                    mm(pbk[3][:, 2 * c:2 * c + 2], tmpF[:, c, :], HS, True, True, [F4[7], 'cm'], [PB[3]])
                cp('act', cbt[:], pbk[3][:, 0:8], [PB[3]], ['cbt'])
                for m in range(4):
                    S.op('dve', lambda e, m=m: e.tensor_tensor_scan(out=csF[:, m, :], data0=f4ones[:], data1=sg[:, m, :], initial=0.0, op0=ALU.mult, op1=ALU.add), r=[F4[0], 'f4ones'], w=[F4[6]])
                E1, E2 = kkF, tmpF
                act(E1[:], csF[:], AF.Exp, [F4[6]], [F4[2]], scale=-C0)
                act(E2[:], csF[:], AF.Exp, [F4[6]], [F4[7]], scale=C0)
                tt('dve', csF[:], csF[:], sg[:], ALU.subtract, [F4[6], F4[0]], [F4[6]])
                act(sg[:], csF[:], AF.Exp, [F4[6]], [F4[0]], scale=-C0)
                E3 = sg
                cp('dve', PCt[:], E1[:, :, 127], [F4[2]], ['PCt'])
                stt(wk['ab'][:], kkn[:], -1.0, E3[:], ALU.mult, ALU.mult, [F4[3], F4[0]], ['wk_ab'])
                tt('dve', wk['rb'][:], rF, E1[:], ALU.mult, ['zs', F4[2]], ['wk_rb'])
                tt('pool', csF[:], bF[:], E2[:], ALU.mult, [F4[5], F4[7]], [F4[6]])
                cp('act', wk['bb'][:], csF[:], [F4[6]], ['wk_bb'])
                tt('pool', wk['bt'][:], csF[:], bc(PCt[:], 2, [128, 4, 128]), ALU.mult, [F4[6], 'PCt'], ['wk_bt'])
                tt('dve', bF[:], kpr[:], E2[:], ALU.mult, [F4[4], F4[7]], [F4[5]])
                cp('act', wk['kb'][:], bF[:], [F4[5]], ['wk_kb'])
                tt('pool', wk['kt'][:], bF[:], bc(PCt[:], 2, [128, 4, 128]), ALU.mult, [F4[5], 'PCt'], ['wk_kt'])
                cp('act', wk['vT'][:], vF, ['zs'], ['wk_vT'])
                for n_ in ('ab', 'bb', 'rb'):
                    cp('pool', wkm[n_][0:64, 0, :, :], wk[n_][0:64, :, :], ['wk_' + n_], ['wkm_' + n_])
                    cp('pool', wkm[n_][64:128, 1, :, :], wk[n_][64:128, :, :], ['wk_' + n_], ['wkm_' + n_])
                ck(6)
                for i, nmk in enumerate(('bt', 'kt', 'vT')):
                    for c in range(4):
                        tr(ptb[:, c, :], wk[nmk][:, c, :], identb[:], ['wk_' + nmk, 'identb'], ['ptb'])
                    cp('act', tok3[:, i, :].rearrange("p (c x) -> p c x", x=128), ptb[:, 0:4, :], ['ptb'], ['tok3_%d' % i])
                Btok = tok3[:, 0, :]; Ktok = tok3[:, 1, :]; Vtok = tok3[:, 2, :]

                ck(7)
                def hop(hh):
                    return hh // 2, (hh % 2) * 64
                for g in range(2):
                    hs = [4 * g + i for i in range(4)]
                    specs = [('bb', 'ab', SU, Nb[0], 'Nb0'), ('ab', 'bb', SL, NTb[0], 'NTb0'),
                             ('kb', 'ab', SU, Am['akT'], 'Am_akT'), ('bb', 'rb', SUI, Am['rbT'], 'Am_rbT'),
                             ('kb', 'rb', SUI, Am['rkT'], 'Am_rkT')]
                    for si, (l_, r_, msk, dst, dk) in enumerate(specs):
                        bank = 2 + (si % 3)
                        for i, hh in enumerate(hs):
                            c, base = hop(hh)
                            mm(pbk[bank][:, i * 128:(i + 1) * 128], wk[l_][:, c, :], wkm[r_][:, hh % 2, c, :], True, True, ['wk_' + l_, 'wkm_' + r_], [PB[bank]])
                        tt('dve', dst[:], pbk[bank][:].rearrange("p (h t) -> p h t", t=128), bc(msk, 1, [128, 4, 128]), ALU.mult, [PB[bank], 'cm'], [dk])
                    tt('pool', Qb[0][:], Nb[0][:], bc(identb[:], 1, [128, 4, 128]), ALU.add, ['Nb0', 'identb'], ['Qb0'])
                    cur = 0
                    for lvl in range(6):
                        nx = 1 - cur
                        last = (lvl == 5)
                        if not last:
                            for i in range(4):
                                mm(pbk[2][:, i * 128:(i + 1) * 128], NTb[cur][:, i, :], Nb[cur][:, i, :], True, True, ['NTb%d' % cur, 'Nb%d' % cur], [PB[2]])
                        for i in range(4):
                            mm(pbk[3][:, i * 128:(i + 1) * 128], Nb[cur][:, i, :], NTb[cur][:, i, :], True, True, ['NTb%d' % cur, 'Nb%d' % cur], [PB[3]])
                        if not last:
                            cp('dve', Nb[nx][:].rearrange("p h t -> p (h t)"), pbk[2][:], [PB[2]], ['Nb%d' % nx])
                        cp('act', NTb[nx][:].rearrange("p h t -> p (h t)"), pbk[3][:], [PB[3]], ['NTb%d' % nx])
                        for i in range(4):
                            mm(pbk[4][:, i * 128:(i + 1) * 128], identb[:], Qb[cur][:, i, :], True, False, ['identb', 'Qb%d' % cur], [PB[4]])
                            mm(pbk[4][:, i * 128:(i + 1) * 128], NTb[nx][:, i, :], Qb[cur][:, i, :], False, True, ['NTb%d' % nx, 'Qb%d' % cur], [PB[4]])
                        cp('act' if lvl % 2 else 'dve', Qb[nx][:].rearrange("p h t -> p (h t)"), pbk[4][:], [PB[4]], ['Qb%d' % nx])
                        cur = nx
                    Qf, QK = Qb[cur], 'Qb%d' % cur
                    for ci in range(2):
                        c = 2 * g + ci
                        mm(pbk[5][:, ci * 128:(ci + 1) * 128], wk['ab'][:, c, :], STb[:, c, :], True, False, ['wk_ab', 'STb'], [PB[5]])
                        for w_ in range(2):
                            i = 2 * ci + w_
                            hh = hs[i]
                            mm(pbk[5][:, i * 64:(i + 1) * 64], Am['akT'][:, i, :], Vtok[:, hh * 64:(hh + 1) * 64], False, w_ == 1, ['Am_akT', 'tok3_2'], [PB[5]])
                    cp('act', Xs[:], pbk[5][:, 0:256], [PB[5]], ['Xs'])
                    for i, hh in enumerate(hs):
                        oc = slice(i * 64, (i + 1) * 64)
                        mm(pbk[5][:, 256 + i * 64:256 + (i + 1) * 64], Qf[:, i, :], Xs[:, oc], True, True, [QK, 'Xs'], [PB[5]])
                    cp('act', Us[:, g * 256:(g + 1) * 256], pbk[5][:, 256:512], [PB[5]], ['Us'])
                    for ci in range(2):
                        c = 2 * g + ci
                        mm(pbk[6][:, c * 128:(c + 1) * 128], wk['rb'][:, c, :], STb[:, c, :], True, False, ['wk_rb', 'STb'], [PB[6]])
                        for w_ in range(2):
                            i = 2 * ci + w_
                            hh = hs[i]
                            hc = slice(hh * 64, (hh + 1) * 64)
                            mm(pbk[6][:, hc], Am['rkT'][:, i, :], Vtok[:, hc], False, False, ['Am_rkT', 'tok3_2'], [PB[6]])
                            mm(pbk[6][:, hc], Am['rbT'][:, i, :], Us[:, hc], False, w_ == 1, ['Am_rbT', 'Us'], [PB[6]])
                for c in range(4):
                    cs_ = slice(c * 128, (c + 1) * 128)
                    mm(pbk[5][:, cs_], Btok[:, cs_], Us[:, cs_], True, False, ['tok3_0', 'Us'], [PB[5]])
                    mm(pbk[5][:, cs_], Ktok[:, cs_], Vtok[:, cs_], False, True, ['tok3_1', 'tok3_2'], [PB[5]])
                tt('pool', STtmp[:], ST32[:], bc(PCt[:], 2, [128, 4, 64]), ALU.mult, ['ST32', 'PCt'], ['STtmp'])
                p5v = pbk[5][:].rearrange("p (c x) -> p c x", x=128)
                tt('dve', ST32[0:64, :, :], STtmp[0:64, :, :], p5v[0:64, :, 0:64], ALU.add, ['STtmp', PB[5]], ['ST32'])
                tt('dve', ST32[64:128, :, :], STtmp[64:128, :, :], p5v[64:128, :, 64:128], ALU.add, ['STtmp', PB[5]], ['ST32'])
                cp('pool', STb[0:64, :, 0:64], ST32[0:64, :, :], ['ST32'], ['STb'])
                cp('pool', STb[64:128, :, 64:128], ST32[64:128, :, :], ['ST32'], ['STb'])
                ck(8)
                o3 = pbk[6][:].rearrange("p (h e) -> p h e", e=64)
                headnorm(o3, PB[6], 2, hn_c[:])
                hc2 = hn_c[:].rearrange("p h e -> p (h e)")
                tt('dve', hc2, hc2, gn3[:, 1, :], ALU.mult, ['hn_c', 'gn3_1'], ['hn_c'])
                tt('dve', hc2, hc2, gn3[:, 2, :], ALU.add, ['hn_c', 'gn3_2'], ['hn_c'])
                tt('pool', hn_sq[:], Vtok.rearrange("p (h e) -> p h e", e=64), bc(cbt[:], 2, [128, 8, 64]), ALU.mult, ['tok3_2', 'cbt'], ['hn_sq'])
                tt('dve', hc2, hc2, hn_sq[:].rearrange("p h e -> p (h e)"), ALU.add, ['hn_c', 'hn_sq'], ['hn_c'])
                tt('dve', y_bf[:], hc2, g_tok[:], ALU.mult, ['hn_c', 'g_tok'], ['y_bf'])
                for c in range(4):
                    tr(ptb[:, c, :], y_bf[:, c * 128:(c + 1) * 128], identb[:], ['y_bf', 'identb'], ['ptb'])
                cp('act', yT[:], ptb[:, 0:4, :], ['ptb'], ['yT'])
                for hf in range(2):
                    for c in range(4):
                        mm(pbk[2 + hf][:], yT[:, c, :], wbr[:, 1, c, hf * 512:(hf + 1) * 512], c == 0, c == 3, ['yT'] + WBR, [PB[2 + hf]])

                ck(9)
                for gi in range(4):
                    bank = 4 + (gi % 2)
                    inproj_tok(8 + gi, bank)
                    act(gate_s[:, gi * 512:(gi + 1) * 512], pbk[bank][:], AF.Sigmoid, [PB[bank]], ['gate_s'])
                for hf in range(2):
                    sl = slice(hf * 512, (hf + 1) * 512)
                    tt('dve', x1[:, sl], pbk[hf][:], gate_s[:, sl], ALU.mult, [PB[hf], 'gate_s'], ['x1'])
                    tt('dve', merged[:, sl], pbk[2 + hf][:], gate_s[:, 1024 + hf * 512:1024 + (hf + 1) * 512], ALU.mult, [PB[2 + hf], 'gate_s'], ['merged'])
                    tt('pool', merged[:, sl], merged[:, sl], x1[:, sl], ALU.add, ['merged', 'x1'], ['merged'])
                for c in range(8):
                    tr(ptb[:, c, :], merged[:, c * 128:(c + 1) * 128], identb[:], ['merged', 'identb'], ['ptb'])
                cp('act', mT[:], ptb[:], ['ptb'], ['mT'])
                for hf in range(2):
                    for c in range(8):
                        mm(pbk[4 + hf][:], mT[:, c, :], wo[:, c, hf * 512:(hf + 1) * 512], c == 0, c == 7, ['mT'] + WO, [PB[4 + hf]])
                    tt('dve', x1[:, hf * 512:(hf + 1) * 512], pbk[4 + hf][:], X[:, hf * 512:(hf + 1) * 512], ALU.add, [PB[4 + hf], XK], ['x1'])
                S.dma('act', lambda e, n=n: e.dma_start(out=out_d[n * 128:(n + 1) * 128, :], in_=x1[:]), r=['x1'], w=['out%d' % n])
          except _Stop:
            pass

        if stage == 'A':
            S.wait_all('sp')
            S.emit()
            return nc
        S.barrier()

        esB = ExitStack()
        with esB:
          try:
            sbB = lambda n, s, d: esB.enter_context(nc.sbuf_tensor("sc_" + n, s, d))
            ptb2 = pbk[6][:].bitcast(BF16).rearrange("p (a b) -> p a b", b=128)
            PTB = [(ptb, 'ptb'), (ptb2, PB[6])]
            g3 = sbB("g3", [128, 3, D], F32)
            for i in range(3):
                ld('sp', g3[:, i, :], g4_d[1 + i, :].partition_broadcast(128), ['g3_%d' % i])
            wpg = sbB("wpg", [128, 8, D], BF16)
            for c in range(8):
                S.dma('pool', lambda e, c=c: e.dma_start(out=wpg[:, c, :], in_=wpg_d[c * 128:(c + 1) * 128, :]), w=['wpg%d' % c])
            WPG = ['wpg%d' % c for c in range(8)]
            wpu = sbB("wpu", [128, 2, D], BF16)
            for c in range(2):
                S.dma('pool', lambda e, c=c: e.dma_start(out=wpu[:, c, :], in_=wpu_d[c * 128:(c + 1) * 128, :]), w=['wpu%d' % c])
            WPU = ['wpu0', 'wpu1']
            skT = sbB("skT", [128, 16, 128], F32)
            s_sb = sbB("s_sb", [128, 16, 128], F32)
            for g in range(16):
                ld('sp', s_sb[:, g, :], sk_d[g, :, :], ['s_sb'])
            for g4i in range(4):
                bank = g4i % 2
                for i in range(4):
                    g = g4i * 4 + i
                    tr(pbk[bank][:, i * 128:(i + 1) * 128], s_sb[:, g, :], identf, ['s_sb', 'cm'], [PB[bank]])
                cp('act', skT[:, g4i * 4:g4i * 4 + 4, :].rearrange("p g k -> p (g k)"), pbk[bank][:], [PB[bank]], ['skT'])

            NSB = 2
            ubuf = [sbB("ubuf%d" % i, [128, 4, D], BF16) for i in range(NSB)]
            vbuf = [sbB("vbuf%d" % i, [128, 4, D], BF16) for i in range(NSB)]
            wqb = [sbB("wqb%d" % i, [128, 8, 256], BF16) for i in range(2)]

            k = 0
            for kc in range(8):
                for hf in range(2):
                    b = k % 2
                    stg = wqb[b][:].rearrange("p a n -> p (a n)")[:, 0:1024]
                    S.dma('pool', lambda e, stg=stg, kc=kc, hf=hf: e.dma_start(out=stg, in_=wpq_d[kc * 128:(kc + 1) * 128, hf * 1024:(hf + 1) * 1024]), w=['wqb%d' % b])
                    S.dma('sp', lambda e, stg=stg, kc=kc, hf=hf: e.dma_start(out=wpqb_d[kc * 128:(kc + 1) * 128, hf * 1024:(hf + 1) * 1024], in_=stg), r=['wqb%d' % b], w=['wpqb'])
                    k += 1
            pu_v = pu_d.rearrange("(i j) d -> j i d", j=128)
            pv_v = pv_d.rearrange("(i j) d -> j i d", j=128)
            for j in range(128):
                b = j % NSB
                ust = ubuf[b][:, 0, :]; uT = ubuf[b][:, 1, :]; vst = ubuf[b][:, 2, :]
                k0, k1, k2 = 'ub%d_0' % b, 'ub%d_1' % b, 'ub%d_2' % b
                S.dma('pool', lambda e, ust=ust, j=j: e.dma_start(out=ust, in_=pu_v[j, :, :]), w=[k0])
                pt_, pk_ = PTB[j % 2]
                for kc in range(8):
                    tr(pt_[:, kc, :], ust[:, kc * 128:(kc + 1) * 128], identb[:], [k0, 'identb'], [pk_])
                cp('act' if j % 2 else 'dve', uT.rearrange("p (a b) -> p a b", b=128), pt_[:, :, :], [pk_], [k1])
                S.dma('sp', lambda e, uT=uT, j=j: e.dma_start(out=utb_d[j, :, :], in_=uT), r=[k1], w=['utb'])
                S.dma('pool', lambda e, vst=vst, j=j: e.dma_start(out=vst, in_=pv_v[j, :, :]), w=[k2])
                S.dma('act', lambda e, vst=vst, j=j: e.dma_start(out=vtb_d[j, :, :], in_=vst), r=[k2], w=['vtb'])
            S.barrier()
            ck(101)

            x1b = [sbB("x1b0", [128, D], F32)] * 2
            ptl = [sbB("ptl%d" % i, [128, 256], F32) for i in range(2)]
            h2b = sbB("h2b", [128, D], BF16)
            h2T = sbB("h2T", [128, 8, 128], BF16)
            qc = sbB("qc", [128, 2048], F32)
            qT = qc[:].rearrange("p (g t) -> p g t", t=128)
            cand = qc[:].rearrange("p (h x) -> p h x", x=256)
            s_rp = sbB("s_rp", [128, 128], F32)
            vals = sbB("vals", [128, 16, 16], F32)
            cand2 = sbB("cand2", [128, 256], F32)
            best = sbB("best", [128, 8, 16], F32)
            gat = sbB("gat", [128, 8, 16], F32)
            gsum = sbB("gsum", [128, 8], F32)
            bias8 = sbB("bias8", [128, 8], F32)
            IB = 16
            Ptok = [sbB("Ptok%d" % i, [128, 128, IB], BF16) for i in range(2)]
            PTt = sbB("PTt", [128, 128, 128], BF16)
            JB = 16
            TG = 512 // JB
            NJB = 128 // JB
            xq = [sbB("xq%d" % i, [128, 16, JB], F32) for i in range(2)]
            eq = [sbB("eq%d" % i, [128, 16, JB], F32) for i in range(2)]
            Qtok = [sbB("Qtok%d" % i, [128, 128, JB], BF16) for i in range(2)]
            QTt = [sbB("QTt%d" % i, [128, 128, JB], BF16) for i in range(2)]
            act_sb = sbB("act_sb", [128, JB, 128], BF16)
            coef = sbB("coef", [128, JB, 128], BF16)
            x2 = sbB("x2", [128, D], F32)
            p_bf = sbB("p_bf", [128, 256], BF16)
            pTt = sbB("pTt", [128, 2, 128], BF16)
            pg_s = sbB("pg_s", [128, D], F32)
            uctr = [0]; vctr = [0]; qctr = [0]; pbc = [0]

            def nextptb():
                r_ = PTB[pbc[0] % 2]
                pbc[0] += 1
                return r_

            for n in range(nt):
                par = n % 2
                X1, X1K = x1b[0], 'x1b0'
                ld('sp', X1[:], out_d[n * 128:(n + 1) * 128, :], [X1K], r=['out%d' % n])
                ld('sp', ptl[par][:], p_d[n * 128:(n + 1) * 128, :], ['ptl%d' % par])
                ck(102)
                rmsnorm(X1[:], X1K, g3[:, 0, :], 'g3_0', h2b[:], 'h2b')
                for c in range(8):
                    tr(ptb[:, c, :], h2b[:, c * 128:(c + 1) * 128], identb[:], ['h2b', 'identb'], ['ptb'])
                cp('act', h2T[:], ptb[:], ['ptb'], ['h2T'])
                for g4i in range(4):
                    bank = 2 + (g4i % 2)
                    for i2 in range(2):
                        wb = qctr[0] % 2
                        qctr[0] += 1
                        c0 = g4i * 512 + i2 * 256
                        S.dma('sp', lambda e, wb=wb, c0=c0: e.dma_start(
                            out=wqb[wb][:], in_=wpqb_d[:, c0:c0 + 256].rearrange("(kc p) n -> p kc n", p=128)),
                            r=['wpqb'], w=['wqb%d' % wb])
                        for i1 in range(2):
                            i = i2 * 2 + i1
                            for kc in range(8):
                                mm(pbk[bank][:, i * 128:(i + 1) * 128], wqb[wb][:, kc, i1 * 128:(i1 + 1) * 128], h2T[:, kc, :], kc == 0, kc == 7, ['h2T', 'wqb%d' % wb], [PB[bank]])
                    cp('act', qT[:, g4i * 4:g4i * 4 + 4, :].rearrange("p g t -> p (g t)"), pbk[bank][:], [PB[bank]], ['qc'])
                for g4i in range(4):
                    bank = 4 + (g4i % 2)
                    for i in range(4):
                        g = g4i * 4 + i
                        mm(pbk[bank][:, i * 128:(i + 1) * 128], qT[:, g, :], skT[:, g, :], True, True, ['qc', 'skT'], [PB[bank]])
                    cp('act', s_sb[:, g4i * 4:g4i * 4 + 4, :].rearrange("p g k -> p (g k)"), pbk[bank][:], [PB[bank]], ['s_sb'])
                ck(103)
                for g in range(16):
                    S.op('dve', lambda e, g=g: e.max(out=vals[:, g, 0:8], in_=s_sb[:, g, :]), r=['s_sb'], w=['vals'])
                    S.op('dve', lambda e, g=g: e.match_replace(out=s_rp[:], in_to_replace=vals[:, g, 0:8], in_values=s_sb[:, g, :], imm_value=-1e30), r=['s_sb', 'vals'], w=['s_rp'])
                    S.op('dve', lambda e, g=g: e.max(out=vals[:, g, 8:16], in_=s_rp[:]), r=['s_rp'], w=['vals'])
                v4 = vals[:].rearrange("p (h c) a -> p h c a", c=2)
                s4 = s_sb[:].rearrange("p (h c) k -> p h c k", c=2)
                for hh in range(8):
                    tt('dve', cand[:, hh, :].rearrange("p (a b) -> p a b", b=16), bc(v4[:, hh, 0, :], 2, [128, 16, 16]), bc(v4[:, hh, 1, :], 1, [128, 16, 16]), ALU.add, ['vals'], ['qc'])
                for hh in range(8):
                    S.op('dve', lambda e, hh=hh: e.max(out=best[:, hh, 0:8], in_=cand[:, hh, :]), r=['qc'], w=['best'])
                    S.op('dve', lambda e, hh=hh: e.match_replace(out=cand2[:], in_to_replace=best[:, hh, 0:8], in_values=cand[:, hh, :], imm_value=-1e30), r=['qc', 'best'], w=['cand2'])
                    S.op('dve', lambda e, hh=hh: e.max(out=best[:, hh, 8:16], in_=cand2[:]), r=['cand2'], w=['best'])
                ck(104)
                tt('dve', gat[:], best[:], bc(best[:, :, 0], 2, [128, 8, 16]), ALU.subtract, ['best'], ['gat'])
                act(gat[:], gat[:], AF.Exp, ['gat'], ['gat'])
                red(gsum[:], gat[:], ALU.add, ['gat'], ['gsum'])
                act(gsum[:], gsum[:], AF.Ln, ['gsum'], ['gsum'])
                stt(bias8[:], best[:, :, 0], -1.0, gsum[:], ALU.mult, ALU.subtract, ['best', 'gsum'], ['bias8'])
                ck(105)

                for ib in range(128 // IB):
                    pb_ = ib % 2
                    tt('dve', Ptok[pb_][:].rearrange("t (h a) i -> t h a i", a=16),
                       bc(s4[:, :, 0, ib * IB:(ib + 1) * IB], 2, [128, 8, 16, IB]),
                       bc(v4[:, :, 0, :], 3, [128, 8, 16, IB]), ALU.is_equal, ['s_sb', 'vals'], ['Ptok%d' % pb_])
                    for i8 in range(IB // 8):
                        pt_, pk_ = nextptb()
                        for il in range(8):
                            tr(pt_[:, il, :], Ptok[pb_][:, :, i8 * 8 + il], identb[:], ['Ptok%d' % pb_, 'identb'], [pk_])
                        i0 = ib * IB + i8 * 8
                        cp('act', PTt[:, :, i0:i0 + 8].rearrange("n t i -> n i t"), pt_[:, :, :], [pk_], ['PTt'])

                def q_build(jb):
                    qb_ = jb % 2
                    for hh in range(8):
                        xb = (jb * 8 + hh) % 2
                        tt('dve', xq[xb][:], bc(v4[:, hh, 0, :], 2, [128, 16, JB]), bc(s4[:, hh, 1, jb * JB:(jb + 1) * JB], 1, [128, 16, JB]), ALU.add, ['vals', 's_sb'], ['xq%d' % xb])
                        act(eq[xb][:], xq[xb][:], AF.Exp, ['xq%d' % xb, 'bias8'], ['eq%d' % xb], bias=bias8[:, hh:hh + 1])
                        stt(Qtok[qb_][:, hh * 16:(hh + 1) * 16, :], xq[xb][:], best[:, hh, 15:16], eq[xb][:], ALU.is_ge, ALU.mult, ['xq%d' % xb, 'eq%d' % xb, 'best'], ['Qtok%d' % qb_])
                    for j8 in range(JB // 8):
                        pt_, pk_ = nextptb()
                        for jl in range(8):
                            tr(pt_[:, jl, :], Qtok[qb_][:, :, j8 * 8 + jl], identb[:], ['Qtok%d' % qb_, 'identb'], [pk_])
                        cp('act', QTt[qb_][:, :, j8 * 8:j8 * 8 + 8].rearrange("n t j -> n j t"), pt_[:, :, :], [pk_], ['QTt%d' % qb_])

                q_build(0)
                for jb in range(NJB):
                    qb_ = jb % 2
                    for jq in range(JB // 4):
                        j0 = jb * JB + jq * 4
                        ub = uctr[0] % NSB
                        uctr[0] += 1
                        S.dma('sp', lambda e, ub=ub, j0=j0: e.dma_start(out=ubuf[ub][:], in_=utb_d[j0:j0 + 4, :, :].rearrange("j d x -> d j x")),
                              r=['utb'], w=['ubuf%d' % ub])
                        bank = 2 + (jq % 2)
                        for jl in range(4):
                            for kc in range(8):
                                mm(pbk[bank][:, jl * 128:(jl + 1) * 128], ubuf[ub][:, jl, kc * 128:(kc + 1) * 128], h2T[:, kc, :], kc == 0, kc == 7, ['ubuf%d' % ub, 'h2T'], [PB[bank]])
                        act(act_sb[:, jq * 4:jq * 4 + 4, :].rearrange("i j t -> i (j t)"), pbk[bank][:], AF.Gelu_apprx_tanh, [PB[bank]], ['act_sb'])
                    if jb + 1 < NJB:
                        q_build(jb + 1)
                    for tg in range(128 // TG):
                        bank = 4 + (tg % 2)
                        for tl in range(TG):
                            t = tg * TG + tl
                            mm(pbk[bank][:, tl * JB:(tl + 1) * JB], PTt[:, t, :], QTt[qb_][:, t, :], True, True, ['PTt', 'QTt%d' % qb_], [PB[bank]])
                        tt('dve', coef[:, :, tg * TG:(tg + 1) * TG], pbk[bank][:].rearrange("i (t j) -> i j t", j=JB), act_sb[:, :, tg * TG:(tg + 1) * TG], ALU.mult, [PB[bank], 'act_sb'], ['coef'])
                    for jq in range(JB // 4):
                        j0 = jb * JB + jq * 4
                        vb = vctr[0] % NSB
                        vctr[0] += 1
                        S.dma('sp', lambda e, vb=vb, j0=j0: e.dma_start(out=vbuf[vb][:], in_=vtb_d[j0:j0 + 4, :, :].rearrange("j i x -> i j x")),
                              r=['vtb'], w=['vbuf%d' % vb])
                        for jl in range(4):
                            j = j0 + jl
                            for hf in range(2):
                                mm(pbk[hf][:], coef[:, jq * 4 + jl, :], vbuf[vb][:, jl, hf * 512:(hf + 1) * 512], j == 0, j == 127, ['coef', 'vbuf%d' % vb], [PB[hf]])
                ck(107)
                for hf in range(2):
                    sl = slice(hf * 512, (hf + 1) * 512)
                    tt('dve', x2[:, sl], pbk[hf][:], X1[:, sl], ALU.add, [PB[hf], X1K], ['x2'])
                rmsnorm(x2[:], 'x2', g3[:, 1, :], 'g3_1', h2b[:], 'h2b')
                for c in range(8):
                    tr(ptb[:, c, :], h2b[:, c * 128:(c + 1) * 128], identb[:], ['h2b', 'identb'], ['ptb'])
                cp('act', h2T[:], ptb[:], ['ptb'], ['h2T'])
                cp('pool', p_bf[:], ptl[par][:], ['ptl%d' % par], ['p_bf'])
                for c in range(2):
                    tr(ptb[:, c, :], p_bf[:, c * 128:(c + 1) * 128], identb[:], ['p_bf', 'identb'], ['ptb'])
                cp('act', pTt[:], ptb[:, 0:2, :], ['ptb'], ['pTt'])
                for hf in range(2):
                    sl = slice(hf * 512, (hf + 1) * 512)
                    for c in range(8):
                        mm(pbk[2 + hf][:], h2T[:, c, :], wpg[:, c, sl], c == 0, c == 7, ['h2T'] + WPG, [PB[2 + hf]])
                    act(pg_s[:, sl], pbk[2 + hf][:], AF.Sigmoid, [PB[2 + hf]], ['pg_s'])
                    for c in range(2):
                        mm(pbk[4 + hf][:], pTt[:, c, :], wpu[:, c, sl], c == 0, c == 1, ['pTt'] + WPU, [PB[4 + hf]])
                    tt('dve', pg_s[:, sl], pg_s[:, sl], pbk[4 + hf][:], ALU.mult, ['pg_s', PB[4 + hf]], ['pg_s'])
                tt('dve', pg_s[:], x2[:], pg_s[:], ALU.add, ['x2', 'pg_s'], ['pg_s'])
                rmsnorm(pg_s[:], 'pg_s', g3[:, 2, :], 'g3_2', x2[:], 'x2')
                S.dma('act', lambda e, n=n: e.dma_start(out=out_d[n * 128:(n + 1) * 128, :], in_=x2[:]), r=['x2'], w=['out%d' % n])
          except _Stop:
            pass

        S.wait_all('sp')
        S.emit()
    return nc


def make_in_maps(inputs, nt, ncores):
    f = np.float32
    c = host_consts(nt)
    g = lambda k: np.asarray(inputs[k], dtype=f)
    S_ = nt * 128
    mu = g('rwkv_mu')[0]
    pp = np.zeros((128, 34), f)
    pp[:, 0:14] = mu.reshape(14, 128).T
    for j, k in enumerate(('rwkv_w0', 'rwkv_a0', 'rwkv_k_k', 'rwkv_k_a')):
        pp[:, 14 + 4 * j:18 + 4 * j] = g(k)[0].reshape(4, 128).T
    pp[:, 30:34] = g('rwkv_r_k')[0].reshape(4, 128).T
    g4 = np.stack([g('g_mix')[0], g('g_ffn')[0], g('g_ple')[0], g('g_final')], 0)
    gn3 = np.stack([g('ret_gn_g')[0], g('rwkv_gn_g')[0], g('rwkv_gn_b')[0]], 0)
    lora = np.zeros((128, 3, 512), f)
    lora[0:64, 0, :] = g('rwkv_w_up')[0]
    lora[64:128, 1, :] = g('rwkv_a_up')[0]
    lora[:, 2, :] = g('rwkv_g_up')[0]
    wbr = np.stack([g('w_ret_br')[0], g('w_rwkv_br')[0]], 0)
    shared = dict(w_in=g('w_in')[0], pp=pp, g4=np.ascontiguousarray(g4), gn3=np.ascontiguousarray(gn3),
                  lora=lora, wbr=np.ascontiguousarray(wbr), w_o=g('w_o')[0], w_pq=g('w_pq')[0],
                  sk=np.ascontiguousarray(g('peer_sub_keys')[0].reshape(16, 128, 128)),
                  peer_u=g('peer_u')[0], peer_v=g('peer_v')[0], w_ple_gate=g('w_ple_gate')[0],
                  w_ple_up=g('w_ple_up')[0], rot=c['rot'], DT=c['DT'], xiT=c['xiT'], CDb=c['CDb'], cm=c['cm'])
    x = g('x'); p = g('p')[0]
    maps = []
    for i in range(ncores):
        m = dict(shared)
        m['x'] = np.ascontiguousarray(x[i, :S_])
        m['p'] = np.ascontiguousarray(p[i, :S_])
        maps.append(m)
    return maps


def kernel(**inputs):
    nt = SEQ // 128
    nc = build(nt)
    in_maps = make_in_maps(inputs, nt, NCORES)
    res = run_bass_kernel_spmd(nc, in_maps, core_ids=list(range(NCORES)))
    out = np.stack([np.asarray(r["out"], dtype=np.float32) for r in res.results], axis=0)
    return out
```

```python
import math
import numpy as np
from contextlib import ExitStack
import concourse.bass as bass
import concourse.mybir as mybir
from concourse.bass_utils import run_bass_kernel_spmd

F32 = mybir.dt.float32
BF16 = mybir.dt.bfloat16
U32 = mybir.dt.uint32
I32 = mybir.dt.int32
AF = mybir.ActivationFunctionType
ALU = mybir.AluOpType
AX = mybir.AxisListType

D = 1024
SEQ = 4096
NCORES = 8
IN_COLS = 5888
C0 = math.exp(-0.5)


class Sched:
    SELF_SYNC = {'pe': False, 'act': True, 'dve': True, 'pool': True, 'sp': True}

    def __init__(self, nc, es, n_dma_sems=8):
        self.nc = nc
        self.ops = {e: [] for e in ('pe', 'act', 'dve', 'pool', 'sp')}
        self.sem = {e: es.enter_context(nc.semaphore('prog_' + e)) for e in self.ops}
        self.cnt = {e: 0 for e in self.ops}
        self.waited = {e: {} for e in self.ops}
        self.last_w = {}
        self.readers = {}
        self.dsem = {}
        self.dcnt = {}
        self.drr = {}
        for q in ('sp', 'act', 'pool'):
            self.dsem[q] = [es.enter_context(nc.semaphore('dma_%s_%d' % (q, i)))
                            for i in range(n_dma_sems)]
            self.dcnt[q] = [0] * n_dma_sems
            self.drr[q] = 0
        self.sem_id = {}
        self.pending = {e: [] for e in self.ops}

    def barrier(self):
        toks = list(self.last_w.values())
        for ts_ in self.readers.values():
            toks.extend(ts_)
        for e in self.ops:
            self.pending[e] = list(toks)

    def _deps(self, r, w):
        toks = []
        for k in r:
            t = self.last_w.get(k)
            if t is not None:
                toks.append(t)
        for k in w:
            t = self.last_w.get(k)
            if t is not None:
                toks.append(t)
            toks.extend(self.readers.get(k, ()))
        return toks

    def _waits(self, e, toks):
        need = {}
        for (sem, val, src) in toks:
            if src == e and not self.SELF_SYNC[e]:
                continue
            key = id(sem)
            self.sem_id[key] = sem
            if self.waited[e].get(key, 0) >= val:
                continue
            if need.get(key, 0) < val:
                need[key] = val
        out = []
        for key, val in need.items():
            self.waited[e][key] = val
            out.append((self.sem_id[key], val))
        return out

    def _commit(self, tok, r, w):
        for k in w:
            self.last_w[k] = tok
            self.readers[k] = []
        for k in r:
            if k in w:
                continue
            self.readers.setdefault(k, []).append(tok)

    def op(self, e, fn, r=(), w=()):
        r = list(r); w = list(w)
        toks = self._deps(r, w) + self.pending[e]
        self.pending[e] = []
        waits = self._waits(e, toks)
        self.cnt[e] += 1
        tok = (self.sem[e], self.cnt[e], e)
        self.ops[e].append((waits, fn, (self.sem[e], 1)))
        self._commit(tok, r, w)
        return tok

    def dma(self, q, fn, r=(), w=()):
        r = list(r); w = list(w)
        j = self.drr[q]
        self.drr[q] = (j + 1) % len(self.dsem[q])
        sem = self.dsem[q][j]
        toks = self._deps(r, w) + self.pending[q]
        self.pending[q] = []
        if self.dcnt[q][j] > 0:
            toks.append((sem, 16 * self.dcnt[q][j], None))
        waits = self._waits(q, toks)
        self.dcnt[q][j] += 1
        tok = (sem, 16 * self.dcnt[q][j], None)
        self.ops[q].append((waits, fn, (sem, 16)))
        self._commit(tok, r, w)
        return tok

    def wait_all(self, e):
        toks = list(self.last_w.values())
        for ts in self.readers.values():
            toks.extend(ts)
        waits = self._waits(e, toks)
        self.ops[e].append((waits, None, None))

    def emit(self):
        nc = self.nc
        with nc.Block() as block:
            def run(e, eng):
                for waits, fn, inc in self.ops[e]:
                    for sem, val in waits:
                        eng.wait_ge(sem, val)
                    if fn is not None:
                        ins = fn(eng)
                        ins.then_inc(inc[0], inc[1])

            @block.sync
            def _(eng):
                run('sp', eng)

            @block.scalar
            def _(eng):
                run('act', eng)

            @block.vector
            def _(eng):
                run('dve', eng)

            @block.gpsimd
            def _(eng):
                run('pool', eng)

            @block.tensor
            def _(eng):
                run('pe', eng)


def host_consts(nt):
    f = np.float32
    S = nt * 128
    half = 32
    inv_freq = (10000.0 ** (-np.arange(half, dtype=f) * f(2.0) / f(64))).astype(f)
    ang = (np.arange(S, dtype=f)[:, None] * inv_freq[None, :]).astype(f)
    cos = np.cos(ang).astype(f); sin = np.sin(ang).astype(f)
    rot = np.zeros((nt, 128, 128), f)
    rot[:, :, 0:32] = cos.reshape(nt, 128, 32)
    rot[:, :, 32:64] = sin.reshape(nt, 128, 32)
    rot[:, :, 64:96] = cos.reshape(nt, 128, 32) * f(0.125)
    rot[:, :, 96:128] = sin.reshape(nt, 128, 32) * f(0.125)
    H = 8
    lg = np.log1p(-(2.0 ** (-5.0 - np.arange(H, dtype=np.float64))))
    idx = np.arange(128, dtype=np.float64)
    diff = idx[None, :] - idx[:, None]
    DT = np.where(diff[:, None, :] >= 0, np.exp(lg[None, :, None] * np.maximum(diff, 0)[:, None, :]), 0.0).astype(f)
    xiT = np.zeros((128, 4, 128), f)
    CDb = np.zeros((128, 4, 64), f)
    for c in range(4):
        for p in range(128):
            h = 2 * c + p // 64
            xiT[p, c, :] = np.exp(lg[h] * (idx + 1.0))
            CDb[p, c, :] = np.exp(lg[h] * 128.0)
    ZT = np.exp(lg[None, :] * (127.0 - idx)[:, None]).astype(f)
    s = np.arange(128)
    SU = (s[None, :] > s[:, None]).astype(f)
    SUI = (s[None, :] >= s[:, None]).astype(f)
    SL = SU.T.copy()
    BO = np.zeros((128, 128), f); BO[:64, :64] = 1; BO[64:, 64:] = 1
    HS = np.zeros((128, 2), f); HS[:64, 0] = 1; HS[64:, 1] = 1
    ident = np.eye(128, dtype=f)
    io16 = np.tile(np.arange(16, dtype=f)[None, :], (128, 1))
    cm = np.concatenate([ident, SU, SUI, SL, BO, ZT, HS, io16, io16 * 16], axis=1)
    return dict(rot=rot, DT=DT.reshape(128, 1024), xiT=xiT.reshape(128, 512),
                CDb=CDb.reshape(128, 256), cm=np.ascontiguousarray(cm))


CM_W = 128 * 5 + 8 + 2 + 16 + 16


class _Stop(Exception):
    pass


def build(nt, stage='full', stop_after=None):
    nc = bass.Bass("TRN2", target_bir_lowering=False)
    S_ = nt * 128
    WDT = BF16
    dram = lambda n, s, d, k="ExternalInput": nc.dram_tensor(n, s, d, kind=k).ap()
    x_d = dram("x", [S_, D], F32)
    p_d = dram("p", [S_, 256], F32)
    w_in_d = dram("w_in", [D, IN_COLS], F32)
    pp_d = dram("pp", [128, 34], F32)
    g4_d = dram("g4", [4, D], F32)
    gn3_d = dram("gn3", [3, 512], F32)
    lora_d = dram("lora", [128, 3, 512], F32)
    wbr_d = dram("wbr", [2, 512, D], F32)
    wo_d = dram("w_o", [D, D], F32)
    wpq_d = dram("w_pq", [D, 2048], F32)
    sk_d = dram("sk", [16, 128, 128], F32)
    pu_d = dram("peer_u", [16384, D], F32)
    pv_d = dram("peer_v", [16384, D], F32)
    wpg_d = dram("w_ple_gate", [D, D], F32)
    wpu_d = dram("w_ple_up", [256, D], F32)
    rot_d = dram("rot", [nt, 128, 128], F32)
    DT_d = dram("DT", [128, 1024], F32)
    xiT_d = dram("xiT", [128, 512], F32)
    CDb_d = dram("CDb", [128, 256], F32)
    cm_d = dram("cm", [128, CM_W], F32)
    out_d = dram("out", [S_, D], F32, "ExternalOutput")
    winb_d = nc.dram_tensor("winb", [D, IN_COLS], BF16, kind="Internal").ap()
    utb_d = nc.dram_tensor("utb", [128, 128, D], BF16, kind="Internal").ap()
    vtb_d = nc.dram_tensor("vtb", [128, 128, D], BF16, kind="Internal").ap()
    wpqb_d = nc.dram_tensor("wpqb", [D, 2048], BF16, kind="Internal").ap()

    es = ExitStack()
    with es:
        S = Sched(nc, es)
        sb = lambda n, s, d: es.enter_context(nc.sbuf_tensor("sb_" + n, s, d))
        ps = lambda n, s, d: es.enter_context(nc.psum_tensor(n, s, d))

        def mm(out, lhsT, rhs, start, stop, r, w):
            S.op('pe', lambda e: e.matmul(out, lhsT=lhsT, rhs=rhs, start=start, stop=stop), r=r, w=w)

        def tr(out, in_, ident, r, w):
            S.op('pe', lambda e: e.transpose(out=out, in_=in_, identity=ident), r=r, w=w)

        def act(out, in_, func, r, w, **kw):
            S.op('act', lambda e: e.activation(out=out, in_=in_, func=func, **kw), r=r, w=w)

        def cp(eng, out, in_, r, w):
            if eng == 'act':
                S.op('act', lambda e: e.copy(out=out, in_=in_), r=r, w=w)
            else:
                S.op(eng, lambda e: e.tensor_copy(out=out, in_=in_), r=r, w=w)

        def tt(eng, out, in0, in1, op, r, w):
            S.op(eng, lambda e: e.tensor_tensor(out=out, in0=in0, in1=in1, op=op), r=r, w=w)

        def ts(eng, out, in0, s1, s2, op0, op1, r, w):
            if op1 is None:
                S.op(eng, lambda e: e.tensor_scalar(out=out, in0=in0, scalar1=s1, scalar2=None, op0=op0), r=r, w=w)
            else:
                S.op(eng, lambda e: e.tensor_scalar(out=out, in0=in0, scalar1=s1, scalar2=s2, op0=op0, op1=op1), r=r, w=w)

        def stt(out, in0, scalar, in1, op0, op1, r, w):
            S.op('dve', lambda e: e.scalar_tensor_tensor(out=out, in0=in0, scalar=scalar, in1=in1, op0=op0, op1=op1), r=r, w=w)

        def red(out, in_, op, r, w, axis=AX.X):
            S.op('dve', lambda e: e.tensor_reduce(out=out, in_=in_, axis=axis, op=op), r=r, w=w)

        def rcp(t, k):
            S.op('dve', lambda e: e.reciprocal(out=t, in_=t), r=[k], w=[k])

        def ld(q, out, in_, w, r=()):
            S.dma(q, lambda e: e.dma_start(out=out, in_=in_), r=r, w=w)

        def bc(ap, axis, shape):
            return ap.unsqueeze(axis).to_broadcast(shape)

        ptb = ps("ptb", [128, 8, 128], BF16)
        pbk = [ps("pb%d" % i, [128, 512], F32) for i in range(7)]
        PB = ['pb%d' % i for i in range(7)]

        cm = sb("cm", [128, CM_W], F32)
        ld('sp', cm[:], cm_d[:, :], ['cm'])
        identf = cm[:, 0:128]
        SU = cm[:, 128:256]; SUI = cm[:, 256:384]; SL = cm[:, 384:512]; BO = cm[:, 512:640]
        ZT = cm[:, 640:648]; HS = cm[:, 648:650]; IO16 = cm[:, 650:666]; IO16X = cm[:, 666:682]
        identb = sb("identb", [128, 128], BF16)
        cp('dve', identb[:], identf, ['cm'], ['identb'])
        eps_t = sb("eps_t", [128, 4], F32)
        S.op('dve', lambda e: e.memset(eps_t[:, 0:1], 1e-6), w=['eps'])
        S.op('dve', lambda e: e.memset(eps_t[:, 1:2], 1e-5), w=['eps'])
        S.op('dve', lambda e: e.memset(eps_t[:, 2:3], 64e-5), w=['eps'])
        sq_junk = sb("sq_junk", [128, D], BF16)
        rs_ss = sb("rs_ss", [128, 1], F32)
        rs_rstd = sb("rs_rstd", [128, 1], F32)

        def rmsnorm(src, src_key, gtab, gkey, dst, dst_key):
            act(sq_junk[:], src, AF.Square, [src_key], ['sq_junk', 'rs_ss'], accum_out=rs_ss[:])
            act(rs_rstd[:], rs_ss[:], AF.Sqrt, ['rs_ss', 'eps'], ['rs_rstd'], scale=1.0 / D, bias=eps_t[:, 0:1])
            rcp(rs_rstd[:], 'rs_rstd')
            stt(dst, src, rs_rstd[:, 0:1], gtab, ALU.mult, ALU.mult, [src_key, 'rs_rstd', gkey], [dst_key])

        def ck(k):
            if stop_after == k:
                raise _Stop()
        esA = ExitStack()
        with esA:
          try:
            sbA = lambda n, s, d: esA.enter_context(nc.sbuf_tensor("sa_" + n, s, d))
            pp = sbA("pp", [128, 34], F32)
            ld('sp', pp[:], pp_d[:, :], ['pp'])
            MU = pp[:, 0:14]; W0 = pp[:, 14:18]; A0 = pp[:, 18:22]; KK_ = pp[:, 22:26]; KA = pp[:, 26:30]; RK = pp[:, 30:34]
            omm = sbA("omm", [128, 14], F32)
            ts('dve', omm[:], MU, -1.0, 1.0, ALU.mult, ALU.add, ['pp'], ['omm'])
            omka = sbA("omka", [128, 4], F32)
            ts('dve', omka[:], KA, -1.0, 1.0, ALU.mult, ALU.add, ['pp'], ['omka'])
            gmix = sbA("gmix", [128, D], F32)
            ld('sp', gmix[:], g4_d[0, :].partition_broadcast(128), ['gmix'])
            gn3 = sbA("gn3", [128, 3, 512], F32)
            for i in range(3):
                ld('sp', gn3[:, i, :], gn3_d[i, :].partition_broadcast(128), ['gn3_%d' % i])

            NWB = 3
            wch = [sbA("wch%d" % i, [128, 8, 512], BF16) for i in range(NWB)]
            k = 0
            for kc in range(8):
                for cb in range(4):
                    b = k % NWB
                    stg = wch[b][:].rearrange("p a n -> p (a n)")[:, 0:1472]
                    S.dma('pool', lambda e, stg=stg, kc=kc, cb=cb: e.dma_start(out=stg, in_=w_in_d[kc * 128:(kc + 1) * 128, cb * 1472:(cb + 1) * 1472]), w=['wch%d' % b])
                    S.dma('sp', lambda e, stg=stg, kc=kc, cb=cb: e.dma_start(out=winb_d[kc * 128:(kc + 1) * 128, cb * 1472:(cb + 1) * 1472], in_=stg), r=['wch%d' % b], w=['winb'])
                    k += 1
            wbr = sbA("wbr", [128, 2, 4, D], BF16)
            for i in range(2):
                for c in range(4):
                    S.dma('pool', lambda e, i=i, c=c: e.dma_start(out=wbr[:, i, c, :], in_=wbr_d[i, c * 128:(c + 1) * 128, :]), w=['wbr%d%d' % (i, c)])
            wo = sbA("wo", [128, 8, D], BF16)
            for c in range(8):
                S.dma('pool', lambda e, c=c: e.dma_start(out=wo[:, c, :], in_=wo_d[c * 128:(c + 1) * 128, :]), w=['wo%d' % c])
            WBR = ['wbr%d%d' % (i, c) for i in range(2) for c in range(4)]
            WO = ['wo%d' % c for c in range(8)]
            lora = sbA("lora", [128, 3, 512], BF16)
            S.dma('pool', lambda e: e.dma_start(out=lora[:], in_=lora_d[:, :, :]), w=['lora'])
            DT = sbA("DT", [128, 8, 128], F32)
            ld('sp', DT[:].rearrange("p h i -> p (h i)"), DT_d[:, :], ['DT'])
            xiT = sbA("xiT", [128, 4, 128], F32)
            ld('sp', xiT[:].rearrange("p c i -> p (c i)"), xiT_d[:, :], ['xiT'])
            CDb = sbA("CDb", [128, 4, 64], F32)
            ld('sp', CDb[:].rearrange("p c i -> p (c i)"), CDb_d[:, :], ['CDb'])

            CH = [(0, 512), (512, 512), (1024, 512), (1536, 512),
                  (2048, 512), (2560, 512), (3072, 512), (3584, 256),
                  (3840, 512), (4352, 512), (4864, 512), (5376, 512)]
            wctr = [k]

            def load_chunk(ci):
                b = wctr[0] % NWB
                wctr[0] += 1
                c0, cw = CH[ci]
                S.dma('sp', lambda e, b=b, c0=c0, cw=cw: e.dma_start(
                    out=wch[b][:, :, 0:cw], in_=winb_d[:, c0:c0 + cw].rearrange("(kc p) n -> p kc n", p=128)),
                    r=['winb'], w=['wch%d' % b])
                return b

            xt = [sbA("xt%d" % i, [128, D], F32) for i in range(2)]
            rot_t = [sbA("rot%d" % i, [128, 128], F32) for i in range(2)]
            h = sbA("h", [128, D], BF16)
            hT = sbA("hT", [128, 8, 128], BF16)
            qk_rot = sbA("qk_rot", [128, 2, 512], BF16)
            rt = [sbA("rt%d" % i, [128, 8, 32], F32) for i in range(4)]
            v_tok = sbA("v_tok", [128, 512], BF16)
            gr_s = sbA("gr_s", [128, 512], BF16)
            gate_s = sbA("gate_s", [128, 2048], BF16)
            qkT = sbA("qkT", [128, 8, 128], BF16)
            qxT = sbA("qxT", [128, 4, 128], BF16)
            kz = sbA("kz", [128, 8, 64], BF16)
            PT = sbA("PT", [128, 8, 128], BF16)
            R32 = sbA("R32", [128, 4, 64], F32)
            Rb = sbA("Rb", [128, 4, 128], BF16)
            qTm = sbA("qTm", [128, 2, 4, 128], BF16)
            S.op('dve', lambda e: e.memset(qTm[:], 0.0), w=['qTm'])
            Rtmp = sbA("Rtmp", [128, 4, 64], F32)
            S.op('dve', lambda e: e.memset(R32[:], 0.0), w=['R32'])
            S.op('dve', lambda e: e.memset(Rb[:], 0.0), w=['Rb'])
            hn_sq = sbA("hn_sq", [128, 8, 64], F32)
            hn_c = sbA("hn_c", [128, 8, 64], F32)
            hn_s = sbA("hn_s", [128, 8], F32)
            hn_q = sbA("hn_q", [128, 8], F32)
            hn_m = sbA("hn_m", [128, 8], F32)
            hn_r = sbA("hn_r", [128, 8], F32)
            y_bf = sbA("y_bf", [128, 512], BF16)
            yT = sbA("yT", [128, 4, 128], BF16)
            ZB = sbA("ZB", [128, 14, 129], F32)
            zlast = sbA("zlast", [128, 14, 1], F32)
            S.op('dve', lambda e: e.memset(zlast[:], 0.0), w=['zlast'])
            zs = sbA("zs", [128, 14, 128], F32)
            lor_in = sbA("lor_in", [128, 2, 128], BF16)
            f4 = [sbA("f4_%d" % i, [128, 4, 128], F32) for i in range(8)]
            F4 = ['f4_%d' % i for i in range(8)]
            f4ones = sbA("f4ones", [128, 128], F32)
            S.op('dve', lambda e: e.memset(f4ones[:], 1.0), w=['f4ones'])
            wk = {n_: sbA("wk_" + n_, [128, 4, 128], WDT) for n_ in ('ab', 'rb', 'bb', 'kb', 'bt', 'kt', 'vT')}
            tok3 = sbA("tok3", [128, 3, 512], WDT)
            Am = {n_: sbA("Am_" + n_, [128, 4, 128], WDT) for n_ in ('akT', 'rbT', 'rkT')}
            Nb = [sbA("Nb%d" % i, [128, 4, 128], WDT) for i in range(2)]
            NTb = [sbA("NTb%d" % i, [128, 4, 128], WDT) for i in range(2)]
            Qb = [sbA("Qb%d" % i, [128, 4, 128], WDT) for i in range(2)]
            BOw = sbA("BOw", [128, 128], F32)
            cp('dve', BOw[:], BO, ['cm'], ['BOw'])
            Xs = sbA("Xs", [128, 256], WDT)
            Us = sbA("Us", [128, 512], WDT)
            ST32 = sbA("ST32", [128, 4, 64], F32)
            STb = sbA("STb", [128, 4, 128], WDT)
            wkm = {n_: sbA("wkm_" + n_, [128, 2, 4, 128], WDT) for n_ in ('ab', 'bb', 'rb')}
            for n_ in ('ab', 'bb', 'rb'):
                S.op('pool', lambda e, n_=n_: e.memset(wkm[n_][:], 0.0), w=['wkm_' + n_])
            STtmp = sbA("STtmp", [128, 4, 64], F32)
            S.op('dve', lambda e: e.memset(ST32[:], 0.0), w=['ST32'])
            S.op('dve', lambda e: e.memset(STb[:], 0.0), w=['STb'])
            PCt = sbA("PCt", [128, 4], F32)
            g_tok = sbA("g_tok", [128, 512], F32)
            cbt = sbA("cbt", [128, 8], F32)
            merged = sbA("merged", [128, D], BF16)
            mT = sbA("mT", [128, 8, 128], BF16)
            x1 = sbA("x1", [128, D], F32)

            def headnorm(ops_ap, ops_key, eps_col, dst_c):
                red(hn_s[:], ops_ap, ALU.add, [ops_key], ['hn_s'])
                act(hn_sq[:], ops_ap, AF.Square, [ops_key], ['hn_sq'])
                red(hn_q[:], hn_sq[:], ALU.add, ['hn_sq'], ['hn_q'])
                ts('dve', hn_m[:], hn_s[:], 1.0 / 64, None, ALU.mult, None, ['hn_s'], ['hn_m'])
                tt('dve', hn_r[:], hn_m[:], hn_m[:], ALU.mult, ['hn_m'], ['hn_r'])
                stt(hn_r[:], hn_q[:], 1.0 / 64, hn_r[:], ALU.mult, ALU.subtract, ['hn_q', 'hn_r'], ['hn_r'])
                act(hn_r[:], hn_r[:], AF.Sqrt, ['hn_r', 'eps'], ['hn_r'], bias=eps_t[:, eps_col:eps_col + 1])
                rcp(hn_r[:], 'hn_r')
                tt('dve', dst_c, ops_ap, bc(hn_m[:], 2, [128, 8, 64]), ALU.subtract, [ops_key, 'hn_m'], ['hn_c'])
                tt('dve', dst_c, dst_c, bc(hn_r[:], 2, [128, 8, 64]), ALU.mult, ['hn_c', 'hn_r'], ['hn_c'])

            ck(1)
            for n in range(nt):
                par = n % 2
                X, XK = xt[par], 'xt%d' % par
                ld('sp', X[:], x_d[n * 128:(n + 1) * 128, :], [XK])
                ld('sp', rot_t[par][:], rot_d[n, :, :], ['rot%d' % par])
                ROT = 'rot%d' % par
                rmsnorm(X[:], XK, gmix[:], 'gmix', h[:], 'h')
                for c in range(8):
                    tr(ptb[:, c, :], h[:, c * 128:(c + 1) * 128], identb[:], ['h', 'identb'], ['ptb'])
                cp('act', hT[:], ptb[:], ['ptb'], ['hT'])

                ck(2)
                def inproj_tok(ci, bank):
                    b = load_chunk(ci)
                    for kc in range(8):
                        mm(pbk[bank][:], hT[:, kc, :], wch[b][:, kc, :], kc == 0, kc == 7, ['hT', 'wch%d' % b], [PB[bank]])

                Cc = rot_t[par][:, 0:32]; Sc = rot_t[par][:, 32:64]
                kCc = rot_t[par][:, 64:96]; kSc = rot_t[par][:, 96:128]
                for qi in range(2):
                    bank = qi
                    inproj_tok(qi, bank)
                    pv = pbk[bank][:].rearrange("p (h two f) -> p h two f", two=2, f=32)
                    q1 = pv[:, :, 0, :]; q2 = pv[:, :, 1, :]
                    cc, ssn = (Cc, Sc) if qi == 0 else (kCc, kSc)
                    cb_ = bc(cc, 1, [128, 8, 32]); sb_ = bc(ssn, 1, [128, 8, 32])
                    ov = qk_rot[:, qi, :].rearrange("p (h two f) -> p h two f", two=2, f=32)
                    tt('dve', rt[0][:], q1, cb_, ALU.mult, [PB[bank], ROT], ['rt0'])
                    tt('dve', rt[1][:], q2, sb_, ALU.mult, [PB[bank], ROT], ['rt1'])
                    tt('dve', rt[2][:], q1, sb_, ALU.mult, [PB[bank], ROT], ['rt2'])
                    tt('dve', rt[3][:], q2, cb_, ALU.mult, [PB[bank], ROT], ['rt3'])
                    tt('pool', ov[:, :, 0, :], rt[0][:], rt[1][:], ALU.subtract, ['rt0', 'rt1'], ['qk_rot'])
                    tt('pool', ov[:, :, 1, :], rt[2][:], rt[3][:], ALU.add, ['rt2', 'rt3'], ['qk_rot'])
                inproj_tok(2, 2)
                cp('act', v_tok[:], pbk[2][:], [PB[2]], ['v_tok'])
                inproj_tok(3, 3)
                act(gr_s[:], pbk[3][:], AF.Silu, [PB[3]], ['gr_s'])

                ck(3)
                for c in range(8):
                    tr(ptb[:, c, :], qk_rot[:, c // 4, (c % 4) * 128:(c % 4 + 1) * 128], identb[:], ['qk_rot', 'identb'], ['ptb'])
                cp('act', qkT[:, 4:8, :], ptb[:, 4:8, :], ['ptb'], ['qkT'])
                cp('act', qTm[0:64, 0, :, :], ptb[0:64, 0:4, :], ['ptb'], ['qTm'])
                cp('act', qTm[64:128, 1, :, :], ptb[64:128, 0:4, :], ['ptb'], ['qTm'])
                tt('dve', qxT[:], ptb[:, 0:4, :], xiT[:], ALU.mult, ['ptb', 'xiT'], ['qxT'])
                tt('pool', kz[:], qk_rot[:, 1, :].rearrange("p (h d) -> p h d", d=64), bc(ZT, 2, [128, 8, 64]), ALU.mult, ['qk_rot', 'cm'], ['kz'])

                ck(31)
                for hh in range(8):
                    c, base = hh // 2, (hh % 2) * 64
                    bank = 4 + hh // 4
                    mm(pbk[bank][:, (hh % 4) * 128:(hh % 4 + 1) * 128], qkT[:, 4 + c, :], qTm[:, hh % 2, c, :], True, True, ['qkT', 'qTm'], [PB[bank]])
                for g in range(2):
                    tt('dve', PT[:, 4 * g:4 * g + 4, :], pbk[4 + g][:].rearrange("p (h i) -> p h i", i=128), DT[:, 4 * g:4 * g + 4, :], ALU.mult, [PB[4 + g], 'DT'], ['PT'])
                ck(32)
                for c in range(4):
                    mm(pbk[6][:, c * 128:(c + 1) * 128], qxT[:, c, :], Rb[:, c, :], True, False, ['qxT', 'Rb'], [PB[6]])
                    for w_ in range(2):
                        hh = 2 * c + w_
                        mm(pbk[6][:, hh * 64:(hh + 1) * 64], PT[:, hh, :], v_tok[:, hh * 64:(hh + 1) * 64], False, w_ == 1, ['PT', 'v_tok'], [PB[6]])
                ck(33)
                for c in range(4):
                    mm(pbk[4][:, c * 128:(c + 1) * 128], kz[:, 2 * c:2 * c + 2, :].rearrange("p a d -> p (a d)"), v_tok[:, c * 128:(c + 1) * 128], True, True, ['kz', 'v_tok'], [PB[4]])
                tt('pool', Rtmp[:], R32[:], CDb[:], ALU.mult, ['R32', 'CDb'], ['Rtmp'])
                p4v = pbk[4][:].rearrange("p (c x) -> p c x", x=128)
                tt('dve', R32[0:64, :, :], Rtmp[0:64, :, :], p4v[0:64, :, 0:64], ALU.add, ['Rtmp', PB[4]], ['R32'])
                tt('dve', R32[64:128, :, :], Rtmp[64:128, :, :], p4v[64:128, :, 64:128], ALU.add, ['Rtmp', PB[4]], ['R32'])
                cp('pool', Rb[0:64, :, 0:64], R32[0:64, :, :], ['R32'], ['Rb'])
                cp('pool', Rb[64:128, :, 64:128], R32[64:128, :, :], ['R32'], ['Rb'])
                ck(34)
                o3 = pbk[6][:].rearrange("p (h e) -> p h e", e=64)
                headnorm(o3, PB[6], 1, hn_c[:])
                hc2 = hn_c[:].rearrange("p h e -> p (h e)")
                tt('dve', hc2, hc2, gn3[:, 0, :], ALU.mult, ['hn_c', 'gn3_0'], ['hn_c'])
                tt('dve', y_bf[:], hc2, gr_s[:], ALU.mult, ['hn_c', 'gr_s'], ['y_bf'])
                ck(35)
                for c in range(4):
                    tr(ptb[:, c, :], y_bf[:, c * 128:(c + 1) * 128], identb[:], ['y_bf', 'identb'], ['ptb'])
                cp('act', yT[:], ptb[:, 0:4, :], ['ptb'], ['yT'])
                for hf in range(2):
                    for c in range(4):
                        mm(pbk[hf][:], yT[:, c, :], wbr[:, 0, c, hf * 512:(hf + 1) * 512], c == 0, c == 3, ['yT'] + WBR, [PB[hf]])

                ck(4)
                for j in range(4):
                    b = load_chunk(4 + j)
                    nm = 4 if j < 3 else 2
                    bank = 2 + (j % 2)
                    for m in range(nm):
                        for kc in range(8):
                            mm(pbk[bank][:, m * 128:(m + 1) * 128], wch[b][:, kc, m * 128:(m + 1) * 128], hT[:, kc, :], kc == 0, kc == 7, ['hT', 'wch%d' % b], [PB[bank]])
                    cp('act', ZB[:, 4 * j:4 * j + nm, 1:129], pbk[bank][:, 0:nm * 128].rearrange("p (m t) -> p m t", t=128), [PB[bank]], ['ZB'])
                cp('pool', ZB[:, :, 0:1], zlast[:], ['zlast'], ['ZB'])
                for (m0, m1) in ((0, 4), (4, 8), (8, 12), (12, 14)):
                    nm = m1 - m0
                    tmp = f4[7][:, 0:nm, :]
                    tt('pool', tmp, ZB[:, m0:m1, 0:128], bc(MU[:, m0:m1], 2, [128, nm, 128]), ALU.mult, ['ZB', 'pp'], [F4[7]])
                    tt('dve', zs[:, m0:m1, :], ZB[:, m0:m1, 1:129], bc(omm[:, m0:m1], 2, [128, nm, 128]), ALU.mult, ['ZB', 'omm'], ['zs'])
                    tt('dve', zs[:, m0:m1, :], zs[:, m0:m1, :], tmp, ALU.add, ['zs', F4[7]], ['zs'])
                cp('pool', zlast[:], ZB[:, :, 128:129], ['ZB'], ['zlast'])
                ck(5)
                rF = zs[:, 0:4, :]; krF = zs[:, 4:8, :]; vF = zs[:, 8:12, :]
                act(lor_in[0:64, 0, :], zs[0:64, 12, :], AF.Tanh, ['zs'], ['lor_in'])
                cp('act', lor_in[64:128, 0, :], zs[64:128, 12, :], ['zs'], ['lor_in'])
                act(lor_in[:, 1, :], zs[:, 13, :], AF.Sigmoid, ['zs'], ['lor_in'])
                for m in range(4):
                    mm(pbk[2][:, m * 128:(m + 1) * 128], lora[:, 0, m * 128:(m + 1) * 128], lor_in[:, 0, :], True, True, ['lora', 'lor_in'], [PB[2]])
                for m in range(4):
                    mm(pbk[3][:, m * 128:(m + 1) * 128], lora[:, 1, m * 128:(m + 1) * 128], lor_in[:, 0, :], True, True, ['lora', 'lor_in'], [PB[3]])
                mm(pbk[4][:], lor_in[:, 1, :], lora[:, 2, :], True, True, ['lora', 'lor_in'], [PB[4]])
                cp('act', g_tok[:], pbk[4][:], [PB[4]], ['g_tok'])
                sg, asig, kkF, kkn, kpr, bF, csF, tmpF = f4
                for m in range(4):
                    act(sg[:, m, :], pbk[2][:, m * 128:(m + 1) * 128], AF.Sigmoid, [PB[2], 'pp'], [F4[0]], bias=W0[:, m:m + 1])
                for m in range(4):
                    act(asig[:, m, :], pbk[3][:, m * 128:(m + 1) * 128], AF.Sigmoid, [PB[3], 'pp'], [F4[1]], bias=A0[:, m:m + 1])
                b4 = lambda t: bc(t, 2, [128, 4, 128])
                f2 = lambda t: t[:].rearrange("p m t -> p (m t)")
                tt('dve', kkF[:], krF, b4(KK_), ALU.mult, ['zs', 'pp'], [F4[2]])
                tt('pool', tmpF[:], kkF[:], kkF[:], ALU.mult, [F4[2]], [F4[7]])
                mm(pbk[2][:], BOw[:], f2(tmpF), True, True, ['BOw', F4[7]], [PB[2]])
                act(f2(kkn), pbk[2][:], AF.Sqrt, [PB[2]], [F4[3]])
                ts('dve', kkn[:], kkn[:], 1e-12, None, ALU.max, None, [F4[3]], [F4[3]])
                rcp(kkn[:], F4[3])
                tt('dve', kkn[:], kkn[:], kkF[:], ALU.mult, [F4[3], F4[2]], [F4[3]])
                tt('pool', kpr[:], asig[:], b4(KA), ALU.mult, [F4[1], 'pp'], [F4[4]])
                tt('pool', kpr[:], kpr[:], b4(omka[:]), ALU.add, [F4[4], 'omka'], [F4[4]])
                tt('dve', kpr[:], kpr[:], krF, ALU.mult, [F4[4], 'zs'], [F4[4]])
                tt('pool', bF[:], kkn[:], asig[:], ALU.mult, [F4[3], F4[1]], [F4[5]])
                tt('pool', tmpF[:], rF, kpr[:], ALU.mult, ['zs', F4[4]], [F4[7]])
                tt('pool', tmpF[:], tmpF[:], b4(RK), ALU.mult, [F4[7], 'pp'], [F4[7]])
                for c in range(4):
                    mm(pbk[3][:, 2 * c:2 * c + 2], tmpF[:, c, :], HS, True, True, [F4[7], 'cm'], [PB[3]])
                cp('act', cbt[:], pbk[3][:, 0:8], [PB[3]], ['cbt'])
                for m in range(4):
                    S.op('dve', lambda e, m=m: e.tensor_tensor_scan(out=csF[:, m, :], data0=f4ones[:], data1=sg[:, m, :], initial=0.0, op0=ALU.mult, op1=ALU.add), r=[F4[0], 'f4ones'], w=[F4[6]])
                E1, E2 = kkF, tmpF
                act(E1[:], csF[:], AF.Exp, [F4[6]], [F4[2]], scale=-C0)
                act(E2[:], csF[:], AF.Exp, [F4[6]], [F4[7]], scale=C0)
                tt('dve', csF[:], csF[:], sg[:], ALU.subtract, [F4[6], F4[0]], [F4[6]])
                act(sg[:], csF[:], AF.Exp, [F4[6]], [F4[0]], scale=-C0)
                E3 = sg
                cp('dve', PCt[:], E1[:, :, 127], [F4[2]], ['PCt'])
                stt(wk['ab'][:], kkn[:], -1.0, E3[:], ALU.mult, ALU.mult, [F4[3], F4[0]], ['wk_ab'])
                tt('dve', wk['rb'][:], rF, E1[:], ALU.mult, ['zs', F4[2]], ['wk_rb'])
                tt('pool', csF[:], bF[:], E2[:], ALU.mult, [F4[5], F4[7]], [F4[6]])
                cp('act', wk['bb'][:], csF[:], [F4[6]], ['wk_bb'])
                tt('pool', wk['bt'][:], csF[:], bc(PCt[:], 2, [128, 4, 128]), ALU.mult, [F4[6], 'PCt'], ['wk_bt'])
                tt('dve', bF[:], kpr[:], E2[:], ALU.mult, [F4[4], F4[7]], [F4[5]])
                cp('act', wk['kb'][:], bF[:], [F4[5]], ['wk_kb'])
                tt('pool', wk['kt'][:], bF[:], bc(PCt[:], 2, [128, 4, 128]), ALU.mult, [F4[5], 'PCt'], ['wk_kt'])
                cp('act', wk['vT'][:], vF, ['zs'], ['wk_vT'])
                for n_ in ('ab', 'bb', 'rb'):
                    cp('pool', wkm[n_][0:64, 0, :, :], wk[n_][0:64, :, :], ['wk_' + n_], ['wkm_' + n_])
                    cp('pool', wkm[n_][64:128, 1, :, :], wk[n_][64:128, :, :], ['wk_' + n_], ['wkm_' + n_])
                ck(6)
                for i, nmk in enumerate(('bt', 'kt', 'vT')):
                    for c in range(4):
                        tr(ptb[:, c, :], wk[nmk][:, c, :], identb[:], ['wk_' + nmk, 'identb'], ['ptb'])
                    cp('act', tok3[:, i, :].rearrange("p (c x) -> p c x", x=128), ptb[:, 0:4, :], ['ptb'], ['tok3_%d' % i])
                Btok = tok3[:, 0, :]; Ktok = tok3[:, 1, :]; Vtok = tok3[:, 2, :]

                ck(7)
                def hop(hh):
                    return hh // 2, (hh % 2) * 64
                for g in range(2):
                    hs = [4 * g + i for i in range(4)]
                    specs = [('bb', 'ab', SU, Nb[0], 'Nb0'), ('ab', 'bb', SL, NTb[0], 'NTb0'),
                             ('kb', 'ab', SU, Am['akT'], 'Am_akT'), ('bb', 'rb', SUI, Am['rbT'], 'Am_rbT'),
                             ('kb', 'rb', SUI, Am['rkT'], 'Am_rkT')]
                    for si, (l_, r_, msk, dst, dk) in enumerate(specs):
                        bank = 2 + (si % 3)
                        for i, hh in enumerate(hs):
                            c, base = hop(hh)
                            mm(pbk[bank][:, i * 128:(i + 1) * 128], wk[l_][:, c, :], wkm[r_][:, hh % 2, c, :], True, True, ['wk_' + l_, 'wkm_' + r_], [PB[bank]])
                        tt('dve', dst[:], pbk[bank][:].rearrange("p (h t) -> p h t", t=128), bc(msk, 1, [128, 4, 128]), ALU.mult, [PB[bank], 'cm'], [dk])
                    tt('pool', Qb[0][:], Nb[0][:], bc(identb[:], 1, [128, 4, 128]), ALU.add, ['Nb0', 'identb'], ['Qb0'])
                    cur = 0
                    for lvl in range(6):
                        nx = 1 - cur
                        last = (lvl == 5)
                        if not last:
                            for i in range(4):
                                mm(pbk[2][:, i * 128:(i + 1) * 128], NTb[cur][:, i, :], Nb[cur][:, i, :], True, True, ['NTb%d' % cur, 'Nb%d' % cur], [PB[2]])
                        for i in range(4):
                            mm(pbk[3][:, i * 128:(i + 1) * 128], Nb[cur][:, i, :], NTb[cur][:, i, :], True, True, ['NTb%d' % cur, 'Nb%d' % cur], [PB[3]])
                        if not last:
                            cp('dve', Nb[nx][:].rearrange("p h t -> p (h t)"), pbk[2][:], [PB[2]], ['Nb%d' % nx])
                        cp('act', NTb[nx][:].rearrange("p h t -> p (h t)"), pbk[3][:], [PB[3]], ['NTb%d' % nx])
                        for i in range(4):
                            mm(pbk[4][:, i * 128:(i + 1) * 128], identb[:], Qb[cur][:, i, :], True, False, ['identb', 'Qb%d' % cur], [PB[4]])
                            mm(pbk[4][:, i * 128:(i + 1) * 128], NTb[nx][:, i, :], Qb[cur][:, i, :], False, True, ['NTb%d' % nx, 'Qb%d' % cur], [PB[4]])
                        cp('act' if lvl % 2 else 'dve', Qb[nx][:].rearrange("p h t -> p (h t)"), pbk[4][:], [PB[4]], ['Qb%d' % nx])
                        cur = nx
                    Qf, QK = Qb[cur], 'Qb%d' % cur
                    for ci in range(2):
                        c = 2 * g + ci
                        mm(pbk[5][:, ci * 128:(ci + 1) * 128], wk['ab'][:, c, :], STb[:, c, :], True, False, ['wk_ab', 'STb'], [PB[5]])
                        for w_ in range(2):
                            i = 2 * ci + w_
                            hh = hs[i]
                            mm(pbk[5][:, i * 64:(i + 1) * 64], Am['akT'][:, i, :], Vtok[:, hh * 64:(hh + 1) * 64], False, w_ == 1, ['Am_akT', 'tok3_2'], [PB[5]])
                    cp('act', Xs[:], pbk[5][:, 0:256], [PB[5]], ['Xs'])
                    for i, hh in enumerate(hs):
                        oc = slice(i * 64, (i + 1) * 64)
                        mm(pbk[5][:, 256 + i * 64:256 + (i + 1) * 64], Qf[:, i, :], Xs[:, oc], True, True, [QK, 'Xs'], [PB[5]])
                    cp('act', Us[:, g * 256:(g + 1) * 256], pbk[5][:, 256:512], [PB[5]], ['Us'])
                    for ci in range(2):
                        c = 2 * g + ci
                        mm(pbk[6][:, c * 128:(c + 1) * 128], wk['rb'][:, c, :], STb[:, c, :], True, False, ['wk_rb', 'STb'], [PB[6]])
                        for w_ in range(2):
                            i = 2 * ci + w_
                            hh = hs[i]
                            hc = slice(hh * 64, (hh + 1) * 64)
                            mm(pbk[6][:, hc], Am['rkT'][:, i, :], Vtok[:, hc], False, False, ['Am_rkT', 'tok3_2'], [PB[6]])
                            mm(pbk[6][:, hc], Am['rbT'][:, i, :], Us[:, hc], False, w_ == 1, ['Am_rbT', 'Us'], [PB[6]])
                for c in range(4):
                    cs_ = slice(c * 128, (c + 1) * 128)
                    mm(pbk[5][:, cs_], Btok[:, cs_], Us[:, cs_], True, False, ['tok3_0', 'Us'], [PB[5]])
                    mm(pbk[5][:, cs_], Ktok[:, cs_], Vtok[:, cs_], False, True, ['tok3_1', 'tok3_2'], [PB[5]])
                tt('pool', STtmp[:], ST32[:], bc(PCt[:], 2, [128, 4, 64]), ALU.mult, ['ST32', 'PCt'], ['STtmp'])
                p5v = pbk[5][:].rearrange("p (c x) -> p c x", x=128)
                tt('dve', ST32[0:64, :, :], STtmp[0:64, :, :], p5v[0:64, :, 0:64], ALU.add, ['STtmp', PB[5]], ['ST32'])
                tt('dve', ST32[64:128, :, :], STtmp[64:128, :, :], p5v[64:128, :, 64:128], ALU.add, ['STtmp', PB[5]], ['ST32'])
                cp('pool', STb[0:64, :, 0:64], ST32[0:64, :, :], ['ST32'], ['STb'])
                cp('pool', STb[64:128, :, 64:128], ST32[64:128, :, :], ['ST32'], ['STb'])
                ck(8)
                o3 = pbk[6][:].rearrange("p (h e) -> p h e", e=64)
                headnorm(o3, PB[6], 2, hn_c[:])
                hc2 = hn_c[:].rearrange("p h e -> p (h e)")
                tt('dve', hc2, hc2, gn3[:, 1, :], ALU.mult, ['hn_c', 'gn3_1'], ['hn_c'])
                tt('dve', hc2, hc2, gn3[:, 2, :], ALU.add, ['hn_c', 'gn3_2'], ['hn_c'])
                tt('pool', hn_sq[:], Vtok.rearrange("p (h e) -> p h e", e=64), bc(cbt[:], 2, [128, 8, 64]), ALU.mult, ['tok3_2', 'cbt'], ['hn_sq'])
                tt('dve', hc2, hc2, hn_sq[:].rearrange("p h e -> p (h e)"), ALU.add, ['hn_c', 'hn_sq'], ['hn_c'])
                tt('dve', y_bf[:], hc2, g_tok[:], ALU.mult, ['hn_c', 'g_tok'], ['y_bf'])
                for c in range(4):
                    tr(ptb[:, c, :], y_bf[:, c * 128:(c + 1) * 128], identb[:], ['y_bf', 'identb'], ['ptb'])
                cp('act', yT[:], ptb[:, 0:4, :], ['ptb'], ['yT'])
                for hf in range(2):
                    for c in range(4):
                        mm(pbk[2 + hf][:], yT[:, c, :], wbr[:, 1, c, hf * 512:(hf + 1) * 512], c == 0, c == 3, ['yT'] + WBR, [PB[2 + hf]])

                ck(9)
                for gi in range(4):
                    bank = 4 + (gi % 2)
                    inproj_tok(8 + gi, bank)
                    act(gate_s[:, gi * 512:(gi + 1) * 512], pbk[bank][:], AF.Sigmoid, [PB[bank]], ['gate_s'])
                for hf in range(2):
                    sl = slice(hf * 512, (hf + 1) * 512)
                    tt('dve', x1[:, sl], pbk[hf][:], gate_s[:, sl], ALU.mult, [PB[hf], 'gate_s'], ['x1'])
                    tt('dve', merged[:, sl], pbk[2 + hf][:], gate_s[:, 1024 + hf * 512:1024 + (hf + 1) * 512], ALU.mult, [PB[2 + hf], 'gate_s'], ['merged'])
                    tt('pool', merged[:, sl], merged[:, sl], x1[:, sl], ALU.add, ['merged', 'x1'], ['merged'])
                for c in range(8):
                    tr(ptb[:, c, :], merged[:, c * 128:(c + 1) * 128], identb[:], ['merged', 'identb'], ['ptb'])
                cp('act', mT[:], ptb[:], ['ptb'], ['mT'])
                for hf in range(2):
                    for c in range(8):
                        mm(pbk[4 + hf][:], mT[:, c, :], wo[:, c, hf * 512:(hf + 1) * 512], c == 0, c == 7, ['mT'] + WO, [PB[4 + hf]])
                    tt('dve', x1[:, hf * 512:(hf + 1) * 512], pbk[4 + hf][:], X[:, hf * 512:(hf + 1) * 512], ALU.add, [PB[4 + hf], XK], ['x1'])
                S.dma('act', lambda e, n=n: e.dma_start(out=out_d[n * 128:(n + 1) * 128, :], in_=x1[:]), r=['x1'], w=['out%d' % n])
          except _Stop:
            pass

        if stage == 'A':
            S.wait_all('sp')
            S.emit()
            return nc
        S.barrier()

        esB = ExitStack()
        with esB:
          try:
            sbB = lambda n, s, d: esB.enter_context(nc.sbuf_tensor("sc_" + n, s, d))
            ptb2 = pbk[6][:].bitcast(BF16).rearrange("p (a b) -> p a b", b=128)
            PTB = [(ptb, 'ptb'), (ptb2, PB[6])]
            g3 = sbB("g3", [128, 3, D], F32)
            for i in range(3):
                ld('sp', g3[:, i, :], g4_d[1 + i, :].partition_broadcast(128), ['g3_%d' % i])
            wpg = sbB("wpg", [128, 8, D], BF16)
            for c in range(8):
                S.dma('pool', lambda e, c=c: e.dma_start(out=wpg[:, c, :], in_=wpg_d[c * 128:(c + 1) * 128, :]), w=['wpg%d' % c])
            WPG = ['wpg%d' % c for c in range(8)]
            wpu = sbB("wpu", [128, 2, D], BF16)
            for c in range(2):
                S.dma('pool', lambda e, c=c: e.dma_start(out=wpu[:, c, :], in_=wpu_d[c * 128:(c + 1) * 128, :]), w=['wpu%d' % c])
            WPU = ['wpu0', 'wpu1']
            skT = sbB("skT", [128, 16, 128], F32)
            s_sb = sbB("s_sb", [128, 16, 128], F32)
            for g in range(16):
                ld('sp', s_sb[:, g, :], sk_d[g, :, :], ['s_sb'])
            for g4i in range(4):
                bank = g4i % 2
                for i in range(4):
                    g = g4i * 4 + i
                    tr(pbk[bank][:, i * 128:(i + 1) * 128], s_sb[:, g, :], identf, ['s_sb', 'cm'], [PB[bank]])
                cp('act', skT[:, g4i * 4:g4i * 4 + 4, :].rearrange("p g k -> p (g k)"), pbk[bank][:], [PB[bank]], ['skT'])

            NSB = 2
            ubuf = [sbB("ubuf%d" % i, [128, 4, D], BF16) for i in range(NSB)]
            vbuf = [sbB("vbuf%d" % i, [128, 4, D], BF16) for i in range(NSB)]
            wqb = [sbB("wqb%d" % i, [128, 8, 256], BF16) for i in range(2)]

            k = 0
            for kc in range(8):
                for hf in range(2):
                    b = k % 2
                    stg = wqb[b][:].rearrange("p a n -> p (a n)")[:, 0:1024]
                    S.dma('pool', lambda e, stg=stg, kc=kc, hf=hf: e.dma_start(out=stg, in_=wpq_d[kc * 128:(kc + 1) * 128, hf * 1024:(hf + 1) * 1024]), w=['wqb%d' % b])
                    S.dma('sp', lambda e, stg=stg, kc=kc, hf=hf: e.dma_start(out=wpqb_d[kc * 128:(kc + 1) * 128, hf * 1024:(hf + 1) * 1024], in_=stg), r=['wqb%d' % b], w=['wpqb'])
                    k += 1
            pu_v = pu_d.rearrange("(i j) d -> j i d", j=128)
            pv_v = pv_d.rearrange("(i j) d -> j i d", j=128)
            for j in range(128):
                b = j % NSB
                ust = ubuf[b][:, 0, :]; uT = ubuf[b][:, 1, :]; vst = ubuf[b][:, 2, :]
                k0, k1, k2 = 'ub%d_0' % b, 'ub%d_1' % b, 'ub%d_2' % b
                S.dma('pool', lambda e, ust=ust, j=j: e.dma_start(out=ust, in_=pu_v[j, :, :]), w=[k0])
                pt_, pk_ = PTB[j % 2]
                for kc in range(8):
                    tr(pt_[:, kc, :], ust[:, kc * 128:(kc + 1) * 128], identb[:], [k0, 'identb'], [pk_])
                cp('act' if j % 2 else 'dve', uT.rearrange("p (a b) -> p a b", b=128), pt_[:, :, :], [pk_], [k1])
                S.dma('sp', lambda e, uT=uT, j=j: e.dma_start(out=utb_d[j, :, :], in_=uT), r=[k1], w=['utb'])
                S.dma('pool', lambda e, vst=vst, j=j: e.dma_start(out=vst, in_=pv_v[j, :, :]), w=[k2])
                S.dma('act', lambda e, vst=vst, j=j: e.dma_start(out=vtb_d[j, :, :], in_=vst), r=[k2], w=['vtb'])
            S.barrier()
            ck(101)

            x1b = [sbB("x1b0", [128, D], F32)] * 2
            ptl = [sbB("ptl%d" % i, [128, 256], F32) for i in range(2)]
            h2b = sbB("h2b", [128, D], BF16)
            h2T = sbB("h2T", [128, 8, 128], BF16)
            qc = sbB("qc", [128, 2048], F32)
            qT = qc[:].rearrange("p (g t) -> p g t", t=128)
            cand = qc[:].rearrange("p (h x) -> p h x", x=256)
            s_rp = sbB("s_rp", [128, 128], F32)
            vals = sbB("vals", [128, 16, 16], F32)
            cand2 = sbB("cand2", [128, 256], F32)
            best = sbB("best", [128, 8, 16], F32)
            gat = sbB("gat", [128, 8, 16], F32)
            gsum = sbB("gsum", [128, 8], F32)
            bias8 = sbB("bias8", [128, 8], F32)
            IB = 16
            Ptok = [sbB("Ptok0", [128, 128, IB], BF16)] * 2
            PTt = sbB("PTt", [128, 128, 128], BF16)
            JB = 16
            TG = 512 // JB
            NJB = 128 // JB
            xq = sbB("xq", [128, 4, 16, JB], F32)
            eq = sbB("eq", [128, 4, 16, JB], BF16)
            Qtok = [sbB("Qtok%d" % i, [128, 128, JB], BF16) for i in range(2)]
            QTt = [sbB("QTt%d" % i, [128, JB, 128], BF16) for i in range(2)]
            act_sb2 = [sbB("act_sb%d" % i, [128, JB, 128], BF16) for i in range(2)]
            coef2 = [sbB("coef%d" % i, [128, JB, 128], BF16) for i in range(2)]
            x2 = sbB("x2", [128, D], F32)
            p_bf = sbB("p_bf", [128, 256], BF16)
            pTt = sbB("pTt", [128, 2, 128], BF16)
            pg_s = sbB("pg_s", [128, D], F32)
            uctr = [0]; vctr = [0]; qctr = [0]; pbc = [0]

            def nextptb():
                r_ = PTB[pbc[0] % 2]
                pbc[0] += 1
                return r_

            for n in range(nt):
                par = n % 2
                X1, X1K = x1b[0], 'x1b0'
                ld('sp', X1[:], out_d[n * 128:(n + 1) * 128, :], [X1K], r=['out%d' % n])
                ld('sp', ptl[par][:], p_d[n * 128:(n + 1) * 128, :], ['ptl%d' % par])
                ck(102)
                rmsnorm(X1[:], X1K, g3[:, 0, :], 'g3_0', h2b[:], 'h2b')
                for c in range(8):
                    tr(ptb[:, c, :], h2b[:, c * 128:(c + 1) * 128], identb[:], ['h2b', 'identb'], ['ptb'])
                cp('act', h2T[:], ptb[:], ['ptb'], ['h2T'])
                for g4i in range(4):
                    bank = 2 + (g4i % 2)
                    for i2 in range(2):
                        wb = qctr[0] % 2
                        qctr[0] += 1
                        c0 = g4i * 512 + i2 * 256
                        S.dma('sp', lambda e, wb=wb, c0=c0: e.dma_start(
                            out=wqb[wb][:], in_=wpqb_d[:, c0:c0 + 256].rearrange("(kc p) n -> p kc n", p=128)),
                            r=['wpqb'], w=['wqb%d' % wb])
                        for i1 in range(2):
                            i = i2 * 2 + i1
                            for kc in range(8):
                                mm(pbk[bank][:, i * 128:(i + 1) * 128], wqb[wb][:, kc, i1 * 128:(i1 + 1) * 128], h2T[:, kc, :], kc == 0, kc == 7, ['h2T', 'wqb%d' % wb], [PB[bank]])
                    cp('act', qT[:, g4i * 4:g4i * 4 + 4, :].rearrange("p g t -> p (g t)"), pbk[bank][:], [PB[bank]], ['qc'])
                for g4i in range(4):
                    bank = 4 + (g4i % 2)
                    for i in range(4):
                        g = g4i * 4 + i
                        mm(pbk[bank][:, i * 128:(i + 1) * 128], qT[:, g, :], skT[:, g, :], True, True, ['qc', 'skT'], [PB[bank]])
                    cp('act', s_sb[:, g4i * 4:g4i * 4 + 4, :].rearrange("p g k -> p (g k)"), pbk[bank][:], [PB[bank]], ['s_sb'])
                ck(103)
                for g in range(16):
                    S.op('dve', lambda e, g=g: e.max(out=vals[:, g, 0:8], in_=s_sb[:, g, :]), r=['s_sb'], w=['vals'])
                    S.op('dve', lambda e, g=g: e.match_replace(out=s_rp[:], in_to_replace=vals[:, g, 0:8], in_values=s_sb[:, g, :], imm_value=-1e30), r=['s_sb', 'vals'], w=['s_rp'])
                    S.op('dve', lambda e, g=g: e.max(out=vals[:, g, 8:16], in_=s_rp[:]), r=['s_rp'], w=['vals'])
                v4 = vals[:].rearrange("p (h c) a -> p h c a", c=2)
                s4 = s_sb[:].rearrange("p (h c) k -> p h c k", c=2)
                for hh in range(8):
                    tt('dve', cand[:, hh, :].rearrange("p (a b) -> p a b", b=16), bc(v4[:, hh, 0, :], 2, [128, 16, 16]), bc(v4[:, hh, 1, :], 1, [128, 16, 16]), ALU.add, ['vals'], ['qc'])
                for hh in range(8):
                    S.op('dve', lambda e, hh=hh: e.max(out=best[:, hh, 0:8], in_=cand[:, hh, :]), r=['qc'], w=['best'])
                    S.op('dve', lambda e, hh=hh: e.match_replace(out=cand2[:], in_to_replace=best[:, hh, 0:8], in_values=cand[:, hh, :], imm_value=-1e30), r=['qc', 'best'], w=['cand2'])
                    S.op('dve', lambda e, hh=hh: e.max(out=best[:, hh, 8:16], in_=cand2[:]), r=['cand2'], w=['best'])
                ck(104)
                tt('dve', gat[:], best[:], bc(best[:, :, 0], 2, [128, 8, 16]), ALU.subtract, ['best'], ['gat'])
                act(gat[:], gat[:], AF.Exp, ['gat'], ['gat'])
                red(gsum[:], gat[:], ALU.add, ['gat'], ['gsum'])
                act(gsum[:], gsum[:], AF.Ln, ['gsum'], ['gsum'])
                stt(bias8[:], best[:, :, 0], -1.0, gsum[:], ALU.mult, ALU.subtract, ['best', 'gsum'], ['bias8'])
                ck(105)

                for ib in range(128 // IB):
                    pb_ = 0
                    tt('dve', Ptok[pb_][:].rearrange("t (h a) i -> t h a i", a=16),
                       bc(s4[:, :, 0, ib * IB:(ib + 1) * IB], 2, [128, 8, 16, IB]),
                       bc(v4[:, :, 0, :], 3, [128, 8, 16, IB]), ALU.is_equal, ['s_sb', 'vals'], ['Ptok%d' % pb_])
                    for i8 in range(IB // 8):
                        pt_, pk_ = nextptb()
                        for il in range(8):
                            tr(pt_[:, il, :], Ptok[pb_][:, :, i8 * 8 + il], identb[:], ['Ptok%d' % pb_, 'identb'], [pk_])
                        i0 = ib * IB + i8 * 8
                        cp('act', PTt[:, i0:i0 + 8, :], pt_[:, :, :], [pk_], ['PTt'])

                def q_elem(jb):
                    qb_ = jb % 2
                    for hg in range(2):
                        hsl = slice(hg * 4, hg * 4 + 4)
                        tt('dve', xq[:], bc(v4[:, hsl, 0, :], 3, [128, 4, 16, JB]), bc(s4[:, hsl, 1, jb * JB:(jb + 1) * JB], 2, [128, 4, 16, JB]), ALU.add, ['vals', 's_sb'], ['xq'])
                        for h_ in range(4):
                            hh = hg * 4 + h_
                            act(eq[:, h_, :, :], xq[:, h_, :, :], AF.Exp, ['xq', 'bias8'], ['eq'], bias=bias8[:, hh:hh + 1])
                        tt('dve', xq[:].rearrange("p h a j -> p h (a j)"), xq[:].rearrange("p h a j -> p h (a j)"), bc(best[:, hsl, 15], 2, [128, 4, 16 * JB]), ALU.is_ge, ['xq', 'eq', 'best'], ['xq'])
                        tt('dve', Qtok[qb_][:, hg * 64:(hg + 1) * 64, :].rearrange("p (h a) j -> p h a j", a=16), xq[:], eq[:], ALU.mult, ['xq', 'eq'], ['Qtok%d' % qb_])

                def q_tr(jb):
                    qb_ = jb % 2
                    for j8 in range(JB // 8):
                        pt_, pk_ = nextptb()
                        for jl in range(8):
                            tr(pt_[:, jl, :], Qtok[qb_][:, :, j8 * 8 + jl], identb[:], ['Qtok%d' % qb_, 'identb'], [pk_])
                        cp('act', QTt[qb_][:, j8 * 8:j8 * 8 + 8, :], pt_[:, :, :], [pk_], ['QTt%d' % qb_])

                def act_blk(jb):
                    ab = jb % 2
                    for jq in range(JB // 4):
                        j0 = jb * JB + jq * 4
                        ub = uctr[0] % NSB
                        uctr[0] += 1
                        S.dma('sp', lambda e, ub=ub, j0=j0: e.dma_start(out=ubuf[ub][:], in_=utb_d[j0:j0 + 4, :, :].rearrange("j d x -> d j x")),
                              r=['utb'], w=['ubuf%d' % ub])
                        bank = 2 + (jq % 2)
                        for jl in range(4):
                            for kc in range(8):
                                mm(pbk[bank][:, jl * 128:(jl + 1) * 128], ubuf[ub][:, jl, kc * 128:(kc + 1) * 128], h2T[:, kc, :], kc == 0, kc == 7, ['ubuf%d' % ub, 'h2T'], [PB[bank]])
                        act(act_sb2[ab][:, jq * 4:jq * 4 + 4, :].rearrange("i j t -> i (j t)"), pbk[bank][:], AF.Gelu_apprx_tanh, [PB[bank]], ['act_sb%d' % ab])

                def w_blk(jb):
                    qb_ = jb % 2
                    for tg in range(128 // TG):
                        bank = 4 + (tg % 2)
                        for tl in range(TG):
                            t = tg * TG + tl
                            mm(pbk[bank][:, tl * JB:(tl + 1) * JB], PTt[:, :, t], QTt[qb_][:, :, t], True, True, ['PTt', 'QTt%d' % qb_], [PB[bank]])
                        tt('dve', coef2[qb_][:, :, tg * TG:(tg + 1) * TG], pbk[bank][:].rearrange("i (t j) -> i j t", j=JB), act_sb2[qb_][:, :, tg * TG:(tg + 1) * TG], ALU.mult, [PB[bank], 'act_sb%d' % qb_], ['coef%d' % qb_])

                def v_blk(jb):
                    qb_ = jb % 2
                    for jq in range(JB // 4):
                        j0 = jb * JB + jq * 4
                        vb = vctr[0] % NSB
                        vctr[0] += 1
                        S.dma('sp', lambda e, vb=vb, j0=j0: e.dma_start(out=vbuf[vb][:], in_=vtb_d[j0:j0 + 4, :, :].rearrange("j i x -> i j x")),
                              r=['vtb'], w=['vbuf%d' % vb])
                        for jl in range(4):
                            j = j0 + jl
                            for hf in range(2):
                                mm(pbk[hf][:], coef2[qb_][:, jq * 4 + jl, :], vbuf[vb][:, jl, hf * 512:(hf + 1) * 512], j == 0, j == 127, ['coef%d' % qb_, 'vbuf%d' % vb], [PB[hf]])

                q_elem(0)
                q_tr(0)
                for jb in range(NJB):
                    if jb + 1 < NJB:
                        q_elem(jb + 1)
                    act_blk(jb)
                    if jb + 1 < NJB:
                        q_tr(jb + 1)
                    w_blk(jb)
                    if jb >= 1:
                        v_blk(jb - 1)
                v_blk(NJB - 1)
                ck(107)
                for hf in range(2):
                    sl = slice(hf * 512, (hf + 1) * 512)
                    tt('dve', x2[:, sl], pbk[hf][:], X1[:, sl], ALU.add, [PB[hf], X1K], ['x2'])
                rmsnorm(x2[:], 'x2', g3[:, 1, :], 'g3_1', h2b[:], 'h2b')
                for c in range(8):
                    tr(ptb[:, c, :], h2b[:, c * 128:(c + 1) * 128], identb[:], ['h2b', 'identb'], ['ptb'])
                cp('act', h2T[:], ptb[:], ['ptb'], ['h2T'])
                cp('pool', p_bf[:], ptl[par][:], ['ptl%d' % par], ['p_bf'])
                for c in range(2):
                    tr(ptb[:, c, :], p_bf[:, c * 128:(c + 1) * 128], identb[:], ['p_bf', 'identb'], ['ptb'])
                cp('act', pTt[:], ptb[:, 0:2, :], ['ptb'], ['pTt'])
                for hf in range(2):
                    sl = slice(hf * 512, (hf + 1) * 512)
                    for c in range(8):
                        mm(pbk[2 + hf][:], h2T[:, c, :], wpg[:, c, sl], c == 0, c == 7, ['h2T'] + WPG, [PB[2 + hf]])
                    act(pg_s[:, sl], pbk[2 + hf][:], AF.Sigmoid, [PB[2 + hf]], ['pg_s'])
                    for c in range(2):
                        mm(pbk[4 + hf][:], pTt[:, c, :], wpu[:, c, sl], c == 0, c == 1, ['pTt'] + WPU, [PB[4 + hf]])
                    tt('dve', pg_s[:, sl], pg_s[:, sl], pbk[4 + hf][:], ALU.mult, ['pg_s', PB[4 + hf]], ['pg_s'])
                tt('dve', pg_s[:], x2[:], pg_s[:], ALU.add, ['x2', 'pg_s'], ['pg_s'])
                rmsnorm(pg_s[:], 'pg_s', g3[:, 2, :], 'g3_2', x2[:], 'x2')
                S.dma('act', lambda e, n=n: e.dma_start(out=out_d[n * 128:(n + 1) * 128, :], in_=x2[:]), r=['x2'], w=['out%d' % n])
          except _Stop:
            pass

        S.wait_all('sp')
        S.emit()
    return nc


def make_in_maps(inputs, nt, ncores):
    f = np.float32
    c = host_consts(nt)
    g = lambda k: np.asarray(inputs[k], dtype=f)
    S_ = nt * 128
    mu = g('rwkv_mu')[0]
    pp = np.zeros((128, 34), f)
    pp[:, 0:14] = mu.reshape(14, 128).T
    for j, k in enumerate(('rwkv_w0', 'rwkv_a0', 'rwkv_k_k', 'rwkv_k_a')):
        pp[:, 14 + 4 * j:18 + 4 * j] = g(k)[0].reshape(4, 128).T
    pp[:, 30:34] = g('rwkv_r_k')[0].reshape(4, 128).T
    g4 = np.stack([g('g_mix')[0], g('g_ffn')[0], g('g_ple')[0], g('g_final')], 0)
    gn3 = np.stack([g('ret_gn_g')[0], g('rwkv_gn_g')[0], g('rwkv_gn_b')[0]], 0)
    lora = np.zeros((128, 3, 512), f)
    lora[0:64, 0, :] = g('rwkv_w_up')[0]
    lora[64:128, 1, :] = g('rwkv_a_up')[0]
    lora[:, 2, :] = g('rwkv_g_up')[0]
    wbr = np.stack([g('w_ret_br')[0], g('w_rwkv_br')[0]], 0)
    shared = dict(w_in=g('w_in')[0], pp=pp, g4=np.ascontiguousarray(g4), gn3=np.ascontiguousarray(gn3),
                  lora=lora, wbr=np.ascontiguousarray(wbr), w_o=g('w_o')[0], w_pq=g('w_pq')[0],
                  sk=np.ascontiguousarray(g('peer_sub_keys')[0].reshape(16, 128, 128)),
                  peer_u=g('peer_u')[0], peer_v=g('peer_v')[0], w_ple_gate=g('w_ple_gate')[0],
                  w_ple_up=g('w_ple_up')[0], rot=c['rot'], DT=c['DT'], xiT=c['xiT'], CDb=c['CDb'], cm=c['cm'])
    x = g('x'); p = g('p')[0]
    maps = []
    for i in range(ncores):
        m = dict(shared)
        m['x'] = np.ascontiguousarray(x[i, :S_])
        m['p'] = np.ascontiguousarray(p[i, :S_])
        maps.append(m)
    return maps


def kernel(**inputs):
    nt = SEQ // 128
    nc = build(nt)
    in_maps = make_in_maps(inputs, nt, NCORES)
    res = run_bass_kernel_spmd(nc, in_maps, core_ids=list(range(NCORES)))
    out = np.stack([np.asarray(r["out"], dtype=np.float32) for r in res.results], axis=0)
    return out
```

```python
import math
import numpy as np
from contextlib import ExitStack
import concourse.bass as bass
import concourse.mybir as mybir
from concourse.bass_utils import run_bass_kernel_spmd

F32 = mybir.dt.float32
BF16 = mybir.dt.bfloat16
U32 = mybir.dt.uint32
I32 = mybir.dt.int32
AF = mybir.ActivationFunctionType
ALU = mybir.AluOpType
AX = mybir.AxisListType

D = 1024
SEQ = 4096
NCORES = 8
IN_COLS = 5888
C0 = math.exp(-0.5)


class Sched:
    SELF_SYNC = {'pe': False, 'act': True, 'dve': True, 'pool': True, 'sp': True}

    def __init__(self, nc, es, n_dma_sems=8):
        self.nc = nc
        self.ops = {e: [] for e in ('pe', 'act', 'dve', 'pool', 'sp')}
        self.sem = {e: es.enter_context(nc.semaphore('prog_' + e)) for e in self.ops}
        self.cnt = {e: 0 for e in self.ops}
        self.waited = {e: {} for e in self.ops}
        self.last_w = {}
        self.readers = {}
        self.dsem = {}
        self.dcnt = {}
        self.drr = {}
        for q in ('sp', 'act', 'pool'):
            self.dsem[q] = [es.enter_context(nc.semaphore('dma_%s_%d' % (q, i)))
                            for i in range(n_dma_sems)]
            self.dcnt[q] = [0] * n_dma_sems
            self.drr[q] = 0
        self.sem_id = {}
        self.pending = {e: [] for e in self.ops}

    def barrier(self):
        toks = list(self.last_w.values())
        for ts_ in self.readers.values():
            toks.extend(ts_)
        for e in self.ops:
            self.pending[e] = list(toks)

    def _deps(self, r, w):
        toks = []
        for k in r:
            t = self.last_w.get(k)
            if t is not None:
                toks.append(t)
        for k in w:
            t = self.last_w.get(k)
            if t is not None:
                toks.append(t)
            toks.extend(self.readers.get(k, ()))
        return toks

    def _waits(self, e, toks):
        need = {}
        for (sem, val, src) in toks:
            if src == e and not self.SELF_SYNC[e]:
                continue
            key = id(sem)
            self.sem_id[key] = sem
            if self.waited[e].get(key, 0) >= val:
                continue
            if need.get(key, 0) < val:
                need[key] = val
        out = []
        for key, val in need.items():
            self.waited[e][key] = val
            out.append((self.sem_id[key], val))
        return out

    def _commit(self, tok, r, w):
        for k in w:
            self.last_w[k] = tok
            self.readers[k] = []
        for k in r:
            if k in w:
                continue
            self.readers.setdefault(k, []).append(tok)

    def op(self, e, fn, r=(), w=()):
        r = list(r); w = list(w)
        toks = self._deps(r, w) + self.pending[e]
        self.pending[e] = []
        waits = self._waits(e, toks)
        self.cnt[e] += 1
        tok = (self.sem[e], self.cnt[e], e)
        self.ops[e].append((waits, fn, (self.sem[e], 1)))
        self._commit(tok, r, w)
        return tok

    def dma(self, q, fn, r=(), w=()):
        r = list(r); w = list(w)
        j = self.drr[q]
        self.drr[q] = (j + 1) % len(self.dsem[q])
        sem = self.dsem[q][j]
        toks = self._deps(r, w) + self.pending[q]
        self.pending[q] = []
        if self.dcnt[q][j] > 0:
            toks.append((sem, 16 * self.dcnt[q][j], None))
        waits = self._waits(q, toks)
        self.dcnt[q][j] += 1
        tok = (sem, 16 * self.dcnt[q][j], None)
        self.ops[q].append((waits, fn, (sem, 16)))
        self._commit(tok, r, w)
        return tok

    def wait_all(self, e):
        toks = list(self.last_w.values())
        for ts in self.readers.values():
            toks.extend(ts)
        waits = self._waits(e, toks)
        self.ops[e].append((waits, None, None))

    def emit(self):
        nc = self.nc
        with nc.Block() as block:
            def run(e, eng):
                for waits, fn, inc in self.ops[e]:
                    for sem, val in waits:
                        eng.wait_ge(sem, val)
                    if fn is not None:
                        ins = fn(eng)
                        ins.then_inc(inc[0], inc[1])

            @block.sync
            def _(eng):
                run('sp', eng)

            @block.scalar
            def _(eng):
                run('act', eng)

            @block.vector
            def _(eng):
                run('dve', eng)

            @block.gpsimd
            def _(eng):
                run('pool', eng)

            @block.tensor
            def _(eng):
                run('pe', eng)


def host_consts(nt):
    f = np.float32
    S = nt * 128
    half = 32
    inv_freq = (10000.0 ** (-np.arange(half, dtype=f) * f(2.0) / f(64))).astype(f)
    ang = (np.arange(S, dtype=f)[:, None] * inv_freq[None, :]).astype(f)
    cos = np.cos(ang).astype(f); sin = np.sin(ang).astype(f)
    rot = np.zeros((nt, 128, 128), f)
    rot[:, :, 0:32] = cos.reshape(nt, 128, 32)
    rot[:, :, 32:64] = sin.reshape(nt, 128, 32)
    rot[:, :, 64:96] = cos.reshape(nt, 128, 32) * f(0.125)
    rot[:, :, 96:128] = sin.reshape(nt, 128, 32) * f(0.125)
    H = 8
    lg = np.log1p(-(2.0 ** (-5.0 - np.arange(H, dtype=np.float64))))
    idx = np.arange(128, dtype=np.float64)
    diff = idx[None, :] - idx[:, None]
    DT = np.where(diff[:, None, :] >= 0, np.exp(lg[None, :, None] * np.maximum(diff, 0)[:, None, :]), 0.0).astype(f)
    xiT = np.zeros((128, 4, 128), f)
    CDb = np.zeros((128, 4, 64), f)
    for c in range(4):
        for p in range(128):
            h = 2 * c + p // 64
            xiT[p, c, :] = np.exp(lg[h] * (idx + 1.0))
            CDb[p, c, :] = np.exp(lg[h] * 128.0)
    ZT = np.exp(lg[None, :] * (127.0 - idx)[:, None]).astype(f)
    s = np.arange(128)
    SU = (s[None, :] > s[:, None]).astype(f)
    SUI = (s[None, :] >= s[:, None]).astype(f)
    SL = SU.T.copy()
    BO = np.zeros((128, 128), f); BO[:64, :64] = 1; BO[64:, 64:] = 1
    HS = np.zeros((128, 2), f); HS[:64, 0] = 1; HS[64:, 1] = 1
    ident = np.eye(128, dtype=f)
    io16 = np.tile(np.arange(16, dtype=f)[None, :], (128, 1))
    cm = np.concatenate([ident, SU, SUI, SL, BO, ZT, HS, io16, io16 * 16], axis=1)
    return dict(rot=rot, DT=DT.reshape(128, 1024), xiT=xiT.reshape(128, 512),
                CDb=CDb.reshape(128, 256), cm=np.ascontiguousarray(cm))


CM_W = 128 * 5 + 8 + 2 + 16 + 16


class _Stop(Exception):
    pass


def build(nt, stage='full', stop_after=None):
    nc = bass.Bass("TRN2", target_bir_lowering=False)
    S_ = nt * 128
    WDT = BF16
    dram = lambda n, s, d, k="ExternalInput": nc.dram_tensor(n, s, d, kind=k).ap()
    x_d = dram("x", [S_, D], F32)
    p_d = dram("p", [S_, 256], F32)
    w_in_d = dram("w_in", [D, IN_COLS], F32)
    pp_d = dram("pp", [128, 34], F32)
    g4_d = dram("g4", [4, D], F32)
    gn3_d = dram("gn3", [3, 512], F32)
    lora_d = dram("lora", [128, 3, 512], F32)
    wbr_d = dram("wbr", [2, 512, D], F32)
    wo_d = dram("w_o", [D, D], F32)
    wpq_d = dram("w_pq", [D, 2048], F32)
    sk_d = dram("sk", [16, 128, 128], F32)
    pu_d = dram("peer_u", [16384, D], F32)
    pv_d = dram("peer_v", [16384, D], F32)
    wpg_d = dram("w_ple_gate", [D, D], F32)
    wpu_d = dram("w_ple_up", [256, D], F32)
    rot_d = dram("rot", [nt, 128, 128], F32)
    DT_d = dram("DT", [128, 1024], F32)
    xiT_d = dram("xiT", [128, 512], F32)
    CDb_d = dram("CDb", [128, 256], F32)
    cm_d = dram("cm", [128, CM_W], F32)
    out_d = dram("out", [S_, D], F32, "ExternalOutput")
    winb_d = nc.dram_tensor("winb", [D, IN_COLS], BF16, kind="Internal").ap()
    utb_d = nc.dram_tensor("utb", [128, 128, D], BF16, kind="Internal").ap()
    vtb_d = nc.dram_tensor("vtb", [128, 128, D], BF16, kind="Internal").ap()
    wpqb_d = nc.dram_tensor("wpqb", [D, 2048], BF16, kind="Internal").ap()

    es = ExitStack()
    with es:
        S = Sched(nc, es)
        sb = lambda n, s, d: es.enter_context(nc.sbuf_tensor("sb_" + n, s, d))
        ps = lambda n, s, d: es.enter_context(nc.psum_tensor(n, s, d))

        def mm(out, lhsT, rhs, start, stop, r, w):
            S.op('pe', lambda e: e.matmul(out, lhsT=lhsT, rhs=rhs, start=start, stop=stop), r=r, w=w)

        def tr(out, in_, ident, r, w):
            S.op('pe', lambda e: e.transpose(out=out, in_=in_, identity=ident), r=r, w=w)

        def act(out, in_, func, r, w, **kw):
            S.op('act', lambda e: e.activation(out=out, in_=in_, func=func, **kw), r=r, w=w)

        def cp(eng, out, in_, r, w):
            if eng == 'act':
                S.op('act', lambda e: e.copy(out=out, in_=in_), r=r, w=w)
            else:
                S.op(eng, lambda e: e.tensor_copy(out=out, in_=in_), r=r, w=w)

        def tt(eng, out, in0, in1, op, r, w):
            S.op(eng, lambda e: e.tensor_tensor(out=out, in0=in0, in1=in1, op=op), r=r, w=w)

        def ts(eng, out, in0, s1, s2, op0, op1, r, w):
            if op1 is None:
                S.op(eng, lambda e: e.tensor_scalar(out=out, in0=in0, scalar1=s1, scalar2=None, op0=op0), r=r, w=w)
            else:
                S.op(eng, lambda e: e.tensor_scalar(out=out, in0=in0, scalar1=s1, scalar2=s2, op0=op0, op1=op1), r=r, w=w)

        def stt(out, in0, scalar, in1, op0, op1, r, w):
            S.op('dve', lambda e: e.scalar_tensor_tensor(out=out, in0=in0, scalar=scalar, in1=in1, op0=op0, op1=op1), r=r, w=w)

        def red(out, in_, op, r, w, axis=AX.X):
            S.op('dve', lambda e: e.tensor_reduce(out=out, in_=in_, axis=axis, op=op), r=r, w=w)

        def rcp(t, k):
            S.op('dve', lambda e: e.reciprocal(out=t, in_=t), r=[k], w=[k])

        def ld(q, out, in_, w, r=()):
            S.dma(q, lambda e: e.dma_start(out=out, in_=in_), r=r, w=w)

        def bc(ap, axis, shape):
            return ap.unsqueeze(axis).to_broadcast(shape)

        ptb = ps("ptb", [128, 8, 128], BF16)
        pbk = [ps("pb%d" % i, [128, 512], F32) for i in range(7)]
        PB = ['pb%d' % i for i in range(7)]

        cm = sb("cm", [128, CM_W], F32)
        ld('sp', cm[:], cm_d[:, :], ['cm'])
        identf = cm[:, 0:128]
        SU = cm[:, 128:256]; SUI = cm[:, 256:384]; SL = cm[:, 384:512]; BO = cm[:, 512:640]
        ZT = cm[:, 640:648]; HS = cm[:, 648:650]; IO16 = cm[:, 650:666]; IO16X = cm[:, 666:682]
        identb = sb("identb", [128, 128], BF16)
        cp('dve', identb[:], identf, ['cm'], ['identb'])
        eps_t = sb("eps_t", [128, 4], F32)
        S.op('dve', lambda e: e.memset(eps_t[:, 0:1], 1e-6), w=['eps'])
        S.op('dve', lambda e: e.memset(eps_t[:, 1:2], 1e-5), w=['eps'])
        S.op('dve', lambda e: e.memset(eps_t[:, 2:3], 64e-5), w=['eps'])
        sq_junk = sb("sq_junk", [128, D], BF16)
        rs_ss = sb("rs_ss", [128, 1], F32)
        rs_rstd = sb("rs_rstd", [128, 1], F32)

        def rmsnorm(src, src_key, gtab, gkey, dst, dst_key):
            act(sq_junk[:], src, AF.Square, [src_key], ['sq_junk', 'rs_ss'], accum_out=rs_ss[:])
            act(rs_rstd[:], rs_ss[:], AF.Sqrt, ['rs_ss', 'eps'], ['rs_rstd'], scale=1.0 / D, bias=eps_t[:, 0:1])
            rcp(rs_rstd[:], 'rs_rstd')
            stt(dst, src, rs_rstd[:, 0:1], gtab, ALU.mult, ALU.mult, [src_key, 'rs_rstd', gkey], [dst_key])

        def ck(k):
            if stop_after == k:
                raise _Stop()
        esA = ExitStack()
        with esA:
          try:
            sbA = lambda n, s, d: esA.enter_context(nc.sbuf_tensor("sa_" + n, s, d))
            pp = sbA("pp", [128, 34], F32)
            ld('sp', pp[:], pp_d[:, :], ['pp'])
            MU = pp[:, 0:14]; W0 = pp[:, 14:18]; A0 = pp[:, 18:22]; KK_ = pp[:, 22:26]; KA = pp[:, 26:30]; RK = pp[:, 30:34]
            omm = sbA("omm", [128, 14], F32)
            ts('dve', omm[:], MU, -1.0, 1.0, ALU.mult, ALU.add, ['pp'], ['omm'])
            omka = sbA("omka", [128, 4], F32)
            ts('dve', omka[:], KA, -1.0, 1.0, ALU.mult, ALU.add, ['pp'], ['omka'])
            gmix = sbA("gmix", [128, D], F32)
            ld('sp', gmix[:], g4_d[0, :].partition_broadcast(128), ['gmix'])
            gn3 = sbA("gn3", [128, 3, 512], F32)
            for i in range(3):
                ld('sp', gn3[:, i, :], gn3_d[i, :].partition_broadcast(128), ['gn3_%d' % i])

            NWB = 3
            wch = [sbA("wch%d" % i, [128, 8, 512], BF16) for i in range(NWB)]
            k = 0
            for kc in range(8):
                for cb in range(4):
                    b = k % NWB
                    stg = wch[b][:].rearrange("p a n -> p (a n)")[:, 0:1472]
                    S.dma('pool', lambda e, stg=stg, kc=kc, cb=cb: e.dma_start(out=stg, in_=w_in_d[kc * 128:(kc + 1) * 128, cb * 1472:(cb + 1) * 1472]), w=['wch%d' % b])
                    S.dma('sp', lambda e, stg=stg, kc=kc, cb=cb: e.dma_start(out=winb_d[kc * 128:(kc + 1) * 128, cb * 1472:(cb + 1) * 1472], in_=stg), r=['wch%d' % b], w=['winb'])
                    k += 1
            wbr = sbA("wbr", [128, 2, 4, D], BF16)
            for i in range(2):
                for c in range(4):
                    S.dma('pool', lambda e, i=i, c=c: e.dma_start(out=wbr[:, i, c, :], in_=wbr_d[i, c * 128:(c + 1) * 128, :]), w=['wbr%d%d' % (i, c)])
            wo = sbA("wo", [128, 8, D], BF16)
            for c in range(8):
                S.dma('pool', lambda e, c=c: e.dma_start(out=wo[:, c, :], in_=wo_d[c * 128:(c + 1) * 128, :]), w=['wo%d' % c])
            WBR = ['wbr%d%d' % (i, c) for i in range(2) for c in range(4)]
            WO = ['wo%d' % c for c in range(8)]
            lora = sbA("lora", [128, 3, 512], BF16)
            S.dma('pool', lambda e: e.dma_start(out=lora[:], in_=lora_d[:, :, :]), w=['lora'])
            DT = sbA("DT", [128, 8, 128], F32)
            ld('sp', DT[:].rearrange("p h i -> p (h i)"), DT_d[:, :], ['DT'])
            xiT = sbA("xiT", [128, 4, 128], F32)
            ld('sp', xiT[:].rearrange("p c i -> p (c i)"), xiT_d[:, :], ['xiT'])
            CDb = sbA("CDb", [128, 4, 64], F32)
            ld('sp', CDb[:].rearrange("p c i -> p (c i)"), CDb_d[:, :], ['CDb'])

            CH = [(0, 512), (512, 512), (1024, 512), (1536, 512),
                  (2048, 512), (2560, 512), (3072, 512), (3584, 256),
                  (3840, 512), (4352, 512), (4864, 512), (5376, 512)]
            wctr = [k]

            def load_chunk(ci):
                b = wctr[0] % NWB
                wctr[0] += 1
                c0, cw = CH[ci]
                S.dma('sp', lambda e, b=b, c0=c0, cw=cw: e.dma_start(
                    out=wch[b][:, :, 0:cw], in_=winb_d[:, c0:c0 + cw].rearrange("(kc p) n -> p kc n", p=128)),
                    r=['winb'], w=['wch%d' % b])
                return b

            xt = [sbA("xt%d" % i, [128, D], F32) for i in range(2)]
            rot_t = [sbA("rot%d" % i, [128, 128], F32) for i in range(2)]
            h = sbA("h", [128, D], BF16)
            hT = sbA("hT", [128, 8, 128], BF16)
            qk_rot = sbA("qk_rot", [128, 2, 512], BF16)
            rt = [sbA("rt%d" % i, [128, 8, 32], F32) for i in range(4)]
            v_tok = sbA("v_tok", [128, 512], BF16)
            gr_s = sbA("gr_s", [128, 512], BF16)
            gate_s = sbA("gate_s", [128, 2048], BF16)
            qkT = sbA("qkT", [128, 8, 128], BF16)
            qxT = sbA("qxT", [128, 4, 128], BF16)
            kz = sbA("kz", [128, 8, 64], BF16)
            PT = sbA("PT", [128, 8, 128], BF16)
            R32 = sbA("R32", [128, 4, 64], F32)
            Rb = sbA("Rb", [128, 4, 128], BF16)
            qTm = sbA("qTm", [128, 2, 4, 128], BF16)
            S.op('dve', lambda e: e.memset(qTm[:], 0.0), w=['qTm'])
            Rtmp = sbA("Rtmp", [128, 4, 64], F32)
            S.op('dve', lambda e: e.memset(R32[:], 0.0), w=['R32'])
            S.op('dve', lambda e: e.memset(Rb[:], 0.0), w=['Rb'])
            hn_sq = sbA("hn_sq", [128, 8, 64], F32)
            hn_c = sbA("hn_c", [128, 8, 64], F32)
            hn_s = sbA("hn_s", [128, 8], F32)
            hn_q = sbA("hn_q", [128, 8], F32)
            hn_m = sbA("hn_m", [128, 8], F32)
            hn_r = sbA("hn_r", [128, 8], F32)
            y_bf = sbA("y_bf", [128, 512], BF16)
            yT = sbA("yT", [128, 4, 128], BF16)
            ZB = sbA("ZB", [128, 14, 129], F32)
            zlast = sbA("zlast", [128, 14, 1], F32)
            S.op('dve', lambda e: e.memset(zlast[:], 0.0), w=['zlast'])
            zs = sbA("zs", [128, 14, 128], F32)
            lor_in = sbA("lor_in", [128, 2, 128], BF16)
            f4 = [sbA("f4_%d" % i, [128, 4, 128], F32) for i in range(8)]
            F4 = ['f4_%d' % i for i in range(8)]
            f4ones = sbA("f4ones", [128, 128], F32)
            S.op('dve', lambda e: e.memset(f4ones[:], 1.0), w=['f4ones'])
            wk = {n_: sbA("wk_" + n_, [128, 4, 128], WDT) for n_ in ('ab', 'rb', 'bb', 'kb', 'bt', 'kt', 'vT')}
            tok3 = sbA("tok3", [128, 3, 512], WDT)
            Am = {n_: sbA("Am_" + n_, [128, 4, 128], WDT) for n_ in ('akT', 'rbT', 'rkT')}
            Nb = [sbA("Nb%d" % i, [128, 4, 128], WDT) for i in range(2)]
            NTb = [sbA("NTb%d" % i, [128, 4, 128], WDT) for i in range(2)]
            Qb = [sbA("Qb%d" % i, [128, 4, 128], WDT) for i in range(2)]
            BOw = sbA("BOw", [128, 128], F32)
            cp('dve', BOw[:], BO, ['cm'], ['BOw'])
            Xs = sbA("Xs", [128, 256], WDT)
            Us = sbA("Us", [128, 512], WDT)
            ST32 = sbA("ST32", [128, 4, 64], F32)
            STb = sbA("STb", [128, 4, 128], WDT)
            wkm = {n_: sbA("wkm_" + n_, [128, 2, 4, 128], WDT) for n_ in ('ab', 'bb', 'rb')}
            for n_ in ('ab', 'bb', 'rb'):
                S.op('pool', lambda e, n_=n_: e.memset(wkm[n_][:], 0.0), w=['wkm_' + n_])
            STtmp = sbA("STtmp", [128, 4, 64], F32)
            S.op('dve', lambda e: e.memset(ST32[:], 0.0), w=['ST32'])
            S.op('dve', lambda e: e.memset(STb[:], 0.0), w=['STb'])
            PCt = sbA("PCt", [128, 4], F32)
            g_tok = sbA("g_tok", [128, 512], F32)
            cbt = sbA("cbt", [128, 8], F32)
            merged = sbA("merged", [128, D], BF16)
            mT = sbA("mT", [128, 8, 128], BF16)
            x1 = sbA("x1", [128, D], F32)

            def headnorm(ops_ap, ops_key, eps_col, dst_c):
                red(hn_s[:], ops_ap, ALU.add, [ops_key], ['hn_s'])
                act(hn_sq[:], ops_ap, AF.Square, [ops_key], ['hn_sq'])
                red(hn_q[:], hn_sq[:], ALU.add, ['hn_sq'], ['hn_q'])
                ts('dve', hn_m[:], hn_s[:], 1.0 / 64, None, ALU.mult, None, ['hn_s'], ['hn_m'])
                tt('dve', hn_r[:], hn_m[:], hn_m[:], ALU.mult, ['hn_m'], ['hn_r'])
                stt(hn_r[:], hn_q[:], 1.0 / 64, hn_r[:], ALU.mult, ALU.subtract, ['hn_q', 'hn_r'], ['hn_r'])
                act(hn_r[:], hn_r[:], AF.Sqrt, ['hn_r', 'eps'], ['hn_r'], bias=eps_t[:, eps_col:eps_col + 1])
                rcp(hn_r[:], 'hn_r')
                tt('dve', dst_c, ops_ap, bc(hn_m[:], 2, [128, 8, 64]), ALU.subtract, [ops_key, 'hn_m'], ['hn_c'])
                tt('dve', dst_c, dst_c, bc(hn_r[:], 2, [128, 8, 64]), ALU.mult, ['hn_c', 'hn_r'], ['hn_c'])

            ck(1)
            for n in range(nt):
                par = n % 2
                X, XK = xt[par], 'xt%d' % par
                ld('sp', X[:], x_d[n * 128:(n + 1) * 128, :], [XK])
                ld('sp', rot_t[par][:], rot_d[n, :, :], ['rot%d' % par])
                ROT = 'rot%d' % par
                rmsnorm(X[:], XK, gmix[:], 'gmix', h[:], 'h')
                for c in range(8):
                    tr(ptb[:, c, :], h[:, c * 128:(c + 1) * 128], identb[:], ['h', 'identb'], ['ptb'])
                cp('act', hT[:], ptb[:], ['ptb'], ['hT'])

                ck(2)
                def inproj_tok(ci, bank):
                    b = load_chunk(ci)
                    for kc in range(8):
                        mm(pbk[bank][:], hT[:, kc, :], wch[b][:, kc, :], kc == 0, kc == 7, ['hT', 'wch%d' % b], [PB[bank]])

                Cc = rot_t[par][:, 0:32]; Sc = rot_t[par][:, 32:64]
                kCc = rot_t[par][:, 64:96]; kSc = rot_t[par][:, 96:128]
                for qi in range(2):
                    bank = qi
                    inproj_tok(qi, bank)
                    pv = pbk[bank][:].rearrange("p (h two f) -> p h two f", two=2, f=32)
                    q1 = pv[:, :, 0, :]; q2 = pv[:, :, 1, :]
                    cc, ssn = (Cc, Sc) if qi == 0 else (kCc, kSc)
                    cb_ = bc(cc, 1, [128, 8, 32]); sb_ = bc(ssn, 1, [128, 8, 32])
                    ov = qk_rot[:, qi, :].rearrange("p (h two f) -> p h two f", two=2, f=32)
                    tt('dve', rt[0][:], q1, cb_, ALU.mult, [PB[bank], ROT], ['rt0'])
                    tt('dve', rt[1][:], q2, sb_, ALU.mult, [PB[bank], ROT], ['rt1'])
                    tt('dve', rt[2][:], q1, sb_, ALU.mult, [PB[bank], ROT], ['rt2'])
                    tt('dve', rt[3][:], q2, cb_, ALU.mult, [PB[bank], ROT], ['rt3'])
                    tt('pool', ov[:, :, 0, :], rt[0][:], rt[1][:], ALU.subtract, ['rt0', 'rt1'], ['qk_rot'])
                    tt('pool', ov[:, :, 1, :], rt[2][:], rt[3][:], ALU.add, ['rt2', 'rt3'], ['qk_rot'])
                inproj_tok(2, 2)
                cp('act', v_tok[:], pbk[2][:], [PB[2]], ['v_tok'])
                inproj_tok(3, 3)
                act(gr_s[:], pbk[3][:], AF.Silu, [PB[3]], ['gr_s'])

                ck(3)
                for c in range(8):
                    tr(ptb[:, c, :], qk_rot[:, c // 4, (c % 4) * 128:(c % 4 + 1) * 128], identb[:], ['qk_rot', 'identb'], ['ptb'])
                cp('act', qkT[:, 4:8, :], ptb[:, 4:8, :], ['ptb'], ['qkT'])
                cp('act', qTm[0:64, 0, :, :], ptb[0:64, 0:4, :], ['ptb'], ['qTm'])
                cp('act', qTm[64:128, 1, :, :], ptb[64:128, 0:4, :], ['ptb'], ['qTm'])
                tt('dve', qxT[:], ptb[:, 0:4, :], xiT[:], ALU.mult, ['ptb', 'xiT'], ['qxT'])
                tt('pool', kz[:], qk_rot[:, 1, :].rearrange("p (h d) -> p h d", d=64), bc(ZT, 2, [128, 8, 64]), ALU.mult, ['qk_rot', 'cm'], ['kz'])

                ck(31)
                for hh in range(8):
                    c, base = hh // 2, (hh % 2) * 64
                    bank = 4 + hh // 4
                    mm(pbk[bank][:, (hh % 4) * 128:(hh % 4 + 1) * 128], qkT[:, 4 + c, :], qTm[:, hh % 2, c, :], True, True, ['qkT', 'qTm'], [PB[bank]])
                for g in range(2):
                    tt('dve', PT[:, 4 * g:4 * g + 4, :], pbk[4 + g][:].rearrange("p (h i) -> p h i", i=128), DT[:, 4 * g:4 * g + 4, :], ALU.mult, [PB[4 + g], 'DT'], ['PT'])
                ck(32)
                for c in range(4):
                    mm(pbk[6][:, c * 128:(c + 1) * 128], qxT[:, c, :], Rb[:, c, :], True, False, ['qxT', 'Rb'], [PB[6]])
                    for w_ in range(2):
                        hh = 2 * c + w_
                        mm(pbk[6][:, hh * 64:(hh + 1) * 64], PT[:, hh, :], v_tok[:, hh * 64:(hh + 1) * 64], False, w_ == 1, ['PT', 'v_tok'], [PB[6]])
                ck(33)
                for c in range(4):
                    mm(pbk[4][:, c * 128:(c + 1) * 128], kz[:, 2 * c:2 * c + 2, :].rearrange("p a d -> p (a d)"), v_tok[:, c * 128:(c + 1) * 128], True, True, ['kz', 'v_tok'], [PB[4]])
                tt('pool', Rtmp[:], R32[:], CDb[:], ALU.mult, ['R32', 'CDb'], ['Rtmp'])
                p4v = pbk[4][:].rearrange("p (c x) -> p c x", x=128)
                tt('dve', R32[0:64, :, :], Rtmp[0:64, :, :], p4v[0:64, :, 0:64], ALU.add, ['Rtmp', PB[4]], ['R32'])
                tt('dve', R32[64:128, :, :], Rtmp[64:128, :, :], p4v[64:128, :, 64:128], ALU.add, ['Rtmp', PB[4]], ['R32'])
                cp('pool', Rb[0:64, :, 0:64], R32[0:64, :, :], ['R32'], ['Rb'])
                cp('pool', Rb[64:128, :, 64:128], R32[64:128, :, :], ['R32'], ['Rb'])
                ck(34)
                o3 = pbk[6][:].rearrange("p (h e) -> p h e", e=64)
                headnorm(o3, PB[6], 1, hn_c[:])
                hc2 = hn_c[:].rearrange("p h e -> p (h e)")
                tt('dve', hc2, hc2, gn3[:, 0, :], ALU.mult, ['hn_c', 'gn3_0'], ['hn_c'])
                tt('dve', y_bf[:], hc2, gr_s[:], ALU.mult, ['hn_c', 'gr_s'], ['y_bf'])
                ck(35)
                for c in range(4):
                    tr(ptb[:, c, :], y_bf[:, c * 128:(c + 1) * 128], identb[:], ['y_bf', 'identb'], ['ptb'])
                cp('act', yT[:], ptb[:, 0:4, :], ['ptb'], ['yT'])
                for hf in range(2):
                    for c in range(4):
                        mm(pbk[hf][:], yT[:, c, :], wbr[:, 0, c, hf * 512:(hf + 1) * 512], c == 0, c == 3, ['yT'] + WBR, [PB[hf]])

                ck(4)
                for j in range(4):
                    b = load_chunk(4 + j)
                    nm = 4 if j < 3 else 2
                    bank = 2 + (j % 2)
                    for m in range(nm):
                        for kc in range(8):
                            mm(pbk[bank][:, m * 128:(m + 1) * 128], wch[b][:, kc, m * 128:(m + 1) * 128], hT[:, kc, :], kc == 0, kc == 7, ['hT', 'wch%d' % b], [PB[bank]])
                    cp('act', ZB[:, 4 * j:4 * j + nm, 1:129], pbk[bank][:, 0:nm * 128].rearrange("p (m t) -> p m t", t=128), [PB[bank]], ['ZB'])
                cp('pool', ZB[:, :, 0:1], zlast[:], ['zlast'], ['ZB'])
                for (m0, m1) in ((0, 4), (4, 8), (8, 12), (12, 14)):
                    nm = m1 - m0
                    tmp = f4[7][:, 0:nm, :]
                    tt('pool', tmp, ZB[:, m0:m1, 0:128], bc(MU[:, m0:m1], 2, [128, nm, 128]), ALU.mult, ['ZB', 'pp'], [F4[7]])
                    tt('dve', zs[:, m0:m1, :], ZB[:, m0:m1, 1:129], bc(omm[:, m0:m1], 2, [128, nm, 128]), ALU.mult, ['ZB', 'omm'], ['zs'])
                    tt('dve', zs[:, m0:m1, :], zs[:, m0:m1, :], tmp, ALU.add, ['zs', F4[7]], ['zs'])
                cp('pool', zlast[:], ZB[:, :, 128:129], ['ZB'], ['zlast'])
                ck(5)
                rF = zs[:, 0:4, :]; krF = zs[:, 4:8, :]; vF = zs[:, 8:12, :]
                act(lor_in[0:64, 0, :], zs[0:64, 12, :], AF.Tanh, ['zs'], ['lor_in'])
                cp('act', lor_in[64:128, 0, :], zs[64:128, 12, :], ['zs'], ['lor_in'])
                act(lor_in[:, 1, :], zs[:, 13, :], AF.Sigmoid, ['zs'], ['lor_in'])
                for m in range(4):
                    mm(pbk[2][:, m * 128:(m + 1) * 128], lora[:, 0, m * 128:(m + 1) * 128], lor_in[:, 0, :], True, True, ['lora', 'lor_in'], [PB[2]])
                for m in range(4):
                    mm(pbk[3][:, m * 128:(m + 1) * 128], lora[:, 1, m * 128:(m + 1) * 128], lor_in[:, 0, :], True, True, ['lora', 'lor_in'], [PB[3]])
                mm(pbk[4][:], lor_in[:, 1, :], lora[:, 2, :], True, True, ['lora', 'lor_in'], [PB[4]])
                cp('act', g_tok[:], pbk[4][:], [PB[4]], ['g_tok'])
                sg, asig, kkF, kkn, kpr, bF, csF, tmpF = f4
                for m in range(4):
                    act(sg[:, m, :], pbk[2][:, m * 128:(m + 1) * 128], AF.Sigmoid, [PB[2], 'pp'], [F4[0]], bias=W0[:, m:m + 1])
                for m in range(4):
                    act(asig[:, m, :], pbk[3][:, m * 128:(m + 1) * 128], AF.Sigmoid, [PB[3], 'pp'], [F4[1]], bias=A0[:, m:m + 1])
                b4 = lambda t: bc(t, 2, [128, 4, 128])
                f2 = lambda t: t[:].rearrange("p m t -> p (m t)")
                tt('dve', kkF[:], krF, b4(KK_), ALU.mult, ['zs', 'pp'], [F4[2]])
                tt('pool', tmpF[:], kkF[:], kkF[:], ALU.mult, [F4[2]], [F4[7]])
                mm(pbk[2][:], BOw[:], f2(tmpF), True, True, ['BOw', F4[7]], [PB[2]])
                act(f2(kkn), pbk[2][:], AF.Sqrt, [PB[2]], [F4[3]])
                ts('dve', kkn[:], kkn[:], 1e-12, None, ALU.max, None, [F4[3]], [F4[3]])
                rcp(kkn[:], F4[3])
                tt('dve', kkn[:], kkn[:], kkF[:], ALU.mult, [F4[3], F4[2]], [F4[3]])
                tt('pool', kpr[:], asig[:], b4(KA), ALU.mult, [F4[1], 'pp'], [F4[4]])
                tt('pool', kpr[:], kpr[:], b4(omka[:]), ALU.add, [F4[4], 'omka'], [F4[4]])
                tt('dve', kpr[:], kpr[:], krF, ALU.mult, [F4[4], 'zs'], [F4[4]])
                tt('pool', bF[:], kkn[:], asig[:], ALU.mult, [F4[3], F4[1]], [F4[5]])
                tt('pool', tmpF[:], rF, kpr[:], ALU.mult, ['zs', F4[4]], [F4[7]])
                tt('pool', tmpF[:], tmpF[:], b4(RK), ALU.mult, [F4[7], 'pp'], [F4[7]])
                for c in range(4):
                    mm(pbk[3][:, 2 * c:2 * c + 2], tmpF[:, c, :], HS, True, True, [F4[7], 'cm'], [PB[3]])
                cp('act', cbt[:], pbk[3][:, 0:8], [PB[3]], ['cbt'])
                for m in range(4):
                    S.op('dve', lambda e, m=m: e.tensor_tensor_scan(out=csF[:, m, :], data0=f4ones[:], data1=sg[:, m, :], initial=0.0, op0=ALU.mult, op1=ALU.add), r=[F4[0], 'f4ones'], w=[F4[6]])
                E1, E2 = kkF, tmpF
                act(E1[:], csF[:], AF.Exp, [F4[6]], [F4[2]], scale=-C0)
                act(E2[:], csF[:], AF.Exp, [F4[6]], [F4[7]], scale=C0)
                tt('dve', csF[:], csF[:], sg[:], ALU.subtract, [F4[6], F4[0]], [F4[6]])
                act(sg[:], csF[:], AF.Exp, [F4[6]], [F4[0]], scale=-C0)
                E3 = sg
                cp('dve', PCt[:], E1[:, :, 127], [F4[2]], ['PCt'])
                stt(wk['ab'][:], kkn[:], -1.0, E3[:], ALU.mult, ALU.mult, [F4[3], F4[0]], ['wk_ab'])
                tt('dve', wk['rb'][:], rF, E1[:], ALU.mult, ['zs', F4[2]], ['wk_rb'])
                tt('pool', csF[:], bF[:], E2[:], ALU.mult, [F4[5], F4[7]], [F4[6]])
                cp('act', wk['bb'][:], csF[:], [F4[6]], ['wk_bb'])
                tt('pool', wk['bt'][:], csF[:], bc(PCt[:], 2, [128, 4, 128]), ALU.mult, [F4[6], 'PCt'], ['wk_bt'])
                tt('dve', bF[:], kpr[:], E2[:], ALU.mult, [F4[4], F4[7]], [F4[5]])
                cp('act', wk['kb'][:], bF[:], [F4[5]], ['wk_kb'])
                tt('pool', wk['kt'][:], bF[:], bc(PCt[:], 2, [128, 4, 128]), ALU.mult, [F4[5], 'PCt'], ['wk_kt'])
                cp('act', wk['vT'][:], vF, ['zs'], ['wk_vT'])
                for n_ in ('ab', 'bb', 'rb'):
                    cp('pool', wkm[n_][0:64, 0, :, :], wk[n_][0:64, :, :], ['wk_' + n_], ['wkm_' + n_])
                    cp('pool', wkm[n_][64:128, 1, :, :], wk[n_][64:128, :, :], ['wk_' + n_], ['wkm_' + n_])
                ck(6)
                for i, nmk in enumerate(('bt', 'kt', 'vT')):
                    for c in range(4):
                        tr(ptb[:, c, :], wk[nmk][:, c, :], identb[:], ['wk_' + nmk, 'identb'], ['ptb'])
                    cp('act', tok3[:, i, :].rearrange("p (c x) -> p c x", x=128), ptb[:, 0:4, :], ['ptb'], ['tok3_%d' % i])
                Btok = tok3[:, 0, :]; Ktok = tok3[:, 1, :]; Vtok = tok3[:, 2, :]

                ck(7)
                def hop(hh):
                    return hh // 2, (hh % 2) * 64
                for g in range(2):
                    hs = [4 * g + i for i in range(4)]
                    specs = [('bb', 'ab', SU, Nb[0], 'Nb0'), ('ab', 'bb', SL, NTb[0], 'NTb0'),
                             ('kb', 'ab', SU, Am['akT'], 'Am_akT'), ('bb', 'rb', SUI, Am['rbT'], 'Am_rbT'),
                             ('kb', 'rb', SUI, Am['rkT'], 'Am_rkT')]
                    for si, (l_, r_, msk, dst, dk) in enumerate(specs):
                        bank = 2 + (si % 3)
                        for i, hh in enumerate(hs):
                            c, base = hop(hh)
                            mm(pbk[bank][:, i * 128:(i + 1) * 128], wk[l_][:, c, :], wkm[r_][:, hh % 2, c, :], True, True, ['wk_' + l_, 'wkm_' + r_], [PB[bank]])
                        tt('dve', dst[:], pbk[bank][:].rearrange("p (h t) -> p h t", t=128), bc(msk, 1, [128, 4, 128]), ALU.mult, [PB[bank], 'cm'], [dk])
                    tt('pool', Qb[0][:], Nb[0][:], bc(identb[:], 1, [128, 4, 128]), ALU.add, ['Nb0', 'identb'], ['Qb0'])
                    cur = 0
                    for lvl in range(6):
                        nx = 1 - cur
                        last = (lvl == 5)
                        if not last:
                            for i in range(4):
                                mm(pbk[2][:, i * 128:(i + 1) * 128], NTb[cur][:, i, :], Nb[cur][:, i, :], True, True, ['NTb%d' % cur, 'Nb%d' % cur], [PB[2]])
                        for i in range(4):
                            mm(pbk[3][:, i * 128:(i + 1) * 128], Nb[cur][:, i, :], NTb[cur][:, i, :], True, True, ['NTb%d' % cur, 'Nb%d' % cur], [PB[3]])
                        if not last:
                            cp('dve', Nb[nx][:].rearrange("p h t -> p (h t)"), pbk[2][:], [PB[2]], ['Nb%d' % nx])
                        cp('act', NTb[nx][:].rearrange("p h t -> p (h t)"), pbk[3][:], [PB[3]], ['NTb%d' % nx])
                        for i in range(4):
                            mm(pbk[4][:, i * 128:(i + 1) * 128], identb[:], Qb[cur][:, i, :], True, False, ['identb', 'Qb%d' % cur], [PB[4]])
                            mm(pbk[4][:, i * 128:(i + 1) * 128], NTb[nx][:, i, :], Qb[cur][:, i, :], False, True, ['NTb%d' % nx, 'Qb%d' % cur], [PB[4]])
                        cp('act' if lvl % 2 else 'dve', Qb[nx][:].rearrange("p h t -> p (h t)"), pbk[4][:], [PB[4]], ['Qb%d' % nx])
                        cur = nx
                    Qf, QK = Qb[cur], 'Qb%d' % cur
                    for ci in range(2):
                        c = 2 * g + ci
                        mm(pbk[5][:, ci * 128:(ci + 1) * 128], wk['ab'][:, c, :], STb[:, c, :], True, False, ['wk_ab', 'STb'], [PB[5]])
                        for w_ in range(2):
                            i = 2 * ci + w_
                            hh = hs[i]
                            mm(pbk[5][:, i * 64:(i + 1) * 64], Am['akT'][:, i, :], Vtok[:, hh * 64:(hh + 1) * 64], False, w_ == 1, ['Am_akT', 'tok3_2'], [PB[5]])
                    cp('act', Xs[:], pbk[5][:, 0:256], [PB[5]], ['Xs'])
                    for i, hh in enumerate(hs):
                        oc = slice(i * 64, (i + 1) * 64)
                        mm(pbk[5][:, 256 + i * 64:256 + (i + 1) * 64], Qf[:, i, :], Xs[:, oc], True, True, [QK, 'Xs'], [PB[5]])
                    cp('act', Us[:, g * 256:(g + 1) * 256], pbk[5][:, 256:512], [PB[5]], ['Us'])
                    for ci in range(2):
                        c = 2 * g + ci
                        mm(pbk[6][:, c * 128:(c + 1) * 128], wk['rb'][:, c, :], STb[:, c, :], True, False, ['wk_rb', 'STb'], [PB[6]])
                        for w_ in range(2):
                            i = 2 * ci + w_
                            hh = hs[i]
                            hc = slice(hh * 64, (hh + 1) * 64)
                            mm(pbk[6][:, hc], Am['rkT'][:, i, :], Vtok[:, hc], False, False, ['Am_rkT', 'tok3_2'], [PB[6]])
                            mm(pbk[6][:, hc], Am['rbT'][:, i, :], Us[:, hc], False, w_ == 1, ['Am_rbT', 'Us'], [PB[6]])
                for c in range(4):
                    cs_ = slice(c * 128, (c + 1) * 128)
                    mm(pbk[5][:, cs_], Btok[:, cs_], Us[:, cs_], True, False, ['tok3_0', 'Us'], [PB[5]])
                    mm(pbk[5][:, cs_], Ktok[:, cs_], Vtok[:, cs_], False, True, ['tok3_1', 'tok3_2'], [PB[5]])
                tt('pool', STtmp[:], ST32[:], bc(PCt[:], 2, [128, 4, 64]), ALU.mult, ['ST32', 'PCt'], ['STtmp'])
                p5v = pbk[5][:].rearrange("p (c x) -> p c x", x=128)
                tt('dve', ST32[0:64, :, :], STtmp[0:64, :, :], p5v[0:64, :, 0:64], ALU.add, ['STtmp', PB[5]], ['ST32'])
                tt('dve', ST32[64:128, :, :], STtmp[64:128, :, :], p5v[64:128, :, 64:128], ALU.add, ['STtmp', PB[5]], ['ST32'])
                cp('pool', STb[0:64, :, 0:64], ST32[0:64, :, :], ['ST32'], ['STb'])
                cp('pool', STb[64:128, :, 64:128], ST32[64:128, :, :], ['ST32'], ['STb'])
                ck(8)
                o3 = pbk[6][:].rearrange("p (h e) -> p h e", e=64)
                headnorm(o3, PB[6], 2, hn_c[:])
                hc2 = hn_c[:].rearrange("p h e -> p (h e)")
                tt('dve', hc2, hc2, gn3[:, 1, :], ALU.mult, ['hn_c', 'gn3_1'], ['hn_c'])
                tt('dve', hc2, hc2, gn3[:, 2, :], ALU.add, ['hn_c', 'gn3_2'], ['hn_c'])
                tt('pool', hn_sq[:], Vtok.rearrange("p (h e) -> p h e", e=64), bc(cbt[:], 2, [128, 8, 64]), ALU.mult, ['tok3_2', 'cbt'], ['hn_sq'])
                tt('dve', hc2, hc2, hn_sq[:].rearrange("p h e -> p (h e)"), ALU.add, ['hn_c', 'hn_sq'], ['hn_c'])
                tt('dve', y_bf[:], hc2, g_tok[:], ALU.mult, ['hn_c', 'g_tok'], ['y_bf'])
                for c in range(4):
                    tr(ptb[:, c, :], y_bf[:, c * 128:(c + 1) * 128], identb[:], ['y_bf', 'identb'], ['ptb'])
                cp('act', yT[:], ptb[:, 0:4, :], ['ptb'], ['yT'])
                for hf in range(2):
                    for c in range(4):
                        mm(pbk[2 + hf][:], yT[:, c, :], wbr[:, 1, c, hf * 512:(hf + 1) * 512], c == 0, c == 3, ['yT'] + WBR, [PB[2 + hf]])

                ck(9)
                for gi in range(4):
                    bank = 4 + (gi % 2)
                    inproj_tok(8 + gi, bank)
                    act(gate_s[:, gi * 512:(gi + 1) * 512], pbk[bank][:], AF.Sigmoid, [PB[bank]], ['gate_s'])
                for hf in range(2):
                    sl = slice(hf * 512, (hf + 1) * 512)
                    tt('dve', x1[:, sl], pbk[hf][:], gate_s[:, sl], ALU.mult, [PB[hf], 'gate_s'], ['x1'])
                    tt('dve', merged[:, sl], pbk[2 + hf][:], gate_s[:, 1024 + hf * 512:1024 + (hf + 1) * 512], ALU.mult, [PB[2 + hf], 'gate_s'], ['merged'])
                    tt('pool', merged[:, sl], merged[:, sl], x1[:, sl], ALU.add, ['merged', 'x1'], ['merged'])
                for c in range(8):
                    tr(ptb[:, c, :], merged[:, c * 128:(c + 1) * 128], identb[:], ['merged', 'identb'], ['ptb'])
                cp('act', mT[:], ptb[:], ['ptb'], ['mT'])
                for hf in range(2):
                    for c in range(8):
                        mm(pbk[4 + hf][:], mT[:, c, :], wo[:, c, hf * 512:(hf + 1) * 512], c == 0, c == 7, ['mT'] + WO, [PB[4 + hf]])
                    tt('dve', x1[:, hf * 512:(hf + 1) * 512], pbk[4 + hf][:], X[:, hf * 512:(hf + 1) * 512], ALU.add, [PB[4 + hf], XK], ['x1'])
                S.dma('act', lambda e, n=n: e.dma_start(out=out_d[n * 128:(n + 1) * 128, :], in_=x1[:]), r=['x1'], w=['out%d' % n])
          except _Stop:
            pass

        if stage == 'A':
            S.wait_all('sp')
            S.emit()
            return nc
        S.barrier()

        esB = ExitStack()
        with esB:
          try:
            sbB = lambda n, s, d: esB.enter_context(nc.sbuf_tensor("sc_" + n, s, d))
            ptb2 = pbk[6][:].bitcast(BF16).rearrange("p (a b) -> p a b", b=128)
            PTB = [(ptb, 'ptb'), (ptb2, PB[6])]
            g3 = sbB("g3", [128, 3, D], F32)
            for i in range(3):
                ld('sp', g3[:, i, :], g4_d[1 + i, :].partition_broadcast(128), ['g3_%d' % i])
            wpg = sbB("wpg", [128, 8, D], BF16)
            for c in range(8):
                S.dma('pool', lambda e, c=c: e.dma_start(out=wpg[:, c, :], in_=wpg_d[c * 128:(c + 1) * 128, :]), w=['wpg%d' % c])
            WPG = ['wpg%d' % c for c in range(8)]
            wpu = sbB("wpu", [128, 2, D], BF16)
            for c in range(2):
                S.dma('pool', lambda e, c=c: e.dma_start(out=wpu[:, c, :], in_=wpu_d[c * 128:(c + 1) * 128, :]), w=['wpu%d' % c])
            WPU = ['wpu0', 'wpu1']
            skT = sbB("skT", [128, 16, 128], F32)
            s_sb2 = [sbB("s_sb%d" % i, [128, 16, 128], F32) for i in range(2)]
            s_sb = s_sb2[0]
            for g in range(16):
                ld('sp', s_sb[:, g, :], sk_d[g, :, :], ['s_sb0'])
            for g4i in range(4):
                bank = g4i % 2
                for i in range(4):
                    g = g4i * 4 + i
                    tr(pbk[bank][:, i * 128:(i + 1) * 128], s_sb[:, g, :], identf, ['s_sb0', 'cm'], [PB[bank]])
                cp('act', skT[:, g4i * 4:g4i * 4 + 4, :].rearrange("p g k -> p (g k)"), pbk[bank][:], [PB[bank]], ['skT'])

            NSB = 3
            ubuf = [sbB("ubuf%d" % i, [128, 2, D], BF16) for i in range(NSB)]
            vbuf = [sbB("vbuf%d" % i, [128, 2, D], BF16) for i in range(NSB)]
            wqb = [sbB("wqb%d" % i, [128, 8, 256], BF16) for i in range(2)]

            k = 0
            for kc in range(8):
                for hf in range(2):
                    b = k % 2
                    stg = wqb[b][:].rearrange("p a n -> p (a n)")[:, 0:1024]
                    S.dma('pool', lambda e, stg=stg, kc=kc, hf=hf: e.dma_start(out=stg, in_=wpq_d[kc * 128:(kc + 1) * 128, hf * 1024:(hf + 1) * 1024]), w=['wqb%d' % b])
                    S.dma('sp', lambda e, stg=stg, kc=kc, hf=hf: e.dma_start(out=wpqb_d[kc * 128:(kc + 1) * 128, hf * 1024:(hf + 1) * 1024], in_=stg), r=['wqb%d' % b], w=['wpqb'])
                    k += 1
            pu_v = pu_d.rearrange("(i j) d -> j i d", j=128)
            pv_v = pv_d.rearrange("(i j) d -> j i d", j=128)
            for j in range(128):
                b = j % 2
                ust = ubuf[b][:, 0, :]; uT = ubuf[b][:, 1, :]; vst = vbuf[b][:, 0, :]
                k0, k1, k2 = 'ub%d_0' % b, 'ub%d_1' % b, 'ub%d_2' % b
                S.dma('pool', lambda e, ust=ust, j=j: e.dma_start(out=ust, in_=pu_v[j, :, :]), w=[k0])
                pt_, pk_ = PTB[j % 2]
                for kc in range(8):
                    tr(pt_[:, kc, :], ust[:, kc * 128:(kc + 1) * 128], identb[:], [k0, 'identb'], [pk_])
                cp('act' if j % 2 else 'dve', uT.rearrange("p (a b) -> p a b", b=128), pt_[:, :, :], [pk_], [k1])
                S.dma('sp', lambda e, uT=uT, j=j: e.dma_start(out=utb_d[j, :, :], in_=uT), r=[k1], w=['utb'])
                S.dma('pool', lambda e, vst=vst, j=j: e.dma_start(out=vst, in_=pv_v[j, :, :]), w=[k2])
                S.dma('act', lambda e, vst=vst, j=j: e.dma_start(out=vtb_d[j, :, :], in_=vst), r=[k2], w=['vtb'])
            S.barrier()
            ck(101)

            x1b = [sbB("x1b%d" % i, [128, D], F32) for i in range(2)]
            ptl = [sbB("ptl%d" % i, [128, 256], F32) for i in range(2)]
            h2b2 = [sbB("h2b%d" % i, [128, D], BF16) for i in range(2)]
            h2T2 = [sbB("h2T%d" % i, [128, 8, 128], BF16) for i in range(2)]
            qc = sbB("qc", [128, 2048], F32)
            qT = qc[:].rearrange("p (g t) -> p g t", t=128)
            cand = qc[:].rearrange("p (h x) -> p h x", x=256)
            s_rp = sbB("s_rp", [128, 128], F32)
            vals2 = [sbB("vals%d" % i, [128, 16, 16], F32) for i in range(2)]
            cand2 = sbB("cand2", [128, 256], F32)
            best2 = [sbB("best%d" % i, [128, 8, 16], F32) for i in range(2)]
            gat = sbB("gat", [128, 8, 16], F32)
            gsum = sbB("gsum", [128, 8], F32)
            bias82 = [sbB("bias8%d" % i, [128, 8], F32) for i in range(2)]
            IB = 16
            Ptok = [sbB("Ptok0", [128, 128, IB], BF16)] * 2
            PTt = sbB("PTt", [128, 128, 128], BF16)
            JB = 16
            TG = 512 // JB
            NJB = 128 // JB
            xq = sbB("xq", [128, 4, 16, JB], F32)
            eq = sbB("eq", [128, 4, 16, JB], BF16)
            Qtok = [sbB("Qtok%d" % i, [128, 128, JB], BF16) for i in range(2)]
            QTt = [sbB("QTt%d" % i, [128, JB, 128], BF16) for i in range(2)]
            act_sb2 = [sbB("act_sb%d" % i, [128, JB, 128], BF16) for i in range(2)]
            coef2 = [sbB("coef%d" % i, [128, JB, 128], BF16) for i in range(2)]
            x2 = sbB("x2", [128, D], F32)
            p_bf = sbB("p_bf", [128, 256], BF16)
            pTt = sbB("pTt", [128, 2, 128], BF16)
            pg_s = sbB("pg_s", [128, D], F32)
            uctr = [0]; vctr = [0]; qctr = [0]; pbc = [0]

            def nextptb():
                r_ = PTB[pbc[0] % 2]
                pbc[0] += 1
                return r_

            def front_end(n, stage):
                par = n % 2
                P_ = str(par)
                X1, X1K = x1b[par], 'x1b%d' % par
                h2b, h2T, s_sb, vals, best, bias8 = h2b2[par], h2T2[par], s_sb2[par], vals2[par], best2[par], bias82[par]
                v4 = vals[:].rearrange("p (h c) a -> p h c a", c=2)
                if stage == 0:
                    ld('sp', X1[:], out_d[n * 128:(n + 1) * 128, :], [X1K], r=['out%d' % n])
                    ld('sp', ptl[par][:], p_d[n * 128:(n + 1) * 128, :], ['ptl%d' % par])
                    rmsnorm(X1[:], X1K, g3[:, 0, :], 'g3_0', h2b[:], 'h2b' + P_)
                elif stage == 1:
                    for c in range(8):
                        tr(ptb[:, c, :], h2b[:, c * 128:(c + 1) * 128], identb[:], ['h2b' + P_, 'identb'], ['ptb'])
                    cp('act', h2T[:], ptb[:], ['ptb'], ['h2T' + P_])
                elif stage in (2, 3):
                    for g4i in ((0, 1) if stage == 2 else (2, 3)):
                        bank = 2 + (g4i % 2)
                        for i2 in range(2):
                            wb = qctr[0] % 2
                            qctr[0] += 1
                            c0 = g4i * 512 + i2 * 256
                            S.dma('sp', lambda e, wb=wb, c0=c0: e.dma_start(
                                out=wqb[wb][:], in_=wpqb_d[:, c0:c0 + 256].rearrange("(kc p) n -> p kc n", p=128)),
                                r=['wpqb'], w=['wqb%d' % wb])
                            for i1 in range(2):
                                i = i2 * 2 + i1
                                for kc in range(8):
                                    mm(pbk[bank][:, i * 128:(i + 1) * 128], wqb[wb][:, kc, i1 * 128:(i1 + 1) * 128], h2T[:, kc, :], kc == 0, kc == 7, ['h2T' + P_, 'wqb%d' % wb], [PB[bank]])
                        cp('act', qT[:, g4i * 4:g4i * 4 + 4, :].rearrange("p g t -> p (g t)"), pbk[bank][:], [PB[bank]], ['qc'])
                elif stage == 4:
                    for g4i in range(4):
                        bank = 4 + (g4i % 2)
                        for i in range(4):
                            g = g4i * 4 + i
                            mm(pbk[bank][:, i * 128:(i + 1) * 128], qT[:, g, :], skT[:, g, :], True, True, ['qc', 'skT'], [PB[bank]])
                        cp('act', s_sb[:, g4i * 4:g4i * 4 + 4, :].rearrange("p g k -> p (g k)"), pbk[bank][:], [PB[bank]], ['s_sb' + P_])
                elif stage == 5:
                    for g in range(16):
                        S.op('dve', lambda e, g=g: e.max(out=vals[:, g, 0:8], in_=s_sb[:, g, :]), r=['s_sb' + P_], w=['vals' + P_])
                        S.op('dve', lambda e, g=g: e.match_replace(out=s_rp[:], in_to_replace=vals[:, g, 0:8], in_values=s_sb[:, g, :], imm_value=-1e30), r=['s_sb' + P_, 'vals' + P_], w=['s_rp'])
                        S.op('dve', lambda e, g=g: e.max(out=vals[:, g, 8:16], in_=s_rp[:]), r=['s_rp'], w=['vals' + P_])
                    for hh in range(8):
                        tt('dve', cand[:, hh, :].rearrange("p (a b) -> p a b", b=16), bc(v4[:, hh, 0, :], 2, [128, 16, 16]), bc(v4[:, hh, 1, :], 1, [128, 16, 16]), ALU.add, ['vals' + P_], ['qc'])
                elif stage == 6:
                    for hh in range(8):
                        S.op('dve', lambda e, hh=hh: e.max(out=best[:, hh, 0:8], in_=cand[:, hh, :]), r=['qc'], w=['best' + P_])
                        S.op('dve', lambda e, hh=hh: e.match_replace(out=cand2[:], in_to_replace=best[:, hh, 0:8], in_values=cand[:, hh, :], imm_value=-1e30), r=['qc', 'best' + P_], w=['cand2'])
                        S.op('dve', lambda e, hh=hh: e.max(out=best[:, hh, 8:16], in_=cand2[:]), r=['cand2'], w=['best' + P_])
                    tt('dve', gat[:], best[:], bc(best[:, :, 0], 2, [128, 8, 16]), ALU.subtract, ['best' + P_], ['gat'])
                    act(gat[:], gat[:], AF.Exp, ['gat'], ['gat'])
                    red(gsum[:], gat[:], ALU.add, ['gat'], ['gsum'])
                    act(gsum[:], gsum[:], AF.Ln, ['gsum'], ['gsum'])
                    stt(bias8[:], best[:, :, 0], -1.0, gsum[:], ALU.mult, ALU.subtract, ['best' + P_, 'gsum'], ['bias8' + P_])

            NFE = 7

            for st_ in range(NFE):
                front_end(0, st_)
            for n in range(nt):
                par = n % 2
                P_ = str(par)
                X1, X1K = x1b[par], 'x1b%d' % par
                h2b, h2T, s_sb, vals, best, bias8 = h2b2[par], h2T2[par], s_sb2[par], vals2[par], best2[par], bias82[par]
                v4 = vals[:].rearrange("p (h c) a -> p h c a", c=2)
                s4 = s_sb[:].rearrange("p (h c) k -> p h c k", c=2)
                def p_build(ibs, v4_, s4_, pk_):
                    for ib in ibs:
                        tt('dve', Ptok[0][:].rearrange("t (h a) i -> t h a i", a=16),
                           bc(s4_[:, :, 0, ib * IB:(ib + 1) * IB], 2, [128, 8, 16, IB]),
                           bc(v4_[:, :, 0, :], 3, [128, 8, 16, IB]), ALU.is_equal, ['s_sb' + pk_, 'vals' + pk_], ['Ptok0'])
                        for i8 in range(IB // 8):
                            pt_, pkk = nextptb()
                            for il in range(8):
                                tr(pt_[:, il, :], Ptok[0][:, :, i8 * 8 + il], identb[:], ['Ptok0', 'identb'], [pkk])
                            i0_ = ib * IB + i8 * 8
                            cp('act' if (i8 % 2) else 'dve', PTt[:, :, i0_:i0_ + 8].rearrange("n t i -> n i t"), pt_[:, :, :], [pkk], ['PTt'])

                if n == 0:
                    p_build(range(128 // IB), v4, s4, P_)

                def q_elem(jb):
                    qb_ = jb % 2
                    for hg in range(2):
                        hsl = slice(hg * 4, hg * 4 + 4)
                        tt('dve', xq[:], bc(v4[:, hsl, 0, :], 3, [128, 4, 16, JB]), bc(s4[:, hsl, 1, jb * JB:(jb + 1) * JB], 2, [128, 4, 16, JB]), ALU.add, ['vals' + P_, 's_sb' + P_], ['xq'])
                        for h_ in range(4):
                            hh = hg * 4 + h_
                            act(eq[:, h_, :, :], xq[:, h_, :, :], AF.Exp, ['xq', 'bias8' + P_], ['eq'], bias=bias8[:, hh:hh + 1])
                        tt('dve', xq[:].rearrange("p h a j -> p h (a j)"), xq[:].rearrange("p h a j -> p h (a j)"), bc(best[:, hsl, 15], 2, [128, 4, 16 * JB]), ALU.is_ge, ['xq', 'eq', 'best' + P_], ['xq'])
                        tt('dve', Qtok[qb_][:, hg * 64:(hg + 1) * 64, :].rearrange("p (h a) j -> p h a j", a=16), xq[:], eq[:], ALU.mult, ['xq', 'eq'], ['Qtok%d' % qb_])

                def q_tr(jb):
                    qb_ = jb % 2
                    for j8 in range(JB // 8):
                        pt_, pk_ = nextptb()
                        for jl in range(8):
                            tr(pt_[:, jl, :], Qtok[qb_][:, :, j8 * 8 + jl], identb[:], ['Qtok%d' % qb_, 'identb'], [pk_])
                        cp('act', QTt[qb_][:, j8 * 8:j8 * 8 + 8, :], pt_[:, :, :], [pk_], ['QTt%d' % qb_])

                def act_blk(jb):
                    ab = jb % 2
                    for jq in range(JB // 4):
                        bank = 2 + (jq % 2)
                        for j2 in range(2):
                            j0 = jb * JB + jq * 4 + j2 * 2
                            ub = uctr[0] % NSB
                            uctr[0] += 1
                            S.dma('sp', lambda e, ub=ub, j0=j0: e.dma_start(out=ubuf[ub][:], in_=utb_d[j0:j0 + 2, :, :].rearrange("j d x -> d j x")),
                                  r=['utb'], w=['ubuf%d' % ub])
                            for jl2 in range(2):
                                jl = j2 * 2 + jl2
                                for kc in range(8):
                                    mm(pbk[bank][:, jl * 128:(jl + 1) * 128], ubuf[ub][:, jl2, kc * 128:(kc + 1) * 128], h2T[:, kc, :], kc == 0, kc == 7, ['ubuf%d' % ub, 'h2T' + P_], [PB[bank]])
                        act(act_sb2[ab][:, jq * 4:jq * 4 + 4, :].rearrange("i j t -> i (j t)"), pbk[bank][:], AF.Gelu_apprx_tanh, [PB[bank]], ['act_sb%d' % ab])

                def w_blk(jb):
                    qb_ = jb % 2
                    for tg in range(128 // TG):
                        bank = 4 + (tg % 2)
                        for tl in range(TG):
                            t = tg * TG + tl
                            mm(pbk[bank][:, tl * JB:(tl + 1) * JB], PTt[:, t, :], QTt[qb_][:, :, t], True, True, ['PTt', 'QTt%d' % qb_], [PB[bank]])
                        tt('dve', coef2[qb_][:, :, tg * TG:(tg + 1) * TG], pbk[bank][:].rearrange("i (t j) -> i j t", j=JB), act_sb2[qb_][:, :, tg * TG:(tg + 1) * TG], ALU.mult, [PB[bank], 'act_sb%d' % qb_], ['coef%d' % qb_])

                def v_blk(jb):
                    qb_ = jb % 2
                    for jq in range(JB // 2):
                        j0 = jb * JB + jq * 2
                        vb = vctr[0] % NSB
                        vctr[0] += 1
                        S.dma('sp', lambda e, vb=vb, j0=j0: e.dma_start(out=vbuf[vb][:], in_=vtb_d[j0:j0 + 2, :, :].rearrange("j i x -> i j x")),
                              r=['vtb'], w=['vbuf%d' % vb])
                        for jl in range(2):
                            j = j0 + jl
                            for hf in range(2):
                                mm(pbk[hf][:], coef2[qb_][:, jq * 2 + jl, :], vbuf[vb][:, jl, hf * 512:(hf + 1) * 512], j == 0, j == 127, ['coef%d' % qb_, 'vbuf%d' % vb], [PB[hf]])

                q_elem(0)
                q_tr(0)
                for jb in range(NJB):
                    if jb + 1 < NJB:
                        q_elem(jb + 1)
                    act_blk(jb)
                    if jb + 1 < NJB:
                        q_tr(jb + 1)
                    w_blk(jb)
                    if jb >= 1:
                        v_blk(jb - 1)
                    if jb < NFE and n + 1 < nt:
                        front_end(n + 1, jb)
                if n + 1 < nt:
                    pn = (n + 1) % 2
                    v4n = vals2[pn][:].rearrange("p (h c) a -> p h c a", c=2)
                    s4n = s_sb2[pn][:].rearrange("p (h c) k -> p h c k", c=2)
                    p_build(range(0, 4), v4n, s4n, str(pn))
                v_blk(NJB - 1)
                if n + 1 < nt:
                    p_build(range(4, 128 // IB), v4n, s4n, str(pn))
                ck(107)
                for hf in range(2):
                    sl = slice(hf * 512, (hf + 1) * 512)
                    tt('dve', x2[:, sl], pbk[hf][:], X1[:, sl], ALU.add, [PB[hf], X1K], ['x2'])
                rmsnorm(x2[:], 'x2', g3[:, 1, :], 'g3_1', h2b[:], 'h2b' + P_)
                for c in range(8):
                    tr(ptb[:, c, :], h2b[:, c * 128:(c + 1) * 128], identb[:], ['h2b' + P_, 'identb'], ['ptb'])
                cp('act', h2T[:], ptb[:], ['ptb'], ['h2T' + P_])
                cp('pool', p_bf[:], ptl[par][:], ['ptl%d' % par], ['p_bf'])
                for c in range(2):
                    tr(ptb[:, c, :], p_bf[:, c * 128:(c + 1) * 128], identb[:], ['p_bf', 'identb'], ['ptb'])
                cp('act', pTt[:], ptb[:, 0:2, :], ['ptb'], ['pTt'])
                for hf in range(2):
                    sl = slice(hf * 512, (hf + 1) * 512)
                    for c in range(8):
                        mm(pbk[2 + hf][:], h2T[:, c, :], wpg[:, c, sl], c == 0, c == 7, ['h2T' + P_] + WPG, [PB[2 + hf]])
                    act(pg_s[:, sl], pbk[2 + hf][:], AF.Sigmoid, [PB[2 + hf]], ['pg_s'])
                    for c in range(2):
                        mm(pbk[4 + hf][:], pTt[:, c, :], wpu[:, c, sl], c == 0, c == 1, ['pTt'] + WPU, [PB[4 + hf]])
                    tt('dve', pg_s[:, sl], pg_s[:, sl], pbk[4 + hf][:], ALU.mult, ['pg_s', PB[4 + hf]], ['pg_s'])
                tt('dve', pg_s[:], x2[:], pg_s[:], ALU.add, ['x2', 'pg_s'], ['pg_s'])
                rmsnorm(pg_s[:], 'pg_s', g3[:, 2, :], 'g3_2', x2[:], 'x2')
                S.dma('act', lambda e, n=n: e.dma_start(out=out_d[n * 128:(n + 1) * 128, :], in_=x2[:]), r=['x2'], w=['out%d' % n])
          except _Stop:
            pass

        S.wait_all('sp')
        S.emit()
    return nc


def make_in_maps(inputs, nt, ncores):
    f = np.float32
    c = host_consts(nt)
    g = lambda k: np.asarray(inputs[k], dtype=f)
    S_ = nt * 128
    mu = g('rwkv_mu')[0]
    pp = np.zeros((128, 34), f)
    pp[:, 0:14] = mu.reshape(14, 128).T
    for j, k in enumerate(('rwkv_w0', 'rwkv_a0', 'rwkv_k_k', 'rwkv_k_a')):
        pp[:, 14 + 4 * j:18 + 4 * j] = g(k)[0].reshape(4, 128).T
    pp[:, 30:34] = g('rwkv_r_k')[0].reshape(4, 128).T
    g4 = np.stack([g('g_mix')[0], g('g_ffn')[0], g('g_ple')[0], g('g_final')], 0)
    gn3 = np.stack([g('ret_gn_g')[0], g('rwkv_gn_g')[0], g('rwkv_gn_b')[0]], 0)
    lora = np.zeros((128, 3, 512), f)
    lora[0:64, 0, :] = g('rwkv_w_up')[0]
    lora[64:128, 1, :] = g('rwkv_a_up')[0]
    lora[:, 2, :] = g('rwkv_g_up')[0]
    wbr = np.stack([g('w_ret_br')[0], g('w_rwkv_br')[0]], 0)
    shared = dict(w_in=g('w_in')[0], pp=pp, g4=np.ascontiguousarray(g4), gn3=np.ascontiguousarray(gn3),
                  lora=lora, wbr=np.ascontiguousarray(wbr), w_o=g('w_o')[0], w_pq=g('w_pq')[0],
                  sk=np.ascontiguousarray(g('peer_sub_keys')[0].reshape(16, 128, 128)),
                  peer_u=g('peer_u')[0], peer_v=g('peer_v')[0], w_ple_gate=g('w_ple_gate')[0],
                  w_ple_up=g('w_ple_up')[0], rot=c['rot'], DT=c['DT'], xiT=c['xiT'], CDb=c['CDb'], cm=c['cm'])
    x = g('x'); p = g('p')[0]
    maps = []
    for i in range(ncores):
        m = dict(shared)
        m['x'] = np.ascontiguousarray(x[i, :S_])
        m['p'] = np.ascontiguousarray(p[i, :S_])
        maps.append(m)
    return maps


def kernel(**inputs):
    nt = SEQ // 128
    nc = build(nt)
    in_maps = make_in_maps(inputs, nt, NCORES)
    res = run_bass_kernel_spmd(nc, in_maps, core_ids=list(range(NCORES)))
    out = np.stack([np.asarray(r["out"], dtype=np.float32) for r in res.results], axis=0)
    return out
```

```python
import math
import numpy as np
from contextlib import ExitStack
import concourse.bass as bass
import concourse.mybir as mybir
from concourse.bass_utils import run_bass_kernel_spmd

F32 = mybir.dt.float32
BF16 = mybir.dt.bfloat16
U32 = mybir.dt.uint32
I32 = mybir.dt.int32
AF = mybir.ActivationFunctionType
ALU = mybir.AluOpType
AX = mybir.AxisListType

D = 1024
SEQ = 4096
NCORES = 8
IN_COLS = 5888
C0 = math.exp(-0.5)


class Sched:
    SELF_SYNC = {'pe': False, 'act': True, 'dve': True, 'pool': True, 'sp': True}

    def __init__(self, nc, es, n_dma_sems=8):
        self.nc = nc
        self.ops = {e: [] for e in ('pe', 'act', 'dve', 'pool', 'sp')}
        self.sem = {e: es.enter_context(nc.semaphore('prog_' + e)) for e in self.ops}
        self.cnt = {e: 0 for e in self.ops}
        self.waited = {e: {} for e in self.ops}
        self.last_w = {}
        self.readers = {}
        self.dsem = {}
        self.dcnt = {}
        self.drr = {}
        for q in ('sp', 'act', 'pool'):
            self.dsem[q] = [es.enter_context(nc.semaphore('dma_%s_%d' % (q, i)))
                            for i in range(n_dma_sems)]
            self.dcnt[q] = [0] * n_dma_sems
            self.drr[q] = 0
        self.sem_id = {}
        self.pending = {e: [] for e in self.ops}

    def barrier(self):
        toks = list(self.last_w.values())
        for ts_ in self.readers.values():
            toks.extend(ts_)
        for e in self.ops:
            self.pending[e] = list(toks)

    def _deps(self, r, w):
        toks = []
        for k in r:
            t = self.last_w.get(k)
            if t is not None:
                toks.append(t)
        for k in w:
            t = self.last_w.get(k)
            if t is not None:
                toks.append(t)
            toks.extend(self.readers.get(k, ()))
        return toks

    def _waits(self, e, toks):
        need = {}
        for (sem, val, src) in toks:
            if src == e and not self.SELF_SYNC[e]:
                continue
            key = id(sem)
            self.sem_id[key] = sem
            if self.waited[e].get(key, 0) >= val:
                continue
            if need.get(key, 0) < val:
                need[key] = val
        out = []
        for key, val in need.items():
            self.waited[e][key] = val
            out.append((self.sem_id[key], val))
        return out

    def _commit(self, tok, r, w):
        for k in w:
            self.last_w[k] = tok
            self.readers[k] = []
        for k in r:
            if k in w:
                continue
            self.readers.setdefault(k, []).append(tok)

    def op(self, e, fn, r=(), w=()):
        r = list(r); w = list(w)
        toks = self._deps(r, w) + self.pending[e]
        self.pending[e] = []
        waits = self._waits(e, toks)
        self.cnt[e] += 1
        tok = (self.sem[e], self.cnt[e], e)
        self.ops[e].append((waits, fn, (self.sem[e], 1)))
        self._commit(tok, r, w)
        return tok

    def dma(self, q, fn, r=(), w=()):
        r = list(r); w = list(w)
        j = self.drr[q]
        self.drr[q] = (j + 1) % len(self.dsem[q])
        sem = self.dsem[q][j]
        toks = self._deps(r, w) + self.pending[q]
        self.pending[q] = []
        if self.dcnt[q][j] > 0:
            toks.append((sem, 16 * self.dcnt[q][j], None))
        waits = self._waits(q, toks)
        self.dcnt[q][j] += 1
        tok = (sem, 16 * self.dcnt[q][j], None)
        self.ops[q].append((waits, fn, (sem, 16)))
        self._commit(tok, r, w)
        return tok

    def wait_all(self, e):
        toks = list(self.last_w.values())
        for ts in self.readers.values():
            toks.extend(ts)
        waits = self._waits(e, toks)
        self.ops[e].append((waits, None, None))

    def emit(self):
        nc = self.nc
        with nc.Block() as block:
            def run(e, eng):
                for waits, fn, inc in self.ops[e]:
                    for sem, val in waits:
                        eng.wait_ge(sem, val)
                    if fn is not None:
                        ins = fn(eng)
                        ins.then_inc(inc[0], inc[1])

            @block.sync
            def _(eng):
                run('sp', eng)

            @block.scalar
            def _(eng):
                run('act', eng)

            @block.vector
            def _(eng):
                run('dve', eng)

            @block.gpsimd
            def _(eng):
                run('pool', eng)

            @block.tensor
            def _(eng):
                run('pe', eng)


def host_consts(nt):
    f = np.float32
    S = nt * 128
    half = 32
    inv_freq = (10000.0 ** (-np.arange(half, dtype=f) * f(2.0) / f(64))).astype(f)
    ang = (np.arange(S, dtype=f)[:, None] * inv_freq[None, :]).astype(f)
    cos = np.cos(ang).astype(f); sin = np.sin(ang).astype(f)
    rot = np.zeros((nt, 128, 128), f)
    rot[:, :, 0:32] = cos.reshape(nt, 128, 32)
    rot[:, :, 32:64] = sin.reshape(nt, 128, 32)
    rot[:, :, 64:96] = cos.reshape(nt, 128, 32) * f(0.125)
    rot[:, :, 96:128] = sin.reshape(nt, 128, 32) * f(0.125)
    H = 8
    lg = np.log1p(-(2.0 ** (-5.0 - np.arange(H, dtype=np.float64))))
    idx = np.arange(128, dtype=np.float64)
    diff = idx[None, :] - idx[:, None]
    DT = np.where(diff[:, None, :] >= 0, np.exp(lg[None, :, None] * np.maximum(diff, 0)[:, None, :]), 0.0).astype(f)
    xiT = np.zeros((128, 4, 128), f)
    CDb = np.zeros((128, 4, 64), f)
    for c in range(4):
        for p in range(128):
            h = 2 * c + p // 64
            xiT[p, c, :] = np.exp(lg[h] * (idx + 1.0))
            CDb[p, c, :] = np.exp(lg[h] * 128.0)
    ZT = np.exp(lg[None, :] * (127.0 - idx)[:, None]).astype(f)
    s = np.arange(128)
    SU = (s[None, :] > s[:, None]).astype(f)
    SUI = (s[None, :] >= s[:, None]).astype(f)
    SL = SU.T.copy()
    BO = np.zeros((128, 128), f); BO[:64, :64] = 1; BO[64:, 64:] = 1
    HS = np.zeros((128, 2), f); HS[:64, 0] = 1; HS[64:, 1] = 1
    ident = np.eye(128, dtype=f)
    io16 = np.tile(np.arange(16, dtype=f)[None, :], (128, 1))
    cm = np.concatenate([ident, SU, SUI, SL, BO, ZT, HS, io16, io16 * 16], axis=1)
    return dict(rot=rot, DT=DT.reshape(128, 1024), xiT=xiT.reshape(128, 512),
                CDb=CDb.reshape(128, 256), cm=np.ascontiguousarray(cm))


CM_W = 128 * 5 + 8 + 2 + 16 + 16


class _Stop(Exception):
    pass


def build(nt, stage='full', stop_after=None):
    nc = bass.Bass("TRN2", target_bir_lowering=False)
    S_ = nt * 128
    WDT = BF16
    dram = lambda n, s, d, k="ExternalInput": nc.dram_tensor(n, s, d, kind=k).ap()
    x_d = dram("x", [S_, D], F32)
    p_d = dram("p", [S_, 256], F32)
    w_in_d = dram("w_in", [D, IN_COLS], F32)
    pp_d = dram("pp", [128, 34], F32)
    g4_d = dram("g4", [4, D], F32)
    gn3_d = dram("gn3", [3, 512], F32)
    lora_d = dram("lora", [128, 3, 512], F32)
    wbr_d = dram("wbr", [2, 512, D], F32)
    wo_d = dram("w_o", [D, D], F32)
    wpq_d = dram("w_pq", [D, 2048], F32)
    sk_d = dram("sk", [16, 128, 128], F32)
    pu_d = dram("peer_u", [16384, D], F32)
    pv_d = dram("peer_v", [16384, D], F32)
    wpg_d = dram("w_ple_gate", [D, D], F32)
    wpu_d = dram("w_ple_up", [256, D], F32)
    rot_d = dram("rot", [nt, 128, 128], F32)
    DT_d = dram("DT", [128, 1024], F32)
    xiT_d = dram("xiT", [128, 512], F32)
    CDb_d = dram("CDb", [128, 256], F32)
    cm_d = dram("cm", [128, CM_W], F32)
    out_d = dram("out", [S_, D], F32, "ExternalOutput")
    winb_d = nc.dram_tensor("winb", [D, IN_COLS], BF16, kind="Internal").ap()
    utb_d = nc.dram_tensor("utb", [128, 128, D], BF16, kind="Internal").ap()
    vtb_d = nc.dram_tensor("vtb", [128, 128, D], BF16, kind="Internal").ap()
    wpqb_d = nc.dram_tensor("wpqb", [D, 2048], BF16, kind="Internal").ap()

    es = ExitStack()
    with es:
        S = Sched(nc, es)
        sb = lambda n, s, d: es.enter_context(nc.sbuf_tensor("sb_" + n, s, d))
        ps = lambda n, s, d: es.enter_context(nc.psum_tensor(n, s, d))

        def mm(out, lhsT, rhs, start, stop, r, w):
            S.op('pe', lambda e: e.matmul(out, lhsT=lhsT, rhs=rhs, start=start, stop=stop), r=r, w=w)

        def tr(out, in_, ident, r, w):
            S.op('pe', lambda e: e.transpose(out=out, in_=in_, identity=ident), r=r, w=w)

        def act(out, in_, func, r, w, **kw):
            S.op('act', lambda e: e.activation(out=out, in_=in_, func=func, **kw), r=r, w=w)

        def cp(eng, out, in_, r, w):
            if eng == 'act':
                S.op('act', lambda e: e.copy(out=out, in_=in_), r=r, w=w)
            else:
                S.op(eng, lambda e: e.tensor_copy(out=out, in_=in_), r=r, w=w)

        def tt(eng, out, in0, in1, op, r, w):
            S.op(eng, lambda e: e.tensor_tensor(out=out, in0=in0, in1=in1, op=op), r=r, w=w)

        def ts(eng, out, in0, s1, s2, op0, op1, r, w):
            if op1 is None:
                S.op(eng, lambda e: e.tensor_scalar(out=out, in0=in0, scalar1=s1, scalar2=None, op0=op0), r=r, w=w)
            else:
                S.op(eng, lambda e: e.tensor_scalar(out=out, in0=in0, scalar1=s1, scalar2=s2, op0=op0, op1=op1), r=r, w=w)

        def stt(out, in0, scalar, in1, op0, op1, r, w):
            S.op('dve', lambda e: e.scalar_tensor_tensor(out=out, in0=in0, scalar=scalar, in1=in1, op0=op0, op1=op1), r=r, w=w)

        def red(out, in_, op, r, w, axis=AX.X):
            S.op('dve', lambda e: e.tensor_reduce(out=out, in_=in_, axis=axis, op=op), r=r, w=w)

        def rcp(t, k):
            S.op('dve', lambda e: e.reciprocal(out=t, in_=t), r=[k], w=[k])

        def ld(q, out, in_, w, r=()):
            S.dma(q, lambda e: e.dma_start(out=out, in_=in_), r=r, w=w)

        def bc(ap, axis, shape):
            return ap.unsqueeze(axis).to_broadcast(shape)

        ptb = ps("ptb", [128, 8, 128], BF16)
        pbk = [ps("pb%d" % i, [128, 512], F32) for i in range(7)]
        PB = ['pb%d' % i for i in range(7)]

        cm = sb("cm", [128, CM_W], F32)
        ld('sp', cm[:], cm_d[:, :], ['cm'])
        identf = cm[:, 0:128]
        SU = cm[:, 128:256]; SUI = cm[:, 256:384]; SL = cm[:, 384:512]; BO = cm[:, 512:640]
        ZT = cm[:, 640:648]; HS = cm[:, 648:650]; IO16 = cm[:, 650:666]; IO16X = cm[:, 666:682]
        identb = sb("identb", [128, 128], BF16)
        cp('dve', identb[:], identf, ['cm'], ['identb'])
        eps_t = sb("eps_t", [128, 4], F32)
        S.op('dve', lambda e: e.memset(eps_t[:, 0:1], 1e-6), w=['eps'])
        S.op('dve', lambda e: e.memset(eps_t[:, 1:2], 1e-5), w=['eps'])
        S.op('dve', lambda e: e.memset(eps_t[:, 2:3], 64e-5), w=['eps'])
        sq_junk = sb("sq_junk", [128, D], BF16)
        rs_ss = sb("rs_ss", [128, 1], F32)
        rs_rstd = sb("rs_rstd", [128, 1], F32)

        def rmsnorm(src, src_key, gtab, gkey, dst, dst_key):
            act(sq_junk[:], src, AF.Square, [src_key], ['sq_junk', 'rs_ss'], accum_out=rs_ss[:])
            act(rs_rstd[:], rs_ss[:], AF.Sqrt, ['rs_ss', 'eps'], ['rs_rstd'], scale=1.0 / D, bias=eps_t[:, 0:1])
            rcp(rs_rstd[:], 'rs_rstd')
            stt(dst, src, rs_rstd[:, 0:1], gtab, ALU.mult, ALU.mult, [src_key, 'rs_rstd', gkey], [dst_key])

        def ck(k):
            if stop_after == k:
                raise _Stop()
        esA = ExitStack()
        with esA:
          try:
            sbA = lambda n, s, d: esA.enter_context(nc.sbuf_tensor("sa_" + n, s, d))
            pp = sbA("pp", [128, 34], F32)
            ld('sp', pp[:], pp_d[:, :], ['pp'])
            MU = pp[:, 0:14]; W0 = pp[:, 14:18]; A0 = pp[:, 18:22]; KK_ = pp[:, 22:26]; KA = pp[:, 26:30]; RK = pp[:, 30:34]
            omm = sbA("omm", [128, 14], F32)
            ts('dve', omm[:], MU, -1.0, 1.0, ALU.mult, ALU.add, ['pp'], ['omm'])
            omka = sbA("omka", [128, 4], F32)
            ts('dve', omka[:], KA, -1.0, 1.0, ALU.mult, ALU.add, ['pp'], ['omka'])
            gmix = sbA("gmix", [128, D], F32)
            ld('sp', gmix[:], g4_d[0, :].partition_broadcast(128), ['gmix'])
            gn3 = sbA("gn3", [128, 3, 512], F32)
            for i in range(3):
                ld('sp', gn3[:, i, :], gn3_d[i, :].partition_broadcast(128), ['gn3_%d' % i])

            NWB = 3
            wch = [sbA("wch%d" % i, [128, 8, 512], BF16) for i in range(NWB)]
            k = 0
            for kc in range(8):
                for cb in range(4):
                    b = k % NWB
                    stg = wch[b][:].rearrange("p a n -> p (a n)")[:, 0:1472]
                    S.dma('pool', lambda e, stg=stg, kc=kc, cb=cb: e.dma_start(out=stg, in_=w_in_d[kc * 128:(kc + 1) * 128, cb * 1472:(cb + 1) * 1472]), w=['wch%d' % b])
                    S.dma('sp', lambda e, stg=stg, kc=kc, cb=cb: e.dma_start(out=winb_d[kc * 128:(kc + 1) * 128, cb * 1472:(cb + 1) * 1472], in_=stg), r=['wch%d' % b], w=['winb'])
                    k += 1
            wbr = sbA("wbr", [128, 2, 4, D], BF16)
            for i in range(2):
                for c in range(4):
                    S.dma('pool', lambda e, i=i, c=c: e.dma_start(out=wbr[:, i, c, :], in_=wbr_d[i, c * 128:(c + 1) * 128, :]), w=['wbr%d%d' % (i, c)])
            wo = sbA("wo", [128, 8, D], BF16)
            for c in range(8):
                S.dma('pool', lambda e, c=c: e.dma_start(out=wo[:, c, :], in_=wo_d[c * 128:(c + 1) * 128, :]), w=['wo%d' % c])
            WBR = ['wbr%d%d' % (i, c) for i in range(2) for c in range(4)]
            WO = ['wo%d' % c for c in range(8)]
            lora = sbA("lora", [128, 3, 512], BF16)
            S.dma('pool', lambda e: e.dma_start(out=lora[:], in_=lora_d[:, :, :]), w=['lora'])
            DT = sbA("DT", [128, 8, 128], F32)
            ld('sp', DT[:].rearrange("p h i -> p (h i)"), DT_d[:, :], ['DT'])
            xiT = sbA("xiT", [128, 4, 128], F32)
            ld('sp', xiT[:].rearrange("p c i -> p (c i)"), xiT_d[:, :], ['xiT'])
            CDb = sbA("CDb", [128, 4, 64], F32)
            ld('sp', CDb[:].rearrange("p c i -> p (c i)"), CDb_d[:, :], ['CDb'])

            CH = [(0, 512), (512, 512), (1024, 512), (1536, 512),
                  (2048, 512), (2560, 512), (3072, 512), (3584, 256),
                  (3840, 512), (4352, 512), (4864, 512), (5376, 512)]
            wctr = [k]

            def load_chunk(ci):
                b = wctr[0] % NWB
                wctr[0] += 1
                c0, cw = CH[ci]
                S.dma('sp', lambda e, b=b, c0=c0, cw=cw: e.dma_start(
                    out=wch[b][:, :, 0:cw], in_=winb_d[:, c0:c0 + cw].rearrange("(kc p) n -> p kc n", p=128)),
                    r=['winb'], w=['wch%d' % b])
                return b

            xt = [sbA("xt%d" % i, [128, D], F32) for i in range(2)]
            rot_t = [sbA("rot%d" % i, [128, 128], F32) for i in range(2)]
            h = sbA("h", [128, D], BF16)
            hT = sbA("hT", [128, 8, 128], BF16)
            qk_rot = sbA("qk_rot", [128, 2, 512], BF16)
            rt = [sbA("rt%d" % i, [128, 8, 32], F32) for i in range(4)]
            v_tok = sbA("v_tok", [128, 512], BF16)
            gr_s = sbA("gr_s", [128, 512], BF16)
            gate_s = sbA("gate_s", [128, 2048], BF16)
            qkT = sbA("qkT", [128, 8, 128], BF16)
            qxT = sbA("qxT", [128, 4, 128], BF16)
            kz = sbA("kz", [128, 8, 64], BF16)
            PT = sbA("PT", [128, 8, 128], BF16)
            R32 = sbA("R32", [128, 4, 64], F32)
            Rb = sbA("Rb", [128, 4, 128], BF16)
            qTm = sbA("qTm", [128, 2, 4, 128], BF16)
            S.op('dve', lambda e: e.memset(qTm[:], 0.0), w=['qTm'])
            Rtmp = sbA("Rtmp", [128, 4, 64], F32)
            S.op('dve', lambda e: e.memset(R32[:], 0.0), w=['R32'])
            S.op('dve', lambda e: e.memset(Rb[:], 0.0), w=['Rb'])
            hn_sq = sbA("hn_sq", [128, 8, 64], F32)
            hn_c = sbA("hn_c", [128, 8, 64], F32)
            hn_s = sbA("hn_s", [128, 8], F32)
            hn_q = sbA("hn_q", [128, 8], F32)
            hn_m = sbA("hn_m", [128, 8], F32)
            hn_r = sbA("hn_r", [128, 8], F32)
            y_bf = sbA("y_bf", [128, 512], BF16)
            yT = sbA("yT", [128, 4, 128], BF16)
            ZB = sbA("ZB", [128, 14, 129], F32)
            zlast = sbA("zlast", [128, 14, 1], F32)
            S.op('dve', lambda e: e.memset(zlast[:], 0.0), w=['zlast'])
            zs = sbA("zs", [128, 14, 128], F32)
            lor_in = sbA("lor_in", [128, 2, 128], BF16)
            f4 = [sbA("f4_%d" % i, [128, 4, 128], F32) for i in range(8)]
            F4 = ['f4_%d' % i for i in range(8)]
            f4ones = sbA("f4ones", [128, 128], F32)
            S.op('dve', lambda e: e.memset(f4ones[:], 1.0), w=['f4ones'])
            wk = {n_: sbA("wk_" + n_, [128, 4, 128], WDT) for n_ in ('ab', 'rb', 'bb', 'kb', 'bt', 'kt', 'vT')}
            tok3 = sbA("tok3", [128, 3, 512], WDT)
            Am = {n_: sbA("Am_" + n_, [128, 4, 128], WDT) for n_ in ('akT', 'rbT', 'rkT')}
            Nb = [sbA("Nb%d" % i, [128, 4, 128], WDT) for i in range(2)]
            NTb = [sbA("NTb%d" % i, [128, 4, 128], WDT) for i in range(2)]
            Qb = [sbA("Qb%d" % i, [128, 4, 128], WDT) for i in range(2)]
            BOw = sbA("BOw", [128, 128], F32)
            cp('dve', BOw[:], BO, ['cm'], ['BOw'])
            Xs = sbA("Xs", [128, 256], WDT)
            Us = sbA("Us", [128, 512], WDT)
            ST32 = sbA("ST32", [128, 4, 64], F32)
            STb = sbA("STb", [128, 4, 128], WDT)
            wkm = {n_: sbA("wkm_" + n_, [128, 2, 4, 128], WDT) for n_ in ('ab', 'bb', 'rb')}
            for n_ in ('ab', 'bb', 'rb'):
                S.op('pool', lambda e, n_=n_: e.memset(wkm[n_][:], 0.0), w=['wkm_' + n_])
            STtmp = sbA("STtmp", [128, 4, 64], F32)
            S.op('dve', lambda e: e.memset(ST32[:], 0.0), w=['ST32'])
            S.op('dve', lambda e: e.memset(STb[:], 0.0), w=['STb'])
            PCt = sbA("PCt", [128, 4], F32)
            g_tok = sbA("g_tok", [128, 512], F32)
            cbt = sbA("cbt", [128, 8], F32)
            merged = sbA("merged", [128, D], BF16)
            mT = sbA("mT", [128, 8, 128], BF16)
            x1 = sbA("x1", [128, D], F32)

            def headnorm(ops_ap, ops_key, eps_col, dst_c):
                red(hn_s[:], ops_ap, ALU.add, [ops_key], ['hn_s'])
                act(hn_sq[:], ops_ap, AF.Square, [ops_key], ['hn_sq'])
                red(hn_q[:], hn_sq[:], ALU.add, ['hn_sq'], ['hn_q'])
                ts('dve', hn_m[:], hn_s[:], 1.0 / 64, None, ALU.mult, None, ['hn_s'], ['hn_m'])
                tt('dve', hn_r[:], hn_m[:], hn_m[:], ALU.mult, ['hn_m'], ['hn_r'])
                stt(hn_r[:], hn_q[:], 1.0 / 64, hn_r[:], ALU.mult, ALU.subtract, ['hn_q', 'hn_r'], ['hn_r'])
                act(hn_r[:], hn_r[:], AF.Sqrt, ['hn_r', 'eps'], ['hn_r'], bias=eps_t[:, eps_col:eps_col + 1])
                rcp(hn_r[:], 'hn_r')
                tt('dve', dst_c, ops_ap, bc(hn_m[:], 2, [128, 8, 64]), ALU.subtract, [ops_key, 'hn_m'], ['hn_c'])
                tt('dve', dst_c, dst_c, bc(hn_r[:], 2, [128, 8, 64]), ALU.mult, ['hn_c', 'hn_r'], ['hn_c'])

            ck(1)
            for n in range(nt):
                par = n % 2
                X, XK = xt[par], 'xt%d' % par
                ld('sp', X[:], x_d[n * 128:(n + 1) * 128, :], [XK])
                ld('sp', rot_t[par][:], rot_d[n, :, :], ['rot%d' % par])
                ROT = 'rot%d' % par
                rmsnorm(X[:], XK, gmix[:], 'gmix', h[:], 'h')
                for c in range(8):
                    tr(ptb[:, c, :], h[:, c * 128:(c + 1) * 128], identb[:], ['h', 'identb'], ['ptb'])
                cp('act', hT[:], ptb[:], ['ptb'], ['hT'])

                ck(2)
                def inproj_tok(ci, bank):
                    b = load_chunk(ci)
                    for kc in range(8):
                        mm(pbk[bank][:], hT[:, kc, :], wch[b][:, kc, :], kc == 0, kc == 7, ['hT', 'wch%d' % b], [PB[bank]])

                Cc = rot_t[par][:, 0:32]; Sc = rot_t[par][:, 32:64]
                kCc = rot_t[par][:, 64:96]; kSc = rot_t[par][:, 96:128]
                for qi in range(2):
                    bank = qi
                    inproj_tok(qi, bank)
                    pv = pbk[bank][:].rearrange("p (h two f) -> p h two f", two=2, f=32)
                    q1 = pv[:, :, 0, :]; q2 = pv[:, :, 1, :]
                    cc, ssn = (Cc, Sc) if qi == 0 else (kCc, kSc)
                    cb_ = bc(cc, 1, [128, 8, 32]); sb_ = bc(ssn, 1, [128, 8, 32])
                    ov = qk_rot[:, qi, :].rearrange("p (h two f) -> p h two f", two=2, f=32)
                    tt('dve', rt[0][:], q1, cb_, ALU.mult, [PB[bank], ROT], ['rt0'])
                    tt('dve', rt[1][:], q2, sb_, ALU.mult, [PB[bank], ROT], ['rt1'])
                    tt('dve', rt[2][:], q1, sb_, ALU.mult, [PB[bank], ROT], ['rt2'])
                    tt('dve', rt[3][:], q2, cb_, ALU.mult, [PB[bank], ROT], ['rt3'])
                    tt('pool', ov[:, :, 0, :], rt[0][:], rt[1][:], ALU.subtract, ['rt0', 'rt1'], ['qk_rot'])
                    tt('pool', ov[:, :, 1, :], rt[2][:], rt[3][:], ALU.add, ['rt2', 'rt3'], ['qk_rot'])
                inproj_tok(2, 2)
                cp('act', v_tok[:], pbk[2][:], [PB[2]], ['v_tok'])
                inproj_tok(3, 3)
                act(gr_s[:], pbk[3][:], AF.Silu, [PB[3]], ['gr_s'])

                ck(3)
                for c in range(8):
                    tr(ptb[:, c, :], qk_rot[:, c // 4, (c % 4) * 128:(c % 4 + 1) * 128], identb[:], ['qk_rot', 'identb'], ['ptb'])
                cp('act', qkT[:, 4:8, :], ptb[:, 4:8, :], ['ptb'], ['qkT'])
                cp('act', qTm[0:64, 0, :, :], ptb[0:64, 0:4, :], ['ptb'], ['qTm'])
                cp('act', qTm[64:128, 1, :, :], ptb[64:128, 0:4, :], ['ptb'], ['qTm'])
                tt('dve', qxT[:], ptb[:, 0:4, :], xiT[:], ALU.mult, ['ptb', 'xiT'], ['qxT'])
                tt('pool', kz[:], qk_rot[:, 1, :].rearrange("p (h d) -> p h d", d=64), bc(ZT, 2, [128, 8, 64]), ALU.mult, ['qk_rot', 'cm'], ['kz'])

                ck(31)
                for hh in range(8):
                    c, base = hh // 2, (hh % 2) * 64
                    bank = 4 + hh // 4
                    mm(pbk[bank][:, (hh % 4) * 128:(hh % 4 + 1) * 128], qkT[:, 4 + c, :], qTm[:, hh % 2, c, :], True, True, ['qkT', 'qTm'], [PB[bank]])
                for g in range(2):
                    tt('dve', PT[:, 4 * g:4 * g + 4, :], pbk[4 + g][:].rearrange("p (h i) -> p h i", i=128), DT[:, 4 * g:4 * g + 4, :], ALU.mult, [PB[4 + g], 'DT'], ['PT'])
                ck(32)
                for c in range(4):
                    mm(pbk[6][:, c * 128:(c + 1) * 128], qxT[:, c, :], Rb[:, c, :], True, False, ['qxT', 'Rb'], [PB[6]])
                    for w_ in range(2):
                        hh = 2 * c + w_
                        mm(pbk[6][:, hh * 64:(hh + 1) * 64], PT[:, hh, :], v_tok[:, hh * 64:(hh + 1) * 64], False, w_ == 1, ['PT', 'v_tok'], [PB[6]])
                ck(33)
                for c in range(4):
                    mm(pbk[4][:, c * 128:(c + 1) * 128], kz[:, 2 * c:2 * c + 2, :].rearrange("p a d -> p (a d)"), v_tok[:, c * 128:(c + 1) * 128], True, True, ['kz', 'v_tok'], [PB[4]])
                tt('pool', Rtmp[:], R32[:], CDb[:], ALU.mult, ['R32', 'CDb'], ['Rtmp'])
                p4v = pbk[4][:].rearrange("p (c x) -> p c x", x=128)
                tt('dve', R32[0:64, :, :], Rtmp[0:64, :, :], p4v[0:64, :, 0:64], ALU.add, ['Rtmp', PB[4]], ['R32'])
                tt('dve', R32[64:128, :, :], Rtmp[64:128, :, :], p4v[64:128, :, 64:128], ALU.add, ['Rtmp', PB[4]], ['R32'])
                cp('pool', Rb[0:64, :, 0:64], R32[0:64, :, :], ['R32'], ['Rb'])
                cp('pool', Rb[64:128, :, 64:128], R32[64:128, :, :], ['R32'], ['Rb'])
                ck(34)
                o3 = pbk[6][:].rearrange("p (h e) -> p h e", e=64)
                headnorm(o3, PB[6], 1, hn_c[:])
                hc2 = hn_c[:].rearrange("p h e -> p (h e)")
                tt('dve', hc2, hc2, gn3[:, 0, :], ALU.mult, ['hn_c', 'gn3_0'], ['hn_c'])
                tt('dve', y_bf[:], hc2, gr_s[:], ALU.mult, ['hn_c', 'gr_s'], ['y_bf'])
                ck(35)
                for c in range(4):
                    tr(ptb[:, c, :], y_bf[:, c * 128:(c + 1) * 128], identb[:], ['y_bf', 'identb'], ['ptb'])
                cp('act', yT[:], ptb[:, 0:4, :], ['ptb'], ['yT'])
                for hf in range(2):
                    for c in range(4):
                        mm(pbk[hf][:], yT[:, c, :], wbr[:, 0, c, hf * 512:(hf + 1) * 512], c == 0, c == 3, ['yT'] + WBR, [PB[hf]])

                ck(4)
                for j in range(4):
                    b = load_chunk(4 + j)
                    nm = 4 if j < 3 else 2
                    bank = 2 + (j % 2)
                    for m in range(nm):
                        for kc in range(8):
                            mm(pbk[bank][:, m * 128:(m + 1) * 128], wch[b][:, kc, m * 128:(m + 1) * 128], hT[:, kc, :], kc == 0, kc == 7, ['hT', 'wch%d' % b], [PB[bank]])
                    cp('act', ZB[:, 4 * j:4 * j + nm, 1:129], pbk[bank][:, 0:nm * 128].rearrange("p (m t) -> p m t", t=128), [PB[bank]], ['ZB'])
                cp('pool', ZB[:, :, 0:1], zlast[:], ['zlast'], ['ZB'])
                for (m0, m1) in ((0, 4), (4, 8), (8, 12), (12, 14)):
                    nm = m1 - m0
                    tmp = f4[7][:, 0:nm, :]
                    tt('pool', tmp, ZB[:, m0:m1, 0:128], bc(MU[:, m0:m1], 2, [128, nm, 128]), ALU.mult, ['ZB', 'pp'], [F4[7]])
                    tt('dve', zs[:, m0:m1, :], ZB[:, m0:m1, 1:129], bc(omm[:, m0:m1], 2, [128, nm, 128]), ALU.mult, ['ZB', 'omm'], ['zs'])
                    tt('dve', zs[:, m0:m1, :], zs[:, m0:m1, :], tmp, ALU.add, ['zs', F4[7]], ['zs'])
                cp('pool', zlast[:], ZB[:, :, 128:129], ['ZB'], ['zlast'])
                ck(5)
                rF = zs[:, 0:4, :]; krF = zs[:, 4:8, :]; vF = zs[:, 8:12, :]
                act(lor_in[0:64, 0, :], zs[0:64, 12, :], AF.Tanh, ['zs'], ['lor_in'])
                cp('act', lor_in[64:128, 0, :], zs[64:128, 12, :], ['zs'], ['lor_in'])
                act(lor_in[:, 1, :], zs[:, 13, :], AF.Sigmoid, ['zs'], ['lor_in'])
                for m in range(4):
                    mm(pbk[2][:, m * 128:(m + 1) * 128], lora[:, 0, m * 128:(m + 1) * 128], lor_in[:, 0, :], True, True, ['lora', 'lor_in'], [PB[2]])
                for m in range(4):
                    mm(pbk[3][:, m * 128:(m + 1) * 128], lora[:, 1, m * 128:(m + 1) * 128], lor_in[:, 0, :], True, True, ['lora', 'lor_in'], [PB[3]])
                mm(pbk[4][:], lor_in[:, 1, :], lora[:, 2, :], True, True, ['lora', 'lor_in'], [PB[4]])
                cp('act', g_tok[:], pbk[4][:], [PB[4]], ['g_tok'])
                sg, asig, kkF, kkn, kpr, bF, csF, tmpF = f4
                for m in range(4):
                    act(sg[:, m, :], pbk[2][:, m * 128:(m + 1) * 128], AF.Sigmoid, [PB[2], 'pp'], [F4[0]], bias=W0[:, m:m + 1])
                for m in range(4):
                    act(asig[:, m, :], pbk[3][:, m * 128:(m + 1) * 128], AF.Sigmoid, [PB[3], 'pp'], [F4[1]], bias=A0[:, m:m + 1])
                b4 = lambda t: bc(t, 2, [128, 4, 128])
                f2 = lambda t: t[:].rearrange("p m t -> p (m t)")
                tt('dve', kkF[:], krF, b4(KK_), ALU.mult, ['zs', 'pp'], [F4[2]])
                tt('pool', tmpF[:], kkF[:], kkF[:], ALU.mult, [F4[2]], [F4[7]])
                mm(pbk[2][:], BOw[:], f2(tmpF), True, True, ['BOw', F4[7]], [PB[2]])
                act(f2(kkn), pbk[2][:], AF.Sqrt, [PB[2]], [F4[3]])
                ts('dve', kkn[:], kkn[:], 1e-12, None, ALU.max, None, [F4[3]], [F4[3]])
                rcp(kkn[:], F4[3])
                tt('dve', kkn[:], kkn[:], kkF[:], ALU.mult, [F4[3], F4[2]], [F4[3]])
                tt('pool', kpr[:], asig[:], b4(KA), ALU.mult, [F4[1], 'pp'], [F4[4]])
                tt('pool', kpr[:], kpr[:], b4(omka[:]), ALU.add, [F4[4], 'omka'], [F4[4]])
                tt('dve', kpr[:], kpr[:], krF, ALU.mult, [F4[4], 'zs'], [F4[4]])
                tt('pool', bF[:], kkn[:], asig[:], ALU.mult, [F4[3], F4[1]], [F4[5]])
                tt('pool', tmpF[:], rF, kpr[:], ALU.mult, ['zs', F4[4]], [F4[7]])
                tt('pool', tmpF[:], tmpF[:], b4(RK), ALU.mult, [F4[7], 'pp'], [F4[7]])
                for c in range(4):
                    mm(pbk[3][:, 2 * c:2 * c + 2], tmpF[:, c, :], HS, True, True, [F4[7], 'cm'], [PB[3]])
                cp('act', cbt[:], pbk[3][:, 0:8], [PB[3]], ['cbt'])
                for m in range(4):
                    S.op('dve', lambda e, m=m: e.tensor_tensor_scan(out=csF[:, m, :], data0=f4ones[:], data1=sg[:, m, :], initial=0.0, op0=ALU.mult, op1=ALU.add), r=[F4[0], 'f4ones'], w=[F4[6]])
                E1, E2 = kkF, tmpF
                act(E1[:], csF[:], AF.Exp, [F4[6]], [F4[2]], scale=-C0)
                act(E2[:], csF[:], AF.Exp, [F4[6]], [F4[7]], scale=C0)
                tt('dve', csF[:], csF[:], sg[:], ALU.subtract, [F4[6], F4[0]], [F4[6]])
                act(sg[:], csF[:], AF.Exp, [F4[6]], [F4[0]], scale=-C0)
                E3 = sg
                cp('dve', PCt[:], E1[:, :, 127], [F4[2]], ['PCt'])
                stt(wk['ab'][:], kkn[:], -1.0, E3[:], ALU.mult, ALU.mult, [F4[3], F4[0]], ['wk_ab'])
                tt('dve', wk['rb'][:], rF, E1[:], ALU.mult, ['zs', F4[2]], ['wk_rb'])
                tt('pool', csF[:], bF[:], E2[:], ALU.mult, [F4[5], F4[7]], [F4[6]])
                cp('act', wk['bb'][:], csF[:], [F4[6]], ['wk_bb'])
                tt('pool', wk['bt'][:], csF[:], bc(PCt[:], 2, [128, 4, 128]), ALU.mult, [F4[6], 'PCt'], ['wk_bt'])
                tt('dve', bF[:], kpr[:], E2[:], ALU.mult, [F4[4], F4[7]], [F4[5]])
                cp('act', wk['kb'][:], bF[:], [F4[5]], ['wk_kb'])
                tt('pool', wk['kt'][:], bF[:], bc(PCt[:], 2, [128, 4, 128]), ALU.mult, [F4[5], 'PCt'], ['wk_kt'])
                cp('act', wk['vT'][:], vF, ['zs'], ['wk_vT'])
                for n_ in ('ab', 'bb', 'rb'):
                    cp('pool', wkm[n_][0:64, 0, :, :], wk[n_][0:64, :, :], ['wk_' + n_], ['wkm_' + n_])
                    cp('pool', wkm[n_][64:128, 1, :, :], wk[n_][64:128, :, :], ['wk_' + n_], ['wkm_' + n_])
                ck(6)
                for i, nmk in enumerate(('bt', 'kt', 'vT')):
                    for c in range(4):
                        tr(ptb[:, c, :], wk[nmk][:, c, :], identb[:], ['wk_' + nmk, 'identb'], ['ptb'])
                    cp('act', tok3[:, i, :].rearrange("p (c x) -> p c x", x=128), ptb[:, 0:4, :], ['ptb'], ['tok3_%d' % i])
                Btok = tok3[:, 0, :]; Ktok = tok3[:, 1, :]; Vtok = tok3[:, 2, :]

                ck(7)
                def hop(hh):
                    return hh // 2, (hh % 2) * 64
                for g in range(2):
                    hs = [4 * g + i for i in range(4)]
                    specs = [('bb', 'ab', SU, Nb[0], 'Nb0'), ('ab', 'bb', SL, NTb[0], 'NTb0'),
                             ('kb', 'ab', SU, Am['akT'], 'Am_akT'), ('bb', 'rb', SUI, Am['rbT'], 'Am_rbT'),
                             ('kb', 'rb', SUI, Am['rkT'], 'Am_rkT')]
                    for si, (l_, r_, msk, dst, dk) in enumerate(specs):
                        bank = 2 + (si % 3)
                        for i, hh in enumerate(hs):
                            c, base = hop(hh)
                            mm(pbk[bank][:, i * 128:(i + 1) * 128], wk[l_][:, c, :], wkm[r_][:, hh % 2, c, :], True, True, ['wk_' + l_, 'wkm_' + r_], [PB[bank]])
                        tt('dve', dst[:], pbk[bank][:].rearrange("p (h t) -> p h t", t=128), bc(msk, 1, [128, 4, 128]), ALU.mult, [PB[bank], 'cm'], [dk])
                    tt('pool', Qb[0][:], Nb[0][:], bc(identb[:], 1, [128, 4, 128]), ALU.add, ['Nb0', 'identb'], ['Qb0'])
                    cur = 0
                    for lvl in range(6):
                        nx = 1 - cur
                        last = (lvl == 5)
                        if not last:
                            for i in range(4):
                                mm(pbk[2][:, i * 128:(i + 1) * 128], NTb[cur][:, i, :], Nb[cur][:, i, :], True, True, ['NTb%d' % cur, 'Nb%d' % cur], [PB[2]])
                        for i in range(4):
                            mm(pbk[3][:, i * 128:(i + 1) * 128], Nb[cur][:, i, :], NTb[cur][:, i, :], True, True, ['NTb%d' % cur, 'Nb%d' % cur], [PB[3]])
                        if not last:
                            cp('dve', Nb[nx][:].rearrange("p h t -> p (h t)"), pbk[2][:], [PB[2]], ['Nb%d' % nx])
                        cp('act', NTb[nx][:].rearrange("p h t -> p (h t)"), pbk[3][:], [PB[3]], ['NTb%d' % nx])
                        for i in range(4):
                            mm(pbk[4][:, i * 128:(i + 1) * 128], identb[:], Qb[cur][:, i, :], True, False, ['identb', 'Qb%d' % cur], [PB[4]])
                            mm(pbk[4][:, i * 128:(i + 1) * 128], NTb[nx][:, i, :], Qb[cur][:, i, :], False, True, ['NTb%d' % nx, 'Qb%d' % cur], [PB[4]])
                        cp('act' if lvl % 2 else 'dve', Qb[nx][:].rearrange("p h t -> p (h t)"), pbk[4][:], [PB[4]], ['Qb%d' % nx])
                        cur = nx
                    Qf, QK = Qb[cur], 'Qb%d' % cur
                    for ci in range(2):
                        c = 2 * g + ci
                        mm(pbk[5][:, ci * 128:(ci + 1) * 128], wk['ab'][:, c, :], STb[:, c, :], True, False, ['wk_ab', 'STb'], [PB[5]])
                        for w_ in range(2):
                            i = 2 * ci + w_
                            hh = hs[i]
                            mm(pbk[5][:, i * 64:(i + 1) * 64], Am['akT'][:, i, :], Vtok[:, hh * 64:(hh + 1) * 64], False, w_ == 1, ['Am_akT', 'tok3_2'], [PB[5]])
                    cp('act', Xs[:], pbk[5][:, 0:256], [PB[5]], ['Xs'])
                    for i, hh in enumerate(hs):
                        oc = slice(i * 64, (i + 1) * 64)
                        mm(pbk[5][:, 256 + i * 64:256 + (i + 1) * 64], Qf[:, i, :], Xs[:, oc], True, True, [QK, 'Xs'], [PB[5]])
                    cp('act', Us[:, g * 256:(g + 1) * 256], pbk[5][:, 256:512], [PB[5]], ['Us'])
                    for ci in range(2):
                        c = 2 * g + ci
                        mm(pbk[6][:, c * 128:(c + 1) * 128], wk['rb'][:, c, :], STb[:, c, :], True, False, ['wk_rb', 'STb'], [PB[6]])
                        for w_ in range(2):
                            i = 2 * ci + w_
                            hh = hs[i]
                            hc = slice(hh * 64, (hh + 1) * 64)
                            mm(pbk[6][:, hc], Am['rkT'][:, i, :], Vtok[:, hc], False, False, ['Am_rkT', 'tok3_2'], [PB[6]])
                            mm(pbk[6][:, hc], Am['rbT'][:, i, :], Us[:, hc], False, w_ == 1, ['Am_rbT', 'Us'], [PB[6]])
                for c in range(4):
                    cs_ = slice(c * 128, (c + 1) * 128)
                    mm(pbk[5][:, cs_], Btok[:, cs_], Us[:, cs_], True, False, ['tok3_0', 'Us'], [PB[5]])
                    mm(pbk[5][:, cs_], Ktok[:, cs_], Vtok[:, cs_], False, True, ['tok3_1', 'tok3_2'], [PB[5]])
                tt('pool', STtmp[:], ST32[:], bc(PCt[:], 2, [128, 4, 64]), ALU.mult, ['ST32', 'PCt'], ['STtmp'])
                p5v = pbk[5][:].rearrange("p (c x) -> p c x", x=128)
                tt('dve', ST32[0:64, :, :], STtmp[0:64, :, :], p5v[0:64, :, 0:64], ALU.add, ['STtmp', PB[5]], ['ST32'])
                tt('dve', ST32[64:128, :, :], STtmp[64:128, :, :], p5v[64:128, :, 64:128], ALU.add, ['STtmp', PB[5]], ['ST32'])
                cp('pool', STb[0:64, :, 0:64], ST32[0:64, :, :], ['ST32'], ['STb'])
                cp('pool', STb[64:128, :, 64:128], ST32[64:128, :, :], ['ST32'], ['STb'])
                ck(8)
                o3 = pbk[6][:].rearrange("p (h e) -> p h e", e=64)
                headnorm(o3, PB[6], 2, hn_c[:])
                hc2 = hn_c[:].rearrange("p h e -> p (h e)")
                tt('dve', hc2, hc2, gn3[:, 1, :], ALU.mult, ['hn_c', 'gn3_1'], ['hn_c'])
                tt('dve', hc2, hc2, gn3[:, 2, :], ALU.add, ['hn_c', 'gn3_2'], ['hn_c'])
                tt('pool', hn_sq[:], Vtok.rearrange("p (h e) -> p h e", e=64), bc(cbt[:], 2, [128, 8, 64]), ALU.mult, ['tok3_2', 'cbt'], ['hn_sq'])
                tt('dve', hc2, hc2, hn_sq[:].rearrange("p h e -> p (h e)"), ALU.add, ['hn_c', 'hn_sq'], ['hn_c'])
                tt('dve', y_bf[:], hc2, g_tok[:], ALU.mult, ['hn_c', 'g_tok'], ['y_bf'])
                for c in range(4):
                    tr(ptb[:, c, :], y_bf[:, c * 128:(c + 1) * 128], identb[:], ['y_bf', 'identb'], ['ptb'])
                cp('act', yT[:], ptb[:, 0:4, :], ['ptb'], ['yT'])
                for hf in range(2):
                    for c in range(4):
                        mm(pbk[2 + hf][:], yT[:, c, :], wbr[:, 1, c, hf * 512:(hf + 1) * 512], c == 0, c == 3, ['yT'] + WBR, [PB[2 + hf]])

                ck(9)
                for gi in range(4):
                    bank = 4 + (gi % 2)
                    inproj_tok(8 + gi, bank)
                    act(gate_s[:, gi * 512:(gi + 1) * 512], pbk[bank][:], AF.Sigmoid, [PB[bank]], ['gate_s'])
                for hf in range(2):
                    sl = slice(hf * 512, (hf + 1) * 512)
                    tt('dve', x1[:, sl], pbk[hf][:], gate_s[:, sl], ALU.mult, [PB[hf], 'gate_s'], ['x1'])
                    tt('dve', merged[:, sl], pbk[2 + hf][:], gate_s[:, 1024 + hf * 512:1024 + (hf + 1) * 512], ALU.mult, [PB[2 + hf], 'gate_s'], ['merged'])
                    tt('pool', merged[:, sl], merged[:, sl], x1[:, sl], ALU.add, ['merged', 'x1'], ['merged'])
                for c in range(8):
                    tr(ptb[:, c, :], merged[:, c * 128:(c + 1) * 128], identb[:], ['merged', 'identb'], ['ptb'])
                cp('act', mT[:], ptb[:], ['ptb'], ['mT'])
                for hf in range(2):
                    for c in range(8):
                        mm(pbk[4 + hf][:], mT[:, c, :], wo[:, c, hf * 512:(hf + 1) * 512], c == 0, c == 7, ['mT'] + WO, [PB[4 + hf]])
                    tt('dve', x1[:, hf * 512:(hf + 1) * 512], pbk[4 + hf][:], X[:, hf * 512:(hf + 1) * 512], ALU.add, [PB[4 + hf], XK], ['x1'])
                S.dma('act', lambda e, n=n: e.dma_start(out=out_d[n * 128:(n + 1) * 128, :], in_=x1[:]), r=['x1'], w=['out%d' % n])
          except _Stop:
            pass

        if stage == 'A':
            S.wait_all('sp')
            S.emit()
            return nc
        S.barrier()

        esB = ExitStack()
        with esB:
          try:
            sbB = lambda n, s, d: esB.enter_context(nc.sbuf_tensor("sc_" + n, s, d))
            ptb2 = pbk[6][:].bitcast(BF16).rearrange("p (a b) -> p a b", b=128)
            PTB = [(ptb, 'ptb'), (ptb2, PB[6])]
            g3 = sbB("g3", [128, 3, D], F32)
            for i in range(3):
                ld('sp', g3[:, i, :], g4_d[1 + i, :].partition_broadcast(128), ['g3_%d' % i])
            wpg = sbB("wpg", [128, 8, D], BF16)
            for c in range(8):
                S.dma('pool', lambda e, c=c: e.dma_start(out=wpg[:, c, :], in_=wpg_d[c * 128:(c + 1) * 128, :]), w=['wpg%d' % c])
            WPG = ['wpg%d' % c for c in range(8)]
            wpu = sbB("wpu", [128, 2, D], BF16)
            for c in range(2):
                S.dma('pool', lambda e, c=c: e.dma_start(out=wpu[:, c, :], in_=wpu_d[c * 128:(c + 1) * 128, :]), w=['wpu%d' % c])
            WPU = ['wpu0', 'wpu1']
            skT = sbB("skT", [128, 16, 128], F32)
            s_sb2 = [sbB("s_sb%d" % i, [128, 16, 128], F32) for i in range(2)]
            s_sb = s_sb2[0]
            for g in range(16):
                ld('sp', s_sb[:, g, :], sk_d[g, :, :], ['s_sb0'])
            for g4i in range(4):
                bank = g4i % 2
                for i in range(4):
                    g = g4i * 4 + i
                    tr(pbk[bank][:, i * 128:(i + 1) * 128], s_sb[:, g, :], identf, ['s_sb0', 'cm'], [PB[bank]])
                cp('act', skT[:, g4i * 4:g4i * 4 + 4, :].rearrange("p g k -> p (g k)"), pbk[bank][:], [PB[bank]], ['skT'])

            NSB = 3
            ubuf = [sbB("ubuf%d" % i, [128, 2, D], BF16) for i in range(NSB)]
            vbuf = [sbB("vbuf%d" % i, [128, 2, D], BF16) for i in range(NSB)]
            wqb = [sbB("wqb%d" % i, [128, 8, 256], BF16) for i in range(2)]

            k = 0
            for kc in range(8):
                for hf in range(2):
                    b = k % 2
                    stg = wqb[b][:].rearrange("p a n -> p (a n)")[:, 0:1024]
                    S.dma('pool', lambda e, stg=stg, kc=kc, hf=hf: e.dma_start(out=stg, in_=wpq_d[kc * 128:(kc + 1) * 128, hf * 1024:(hf + 1) * 1024]), w=['wqb%d' % b])
                    S.dma('sp', lambda e, stg=stg, kc=kc, hf=hf: e.dma_start(out=wpqb_d[kc * 128:(kc + 1) * 128, hf * 1024:(hf + 1) * 1024], in_=stg), r=['wqb%d' % b], w=['wpqb'])
                    k += 1
            pu_v = pu_d.rearrange("(i j) d -> j i d", j=128)
            pv_v = pv_d.rearrange("(i j) d -> j i d", j=128)
            for j in range(128):
                b = j % 2
                ust = ubuf[b][:, 0, :]; uT = ubuf[b][:, 1, :]; vst = vbuf[b][:, 0, :]
                k0, k1, k2 = 'ub%d_0' % b, 'ub%d_1' % b, 'ub%d_2' % b
                S.dma('pool', lambda e, ust=ust, j=j: e.dma_start(out=ust, in_=pu_v[j, :, :]), w=[k0])
                pt_, pk_ = PTB[j % 2]
                for kc in range(8):
                    tr(pt_[:, kc, :], ust[:, kc * 128:(kc + 1) * 128], identb[:], [k0, 'identb'], [pk_])
                cp('act' if j % 2 else 'dve', uT.rearrange("p (a b) -> p a b", b=128), pt_[:, :, :], [pk_], [k1])
                S.dma('sp', lambda e, uT=uT, j=j: e.dma_start(out=utb_d[j, :, :], in_=uT), r=[k1], w=['utb'])
                S.dma('pool', lambda e, vst=vst, j=j: e.dma_start(out=vst, in_=pv_v[j, :, :]), w=[k2])
                S.dma('act', lambda e, vst=vst, j=j: e.dma_start(out=vtb_d[j, :, :], in_=vst), r=[k2], w=['vtb'])
            S.barrier()
            ck(101)

            x1b = [sbB("x1b%d" % i, [128, D], F32) for i in range(2)]
            ptl = [sbB("ptl%d" % i, [128, 256], F32) for i in range(2)]
            h2b2 = [sbB("h2b%d" % i, [128, D], BF16) for i in range(2)]
            h2T2 = [sbB("h2T%d" % i, [128, 8, 128], BF16) for i in range(2)]
            qc = sbB("qc", [128, 2048], F32)
            qT = qc[:].rearrange("p (g t) -> p g t", t=128)
            cand = qc[:].rearrange("p (h x) -> p h x", x=256)
            s_rp = sbB("s_rp", [128, 128], F32)
            vals2 = [sbB("vals%d" % i, [128, 16, 16], F32) for i in range(2)]
            cand2 = sbB("cand2", [128, 256], F32)
            best2 = [sbB("best%d" % i, [128, 8, 16], F32) for i in range(2)]
            gat = sbB("gat", [128, 8, 16], F32)
            gsum = sbB("gsum", [128, 8], F32)
            bias82 = [sbB("bias8%d" % i, [128, 8], F32) for i in range(2)]
            IB = 16
            Ptok = [sbB("Ptok0", [128, 128, IB], BF16)] * 2
            PTt = sbB("PTt", [128, 128, 128], BF16)
            JB = 16
            TG = 512 // JB
            NJB = 128 // JB
            xq = sbB("xq", [128, 4, 16, JB], F32)
            eq = sbB("eq", [128, 4, 16, JB], BF16)
            Qtok = [sbB("Qtok%d" % i, [128, 128, JB], BF16) for i in range(2)]
            QTt = [sbB("QTt%d" % i, [128, JB, 128], BF16) for i in range(2)]
            act_sb2 = [sbB("act_sb%d" % i, [128, JB, 128], BF16) for i in range(2)]
            coef2 = [sbB("coef%d" % i, [128, JB, 128], BF16) for i in range(2)]
            x2 = sbB("x2", [128, D], F32)
            p_bf = sbB("p_bf", [128, 256], BF16)
            pTt = sbB("pTt", [128, 2, 128], BF16)
            pg_s = sbB("pg_s", [128, D], F32)
            uctr = [0]; vctr = [0]; qctr = [0]; pbc = [0]

            def nextptb():
                r_ = PTB[pbc[0] % 2]
                pbc[0] += 1
                return r_

            def front_end(n, stage):
                par = n % 2
                P_ = str(par)
                X1, X1K = x1b[par], 'x1b%d' % par
                h2b, h2T, s_sb, vals, best, bias8 = h2b2[par], h2T2[par], s_sb2[par], vals2[par], best2[par], bias82[par]
                v4 = vals[:].rearrange("p (h c) a -> p h c a", c=2)
                if stage == 0:
                    ld('sp', X1[:], out_d[n * 128:(n + 1) * 128, :], [X1K], r=['out%d' % n])
                    ld('sp', ptl[par][:], p_d[n * 128:(n + 1) * 128, :], ['ptl%d' % par])
                    rmsnorm(X1[:], X1K, g3[:, 0, :], 'g3_0', h2b[:], 'h2b' + P_)
                elif stage == 1:
                    for c in range(8):
                        tr(ptb[:, c, :], h2b[:, c * 128:(c + 1) * 128], identb[:], ['h2b' + P_, 'identb'], ['ptb'])
                    cp('act', h2T[:], ptb[:], ['ptb'], ['h2T' + P_])
                elif stage in (2, 3, 4, 5):
                    for g4i in (stage - 2,):
                        bank = 2 + (g4i % 2)
                        for i2 in range(2):
                            wb = qctr[0] % 2
                            qctr[0] += 1
                            c0 = g4i * 512 + i2 * 256
                            S.dma('sp', lambda e, wb=wb, c0=c0: e.dma_start(
                                out=wqb[wb][:], in_=wpqb_d[:, c0:c0 + 256].rearrange("(kc p) n -> p kc n", p=128)),
                                r=['wpqb'], w=['wqb%d' % wb])
                            for i1 in range(2):
                                i = i2 * 2 + i1
                                for kc in range(8):
                                    mm(pbk[bank][:, i * 128:(i + 1) * 128], wqb[wb][:, kc, i1 * 128:(i1 + 1) * 128], h2T[:, kc, :], kc == 0, kc == 7, ['h2T' + P_, 'wqb%d' % wb], [PB[bank]])
                        cp('act', qT[:, g4i * 4:g4i * 4 + 4, :].rearrange("p g t -> p (g t)"), pbk[bank][:], [PB[bank]], ['qc'])
                elif stage == 6:
                    for g4i in range(4):
                        bank = 4 + (g4i % 2)
                        for i in range(4):
                            g = g4i * 4 + i
                            mm(pbk[bank][:, i * 128:(i + 1) * 128], qT[:, g, :], skT[:, g, :], True, True, ['qc', 'skT'], [PB[bank]])
                        cp('act', s_sb[:, g4i * 4:g4i * 4 + 4, :].rearrange("p g k -> p (g k)"), pbk[bank][:], [PB[bank]], ['s_sb' + P_])
                elif stage in (7, 8, 9, 10):
                    for g in range((stage - 7) * 4, (stage - 6) * 4):
                        S.op('dve', lambda e, g=g: e.max(out=vals[:, g, 0:8], in_=s_sb[:, g, :]), r=['s_sb' + P_], w=['vals' + P_])
                        S.op('dve', lambda e, g=g: e.match_replace(out=s_rp[:], in_to_replace=vals[:, g, 0:8], in_values=s_sb[:, g, :], imm_value=-1e30), r=['s_sb' + P_, 'vals' + P_], w=['s_rp'])
                        S.op('dve', lambda e, g=g: e.max(out=vals[:, g, 8:16], in_=s_rp[:]), r=['s_rp'], w=['vals' + P_])
                elif stage == 11:
                    for hh in range(8):
                        tt('dve', cand[:, hh, :].rearrange("p (a b) -> p a b", b=16), bc(v4[:, hh, 0, :], 2, [128, 16, 16]), bc(v4[:, hh, 1, :], 1, [128, 16, 16]), ALU.add, ['vals' + P_], ['qc'])
                elif stage in (12, 13):
                    for hh in range((stage - 12) * 4, (stage - 11) * 4):
                        S.op('dve', lambda e, hh=hh: e.max(out=best[:, hh, 0:8], in_=cand[:, hh, :]), r=['qc'], w=['best' + P_])
                        S.op('dve', lambda e, hh=hh: e.match_replace(out=cand2[:], in_to_replace=best[:, hh, 0:8], in_values=cand[:, hh, :], imm_value=-1e30), r=['qc', 'best' + P_], w=['cand2'])
                        S.op('dve', lambda e, hh=hh: e.max(out=best[:, hh, 8:16], in_=cand2[:]), r=['cand2'], w=['best' + P_])
                elif stage == 14:
                    tt('dve', gat[:], best[:], bc(best[:, :, 0], 2, [128, 8, 16]), ALU.subtract, ['best' + P_], ['gat'])
                    act(gat[:], gat[:], AF.Exp, ['gat'], ['gat'])
                    red(gsum[:], gat[:], ALU.add, ['gat'], ['gsum'])
                    act(gsum[:], gsum[:], AF.Ln, ['gsum'], ['gsum'])
                    stt(bias8[:], best[:, :, 0], -1.0, gsum[:], ALU.mult, ALU.subtract, ['best' + P_, 'gsum'], ['bias8' + P_])

            NFE = 15

            for st_ in range(NFE):
                front_end(0, st_)
            for n in range(nt):
                par = n % 2
                P_ = str(par)
                X1, X1K = x1b[par], 'x1b%d' % par
                h2b, h2T, s_sb, vals, best, bias8 = h2b2[par], h2T2[par], s_sb2[par], vals2[par], best2[par], bias82[par]
                v4 = vals[:].rearrange("p (h c) a -> p h c a", c=2)
                s4 = s_sb[:].rearrange("p (h c) k -> p h c k", c=2)
                def p_build(ibs, v4_, s4_, pk_):
                    for ib in ibs:
                        tt('dve', Ptok[0][:].rearrange("t (h a) i -> t h a i", a=16),
                           bc(s4_[:, :, 0, ib * IB:(ib + 1) * IB], 2, [128, 8, 16, IB]),
                           bc(v4_[:, :, 0, :], 3, [128, 8, 16, IB]), ALU.is_equal, ['s_sb' + pk_, 'vals' + pk_], ['Ptok0'])
                        for i8 in range(IB // 8):
                            pt_, pkk = nextptb()
                            for il in range(8):
                                tr(pt_[:, il, :], Ptok[0][:, :, i8 * 8 + il], identb[:], ['Ptok0', 'identb'], [pkk])
                            i0_ = ib * IB + i8 * 8
                            cp('act' if (i8 % 2) else 'dve', PTt[:, :, i0_:i0_ + 8].rearrange("n t i -> n i t"), pt_[:, :, :], [pkk], ['PTt'])

                if n == 0:
                    p_build(range(128 // IB), v4, s4, P_)

                def q_elem(jb):
                    qb_ = jb % 2
                    for hg in range(2):
                        hsl = slice(hg * 4, hg * 4 + 4)
                        tt('dve', xq[:], bc(v4[:, hsl, 0, :], 3, [128, 4, 16, JB]), bc(s4[:, hsl, 1, jb * JB:(jb + 1) * JB], 2, [128, 4, 16, JB]), ALU.add, ['vals' + P_, 's_sb' + P_], ['xq'])
                        for h_ in range(4):
                            hh = hg * 4 + h_
                            act(eq[:, h_, :, :], xq[:, h_, :, :], AF.Exp, ['xq', 'bias8' + P_], ['eq'], bias=bias8[:, hh:hh + 1])
                        tt('dve', xq[:].rearrange("p h a j -> p h (a j)"), xq[:].rearrange("p h a j -> p h (a j)"), bc(best[:, hsl, 15], 2, [128, 4, 16 * JB]), ALU.is_ge, ['xq', 'eq', 'best' + P_], ['xq'])
                        tt('dve', Qtok[qb_][:, hg * 64:(hg + 1) * 64, :].rearrange("p (h a) j -> p h a j", a=16), xq[:], eq[:], ALU.mult, ['xq', 'eq'], ['Qtok%d' % qb_])

                def q_tr(jb):
                    qb_ = jb % 2
                    for j8 in range(JB // 8):
                        pt_, pk_ = nextptb()
                        for jl in range(8):
                            tr(pt_[:, jl, :], Qtok[qb_][:, :, j8 * 8 + jl], identb[:], ['Qtok%d' % qb_, 'identb'], [pk_])
                        cp('act', QTt[qb_][:, j8 * 8:j8 * 8 + 8, :], pt_[:, :, :], [pk_], ['QTt%d' % qb_])

                def act_blk(jb):
                    ab = jb % 2
                    for jq in range(JB // 4):
                        bank = 2 + (jq % 2)
                        for j2 in range(2):
                            j0 = jb * JB + jq * 4 + j2 * 2
                            ub = uctr[0] % NSB
                            uctr[0] += 1
                            S.dma('sp', lambda e, ub=ub, j0=j0: e.dma_start(out=ubuf[ub][:], in_=utb_d[j0:j0 + 2, :, :].rearrange("j d x -> d j x")),
                                  r=['utb'], w=['ubuf%d' % ub])
                            for jl2 in range(2):
                                jl = j2 * 2 + jl2
                                for kc in range(8):
                                    mm(pbk[bank][:, jl * 128:(jl + 1) * 128], ubuf[ub][:, jl2, kc * 128:(kc + 1) * 128], h2T[:, kc, :], kc == 0, kc == 7, ['ubuf%d' % ub, 'h2T' + P_], [PB[bank]])
                        act(act_sb2[ab][:, jq * 4:jq * 4 + 4, :].rearrange("i j t -> i (j t)"), pbk[bank][:], AF.Gelu_apprx_tanh, [PB[bank]], ['act_sb%d' % ab])

                def v_part(jb, jqs):
                    qb_ = jb % 2
                    for jq in jqs:
                        j0 = jb * JB + jq * 2
                        vb = vctr[0] % NSB
                        vctr[0] += 1
                        S.dma('sp', lambda e, vb=vb, j0=j0: e.dma_start(out=vbuf[vb][:], in_=vtb_d[j0:j0 + 2, :, :].rearrange("j i x -> i j x")),
                              r=['vtb'], w=['vbuf%d' % vb])
                        for jl in range(2):
                            j = j0 + jl
                            for hf in range(2):
                                mm(pbk[hf][:], coef2[qb_][:, jq * 2 + jl, :], vbuf[vb][:, jl, hf * 512:(hf + 1) * 512], j == 0, j == 127, ['coef%d' % qb_, 'vbuf%d' % vb], [PB[hf]])

                def w_blk(jb, vjb, hook=None):
                    qb_ = jb % 2
                    ntg = 128 // TG
                    nvq = (JB // 2) // ntg
                    for tg in range(ntg):
                        bank = 4 + (tg % 2)
                        for tl in range(TG):
                            t = tg * TG + tl
                            mm(pbk[bank][:, tl * JB:(tl + 1) * JB], PTt[:, t, :], QTt[qb_][:, :, t], True, True, ['PTt', 'QTt%d' % qb_], [PB[bank]])
                        tt('dve', coef2[qb_][:, :, tg * TG:(tg + 1) * TG], pbk[bank][:].rearrange("i (t j) -> i j t", j=JB), act_sb2[qb_][:, :, tg * TG:(tg + 1) * TG], ALU.mult, [PB[bank], 'act_sb%d' % qb_], ['coef%d' % qb_])
                        if vjb is not None:
                            v_part(vjb, range(tg * nvq, (tg + 1) * nvq))
                        if hook is not None:
                            hook(jb * ntg + tg)

                def v_blk(jb):
                    v_part(jb, range(JB // 2))

                q_elem(0)
                q_tr(0)
                for jb in range(NJB):
                    if jb + 1 < NJB:
                        q_elem(jb + 1)
                    act_blk(jb)
                    if jb + 1 < NJB:
                        q_tr(jb + 1)
                    def hook(slot, n=n):
                        if n + 1 < nt and slot % 2 == 0 and slot // 2 < NFE:
                            front_end(n + 1, slot // 2)
                    w_blk(jb, jb - 1 if jb >= 1 else None, hook)
                if n + 1 < nt:
                    pn = (n + 1) % 2
                    v4n = vals2[pn][:].rearrange("p (h c) a -> p h c a", c=2)
                    s4n = s_sb2[pn][:].rearrange("p (h c) k -> p h c k", c=2)
                    p_build(range(0, 4), v4n, s4n, str(pn))
                v_blk(NJB - 1)
                if n + 1 < nt:
                    p_build(range(4, 128 // IB), v4n, s4n, str(pn))
                ck(107)
                for hf in range(2):
                    sl = slice(hf * 512, (hf + 1) * 512)
                    tt('dve', x2[:, sl], pbk[hf][:], X1[:, sl], ALU.add, [PB[hf], X1K], ['x2'])
                rmsnorm(x2[:], 'x2', g3[:, 1, :], 'g3_1', h2b[:], 'h2b' + P_)
                for c in range(8):
                    tr(ptb[:, c, :], h2b[:, c * 128:(c + 1) * 128], identb[:], ['h2b' + P_, 'identb'], ['ptb'])
                cp('act', h2T[:], ptb[:], ['ptb'], ['h2T' + P_])
                cp('pool', p_bf[:], ptl[par][:], ['ptl%d' % par], ['p_bf'])
                for c in range(2):
                    tr(ptb[:, c, :], p_bf[:, c * 128:(c + 1) * 128], identb[:], ['p_bf', 'identb'], ['ptb'])
                cp('act', pTt[:], ptb[:, 0:2, :], ['ptb'], ['pTt'])
                for hf in range(2):
                    sl = slice(hf * 512, (hf + 1) * 512)
                    for c in range(8):
                        mm(pbk[2 + hf][:], h2T[:, c, :], wpg[:, c, sl], c == 0, c == 7, ['h2T' + P_] + WPG, [PB[2 + hf]])
                    act(pg_s[:, sl], pbk[2 + hf][:], AF.Sigmoid, [PB[2 + hf]], ['pg_s'])
                    for c in range(2):
                        mm(pbk[4 + hf][:], pTt[:, c, :], wpu[:, c, sl], c == 0, c == 1, ['pTt'] + WPU, [PB[4 + hf]])
                    tt('dve', pg_s[:, sl], pg_s[:, sl], pbk[4 + hf][:], ALU.mult, ['pg_s', PB[4 + hf]], ['pg_s'])
                tt('dve', pg_s[:], x2[:], pg_s[:], ALU.add, ['x2', 'pg_s'], ['pg_s'])
                rmsnorm(pg_s[:], 'pg_s', g3[:, 2, :], 'g3_2', x2[:], 'x2')
                S.dma('act', lambda e, n=n: e.dma_start(out=out_d[n * 128:(n + 1) * 128, :], in_=x2[:]), r=['x2'], w=['out%d' % n])
          except _Stop:
            pass

        S.wait_all('sp')
        S.emit()
    return nc


def make_in_maps(inputs, nt, ncores):
    f = np.float32
    c = host_consts(nt)
    g = lambda k: np.asarray(inputs[k], dtype=f)
    S_ = nt * 128
    mu = g('rwkv_mu')[0]
    pp = np.zeros((128, 34), f)
    pp[:, 0:14] = mu.reshape(14, 128).T
    for j, k in enumerate(('rwkv_w0', 'rwkv_a0', 'rwkv_k_k', 'rwkv_k_a')):
        pp[:, 14 + 4 * j:18 + 4 * j] = g(k)[0].reshape(4, 128).T
    pp[:, 30:34] = g('rwkv_r_k')[0].reshape(4, 128).T
    g4 = np.stack([g('g_mix')[0], g('g_ffn')[0], g('g_ple')[0], g('g_final')], 0)
    gn3 = np.stack([g('ret_gn_g')[0], g('rwkv_gn_g')[0], g('rwkv_gn_b')[0]], 0)
    lora = np.zeros((128, 3, 512), f)
    lora[0:64, 0, :] = g('rwkv_w_up')[0]
    lora[64:128, 1, :] = g('rwkv_a_up')[0]
    lora[:, 2, :] = g('rwkv_g_up')[0]
    wbr = np.stack([g('w_ret_br')[0], g('w_rwkv_br')[0]], 0)
    shared = dict(w_in=g('w_in')[0], pp=pp, g4=np.ascontiguousarray(g4), gn3=np.ascontiguousarray(gn3),
                  lora=lora, wbr=np.ascontiguousarray(wbr), w_o=g('w_o')[0], w_pq=g('w_pq')[0],
                  sk=np.ascontiguousarray(g('peer_sub_keys')[0].reshape(16, 128, 128)),
                  peer_u=g('peer_u')[0], peer_v=g('peer_v')[0], w_ple_gate=g('w_ple_gate')[0],
                  w_ple_up=g('w_ple_up')[0], rot=c['rot'], DT=c['DT'], xiT=c['xiT'], CDb=c['CDb'], cm=c['cm'])
    x = g('x'); p = g('p')[0]
    maps = []
    for i in range(ncores):
        m = dict(shared)
        m['x'] = np.ascontiguousarray(x[i, :S_])
        m['p'] = np.ascontiguousarray(p[i, :S_])
        maps.append(m)
    return maps


def kernel(**inputs):
    nt = SEQ // 128
    nc = build(nt)
    in_maps = make_in_maps(inputs, nt, NCORES)
    res = run_bass_kernel_spmd(nc, in_maps, core_ids=list(range(NCORES)))
    out = np.stack([np.asarray(r["out"], dtype=np.float32) for r in res.results], axis=0)
    return out
```

```python
import math
import numpy as np
from contextlib import ExitStack
import concourse.bass as bass
import concourse.mybir as mybir
from concourse.bass_utils import run_bass_kernel_spmd

F32 = mybir.dt.float32
BF16 = mybir.dt.bfloat16
U32 = mybir.dt.uint32
I32 = mybir.dt.int32
AF = mybir.ActivationFunctionType
ALU = mybir.AluOpType
AX = mybir.AxisListType

D = 1024
SEQ = 4096
NCORES = 8
IN_COLS = 5888
C0 = math.exp(-0.5)


class Sched:
    SELF_SYNC = {'pe': False, 'act': True, 'dve': True, 'pool': True, 'sp': True}

    def __init__(self, nc, es, n_dma_sems=8):
        self.nc = nc
        self.ops = {e: [] for e in ('pe', 'act', 'dve', 'pool', 'sp')}
        self.sem = {e: es.enter_context(nc.semaphore('prog_' + e)) for e in self.ops}
        self.cnt = {e: 0 for e in self.ops}
        self.waited = {e: {} for e in self.ops}
        self.last_w = {}
        self.readers = {}
        self.dsem = {}
        self.dcnt = {}
        self.drr = {}
        for q in ('sp', 'act', 'pool'):
            self.dsem[q] = [es.enter_context(nc.semaphore('dma_%s_%d' % (q, i)))
                            for i in range(n_dma_sems)]
            self.dcnt[q] = [0] * n_dma_sems
            self.drr[q] = 0
        self.sem_id = {}
        self.pending = {e: [] for e in self.ops}

    def barrier(self):
        toks = list(self.last_w.values())
        for ts_ in self.readers.values():
            toks.extend(ts_)
        for e in self.ops:
            self.pending[e] = list(toks)

    def _deps(self, r, w):
        toks = []
        for k in r:
            t = self.last_w.get(k)
            if t is not None:
                toks.append(t)
        for k in w:
            t = self.last_w.get(k)
            if t is not None:
                toks.append(t)
            toks.extend(self.readers.get(k, ()))
        return toks

    def _waits(self, e, toks):
        need = {}
        for (sem, val, src) in toks:
            if src == e and not self.SELF_SYNC[e]:
                continue
            key = id(sem)
            self.sem_id[key] = sem
            if self.waited[e].get(key, 0) >= val:
                continue
            if need.get(key, 0) < val:
                need[key] = val
        out = []
        for key, val in need.items():
            self.waited[e][key] = val
            out.append((self.sem_id[key], val))
        return out

    def _commit(self, tok, r, w):
        for k in w:
            self.last_w[k] = tok
            self.readers[k] = []
        for k in r:
            if k in w:
                continue
            self.readers.setdefault(k, []).append(tok)

    def op(self, e, fn, r=(), w=()):
        r = list(r); w = list(w)
        toks = self._deps(r, w) + self.pending[e]
        self.pending[e] = []
        waits = self._waits(e, toks)
        self.cnt[e] += 1
        tok = (self.sem[e], self.cnt[e], e)
        self.ops[e].append((waits, fn, (self.sem[e], 1)))
        self._commit(tok, r, w)
        return tok

    def dma(self, q, fn, r=(), w=()):
        r = list(r); w = list(w)
        j = self.drr[q]
        self.drr[q] = (j + 1) % len(self.dsem[q])
        sem = self.dsem[q][j]
        toks = self._deps(r, w) + self.pending[q]
        self.pending[q] = []
        if self.dcnt[q][j] > 0:
            toks.append((sem, 16 * self.dcnt[q][j], None))
        waits = self._waits(q, toks)
        self.dcnt[q][j] += 1
        tok = (sem, 16 * self.dcnt[q][j], None)
        self.ops[q].append((waits, fn, (sem, 16)))
        self._commit(tok, r, w)
        return tok

    def wait_all(self, e):
        toks = list(self.last_w.values())
        for ts in self.readers.values():
            toks.extend(ts)
        waits = self._waits(e, toks)
        self.ops[e].append((waits, None, None))

    def emit(self):
        nc = self.nc
        with nc.Block() as block:
            def run(e, eng):
                for waits, fn, inc in self.ops[e]:
                    for sem, val in waits:
                        eng.wait_ge(sem, val)
                    if fn is not None:
                        ins = fn(eng)
                        ins.then_inc(inc[0], inc[1])

            @block.sync
            def _(eng):
                run('sp', eng)

            @block.scalar
            def _(eng):
                run('act', eng)

            @block.vector
            def _(eng):
                run('dve', eng)

            @block.gpsimd
            def _(eng):
                run('pool', eng)

            @block.tensor
            def _(eng):
                run('pe', eng)


def host_consts(nt):
    f = np.float32
    S = nt * 128
    half = 32
    inv_freq = (10000.0 ** (-np.arange(half, dtype=f) * f(2.0) / f(64))).astype(f)
    ang = (np.arange(S, dtype=f)[:, None] * inv_freq[None, :]).astype(f)
    cos = np.cos(ang).astype(f); sin = np.sin(ang).astype(f)
    rot = np.zeros((nt, 128, 128), f)
    rot[:, :, 0:32] = cos.reshape(nt, 128, 32)
    rot[:, :, 32:64] = sin.reshape(nt, 128, 32)
    rot[:, :, 64:96] = cos.reshape(nt, 128, 32) * f(0.125)
    rot[:, :, 96:128] = sin.reshape(nt, 128, 32) * f(0.125)
    H = 8
    lg = np.log1p(-(2.0 ** (-5.0 - np.arange(H, dtype=np.float64))))
    idx = np.arange(128, dtype=np.float64)
    diff = idx[None, :] - idx[:, None]
    DT = np.where(diff[:, None, :] >= 0, np.exp(lg[None, :, None] * np.maximum(diff, 0)[:, None, :]), 0.0).astype(f)
    xiT = np.zeros((128, 4, 128), f)
    CDb = np.zeros((128, 4, 64), f)
    for c in range(4):
        for p in range(128):
            h = 2 * c + p // 64
            xiT[p, c, :] = np.exp(lg[h] * (idx + 1.0))
            CDb[p, c, :] = np.exp(lg[h] * 128.0)
    ZT = np.exp(lg[None, :] * (127.0 - idx)[:, None]).astype(f)
    s = np.arange(128)
    SU = (s[None, :] > s[:, None]).astype(f)
    SUI = (s[None, :] >= s[:, None]).astype(f)
    SL = SU.T.copy()
    BO = np.zeros((128, 128), f); BO[:64, :64] = 1; BO[64:, 64:] = 1
    HS = np.zeros((128, 2), f); HS[:64, 0] = 1; HS[64:, 1] = 1
    ident = np.eye(128, dtype=f)
    io16 = np.tile(np.arange(16, dtype=f)[None, :], (128, 1))
    cm = np.concatenate([ident, SU, SUI, SL, BO, ZT, HS, io16, io16 * 16], axis=1)
    return dict(rot=rot, DT=DT.reshape(128, 1024), xiT=xiT.reshape(128, 512),
                CDb=CDb.reshape(128, 256), cm=np.ascontiguousarray(cm))


CM_W = 128 * 5 + 8 + 2 + 16 + 16


class _Stop(Exception):
    pass


def build(nt, stage='full', stop_after=None):
    nc = bass.Bass("TRN2", target_bir_lowering=False)
    S_ = nt * 128
    WDT = BF16
    dram = lambda n, s, d, k="ExternalInput": nc.dram_tensor(n, s, d, kind=k).ap()
    x_d = dram("x", [S_, D], F32)
    p_d = dram("p", [S_, 256], F32)
    w_in_d = dram("w_in", [D, IN_COLS], F32)
    pp_d = dram("pp", [128, 34], F32)
    g4_d = dram("g4", [4, D], F32)
    gn3_d = dram("gn3", [3, 512], F32)
    lora_d = dram("lora", [128, 3, 512], F32)
    wbr_d = dram("wbr", [2, 512, D], F32)
    wo_d = dram("w_o", [D, D], F32)
    wpq_d = dram("w_pq", [D, 2048], F32)
    sk_d = dram("sk", [16, 128, 128], F32)
    pu_d = dram("peer_u", [16384, D], F32)
    pv_d = dram("peer_v", [16384, D], F32)
    wpg_d = dram("w_ple_gate", [D, D], F32)
    wpu_d = dram("w_ple_up", [256, D], F32)
    rot_d = dram("rot", [nt, 128, 128], F32)
    DT_d = dram("DT", [128, 1024], F32)
    xiT_d = dram("xiT", [128, 512], F32)
    CDb_d = dram("CDb", [128, 256], F32)
    cm_d = dram("cm", [128, CM_W], F32)
    out_d = dram("out", [S_, D], F32, "ExternalOutput")
    winb_d = nc.dram_tensor("winb", [D, IN_COLS], BF16, kind="Internal").ap()
    utb_d = nc.dram_tensor("utb", [128, 128, D], BF16, kind="Internal").ap()
    vtb_d = nc.dram_tensor("vtb", [128, 128, D], BF16, kind="Internal").ap()
    wpqb_d = nc.dram_tensor("wpqb", [D, 2048], BF16, kind="Internal").ap()

    es = ExitStack()
    with es:
        S = Sched(nc, es)
        sb = lambda n, s, d: es.enter_context(nc.sbuf_tensor("sb_" + n, s, d))
        ps = lambda n, s, d: es.enter_context(nc.psum_tensor(n, s, d))

        def mm(out, lhsT, rhs, start, stop, r, w):
            S.op('pe', lambda e: e.matmul(out, lhsT=lhsT, rhs=rhs, start=start, stop=stop), r=r, w=w)

        def tr(out, in_, ident, r, w):
            S.op('pe', lambda e: e.transpose(out=out, in_=in_, identity=ident), r=r, w=w)

        def act(out, in_, func, r, w, **kw):
            S.op('act', lambda e: e.activation(out=out, in_=in_, func=func, **kw), r=r, w=w)

        def cp(eng, out, in_, r, w):
            if eng == 'act':
                S.op('act', lambda e: e.copy(out=out, in_=in_), r=r, w=w)
            else:
                S.op(eng, lambda e: e.tensor_copy(out=out, in_=in_), r=r, w=w)

        def tt(eng, out, in0, in1, op, r, w):
            S.op(eng, lambda e: e.tensor_tensor(out=out, in0=in0, in1=in1, op=op), r=r, w=w)

        def ts(eng, out, in0, s1, s2, op0, op1, r, w):
            if op1 is None:
                S.op(eng, lambda e: e.tensor_scalar(out=out, in0=in0, scalar1=s1, scalar2=None, op0=op0), r=r, w=w)
            else:
                S.op(eng, lambda e: e.tensor_scalar(out=out, in0=in0, scalar1=s1, scalar2=s2, op0=op0, op1=op1), r=r, w=w)

        def stt(out, in0, scalar, in1, op0, op1, r, w):
            S.op('dve', lambda e: e.scalar_tensor_tensor(out=out, in0=in0, scalar=scalar, in1=in1, op0=op0, op1=op1), r=r, w=w)

        def red(out, in_, op, r, w, axis=AX.X):
            S.op('dve', lambda e: e.tensor_reduce(out=out, in_=in_, axis=axis, op=op), r=r, w=w)

        def rcp(t, k):
            S.op('dve', lambda e: e.reciprocal(out=t, in_=t), r=[k], w=[k])

        def ld(q, out, in_, w, r=()):
            S.dma(q, lambda e: e.dma_start(out=out, in_=in_), r=r, w=w)

        def bc(ap, axis, shape):
            return ap.unsqueeze(axis).to_broadcast(shape)

        ptb = ps("ptb", [128, 8, 128], BF16)
        pbk = [ps("pb%d" % i, [128, 512], F32) for i in range(7)]
        PB = ['pb%d' % i for i in range(7)]

        cm = sb("cm", [128, CM_W], F32)
        ld('sp', cm[:], cm_d[:, :], ['cm'])
        identf = cm[:, 0:128]
        SU = cm[:, 128:256]; SUI = cm[:, 256:384]; SL = cm[:, 384:512]; BO = cm[:, 512:640]
        ZT = cm[:, 640:648]; HS = cm[:, 648:650]; IO16 = cm[:, 650:666]; IO16X = cm[:, 666:682]
        identb = sb("identb", [128, 128], BF16)
        cp('dve', identb[:], identf, ['cm'], ['identb'])
        eps_t = sb("eps_t", [128, 4], F32)
        S.op('dve', lambda e: e.memset(eps_t[:, 0:1], 1e-6), w=['eps'])
        S.op('dve', lambda e: e.memset(eps_t[:, 1:2], 1e-5), w=['eps'])
        S.op('dve', lambda e: e.memset(eps_t[:, 2:3], 64e-5), w=['eps'])
        sq_junk = sb("sq_junk", [128, D], BF16)
        rs_ss = sb("rs_ss", [128, 1], F32)
        rs_rstd = sb("rs_rstd", [128, 1], F32)

        def rmsnorm(src, src_key, gtab, gkey, dst, dst_key):
            act(sq_junk[:], src, AF.Square, [src_key], ['sq_junk', 'rs_ss'], accum_out=rs_ss[:])
            act(rs_rstd[:], rs_ss[:], AF.Sqrt, ['rs_ss', 'eps'], ['rs_rstd'], scale=1.0 / D, bias=eps_t[:, 0:1])
            rcp(rs_rstd[:], 'rs_rstd')
            stt(dst, src, rs_rstd[:, 0:1], gtab, ALU.mult, ALU.mult, [src_key, 'rs_rstd', gkey], [dst_key])

        def ck(k):
            if stop_after == k:
                raise _Stop()
        esA = ExitStack()
        with esA:
          try:
            sbA = lambda n, s, d: esA.enter_context(nc.sbuf_tensor("sa_" + n, s, d))
            pp = sbA("pp", [128, 34], F32)
            ld('sp', pp[:], pp_d[:, :], ['pp'])
            MU = pp[:, 0:14]; W0 = pp[:, 14:18]; A0 = pp[:, 18:22]; KK_ = pp[:, 22:26]; KA = pp[:, 26:30]; RK = pp[:, 30:34]
            omm = sbA("omm", [128, 14], F32)
            ts('dve', omm[:], MU, -1.0, 1.0, ALU.mult, ALU.add, ['pp'], ['omm'])
            omka = sbA("omka", [128, 4], F32)
            ts('dve', omka[:], KA, -1.0, 1.0, ALU.mult, ALU.add, ['pp'], ['omka'])
            gmix = sbA("gmix", [128, D], F32)
            ld('sp', gmix[:], g4_d[0, :].partition_broadcast(128), ['gmix'])
            gn3 = sbA("gn3", [128, 3, 512], F32)
            for i in range(3):
                ld('sp', gn3[:, i, :], gn3_d[i, :].partition_broadcast(128), ['gn3_%d' % i])

            NWB = 3
            wch = [sbA("wch%d" % i, [128, 8, 512], BF16) for i in range(NWB)]
            k = 0
            for kc in range(8):
                for cb in range(4):
                    b = k % NWB
                    stg = wch[b][:].rearrange("p a n -> p (a n)")[:, 0:1472]
                    S.dma('pool', lambda e, stg=stg, kc=kc, cb=cb: e.dma_start(out=stg, in_=w_in_d[kc * 128:(kc + 1) * 128, cb * 1472:(cb + 1) * 1472]), w=['wch%d' % b])
                    S.dma('sp', lambda e, stg=stg, kc=kc, cb=cb: e.dma_start(out=winb_d[kc * 128:(kc + 1) * 128, cb * 1472:(cb + 1) * 1472], in_=stg), r=['wch%d' % b], w=['winb'])
                    k += 1
            wbr = sbA("wbr", [128, 2, 4, D], BF16)
            for i in range(2):
                for c in range(4):
                    S.dma('pool', lambda e, i=i, c=c: e.dma_start(out=wbr[:, i, c, :], in_=wbr_d[i, c * 128:(c + 1) * 128, :]), w=['wbr%d%d' % (i, c)])
            wo = sbA("wo", [128, 8, D], BF16)
            for c in range(8):
                S.dma('pool', lambda e, c=c: e.dma_start(out=wo[:, c, :], in_=wo_d[c * 128:(c + 1) * 128, :]), w=['wo%d' % c])
            WBR = ['wbr%d%d' % (i, c) for i in range(2) for c in range(4)]
            WO = ['wo%d' % c for c in range(8)]
            lora = sbA("lora", [128, 3, 512], BF16)
            S.dma('pool', lambda e: e.dma_start(out=lora[:], in_=lora_d[:, :, :]), w=['lora'])
            DT = sbA("DT", [128, 8, 128], F32)
            ld('sp', DT[:].rearrange("p h i -> p (h i)"), DT_d[:, :], ['DT'])
            xiT = sbA("xiT", [128, 4, 128], F32)
            ld('sp', xiT[:].rearrange("p c i -> p (c i)"), xiT_d[:, :], ['xiT'])
            CDb = sbA("CDb", [128, 4, 64], F32)
            ld('sp', CDb[:].rearrange("p c i -> p (c i)"), CDb_d[:, :], ['CDb'])

            CH = [(0, 512), (512, 512), (1024, 512), (1536, 512),
                  (2048, 512), (2560, 512), (3072, 512), (3584, 256),
                  (3840, 512), (4352, 512), (4864, 512), (5376, 512)]
            wctr = [k]

            def load_chunk(ci):
                b = wctr[0] % NWB
                wctr[0] += 1
                c0, cw = CH[ci]
                S.dma('sp', lambda e, b=b, c0=c0, cw=cw: e.dma_start(
                    out=wch[b][:, :, 0:cw], in_=winb_d[:, c0:c0 + cw].rearrange("(kc p) n -> p kc n", p=128)),
                    r=['winb'], w=['wch%d' % b])
                return b

            xt = [sbA("xt%d" % i, [128, D], F32) for i in range(2)]
            rot_t = [sbA("rot%d" % i, [128, 128], F32) for i in range(2)]
            h = sbA("h", [128, D], BF16)
            hT = sbA("hT", [128, 8, 128], BF16)
            qk_rot = sbA("qk_rot", [128, 2, 512], BF16)
            rt = [sbA("rt%d" % i, [128, 8, 32], F32) for i in range(4)]
            v_tok = sbA("v_tok", [128, 512], BF16)
            gr_s = sbA("gr_s", [128, 512], BF16)
            gate_s = sbA("gate_s", [128, 2048], BF16)
            qkT = sbA("qkT", [128, 8, 128], BF16)
            qxT = sbA("qxT", [128, 4, 128], BF16)
            kz = sbA("kz", [128, 8, 64], BF16)
            PT = sbA("PT", [128, 8, 128], BF16)
            R32 = sbA("R32", [128, 4, 64], F32)
            Rb = sbA("Rb", [128, 4, 128], BF16)
            qTm = sbA("qTm", [128, 2, 4, 128], BF16)
            S.op('dve', lambda e: e.memset(qTm[:], 0.0), w=['qTm'])
            Rtmp = sbA("Rtmp", [128, 4, 64], F32)
            S.op('dve', lambda e: e.memset(R32[:], 0.0), w=['R32'])
            S.op('dve', lambda e: e.memset(Rb[:], 0.0), w=['Rb'])
            hn_sq = sbA("hn_sq", [128, 8, 64], F32)
            hn_c = sbA("hn_c", [128, 8, 64], F32)
            hn_s = sbA("hn_s", [128, 8], F32)
            hn_q = sbA("hn_q", [128, 8], F32)
            hn_m = sbA("hn_m", [128, 8], F32)
            hn_r = sbA("hn_r", [128, 8], F32)
            y_bf = sbA("y_bf", [128, 512], BF16)
            yT = sbA("yT", [128, 4, 128], BF16)
            ZB = sbA("ZB", [128, 14, 129], F32)
            zlast = sbA("zlast", [128, 14, 1], F32)
            S.op('dve', lambda e: e.memset(zlast[:], 0.0), w=['zlast'])
            zs = sbA("zs", [128, 14, 128], F32)
            lor_in = sbA("lor_in", [128, 2, 128], BF16)
            f4 = [sbA("f4_%d" % i, [128, 4, 128], F32) for i in range(8)]
            F4 = ['f4_%d' % i for i in range(8)]
            f4ones = sbA("f4ones", [128, 128], F32)
            S.op('dve', lambda e: e.memset(f4ones[:], 1.0), w=['f4ones'])
            wk = {n_: sbA("wk_" + n_, [128, 4, 128], WDT) for n_ in ('ab', 'rb', 'bb', 'kb', 'bt', 'kt', 'vT')}
            tok3 = sbA("tok3", [128, 3, 512], WDT)
            Am = {n_: sbA("Am_" + n_, [128, 4, 128], WDT) for n_ in ('akT', 'rbT', 'rkT')}
            Nb = [sbA("Nb%d" % i, [128, 4, 128], WDT) for i in range(2)]
            NTb = [sbA("NTb%d" % i, [128, 4, 128], WDT) for i in range(2)]
            Qb = [sbA("Qb%d" % i, [128, 4, 128], WDT) for i in range(2)]
            BOw = sbA("BOw", [128, 128], F32)
            cp('dve', BOw[:], BO, ['cm'], ['BOw'])
            Xs = sbA("Xs", [128, 256], WDT)
            Us = sbA("Us", [128, 512], WDT)
            ST32 = sbA("ST32", [128, 4, 64], F32)
            STb = sbA("STb", [128, 4, 128], WDT)
            wkm = {n_: sbA("wkm_" + n_, [128, 2, 4, 128], WDT) for n_ in ('ab', 'bb', 'rb')}
            for n_ in ('ab', 'bb', 'rb'):
                S.op('pool', lambda e, n_=n_: e.memset(wkm[n_][:], 0.0), w=['wkm_' + n_])
            STtmp = sbA("STtmp", [128, 4, 64], F32)
            S.op('dve', lambda e: e.memset(ST32[:], 0.0), w=['ST32'])
            S.op('dve', lambda e: e.memset(STb[:], 0.0), w=['STb'])
            PCt = sbA("PCt", [128, 4], F32)
            g_tok = sbA("g_tok", [128, 512], F32)
            cbt = sbA("cbt", [128, 8], F32)
            merged = sbA("merged", [128, D], BF16)
            mT = sbA("mT", [128, 8, 128], BF16)
            x1 = sbA("x1", [128, D], F32)

            def headnorm(ops_ap, ops_key, eps_col, dst_c):
                red(hn_s[:], ops_ap, ALU.add, [ops_key], ['hn_s'])
                act(hn_sq[:], ops_ap, AF.Square, [ops_key], ['hn_sq'])
                red(hn_q[:], hn_sq[:], ALU.add, ['hn_sq'], ['hn_q'])
                ts('dve', hn_m[:], hn_s[:], 1.0 / 64, None, ALU.mult, None, ['hn_s'], ['hn_m'])
                tt('dve', hn_r[:], hn_m[:], hn_m[:], ALU.mult, ['hn_m'], ['hn_r'])
                stt(hn_r[:], hn_q[:], 1.0 / 64, hn_r[:], ALU.mult, ALU.subtract, ['hn_q', 'hn_r'], ['hn_r'])
                act(hn_r[:], hn_r[:], AF.Sqrt, ['hn_r', 'eps'], ['hn_r'], bias=eps_t[:, eps_col:eps_col + 1])
                rcp(hn_r[:], 'hn_r')
                tt('dve', dst_c, ops_ap, bc(hn_m[:], 2, [128, 8, 64]), ALU.subtract, [ops_key, 'hn_m'], ['hn_c'])
                tt('dve', dst_c, dst_c, bc(hn_r[:], 2, [128, 8, 64]), ALU.mult, ['hn_c', 'hn_r'], ['hn_c'])

            ck(1)
            for n in range(nt):
                par = n % 2
                X, XK = xt[par], 'xt%d' % par
                ld('sp', X[:], x_d[n * 128:(n + 1) * 128, :], [XK])
                ld('sp', rot_t[par][:], rot_d[n, :, :], ['rot%d' % par])
                ROT = 'rot%d' % par
                rmsnorm(X[:], XK, gmix[:], 'gmix', h[:], 'h')
                for c in range(8):
                    tr(ptb[:, c, :], h[:, c * 128:(c + 1) * 128], identb[:], ['h', 'identb'], ['ptb'])
                cp('act', hT[:], ptb[:], ['ptb'], ['hT'])

                ck(2)
                def inproj_tok(ci, bank):
                    b = load_chunk(ci)
                    for kc in range(8):
                        mm(pbk[bank][:], hT[:, kc, :], wch[b][:, kc, :], kc == 0, kc == 7, ['hT', 'wch%d' % b], [PB[bank]])

                Cc = rot_t[par][:, 0:32]; Sc = rot_t[par][:, 32:64]
                kCc = rot_t[par][:, 64:96]; kSc = rot_t[par][:, 96:128]
                for qi in range(2):
                    bank = qi
                    inproj_tok(qi, bank)
                    pv = pbk[bank][:].rearrange("p (h two f) -> p h two f", two=2, f=32)
                    q1 = pv[:, :, 0, :]; q2 = pv[:, :, 1, :]
                    cc, ssn = (Cc, Sc) if qi == 0 else (kCc, kSc)
                    cb_ = bc(cc, 1, [128, 8, 32]); sb_ = bc(ssn, 1, [128, 8, 32])
                    ov = qk_rot[:, qi, :].rearrange("p (h two f) -> p h two f", two=2, f=32)
                    tt('dve', rt[0][:], q1, cb_, ALU.mult, [PB[bank], ROT], ['rt0'])
                    tt('dve', rt[1][:], q2, sb_, ALU.mult, [PB[bank], ROT], ['rt1'])
                    tt('dve', rt[2][:], q1, sb_, ALU.mult, [PB[bank], ROT], ['rt2'])
                    tt('dve', rt[3][:], q2, cb_, ALU.mult, [PB[bank], ROT], ['rt3'])
                    tt('pool', ov[:, :, 0, :], rt[0][:], rt[1][:], ALU.subtract, ['rt0', 'rt1'], ['qk_rot'])
                    tt('pool', ov[:, :, 1, :], rt[2][:], rt[3][:], ALU.add, ['rt2', 'rt3'], ['qk_rot'])
                inproj_tok(2, 2)
                cp('act', v_tok[:], pbk[2][:], [PB[2]], ['v_tok'])
                inproj_tok(3, 3)
                act(gr_s[:], pbk[3][:], AF.Silu, [PB[3]], ['gr_s'])

                ck(3)
                for c in range(8):
                    tr(ptb[:, c, :], qk_rot[:, c // 4, (c % 4) * 128:(c % 4 + 1) * 128], identb[:], ['qk_rot', 'identb'], ['ptb'])
                cp('act', qkT[:, 4:8, :], ptb[:, 4:8, :], ['ptb'], ['qkT'])
                cp('act', qTm[0:64, 0, :, :], ptb[0:64, 0:4, :], ['ptb'], ['qTm'])
                cp('act', qTm[64:128, 1, :, :], ptb[64:128, 0:4, :], ['ptb'], ['qTm'])
                tt('dve', qxT[:], ptb[:, 0:4, :], xiT[:], ALU.mult, ['ptb', 'xiT'], ['qxT'])
                tt('pool', kz[:], qk_rot[:, 1, :].rearrange("p (h d) -> p h d", d=64), bc(ZT, 2, [128, 8, 64]), ALU.mult, ['qk_rot', 'cm'], ['kz'])

                ck(31)
                for hh in range(8):
                    c, base = hh // 2, (hh % 2) * 64
                    bank = 4 + hh // 4
                    mm(pbk[bank][:, (hh % 4) * 128:(hh % 4 + 1) * 128], qkT[:, 4 + c, :], qTm[:, hh % 2, c, :], True, True, ['qkT', 'qTm'], [PB[bank]])
                for g in range(2):
                    tt('dve', PT[:, 4 * g:4 * g + 4, :], pbk[4 + g][:].rearrange("p (h i) -> p h i", i=128), DT[:, 4 * g:4 * g + 4, :], ALU.mult, [PB[4 + g], 'DT'], ['PT'])
                ck(32)
                for c in range(4):
                    mm(pbk[6][:, c * 128:(c + 1) * 128], qxT[:, c, :], Rb[:, c, :], True, False, ['qxT', 'Rb'], [PB[6]])
                    for w_ in range(2):
                        hh = 2 * c + w_
                        mm(pbk[6][:, hh * 64:(hh + 1) * 64], PT[:, hh, :], v_tok[:, hh * 64:(hh + 1) * 64], False, w_ == 1, ['PT', 'v_tok'], [PB[6]])
                ck(33)
                for c in range(4):
                    mm(pbk[4][:, c * 128:(c + 1) * 128], kz[:, 2 * c:2 * c + 2, :].rearrange("p a d -> p (a d)"), v_tok[:, c * 128:(c + 1) * 128], True, True, ['kz', 'v_tok'], [PB[4]])
                tt('pool', Rtmp[:], R32[:], CDb[:], ALU.mult, ['R32', 'CDb'], ['Rtmp'])
                p4v = pbk[4][:].rearrange("p (c x) -> p c x", x=128)
                tt('dve', R32[0:64, :, :], Rtmp[0:64, :, :], p4v[0:64, :, 0:64], ALU.add, ['Rtmp', PB[4]], ['R32'])
                tt('dve', R32[64:128, :, :], Rtmp[64:128, :, :], p4v[64:128, :, 64:128], ALU.add, ['Rtmp', PB[4]], ['R32'])
                cp('pool', Rb[0:64, :, 0:64], R32[0:64, :, :], ['R32'], ['Rb'])
                cp('pool', Rb[64:128, :, 64:128], R32[64:128, :, :], ['R32'], ['Rb'])
                ck(34)
                o3 = pbk[6][:].rearrange("p (h e) -> p h e", e=64)
                headnorm(o3, PB[6], 1, hn_c[:])
                hc2 = hn_c[:].rearrange("p h e -> p (h e)")
                tt('dve', hc2, hc2, gn3[:, 0, :], ALU.mult, ['hn_c', 'gn3_0'], ['hn_c'])
                tt('dve', y_bf[:], hc2, gr_s[:], ALU.mult, ['hn_c', 'gr_s'], ['y_bf'])
                ck(35)
                for c in range(4):
                    tr(ptb[:, c, :], y_bf[:, c * 128:(c + 1) * 128], identb[:], ['y_bf', 'identb'], ['ptb'])
                cp('act', yT[:], ptb[:, 0:4, :], ['ptb'], ['yT'])
                for hf in range(2):
                    for c in range(4):
                        mm(pbk[hf][:], yT[:, c, :], wbr[:, 0, c, hf * 512:(hf + 1) * 512], c == 0, c == 3, ['yT'] + WBR, [PB[hf]])

                ck(4)
                for j in range(4):
                    b = load_chunk(4 + j)
                    nm = 4 if j < 3 else 2
                    bank = 2 + (j % 2)
                    for m in range(nm):
                        for kc in range(8):
                            mm(pbk[bank][:, m * 128:(m + 1) * 128], wch[b][:, kc, m * 128:(m + 1) * 128], hT[:, kc, :], kc == 0, kc == 7, ['hT', 'wch%d' % b], [PB[bank]])
                    cp('act', ZB[:, 4 * j:4 * j + nm, 1:129], pbk[bank][:, 0:nm * 128].rearrange("p (m t) -> p m t", t=128), [PB[bank]], ['ZB'])
                cp('pool', ZB[:, :, 0:1], zlast[:], ['zlast'], ['ZB'])
                for (m0, m1) in ((0, 4), (4, 8), (8, 12), (12, 14)):
                    nm = m1 - m0
                    tmp = f4[7][:, 0:nm, :]
                    tt('pool', tmp, ZB[:, m0:m1, 0:128], bc(MU[:, m0:m1], 2, [128, nm, 128]), ALU.mult, ['ZB', 'pp'], [F4[7]])
                    tt('dve', zs[:, m0:m1, :], ZB[:, m0:m1, 1:129], bc(omm[:, m0:m1], 2, [128, nm, 128]), ALU.mult, ['ZB', 'omm'], ['zs'])
                    tt('dve', zs[:, m0:m1, :], zs[:, m0:m1, :], tmp, ALU.add, ['zs', F4[7]], ['zs'])
                cp('pool', zlast[:], ZB[:, :, 128:129], ['ZB'], ['zlast'])
                ck(5)
                rF = zs[:, 0:4, :]; krF = zs[:, 4:8, :]; vF = zs[:, 8:12, :]
                act(lor_in[0:64, 0, :], zs[0:64, 12, :], AF.Tanh, ['zs'], ['lor_in'])
                cp('act', lor_in[64:128, 0, :], zs[64:128, 12, :], ['zs'], ['lor_in'])
                act(lor_in[:, 1, :], zs[:, 13, :], AF.Sigmoid, ['zs'], ['lor_in'])
                for m in range(4):
                    mm(pbk[2][:, m * 128:(m + 1) * 128], lora[:, 0, m * 128:(m + 1) * 128], lor_in[:, 0, :], True, True, ['lora', 'lor_in'], [PB[2]])
                for m in range(4):
                    mm(pbk[3][:, m * 128:(m + 1) * 128], lora[:, 1, m * 128:(m + 1) * 128], lor_in[:, 0, :], True, True, ['lora', 'lor_in'], [PB[3]])
                mm(pbk[4][:], lor_in[:, 1, :], lora[:, 2, :], True, True, ['lora', 'lor_in'], [PB[4]])
                cp('act', g_tok[:], pbk[4][:], [PB[4]], ['g_tok'])
                sg, asig, kkF, kkn, kpr, bF, csF, tmpF = f4
                for m in range(4):
                    act(sg[:, m, :], pbk[2][:, m * 128:(m + 1) * 128], AF.Sigmoid, [PB[2], 'pp'], [F4[0]], bias=W0[:, m:m + 1])
                for m in range(4):
                    act(asig[:, m, :], pbk[3][:, m * 128:(m + 1) * 128], AF.Sigmoid, [PB[3], 'pp'], [F4[1]], bias=A0[:, m:m + 1])
                b4 = lambda t: bc(t, 2, [128, 4, 128])
                f2 = lambda t: t[:].rearrange("p m t -> p (m t)")
                tt('dve', kkF[:], krF, b4(KK_), ALU.mult, ['zs', 'pp'], [F4[2]])
                tt('pool', tmpF[:], kkF[:], kkF[:], ALU.mult, [F4[2]], [F4[7]])
                mm(pbk[2][:], BOw[:], f2(tmpF), True, True, ['BOw', F4[7]], [PB[2]])
                act(f2(kkn), pbk[2][:], AF.Sqrt, [PB[2]], [F4[3]])
                ts('dve', kkn[:], kkn[:], 1e-12, None, ALU.max, None, [F4[3]], [F4[3]])
                rcp(kkn[:], F4[3])
                tt('dve', kkn[:], kkn[:], kkF[:], ALU.mult, [F4[3], F4[2]], [F4[3]])
                tt('pool', kpr[:], asig[:], b4(KA), ALU.mult, [F4[1], 'pp'], [F4[4]])
                tt('pool', kpr[:], kpr[:], b4(omka[:]), ALU.add, [F4[4], 'omka'], [F4[4]])
                tt('dve', kpr[:], kpr[:], krF, ALU.mult, [F4[4], 'zs'], [F4[4]])
                tt('pool', bF[:], kkn[:], asig[:], ALU.mult, [F4[3], F4[1]], [F4[5]])
                tt('pool', tmpF[:], rF, kpr[:], ALU.mult, ['zs', F4[4]], [F4[7]])
                tt('pool', tmpF[:], tmpF[:], b4(RK), ALU.mult, [F4[7], 'pp'], [F4[7]])
                for c in range(4):
                    mm(pbk[3][:, 2 * c:2 * c + 2], tmpF[:, c, :], HS, True, True, [F4[7], 'cm'], [PB[3]])
                cp('act', cbt[:], pbk[3][:, 0:8], [PB[3]], ['cbt'])
                for m in range(4):
                    S.op('dve', lambda e, m=m: e.tensor_tensor_scan(out=csF[:, m, :], data0=f4ones[:], data1=sg[:, m, :], initial=0.0, op0=ALU.mult, op1=ALU.add), r=[F4[0], 'f4ones'], w=[F4[6]])
                E1, E2 = kkF, tmpF
                act(E1[:], csF[:], AF.Exp, [F4[6]], [F4[2]], scale=-C0)
                act(E2[:], csF[:], AF.Exp, [F4[6]], [F4[7]], scale=C0)
                tt('dve', csF[:], csF[:], sg[:], ALU.subtract, [F4[6], F4[0]], [F4[6]])
                act(sg[:], csF[:], AF.Exp, [F4[6]], [F4[0]], scale=-C0)
                E3 = sg
                cp('dve', PCt[:], E1[:, :, 127], [F4[2]], ['PCt'])
                stt(wk['ab'][:], kkn[:], -1.0, E3[:], ALU.mult, ALU.mult, [F4[3], F4[0]], ['wk_ab'])
                tt('dve', wk['rb'][:], rF, E1[:], ALU.mult, ['zs', F4[2]], ['wk_rb'])
                tt('pool', csF[:], bF[:], E2[:], ALU.mult, [F4[5], F4[7]], [F4[6]])
                cp('act', wk['bb'][:], csF[:], [F4[6]], ['wk_bb'])
                tt('pool', wk['bt'][:], csF[:], bc(PCt[:], 2, [128, 4, 128]), ALU.mult, [F4[6], 'PCt'], ['wk_bt'])
                tt('dve', bF[:], kpr[:], E2[:], ALU.mult, [F4[4], F4[7]], [F4[5]])
                cp('act', wk['kb'][:], bF[:], [F4[5]], ['wk_kb'])
                tt('pool', wk['kt'][:], bF[:], bc(PCt[:], 2, [128, 4, 128]), ALU.mult, [F4[5], 'PCt'], ['wk_kt'])
                cp('act', wk['vT'][:], vF, ['zs'], ['wk_vT'])
                for n_ in ('ab', 'bb', 'rb'):
                    cp('pool', wkm[n_][0:64, 0, :, :], wk[n_][0:64, :, :], ['wk_' + n_], ['wkm_' + n_])
                    cp('pool', wkm[n_][64:128, 1, :, :], wk[n_][64:128, :, :], ['wk_' + n_], ['wkm_' + n_])
                ck(6)
                for i, nmk in enumerate(('bt', 'kt', 'vT')):
                    for c in range(4):
                        tr(ptb[:, c, :], wk[nmk][:, c, :], identb[:], ['wk_' + nmk, 'identb'], ['ptb'])
                    cp('act', tok3[:, i, :].rearrange("p (c x) -> p c x", x=128), ptb[:, 0:4, :], ['ptb'], ['tok3_%d' % i])
                Btok = tok3[:, 0, :]; Ktok = tok3[:, 1, :]; Vtok = tok3[:, 2, :]

                ck(7)
                def hop(hh):
                    return hh // 2, (hh % 2) * 64
                for g in range(2):
                    hs = [4 * g + i for i in range(4)]
                    specs = [('bb', 'ab', SU, Nb[0], 'Nb0'), ('ab', 'bb', SL, NTb[0], 'NTb0'),
                             ('kb', 'ab', SU, Am['akT'], 'Am_akT'), ('bb', 'rb', SUI, Am['rbT'], 'Am_rbT'),
                             ('kb', 'rb', SUI, Am['rkT'], 'Am_rkT')]
                    for si, (l_, r_, msk, dst, dk) in enumerate(specs):
                        bank = 2 + (si % 3)
                        for i, hh in enumerate(hs):
                            c, base = hop(hh)
                            mm(pbk[bank][:, i * 128:(i + 1) * 128], wk[l_][:, c, :], wkm[r_][:, hh % 2, c, :], True, True, ['wk_' + l_, 'wkm_' + r_], [PB[bank]])
                        tt('dve', dst[:], pbk[bank][:].rearrange("p (h t) -> p h t", t=128), bc(msk, 1, [128, 4, 128]), ALU.mult, [PB[bank], 'cm'], [dk])
                    tt('pool', Qb[0][:], Nb[0][:], bc(identb[:], 1, [128, 4, 128]), ALU.add, ['Nb0', 'identb'], ['Qb0'])
                    cur = 0
                    for lvl in range(6):
                        nx = 1 - cur
                        last = (lvl == 5)
                        if not last:
                            for i in range(4):
                                mm(pbk[2][:, i * 128:(i + 1) * 128], NTb[cur][:, i, :], Nb[cur][:, i, :], True, True, ['NTb%d' % cur, 'Nb%d' % cur], [PB[2]])
                        for i in range(4):
                            mm(pbk[3][:, i * 128:(i + 1) * 128], Nb[cur][:, i, :], NTb[cur][:, i, :], True, True, ['NTb%d' % cur, 'Nb%d' % cur], [PB[3]])
                        if not last:
                            cp('dve', Nb[nx][:].rearrange("p h t -> p (h t)"), pbk[2][:], [PB[2]], ['Nb%d' % nx])
                        cp('act', NTb[nx][:].rearrange("p h t -> p (h t)"), pbk[3][:], [PB[3]], ['NTb%d' % nx])
                        for i in range(4):
                            mm(pbk[4][:, i * 128:(i + 1) * 128], identb[:], Qb[cur][:, i, :], True, False, ['identb', 'Qb%d' % cur], [PB[4]])
                            mm(pbk[4][:, i * 128:(i + 1) * 128], NTb[nx][:, i, :], Qb[cur][:, i, :], False, True, ['NTb%d' % nx, 'Qb%d' % cur], [PB[4]])
                        cp('act' if lvl % 2 else 'dve', Qb[nx][:].rearrange("p h t -> p (h t)"), pbk[4][:], [PB[4]], ['Qb%d' % nx])
                        cur = nx
                    Qf, QK = Qb[cur], 'Qb%d' % cur
                    for ci in range(2):
                        c = 2 * g + ci
                        mm(pbk[5][:, ci * 128:(ci + 1) * 128], wk['ab'][:, c, :], STb[:, c, :], True, False, ['wk_ab', 'STb'], [PB[5]])
                        for w_ in range(2):
                            i = 2 * ci + w_
                            hh = hs[i]
                            mm(pbk[5][:, i * 64:(i + 1) * 64], Am['akT'][:, i, :], Vtok[:, hh * 64:(hh + 1) * 64], False, w_ == 1, ['Am_akT', 'tok3_2'], [PB[5]])
                    cp('act', Xs[:], pbk[5][:, 0:256], [PB[5]], ['Xs'])
                    for i, hh in enumerate(hs):
                        oc = slice(i * 64, (i + 1) * 64)
                        mm(pbk[5][:, 256 + i * 64:256 + (i + 1) * 64], Qf[:, i, :], Xs[:, oc], True, True, [QK, 'Xs'], [PB[5]])
                    cp('act', Us[:, g * 256:(g + 1) * 256], pbk[5][:, 256:512], [PB[5]], ['Us'])
                    for ci in range(2):
                        c = 2 * g + ci
                        mm(pbk[6][:, c * 128:(c + 1) * 128], wk['rb'][:, c, :], STb[:, c, :], True, False, ['wk_rb', 'STb'], [PB[6]])
                        for w_ in range(2):
                            i = 2 * ci + w_
                            hh = hs[i]
                            hc = slice(hh * 64, (hh + 1) * 64)
                            mm(pbk[6][:, hc], Am['rkT'][:, i, :], Vtok[:, hc], False, False, ['Am_rkT', 'tok3_2'], [PB[6]])
                            mm(pbk[6][:, hc], Am['rbT'][:, i, :], Us[:, hc], False, w_ == 1, ['Am_rbT', 'Us'], [PB[6]])
                for c in range(4):
                    cs_ = slice(c * 128, (c + 1) * 128)
                    mm(pbk[5][:, cs_], Btok[:, cs_], Us[:, cs_], True, False, ['tok3_0', 'Us'], [PB[5]])
                    mm(pbk[5][:, cs_], Ktok[:, cs_], Vtok[:, cs_], False, True, ['tok3_1', 'tok3_2'], [PB[5]])
                tt('pool', STtmp[:], ST32[:], bc(PCt[:], 2, [128, 4, 64]), ALU.mult, ['ST32', 'PCt'], ['STtmp'])
                p5v = pbk[5][:].rearrange("p (c x) -> p c x", x=128)
                tt('dve', ST32[0:64, :, :], STtmp[0:64, :, :], p5v[0:64, :, 0:64], ALU.add, ['STtmp', PB[5]], ['ST32'])
                tt('dve', ST32[64:128, :, :], STtmp[64:128, :, :], p5v[64:128, :, 64:128], ALU.add, ['STtmp', PB[5]], ['ST32'])
                cp('pool', STb[0:64, :, 0:64], ST32[0:64, :, :], ['ST32'], ['STb'])
                cp('pool', STb[64:128, :, 64:128], ST32[64:128, :, :], ['ST32'], ['STb'])
                ck(8)
                o3 = pbk[6][:].rearrange("p (h e) -> p h e", e=64)
                headnorm(o3, PB[6], 2, hn_c[:])
                hc2 = hn_c[:].rearrange("p h e -> p (h e)")
                tt('dve', hc2, hc2, gn3[:, 1, :], ALU.mult, ['hn_c', 'gn3_1'], ['hn_c'])
                tt('dve', hc2, hc2, gn3[:, 2, :], ALU.add, ['hn_c', 'gn3_2'], ['hn_c'])
                tt('pool', hn_sq[:], Vtok.rearrange("p (h e) -> p h e", e=64), bc(cbt[:], 2, [128, 8, 64]), ALU.mult, ['tok3_2', 'cbt'], ['hn_sq'])
                tt('dve', hc2, hc2, hn_sq[:].rearrange("p h e -> p (h e)"), ALU.add, ['hn_c', 'hn_sq'], ['hn_c'])
                tt('dve', y_bf[:], hc2, g_tok[:], ALU.mult, ['hn_c', 'g_tok'], ['y_bf'])
                for c in range(4):
                    tr(ptb[:, c, :], y_bf[:, c * 128:(c + 1) * 128], identb[:], ['y_bf', 'identb'], ['ptb'])
                cp('act', yT[:], ptb[:, 0:4, :], ['ptb'], ['yT'])
                for hf in range(2):
                    for c in range(4):
                        mm(pbk[2 + hf][:], yT[:, c, :], wbr[:, 1, c, hf * 512:(hf + 1) * 512], c == 0, c == 3, ['yT'] + WBR, [PB[2 + hf]])

                ck(9)
                for gi in range(4):
                    bank = 4 + (gi % 2)
                    inproj_tok(8 + gi, bank)
                    act(gate_s[:, gi * 512:(gi + 1) * 512], pbk[bank][:], AF.Sigmoid, [PB[bank]], ['gate_s'])
                for hf in range(2):
                    sl = slice(hf * 512, (hf + 1) * 512)
                    tt('dve', x1[:, sl], pbk[hf][:], gate_s[:, sl], ALU.mult, [PB[hf], 'gate_s'], ['x1'])
                    tt('dve', merged[:, sl], pbk[2 + hf][:], gate_s[:, 1024 + hf * 512:1024 + (hf + 1) * 512], ALU.mult, [PB[2 + hf], 'gate_s'], ['merged'])
                    tt('pool', merged[:, sl], merged[:, sl], x1[:, sl], ALU.add, ['merged', 'x1'], ['merged'])
                for c in range(8):
                    tr(ptb[:, c, :], merged[:, c * 128:(c + 1) * 128], identb[:], ['merged', 'identb'], ['ptb'])
                cp('act', mT[:], ptb[:], ['ptb'], ['mT'])
                for hf in range(2):
                    for c in range(8):
                        mm(pbk[4 + hf][:], mT[:, c, :], wo[:, c, hf * 512:(hf + 1) * 512], c == 0, c == 7, ['mT'] + WO, [PB[4 + hf]])
                    tt('dve', x1[:, hf * 512:(hf + 1) * 512], pbk[4 + hf][:], X[:, hf * 512:(hf + 1) * 512], ALU.add, [PB[4 + hf], XK], ['x1'])
                S.dma('act', lambda e, n=n: e.dma_start(out=out_d[n * 128:(n + 1) * 128, :], in_=x1[:]), r=['x1'], w=['out%d' % n])
          except _Stop:
            pass

        if stage == 'A':
            S.wait_all('sp')
            S.emit()
            return nc
        S.barrier()

        esB = ExitStack()
        with esB:
          try:
            sbB = lambda n, s, d: esB.enter_context(nc.sbuf_tensor("sc_" + n, s, d))
            ptb2 = pbk[6][:].bitcast(BF16).rearrange("p (a b) -> p a b", b=128)
            PTB = [(ptb, 'ptb'), (ptb2, PB[6])]
            g3 = sbB("g3", [128, 3, D], F32)
            for i in range(3):
                ld('sp', g3[:, i, :], g4_d[1 + i, :].partition_broadcast(128), ['g3_%d' % i])
            wpg = sbB("wpg", [128, 8, D], BF16)
            for c in range(8):
                S.dma('pool', lambda e, c=c: e.dma_start(out=wpg[:, c, :], in_=wpg_d[c * 128:(c + 1) * 128, :]), w=['wpg%d' % c])
            WPG = ['wpg%d' % c for c in range(8)]
            wpu = sbB("wpu", [128, 2, D], BF16)
            for c in range(2):
                S.dma('pool', lambda e, c=c: e.dma_start(out=wpu[:, c, :], in_=wpu_d[c * 128:(c + 1) * 128, :]), w=['wpu%d' % c])
            WPU = ['wpu0', 'wpu1']
            skT = sbB("skT", [128, 16, 128], F32)
            s_sb2 = [sbB("s_sb%d" % i, [128, 16, 128], F32) for i in range(2)]
            s_sb = s_sb2[0]
            for g in range(16):
                ld('sp', s_sb[:, g, :], sk_d[g, :, :], ['s_sb0'])
            for g4i in range(4):
                bank = g4i % 2
                for i in range(4):
                    g = g4i * 4 + i
                    tr(pbk[bank][:, i * 128:(i + 1) * 128], s_sb[:, g, :], identf, ['s_sb0', 'cm'], [PB[bank]])
                cp('act', skT[:, g4i * 4:g4i * 4 + 4, :].rearrange("p g k -> p (g k)"), pbk[bank][:], [PB[bank]], ['skT'])

            NSB = 3
            ubuf = [sbB("ubuf%d" % i, [128, 2, D], BF16) for i in range(NSB)]
            vbuf = [sbB("vbuf%d" % i, [128, 2, D], BF16) for i in range(NSB)]
            wqb = [sbB("wqb%d" % i, [128, 8, 256], BF16) for i in range(2)]

            k = 0
            for kc in range(8):
                for hf in range(2):
                    b = k % 2
                    stg = wqb[b][:].rearrange("p a n -> p (a n)")[:, 0:1024]
                    S.dma('pool', lambda e, stg=stg, kc=kc, hf=hf: e.dma_start(out=stg, in_=wpq_d[kc * 128:(kc + 1) * 128, hf * 1024:(hf + 1) * 1024]), w=['wqb%d' % b])
                    S.dma('sp', lambda e, stg=stg, kc=kc, hf=hf: e.dma_start(out=wpqb_d[kc * 128:(kc + 1) * 128, hf * 1024:(hf + 1) * 1024], in_=stg), r=['wqb%d' % b], w=['wpqb'])
                    k += 1
            pu_v = pu_d.rearrange("(i j) d -> j i d", j=128)
            pv_v = pv_d.rearrange("(i j) d -> j i d", j=128)
            for j in range(128):
                b = j % 2
                ust = ubuf[b][:, 0, :]; uT = ubuf[b][:, 1, :]; vst = vbuf[b][:, 0, :]
                k0, k1, k2 = 'ub%d_0' % b, 'ub%d_1' % b, 'ub%d_2' % b
                S.dma('pool', lambda e, ust=ust, j=j: e.dma_start(out=ust, in_=pu_v[j, :, :]), w=[k0])
                pt_, pk_ = PTB[j % 2]
                for kc in range(8):
                    tr(pt_[:, kc, :], ust[:, kc * 128:(kc + 1) * 128], identb[:], [k0, 'identb'], [pk_])
                cp('act' if j % 2 else 'dve', uT.rearrange("p (a b) -> p a b", b=128), pt_[:, :, :], [pk_], [k1])
                S.dma('sp', lambda e, uT=uT, j=j: e.dma_start(out=utb_d[j, :, :], in_=uT), r=[k1], w=['utb'])
                S.dma('pool', lambda e, vst=vst, j=j: e.dma_start(out=vst, in_=pv_v[j, :, :]), w=[k2])
                S.dma('act', lambda e, vst=vst, j=j: e.dma_start(out=vtb_d[j, :, :], in_=vst), r=[k2], w=['vtb'])
            S.barrier()
            ck(101)

            x1b = [sbB("x1b%d" % i, [128, D], F32) for i in range(2)]
            ptl = [sbB("ptl%d" % i, [128, 256], F32) for i in range(2)]
            h2b2 = [sbB("h2b%d" % i, [128, D], BF16) for i in range(2)]
            h2T2 = [sbB("h2T%d" % i, [128, 8, 128], BF16) for i in range(2)]
            qc = sbB("qc", [128, 2048], F32)
            qT = qc[:].rearrange("p (g t) -> p g t", t=128)
            cand = qc[:].rearrange("p (h x) -> p h x", x=256)
            s_rp = sbB("s_rp", [128, 128], F32)
            vals2 = [sbB("vals%d" % i, [128, 16, 16], F32) for i in range(2)]
            cand2 = sbB("cand2", [128, 256], F32)
            best2 = [sbB("best%d" % i, [128, 8, 16], F32) for i in range(2)]
            gat = sbB("gat", [128, 8, 16], F32)
            gsum = sbB("gsum", [128, 8], F32)
            bias82 = [sbB("bias8%d" % i, [128, 8], F32) for i in range(2)]
            IB = 16
            Ptok = [sbB("Ptok0", [128, 128, IB], BF16)] * 2
            PTt = sbB("PTt", [128, 128, 128], BF16)
            JB = 16
            TG = 512 // JB
            NJB = 128 // JB
            xq = sbB("xq", [128, 4, 16, JB], F32)
            eq = sbB("eq", [128, 4, 16, JB], BF16)
            Qtok = [sbB("Qtok%d" % i, [128, 128, JB], BF16) for i in range(2)]
            QTt = [sbB("QTt%d" % i, [128, JB, 128], BF16) for i in range(2)]
            act_sb2 = [sbB("act_sb%d" % i, [128, JB, 128], BF16) for i in range(2)]
            coef2 = [sbB("coef%d" % i, [128, JB, 128], BF16) for i in range(2)]
            x2 = sbB("x2", [128, D], F32)
            p_bf = sbB("p_bf", [128, 256], BF16)
            pTt = sbB("pTt", [128, 2, 128], BF16)
            pg_s = sbB("pg_s", [128, D], F32)
            uctr = [0]; vctr = [0]; qctr = [0]; pbc = [0]

            def nextptb():
                r_ = PTB[pbc[0] % 2]
                pbc[0] += 1
                return r_

            def front_end(n, stage):
                par = n % 2
                P_ = str(par)
                X1, X1K = x1b[par], 'x1b%d' % par
                h2b, h2T, s_sb, vals, best, bias8 = h2b2[par], h2T2[par], s_sb2[par], vals2[par], best2[par], bias82[par]
                v4 = vals[:].rearrange("p (h c) a -> p h c a", c=2)
                if stage == 0:
                    ld('sp', X1[:], out_d[n * 128:(n + 1) * 128, :], [X1K], r=['out%d' % n])
                    ld('sp', ptl[par][:], p_d[n * 128:(n + 1) * 128, :], ['ptl%d' % par])
                    rmsnorm(X1[:], X1K, g3[:, 0, :], 'g3_0', h2b[:], 'h2b' + P_)
                elif stage == 1:
                    for c in range(8):
                        tr(ptb[:, c, :], h2b[:, c * 128:(c + 1) * 128], identb[:], ['h2b' + P_, 'identb'], ['ptb'])
                    cp('act', h2T[:], ptb[:], ['ptb'], ['h2T' + P_])
                elif stage in (2, 3, 4, 5):
                    for g4i in (stage - 2,):
                        bank = 2 + (g4i % 2)
                        for i2 in range(2):
                            wb = qctr[0] % 2
                            qctr[0] += 1
                            c0 = g4i * 512 + i2 * 256
                            S.dma('sp', lambda e, wb=wb, c0=c0: e.dma_start(
                                out=wqb[wb][:], in_=wpqb_d[:, c0:c0 + 256].rearrange("(kc p) n -> p kc n", p=128)),
                                r=['wpqb'], w=['wqb%d' % wb])
                            for i1 in range(2):
                                i = i2 * 2 + i1
                                for kc in range(8):
                                    mm(pbk[bank][:, i * 128:(i + 1) * 128], wqb[wb][:, kc, i1 * 128:(i1 + 1) * 128], h2T[:, kc, :], kc == 0, kc == 7, ['h2T' + P_, 'wqb%d' % wb], [PB[bank]])
                        cp('act', qT[:, g4i * 4:g4i * 4 + 4, :].rearrange("p g t -> p (g t)"), pbk[bank][:], [PB[bank]], ['qc'])
                elif stage == 6:
                    for g4i in range(4):
                        bank = 4 + (g4i % 2)
                        for i in range(4):
                            g = g4i * 4 + i
                            mm(pbk[bank][:, i * 128:(i + 1) * 128], qT[:, g, :], skT[:, g, :], True, True, ['qc', 'skT'], [PB[bank]])
                        cp('act', s_sb[:, g4i * 4:g4i * 4 + 4, :].rearrange("p g k -> p (g k)"), pbk[bank][:], [PB[bank]], ['s_sb' + P_])
                elif stage in (7, 8, 9, 10):
                    for g in range((stage - 7) * 4, (stage - 6) * 4):
                        S.op('dve', lambda e, g=g: e.max(out=vals[:, g, 0:8], in_=s_sb[:, g, :]), r=['s_sb' + P_], w=['vals' + P_])
                        S.op('dve', lambda e, g=g: e.match_replace(out=s_rp[:], in_to_replace=vals[:, g, 0:8], in_values=s_sb[:, g, :], imm_value=-1e30), r=['s_sb' + P_, 'vals' + P_], w=['s_rp'])
                        S.op('dve', lambda e, g=g: e.max(out=vals[:, g, 8:16], in_=s_rp[:]), r=['s_rp'], w=['vals' + P_])
                elif stage == 11:
                    for hh in range(8):
                        tt('dve', cand[:, hh, :].rearrange("p (a b) -> p a b", b=16), bc(v4[:, hh, 0, :], 2, [128, 16, 16]), bc(v4[:, hh, 1, :], 1, [128, 16, 16]), ALU.add, ['vals' + P_], ['qc'])
                elif stage in (12, 13):
                    for hh in range((stage - 12) * 4, (stage - 11) * 4):
                        S.op('dve', lambda e, hh=hh: e.max(out=best[:, hh, 0:8], in_=cand[:, hh, :]), r=['qc'], w=['best' + P_])
                        S.op('dve', lambda e, hh=hh: e.match_replace(out=cand2[:], in_to_replace=best[:, hh, 0:8], in_values=cand[:, hh, :], imm_value=-1e30), r=['qc', 'best' + P_], w=['cand2'])
                        S.op('dve', lambda e, hh=hh: e.max(out=best[:, hh, 8:16], in_=cand2[:]), r=['cand2'], w=['best' + P_])
                elif stage == 14:
                    tt('dve', gat[:], best[:], bc(best[:, :, 0], 2, [128, 8, 16]), ALU.subtract, ['best' + P_], ['gat'])
                    act(gat[:], gat[:], AF.Exp, ['gat'], ['gat'])
                    red(gsum[:], gat[:], ALU.add, ['gat'], ['gsum'])
                    act(gsum[:], gsum[:], AF.Ln, ['gsum'], ['gsum'])
                    stt(bias8[:], best[:, :, 0], -1.0, gsum[:], ALU.mult, ALU.subtract, ['best' + P_, 'gsum'], ['bias8' + P_])

            NFE = 15

            def tail(n, stage):
                par = n % 2
                P_ = str(par)
                X1, X1K = x1b[par], 'x1b%d' % par
                h2b, h2T = h2b2[par], h2T2[par]
                if stage == 0:
                    for hf in range(2):
                        sl = slice(hf * 512, (hf + 1) * 512)
                        tt('dve', x2[:, sl], pbk[hf][:], X1[:, sl], ALU.add, [PB[hf], X1K], ['x2'])
                    rmsnorm(x2[:], 'x2', g3[:, 1, :], 'g3_1', h2b[:], 'h2b' + P_)
                    cp('pool', p_bf[:], ptl[par][:], ['ptl%d' % par], ['p_bf'])
                elif stage == 1:
                    for c in range(8):
                        tr(ptb[:, c, :], h2b[:, c * 128:(c + 1) * 128], identb[:], ['h2b' + P_, 'identb'], ['ptb'])
                    cp('act', h2T[:], ptb[:], ['ptb'], ['h2T' + P_])
                    for c in range(2):
                        tr(ptb[:, c, :], p_bf[:, c * 128:(c + 1) * 128], identb[:], ['p_bf', 'identb'], ['ptb'])
                    cp('act', pTt[:], ptb[:, 0:2, :], ['ptb'], ['pTt'])
                elif stage in (2, 3):
                    hf = stage - 2
                    sl = slice(hf * 512, (hf + 1) * 512)
                    for c in range(8):
                        mm(pbk[2 + hf][:], h2T[:, c, :], wpg[:, c, sl], c == 0, c == 7, ['h2T' + P_] + WPG, [PB[2 + hf]])
                    act(pg_s[:, sl], pbk[2 + hf][:], AF.Sigmoid, [PB[2 + hf]], ['pg_s'])
                    for c in range(2):
                        mm(pbk[4 + hf][:], pTt[:, c, :], wpu[:, c, sl], c == 0, c == 1, ['pTt'] + WPU, [PB[4 + hf]])
                    tt('dve', pg_s[:, sl], pg_s[:, sl], pbk[4 + hf][:], ALU.mult, ['pg_s', PB[4 + hf]], ['pg_s'])
                elif stage == 4:
                    tt('dve', pg_s[:], x2[:], pg_s[:], ALU.add, ['x2', 'pg_s'], ['pg_s'])
                    rmsnorm(pg_s[:], 'pg_s', g3[:, 2, :], 'g3_2', x2[:], 'x2')
                    S.dma('act', lambda e, n=n: e.dma_start(out=out_d[n * 128:(n + 1) * 128, :], in_=x2[:]), r=['x2'], w=['out%d' % n])

            NTAIL = 5
            FE_SLOT = {5 + (k * 9) // 5: k for k in range(15)}

            for st_ in range(NFE):
                front_end(0, st_)
            for n in range(nt):
                par = n % 2
                P_ = str(par)
                X1, X1K = x1b[par], 'x1b%d' % par
                h2b, h2T, s_sb, vals, best, bias8 = h2b2[par], h2T2[par], s_sb2[par], vals2[par], best2[par], bias82[par]
                v4 = vals[:].rearrange("p (h c) a -> p h c a", c=2)
                s4 = s_sb[:].rearrange("p (h c) k -> p h c k", c=2)
                def p_build(ibs, v4_, s4_, pk_):
                    for ib in ibs:
                        tt('dve', Ptok[0][:].rearrange("t (h a) i -> t h a i", a=16),
                           bc(s4_[:, :, 0, ib * IB:(ib + 1) * IB], 2, [128, 8, 16, IB]),
                           bc(v4_[:, :, 0, :], 3, [128, 8, 16, IB]), ALU.is_equal, ['s_sb' + pk_, 'vals' + pk_], ['Ptok0'])
                        for i8 in range(IB // 8):
                            pt_, pkk = nextptb()
                            for il in range(8):
                                tr(pt_[:, il, :], Ptok[0][:, :, i8 * 8 + il], identb[:], ['Ptok0', 'identb'], [pkk])
                            i0_ = ib * IB + i8 * 8
                            cp('act' if (i8 % 2) else 'dve', PTt[:, :, i0_:i0_ + 8].rearrange("n t i -> n i t"), pt_[:, :, :], [pkk], ['PTt'])

                if n == 0:
                    p_build(range(128 // IB), v4, s4, P_)

                def q_elem(jb):
                    qb_ = jb % 2
                    for hg in range(2):
                        hsl = slice(hg * 4, hg * 4 + 4)
                        tt('dve', xq[:], bc(v4[:, hsl, 0, :], 3, [128, 4, 16, JB]), bc(s4[:, hsl, 1, jb * JB:(jb + 1) * JB], 2, [128, 4, 16, JB]), ALU.add, ['vals' + P_, 's_sb' + P_], ['xq'])
                        for h_ in range(4):
                            hh = hg * 4 + h_
                            act(eq[:, h_, :, :], xq[:, h_, :, :], AF.Exp, ['xq', 'bias8' + P_], ['eq'], bias=bias8[:, hh:hh + 1])
                        tt('dve', xq[:].rearrange("p h a j -> p h (a j)"), xq[:].rearrange("p h a j -> p h (a j)"), bc(best[:, hsl, 15], 2, [128, 4, 16 * JB]), ALU.is_ge, ['xq', 'eq', 'best' + P_], ['xq'])
                        tt('dve', Qtok[qb_][:, hg * 64:(hg + 1) * 64, :].rearrange("p (h a) j -> p h a j", a=16), xq[:], eq[:], ALU.mult, ['xq', 'eq'], ['Qtok%d' % qb_])

                def q_tr(jb):
                    qb_ = jb % 2
                    for j8 in range(JB // 8):
                        pt_, pk_ = nextptb()
                        for jl in range(8):
                            tr(pt_[:, jl, :], Qtok[qb_][:, :, j8 * 8 + jl], identb[:], ['Qtok%d' % qb_, 'identb'], [pk_])
                        cp('act', QTt[qb_][:, j8 * 8:j8 * 8 + 8, :], pt_[:, :, :], [pk_], ['QTt%d' % qb_])

                def act_blk(jb):
                    ab = jb % 2
                    for jq in range(JB // 4):
                        bank = 2 + (jq % 2)
                        for j2 in range(2):
                            j0 = jb * JB + jq * 4 + j2 * 2
                            ub = uctr[0] % NSB
                            uctr[0] += 1
                            S.dma('sp', lambda e, ub=ub, j0=j0: e.dma_start(out=ubuf[ub][:], in_=utb_d[j0:j0 + 2, :, :].rearrange("j d x -> d j x")),
                                  r=['utb'], w=['ubuf%d' % ub])
                            for jl2 in range(2):
                                jl = j2 * 2 + jl2
                                for kc in range(8):
                                    mm(pbk[bank][:, jl * 128:(jl + 1) * 128], ubuf[ub][:, jl2, kc * 128:(kc + 1) * 128], h2T[:, kc, :], kc == 0, kc == 7, ['ubuf%d' % ub, 'h2T' + P_], [PB[bank]])
                        act(act_sb2[ab][:, jq * 4:jq * 4 + 4, :].rearrange("i j t -> i (j t)"), pbk[bank][:], AF.Gelu_apprx_tanh, [PB[bank]], ['act_sb%d' % ab])

                def v_part(jb, jqs):
                    qb_ = jb % 2
                    for jq in jqs:
                        j0 = jb * JB + jq * 2
                        vb = vctr[0] % NSB
                        vctr[0] += 1
                        S.dma('sp', lambda e, vb=vb, j0=j0: e.dma_start(out=vbuf[vb][:], in_=vtb_d[j0:j0 + 2, :, :].rearrange("j i x -> i j x")),
                              r=['vtb'], w=['vbuf%d' % vb])
                        for jl in range(2):
                            j = j0 + jl
                            for hf in range(2):
                                mm(pbk[hf][:], coef2[qb_][:, jq * 2 + jl, :], vbuf[vb][:, jl, hf * 512:(hf + 1) * 512], j == 0, j == 127, ['coef%d' % qb_, 'vbuf%d' % vb], [PB[hf]])

                def w_blk(jb, vjb, hook=None):
                    qb_ = jb % 2
                    ntg = 128 // TG
                    nvq = (JB // 2) // ntg
                    for tg in range(ntg):
                        bank = 4 + (tg % 2)
                        for tl in range(TG):
                            t = tg * TG + tl
                            mm(pbk[bank][:, tl * JB:(tl + 1) * JB], PTt[:, t, :], QTt[qb_][:, :, t], True, True, ['PTt', 'QTt%d' % qb_], [PB[bank]])
                        tt('dve', coef2[qb_][:, :, tg * TG:(tg + 1) * TG], pbk[bank][:].rearrange("i (t j) -> i j t", j=JB), act_sb2[qb_][:, :, tg * TG:(tg + 1) * TG], ALU.mult, [PB[bank], 'act_sb%d' % qb_], ['coef%d' % qb_])
                        if vjb is not None:
                            v_part(vjb, range(tg * nvq, (tg + 1) * nvq))
                        if hook is not None:
                            hook(jb * ntg + tg)

                def v_blk(jb):
                    v_part(jb, range(JB // 2))

                q_elem(0)
                act_blk(0)
                q_tr(0)
                for jb in range(NJB):
                    if jb + 1 < NJB:
                        q_elem(jb + 1)
                        act_blk(jb + 1)
                        q_tr(jb + 1)
                    def hook(slot, n=n):
                        if n >= 1 and slot < NTAIL:
                            tail(n - 1, slot)
                        if n + 1 < nt and slot in FE_SLOT:
                            front_end(n + 1, FE_SLOT[slot])
                    w_blk(jb, jb - 1 if jb >= 1 else None, hook)
                if n + 1 < nt:
                    pn = (n + 1) % 2
                    v4n = vals2[pn][:].rearrange("p (h c) a -> p h c a", c=2)
                    s4n = s_sb2[pn][:].rearrange("p (h c) k -> p h c k", c=2)
                    p_build(range(0, 4), v4n, s4n, str(pn))
                v_blk(NJB - 1)
                if n + 1 < nt:
                    p_build(range(4, 128 // IB), v4n, s4n, str(pn))
            for st_ in range(NTAIL):
                tail(nt - 1, st_)
          except _Stop:
            pass

        S.wait_all('sp')
        S.emit()
    return nc


def make_in_maps(inputs, nt, ncores):
    f = np.float32
    c = host_consts(nt)
    g = lambda k: np.asarray(inputs[k], dtype=f)
    S_ = nt * 128
    mu = g('rwkv_mu')[0]
    pp = np.zeros((128, 34), f)
    pp[:, 0:14] = mu.reshape(14, 128).T
    for j, k in enumerate(('rwkv_w0', 'rwkv_a0', 'rwkv_k_k', 'rwkv_k_a')):
        pp[:, 14 + 4 * j:18 + 4 * j] = g(k)[0].reshape(4, 128).T
    pp[:, 30:34] = g('rwkv_r_k')[0].reshape(4, 128).T
    g4 = np.stack([g('g_mix')[0], g('g_ffn')[0], g('g_ple')[0], g('g_final')], 0)
    gn3 = np.stack([g('ret_gn_g')[0], g('rwkv_gn_g')[0], g('rwkv_gn_b')[0]], 0)
    lora = np.zeros((128, 3, 512), f)
    lora[0:64, 0, :] = g('rwkv_w_up')[0]
    lora[64:128, 1, :] = g('rwkv_a_up')[0]
    lora[:, 2, :] = g('rwkv_g_up')[0]
    wbr = np.stack([g('w_ret_br')[0], g('w_rwkv_br')[0]], 0)
    shared = dict(w_in=g('w_in')[0], pp=pp, g4=np.ascontiguousarray(g4), gn3=np.ascontiguousarray(gn3),
                  lora=lora, wbr=np.ascontiguousarray(wbr), w_o=g('w_o')[0], w_pq=g('w_pq')[0],
                  sk=np.ascontiguousarray(g('peer_sub_keys')[0].reshape(16, 128, 128)),
                  peer_u=g('peer_u')[0], peer_v=g('peer_v')[0], w_ple_gate=g('w_ple_gate')[0],
                  w_ple_up=g('w_ple_up')[0], rot=c['rot'], DT=c['DT'], xiT=c['xiT'], CDb=c['CDb'], cm=c['cm'])
    x = g('x'); p = g('p')[0]
    maps = []
    for i in range(ncores):
        m = dict(shared)
        m['x'] = np.ascontiguousarray(x[i, :S_])
        m['p'] = np.ascontiguousarray(p[i, :S_])
        maps.append(m)
    return maps


def kernel(**inputs):
    nt = SEQ // 128
    nc = build(nt)
    in_maps = make_in_maps(inputs, nt, NCORES)
    res = run_bass_kernel_spmd(nc, in_maps, core_ids=list(range(NCORES)))
    out = np.stack([np.asarray(r["out"], dtype=np.float32) for r in res.results], axis=0)
    return out
```
